# Optimizing a Trainium2 kernel written in Bass

```python
import math
import jax, jax.numpy as jnp
from jax import lax
import numpy as np

D_MODEL = 1024
BATCH = 8
SEQ = 4096
DEPTH = 2

MIX_WIDTH = D_MODEL
DIFF_HEADS = 4
DIFF_QK_DIM = 64
DIFF_V_DIM = 2 * DIFF_QK_DIM
DIFF_ROT_DIM = DIFF_QK_DIM // 4
ROPE_THETA = 500000.0
MLA_HEADS = 4
MLA_Q_RANK = 256
MLA_KV_RANK = 128
MLA_NOPE_DIM = 128
MLA_ROPE_DIM = 64
MLA_QK_DIM = MLA_NOPE_DIM + MLA_ROPE_DIM
MLA_V_DIM = 128
MLA_ROPE_THETA = 10000.0
DIFF_QK_COLS = DIFF_HEADS * 2 * DIFF_QK_DIM
DIFF_V_COLS = DIFF_HEADS * DIFF_V_DIM
IN_SPLITS = [
    DIFF_QK_COLS,
    2 * DIFF_QK_COLS,
    2 * DIFF_QK_COLS + DIFF_V_COLS,
    2 * DIFF_QK_COLS + DIFF_V_COLS + MLA_Q_RANK,
    2 * DIFF_QK_COLS + DIFF_V_COLS + MLA_Q_RANK + MLA_KV_RANK,
]
IN_COLS = IN_SPLITS[-1] + MLA_ROPE_DIM
OUT_COLS = DIFF_HEADS * DIFF_V_DIM + MLA_HEADS * MLA_V_DIM
D_FF = 2816
N_EXPERTS = 8
TOP_K = 2
D_FF_EXPERT = D_FF // TOP_K
N_DENSE_LAYERS = (DEPTH + 1) // 2
N_MOE_LAYERS = DEPTH // 2
Q_BLOCK = 128
NORM_EPS = 1e-6

kernel_name = "hybrid_diffattn_mla_moe_block"


def rms_norm(x, g):
    xf = x.astype(jnp.float32)
    y = xf * lax.rsqrt(jnp.mean(xf * xf, axis=-1, keepdims=True) + NORM_EPS)
    return (y * g.astype(jnp.float32)).astype(x.dtype)


def rope(x, positions, theta):
    rot = x.shape[-1]
    half = rot // 2
    inv_freq = 1.0 / (theta ** (jnp.arange(half, dtype=jnp.float32) * (2.0 / rot)))
    ang = positions.astype(jnp.float32)[:, None] * inv_freq[None, :]
    bshape = (1, ang.shape[0]) + (1,) * (x.ndim - 3) + (half,)
    cos = jnp.cos(ang).reshape(bshape)
    sin = jnp.sin(ang).reshape(bshape)
    xf = x.astype(jnp.float32)
    x1, x2 = xf[..., :half], xf[..., half:]
    return jnp.concatenate([x1 * cos - x2 * sin, x2 * cos + x1 * sin], axis=-1).astype(x.dtype)


def causal_mask(i, seq):
    qpos = i * Q_BLOCK + jnp.arange(Q_BLOCK)
    kpos = jnp.arange(seq)
    return kpos[None, :] <= qpos[:, None]


def masked_softmax(s, mask):
    return jax.nn.softmax(jnp.where(mask, s, -jnp.inf), axis=-1)


def causal_query_blocks(block_fn, queries):
    b, s = queries[0].shape[:2]
    nb = s // Q_BLOCK
    qb = tuple(jnp.moveaxis(a.reshape(b, nb, Q_BLOCK, *a.shape[2:]), 1, 0) for a in queries)
    out = lax.map(lambda args: block_fn(args[0], args[1]), (qb, jnp.arange(nb)))
    out = jnp.moveaxis(out, 0, 1)
    return out.reshape(b, s, *out.shape[3:])


def differential_attention(q, k, v, q_g, k_g, lam_params, subln_g, layer_idx, positions):
    b, s, _ = q.shape
    q = q.reshape(b, s, DIFF_HEADS, 2, DIFF_QK_DIM)
    k = k.reshape(b, s, DIFF_HEADS, 2, DIFF_QK_DIM)
    v = v.reshape(b, s, DIFF_HEADS, DIFF_V_DIM)
    q = rms_norm(q, q_g)
    k = rms_norm(k, k_g)
    q = jnp.concatenate([rope(q[..., :DIFF_ROT_DIM], positions, ROPE_THETA), q[..., DIFF_ROT_DIM:]], axis=-1)
    k = jnp.concatenate([rope(k[..., :DIFF_ROT_DIM], positions, ROPE_THETA), k[..., DIFF_ROT_DIM:]], axis=-1)
    lam_init = 0.8 - 0.6 * math.exp(-0.3 * layer_idx)
    lp = lam_params.astype(jnp.float32)
    lam = jnp.exp(jnp.sum(lp[0] * lp[1])) - jnp.exp(jnp.sum(lp[2] * lp[3])) + lam_init
    scale = DIFF_QK_DIM ** -0.5
    k1, k2 = k[..., 0, :], k[..., 1, :]

    def block(qs, i):
        q1b, q2b = qs
        mask = causal_mask(i, s)
        s1 = jnp.einsum('bqhd,bkhd->bhqk', q1b, k1).astype(jnp.float32) * scale
        s2 = jnp.einsum('bqhd,bkhd->bhqk', q2b, k2).astype(jnp.float32) * scale
        p = masked_softmax(s1, mask) - lam * masked_softmax(s2, mask)
        return jnp.einsum('bhqk,bkhe->bqhe', p.astype(v.dtype), v)

    o = causal_query_blocks(block, (q[..., 0, :], q[..., 1, :]))
    o = rms_norm(o, subln_g) * (1.0 - lam_init)
    return o.reshape(b, s, DIFF_HEADS * DIFF_V_DIM)


def latent_attention(c_q, c_kv, k_pe, q_ln_g, w_uq, kv_ln_g, w_ukv, qk_g, positions):
    b, s, _ = c_q.shape
    q = (rms_norm(c_q, q_ln_g) @ w_uq).reshape(b, s, MLA_HEADS, MLA_QK_DIM)
    kv = (rms_norm(c_kv, kv_ln_g) @ w_ukv).reshape(b, s, MLA_HEADS, MLA_NOPE_DIM + MLA_V_DIM)
    k_nope, v = kv[..., :MLA_NOPE_DIM], kv[..., MLA_NOPE_DIM:]
    k_pe = jnp.broadcast_to(k_pe[:, :, None, :], (b, s, MLA_HEADS, MLA_ROPE_DIM))
    k = jnp.concatenate([k_nope, k_pe], axis=-1)
    q = rms_norm(q, qk_g[0])
    k = rms_norm(k, qk_g[1])
    q = jnp.concatenate([q[..., :MLA_NOPE_DIM], rope(q[..., MLA_NOPE_DIM:], positions, MLA_ROPE_THETA)], axis=-1)
    k = jnp.concatenate([k[..., :MLA_NOPE_DIM], rope(k[..., MLA_NOPE_DIM:], positions, MLA_ROPE_THETA)], axis=-1)
    scale = MLA_QK_DIM ** -0.5

    def block(qs, i):
        (qb,) = qs
        sc = jnp.einsum('bqhd,bkhd->bhqk', qb, k).astype(jnp.float32) * scale
        p = masked_softmax(sc, causal_mask(i, s))
        return jnp.einsum('bhqk,bkhe->bqhe', p.astype(v.dtype), v)

    o = causal_query_blocks(block, (q,))
    return o.reshape(b, s, MLA_HEADS * MLA_V_DIM)


def swiglu(h, wg, wu, wd):
    return (jax.nn.silu(h @ wg) * (h @ wu)) @ wd


def moe_swiglu(h, router_w, wg, wu, wd):
    logits = (h @ router_w).astype(jnp.float32)
    top_logits, top_idx = lax.top_k(logits, TOP_K)
    gates = jax.nn.softmax(top_logits, axis=-1)
    combine = jnp.einsum('bsk,bske->bse', gates,
                         jax.nn.one_hot(top_idx, N_EXPERTS, dtype=jnp.float32)).astype(h.dtype)
    out = jnp.zeros_like(h)
    for e in range(N_EXPERTS):
        out = out + combine[..., e:e + 1] * swiglu(h, wg[e], wu[e], wd[e])
    return out


def setup_inputs(seed: int = 0) -> dict:
    key = jax.random.key(seed)
    ks = jax.random.split(key, 24)
    f32 = jnp.float32

    def nrm(k, shape, scale):
        return jax.random.normal(k, shape, f32) * scale

    def gain(k, shape):
        return 1.0 + 0.02 * jax.random.normal(k, shape, f32)

    return {
        "x": nrm(ks[0], (BATCH, SEQ, D_MODEL), 1.0),
        "attn_norm_g": gain(ks[1], (DEPTH, D_MODEL)),
        "w_in": nrm(ks[2], (DEPTH, D_MODEL, IN_COLS), D_MODEL ** -0.5),
        "diff_q_norm_g": gain(ks[3], (DEPTH, DIFF_QK_DIM)),
        "diff_k_norm_g": gain(ks[4], (DEPTH, DIFF_QK_DIM)),
        "diff_lambda": nrm(ks[5], (DEPTH, 4, DIFF_QK_DIM), 0.1),
        "diff_subln_g": gain(ks[6], (DEPTH, DIFF_V_DIM)),
        "mla_q_ln_g": gain(ks[7], (DEPTH, MLA_Q_RANK)),
        "w_uq": nrm(ks[8], (DEPTH, MLA_Q_RANK, MLA_HEADS * MLA_QK_DIM), MLA_Q_RANK ** -0.5),
        "mla_kv_ln_g": gain(ks[9], (DEPTH, MLA_KV_RANK)),
        "w_ukv": nrm(ks[10], (DEPTH, MLA_KV_RANK, MLA_HEADS * (MLA_NOPE_DIM + MLA_V_DIM)), MLA_KV_RANK ** -0.5),
        "mla_qk_norm_g": gain(ks[11], (DEPTH, 2, MLA_QK_DIM)),
        "w_o": nrm(ks[12], (DEPTH, OUT_COLS, D_MODEL), OUT_COLS ** -0.5),
        "ffn_norm_g": gain(ks[13], (DEPTH, D_MODEL)),
        "dense_w_gate": nrm(ks[14], (N_DENSE_LAYERS, D_MODEL, D_FF), D_MODEL ** -0.5),
        "dense_w_up": nrm(ks[15], (N_DENSE_LAYERS, D_MODEL, D_FF), D_MODEL ** -0.5),
        "dense_w_down": nrm(ks[16], (N_DENSE_LAYERS, D_FF, D_MODEL), D_FF ** -0.5),
        "router_w": nrm(ks[17], (N_MOE_LAYERS, D_MODEL, N_EXPERTS), D_MODEL ** -0.5),
        "moe_w_gate": nrm(ks[18], (N_MOE_LAYERS, N_EXPERTS, D_MODEL, D_FF_EXPERT), D_MODEL ** -0.5),
        "moe_w_up": nrm(ks[19], (N_MOE_LAYERS, N_EXPERTS, D_MODEL, D_FF_EXPERT), D_MODEL ** -0.5),
        "moe_w_down": nrm(ks[20], (N_MOE_LAYERS, N_EXPERTS, D_FF_EXPERT, D_MODEL), D_FF_EXPERT ** -0.5),
    }


def reference(x, attn_norm_g, w_in, diff_q_norm_g, diff_k_norm_g, diff_lambda, diff_subln_g,
              mla_q_ln_g, w_uq, mla_kv_ln_g, w_ukv, mla_qk_norm_g, w_o, ffn_norm_g,
              dense_w_gate, dense_w_up, dense_w_down, router_w, moe_w_gate, moe_w_up, moe_w_down):
    positions = jnp.arange(x.shape[1], dtype=jnp.int32)
    for l in range(DEPTH):
        h = rms_norm(x, attn_norm_g[l])
        proj = h @ w_in[l]
        dq, dk, dv, c_q, c_kv, k_pe = jnp.split(proj, IN_SPLITS, axis=-1)
        a_out = differential_attention(dq, dk, dv, diff_q_norm_g[l], diff_k_norm_g[l],
                                       diff_lambda[l], diff_subln_g[l], l, positions)
        b_out = latent_attention(c_q, c_kv, k_pe, mla_q_ln_g[l], w_uq[l], mla_kv_ln_g[l],
                                 w_ukv[l], mla_qk_norm_g[l], positions)
        x = x + jnp.concatenate([a_out, b_out], axis=-1) @ w_o[l]
        h = rms_norm(x, ffn_norm_g[l])
        j = l // 2
        if l % 2 == 0:
            x = x + swiglu(h, dense_w_gate[j], dense_w_up[j], dense_w_down[j])
        else:
            x = x + moe_swiglu(h, router_w[j], moe_w_gate[j], moe_w_up[j], moe_w_down[j])
    return x
```

```python
import math
from contextlib import ExitStack
import numpy as np
import ml_dtypes
import concourse.bass as bass
import concourse.mybir as mybir
from concourse.bass_utils import run_bass_kernel_spmd

F32 = mybir.dt.float32
BF16 = mybir.dt.bfloat16
ALU = mybir.AluOpType
AF = mybir.ActivationFunctionType
AX = mybir.AxisListType

S = 4096
D = 1024
NT = S // 128
DEPTH = 2
EPS = 1e-6
INC = 1984
FFE = 1408
NCH = FFE // 128
ROPE_THETA = 500000.0
MLA_ROPE_THETA = 10000.0


class DSem:
    def __init__(self, h):
        self.h = h
        self.v = 0


class Slot:
    __slots__ = ("wr", "rd")

    def __init__(self):
        self.wr = []
        self.rd = []


class KB:
    def __init__(self, depth=DEPTH, stop_after=None, debug=False):
        self.depth = depth
        self.stop_after = stop_after
        self.debug = debug
        nc = self.nc = bass.Bass("TRN2", target_bir_lowering=False)
        self.E = dict(pe=nc.tensor, dve=nc.vector, act=nc.scalar, pool=nc.gpsimd, sp=nc.sync)
        self.csem = {k: nc.alloc_semaphore("c_" + k) for k in ("pe", "dve", "act", "pool")}
        self.ccnt = dict.fromkeys(self.csem, 0)
        self.waited = {}
        self.dsems = [DSem(nc.alloc_semaphore("d%d" % i)) for i in range(72)]
        self.dptr = 0
        self.banks = [nc.alloc_psum_tensor("bank%d" % i, [128, 512], F32) for i in range(8)]
        self.bank_s = [Slot() for _ in range(8)]
        self.stack = None
        self.uid = 0
        self.const_s = None

    def sig(self, e, ins):
        self.ccnt[e] += 1
        ins.then_inc(self.csem[e], 1)
        return (self.csem[e], self.ccnt[e])

    def wait(self, e, toks):
        for tok in toks:
            sem, v = tok
            key = (e, sem.num)
            if self.waited.get(key, 0) >= v:
                continue
            self.waited[key] = v
            self.E[e].wait_ge(sem, v)

    def _deps(self, e, reads, writes):
        toks = []
        for s in reads:
            toks.extend(s.wr)
        for s in writes:
            toks.extend(s.rd)
            toks.extend(s.wr)
        self.wait(e, toks)

    def _commit(self, tok, reads, writes):
        for s in reads:
            if s is not self.const_s:
                s.rd.append(tok)
        for s in writes:
            s.wr = [tok]
            s.rd = []

    def op(self, e, fn, reads=(), writes=()):
        self._deps(e, reads, writes)
        ins = fn(self.E[e])
        tok = self.sig(e, ins)
        self._commit(tok, reads, writes)
        return tok

    def dsem(self):
        d = self.dsems[self.dptr]
        self.dptr += 1
        return d

    def dma(self, q, out, in_, ds, reads=(), writes=(), **kw):
        self._deps(q, reads, writes)
        ins = self.E[q].dma_start(out=out, in_=in_, **kw)
        ins.then_inc(ds.h, 16)
        ds.v += 16
        tok = (ds.h, ds.v)
        self._commit(tok, reads, writes)
        return tok

    def mm_group(self, bank_i, mms, reads=()):
        bs = self.bank_s[bank_i]
        self._deps("pe", reads, [bs])
        ins = None
        for (o, l, r, st, sp) in mms:
            ins = self.nc.tensor.matmul(o, lhsT=l, rhs=r, start=st, stop=sp)
        tok = self.sig("pe", ins)
        self._commit(tok, reads, [bs])
        return tok

    def tr_group(self, bank_i, trs, ident, reads=()):
        bs = self.bank_s[bank_i]
        self._deps("pe", reads, [bs])
        ins = None
        for (o, i) in trs:
            ins = self.nc.tensor.transpose(out=o, in_=i, identity=ident)
        tok = self.sig("pe", ins)
        self._commit(tok, reads, [bs])
        return tok

    def barrier(self):
        toks = [(self.csem[c], self.ccnt[c]) for c in self.csem if self.ccnt[c] > 0]
        toks += [(d.h, d.v) for d in self.dsems if d.v > 0]
        for e in self.E:
            self.wait(e, toks)

    def begin_phase(self):
        self.barrier()
        self.stack = ExitStack()
        self.dptr = self.dkeep
        for b in self.bank_s:
            b.wr = []
            b.rd = []

    def end_phase(self):
        self.barrier()
        self.stack.close()
        self.stack = None

    def sb(self, name, shape, dt):
        self.uid += 1
        return self.stack.enter_context(self.nc.sbuf_tensor("%s_%d" % (name, self.uid), shape, dt))

    def bank_bf(self, i):
        return self.banks[i][:].bitcast(BF16)

    def build(self):
        nc = self.nc

        def din(name, shape):
            return nc.dram_tensor(name, shape, F32, kind="ExternalInput").ap()

        self.x = din("x", [S, D])
        self.attn_g = din("attn_norm_g", [DEPTH, D])
        self.w_in = din("w_in", [DEPTH, D, INC])
        self.dq_g = din("diff_q_norm_g", [DEPTH, 64])
        self.dk_g = din("diff_k_norm_g", [DEPTH, 64])
        self.dlam = din("diff_lambda", [DEPTH, 256])
        self.subln_g = din("diff_subln_g", [DEPTH, 128])
        self.qln_g = din("mla_q_ln_g", [DEPTH, 256])
        self.w_uq = din("w_uq", [DEPTH, 256, 768])
        self.kvln_g = din("mla_kv_ln_g", [DEPTH, 128])
        self.w_ukv = din("w_ukv", [DEPTH, 128, 1024])
        self.mqk_g = din("mla_qk_norm_g", [DEPTH, 2 * 192])
        self.w_o = din("w_o", [DEPTH, D, D])
        self.ffn_g = din("ffn_norm_g", [DEPTH, D])
        self.dwg = din("dense_w_gate", [D, 2816])
        self.dwu = din("dense_w_up", [D, 2816])
        self.dwd = din("dense_w_down", [2816, D])
        self.rw = din("router_w", [D, 8])
        self.mwg = din("moe_w_gate", [8, D, FFE])
        self.mwu = din("moe_w_up", [8, D, FFE])
        self.mwd = din("moe_w_down", [8, FFE, D])
        self.cs_d = din("cs_d", [S, 32])
        self.cs_m = din("cs_m", [S, 128])
        self.ident_in = din("ident", [128, 128])
        self.y = nc.dram_tensor("y", [S, D], F32, kind="ExternalOutput").ap()
        xr = self.xr = self.y

        kind = "ExternalOutput" if self.debug else "Internal"

        def dscr(name, shape, dt):
            return nc.dram_tensor(name, shape, dt, kind=kind).ap()

        self.qkt = dscr("qkt", [20, 128, S], BF16)
        self.vd = dscr("vd", [S, 512], BF16)
        self.vm = dscr("vm", [S, 512], BF16)
        self.ocat = dscr("ocat", [S, D], BF16)
        self.h2t = dscr("h2t", [8, 128, S], BF16)
        self.b_win = dscr("b_win", [DEPTH, D, INC], BF16)
        self.b_wuq = dscr("b_wuq", [DEPTH, 256, 768], BF16)
        self.b_wukv = dscr("b_wukv", [DEPTH, 128, 1024], BF16)
        self.b_wo = dscr("b_wo", [DEPTH, D, D], BF16)
        self.b_dwg = dscr("b_dwg", [D, 2816], BF16)
        self.b_dwu = dscr("b_dwu", [D, 2816], BF16)
        self.b_dwd = dscr("b_dwd", [2816, D], BF16)
        self.b_mwg = dscr("b_mwg", [8, D, FFE], BF16)
        self.b_mwu = dscr("b_mwu", [8, D, FFE], BF16)
        self.b_mwd = dscr("b_mwd", [8, FFE, D], BF16)

        A = nc.alloc_sbuf_tensor
        self.ident_f = A("ident_f", [128, 128], F32)
        self.ident_b = A("ident_b", [128, 128], BF16)
        self.sc_attn = A("sc_attn", [128, DEPTH, 8], F32)
        self.sc_ffn = A("sc_ffn", [128, DEPTH, 8], F32)
        self.sc_qln = A("sc_qln", [128, DEPTH, 2], F32)
        self.sc_kvln = A("sc_kvln", [128, DEPTH, 1], F32)
        self.sc_wo = A("sc_wo", [128, DEPTH, 8], F32)
        self.ones = A("ones", [128, 24], F32)
        self.gd = A("gd", [128, DEPTH, 2, 64], F32)
        self.gm = A("gm", [128, DEPTH, 2, 192], F32)
        self.lamt = A("lamt", [128, DEPTH, 256], F32)
        self.lamw = A("lamw", [128, 8], F32)
        self.neglam = A("neglam", [128, DEPTH], F32)
        self.negM = A("negM", [128, DEPTH, 2], F32)
        self.gmax = A("gmax", [128, 8], F32)
        self.rw_s = A("rw_s", [128, 8, 8], F32)
        self.comb = A("comb", [128, NT, 8], F32)
        self.junk = A("junk", [128, 1024], BF16)
        self.const_s = Slot()

        self.setup_consts()
        self.setup_wjobs()
        self.dkeep = self.dptr
        for l in range(self.depth):
            self.phase1(l)
            if self.stop_after == "p1_%d" % l:
                return self.finish()
            self.phase2(l)
            if self.stop_after == "p2_%d" % l:
                return self.finish()
            self.phase3a(l)
            if self.stop_after == "p3a_%d" % l:
                return self.finish()
            self.phase3b(l)
            if self.stop_after == "p3b_%d" % l:
                return self.finish()
        return self.finish()

    def finish(self):
        self.barrier()
        return self.nc

    def setup_consts(self):
        nc = self.nc
        cs = self.const_s
        ds = self.dsem()
        toks = []

        def ld(out, in_, **kw):
            ins = nc.sync.dma_start(out=out, in_=in_, **kw)
            ins.then_inc(ds.h, 16)
            ds.v += 16

        self.xinit = self.dsem()
        for i in range(4):
            ins = nc.sync.dma_start(out=self.y[i * 1024:(i + 1) * 1024, :], in_=self.x[i * 1024:(i + 1) * 1024, :])
            ins.then_inc(self.xinit.h, 16)
            self.xinit.v += 16

        ld(self.ident_f[:], self.ident_in[:, :])
        for l in range(DEPTH):
            ld(self.sc_attn[:, l, :], self.attn_g[l].rearrange("(c p) -> p c", p=128), allow_slow_non_contiguous=True)
            ld(self.sc_ffn[:, l, :], self.ffn_g[l].rearrange("(c p) -> p c", p=128), allow_slow_non_contiguous=True)
            ld(self.sc_qln[:, l, :], self.qln_g[l].rearrange("(c p) -> p c", p=128), allow_slow_non_contiguous=True)
            ld(self.sc_kvln[:, l, :], self.kvln_g[l].rearrange("(c p) -> p c", p=128), allow_slow_non_contiguous=True)
            ld(self.sc_wo[:, l, 0:1], self.subln_g[l].rearrange("(c p) -> p c", p=128), allow_slow_non_contiguous=True)
            ld(self.gd[:, l, 0, :], self.dq_g[l].partition_broadcast(128))
            ld(self.gd[:, l, 1, :], self.dk_g[l].partition_broadcast(128))
            ld(self.gm[:, l, :, :].rearrange("p a b -> p (a b)"), self.mqk_g[l].partition_broadcast(128))
            ld(self.lamt[:, l, :], self.dlam[l].partition_broadcast(128))
        ld(self.rw_s[:], self.rw.rearrange("(c p) n -> p c n", p=128))
        nc.vector.wait_ge(ds.h, ds.v)
        nc.gpsimd.wait_ge(ds.h, ds.v)
        nc.scalar.wait_ge(ds.h, ds.v)
        V = nc.vector
        sv = self.csem["dve"]

        def vop(ins):
            self.ccnt["dve"] += 1
            ins.then_inc(sv, 1)
            V.wait_ge(sv, self.ccnt["dve"])

        def aop(ins):
            self.ccnt["act"] += 1
            ins.then_inc(self.csem["act"], 1)
            nc.scalar.wait_ge(self.csem["act"], self.ccnt["act"])
            V.wait_ge(self.csem["act"], self.ccnt["act"])

        vop(V.tensor_copy(out=self.ident_b[:], in_=self.ident_f[:]))
        vop(V.memset(self.ones[:], 1.0))
        for l in range(DEPTH):
            lam_init = 0.8 - 0.6 * math.exp(-0.3 * l)
            vop(V.tensor_scalar(out=self.sc_wo[:, l, 0:1], in0=self.sc_wo[:, l, 0:1], scalar1=float(1.0 - lam_init),
                                scalar2=None, op0=ALU.mult))
            for c in range(1, 4):
                vop(V.tensor_copy(out=self.sc_wo[:, l, c:c + 1], in_=self.sc_wo[:, l, 0:1]))
            vop(V.memset(self.sc_wo[:, l, 4:8], 1.0))
        for l in range(DEPTH):
            lam_init = 0.8 - 0.6 * math.exp(-0.3 * l)
            vop(V.tensor_tensor(out=self.lamt[:, l, 0:64], in0=self.lamt[:, l, 0:64], in1=self.lamt[:, l, 64:128], op=ALU.mult))
            vop(V.tensor_tensor(out=self.lamt[:, l, 128:192], in0=self.lamt[:, l, 128:192], in1=self.lamt[:, l, 192:256], op=ALU.mult))
            vop(V.tensor_reduce(out=self.lamw[:, 0:1], in_=self.lamt[:, l, 0:64], axis=AX.X, op=ALU.add))
            vop(V.tensor_reduce(out=self.lamw[:, 1:2], in_=self.lamt[:, l, 128:192], axis=AX.X, op=ALU.add))
            nc.scalar.wait_ge(sv, self.ccnt["dve"])
            aop(nc.scalar.activation(out=self.lamw[:, 2:4], in_=self.lamw[:, 0:2], func=AF.Exp))
            vop(V.tensor_tensor(out=self.lamw[:, 4:5], in0=self.lamw[:, 3:4], in1=self.lamw[:, 2:3], op=ALU.subtract))
            vop(V.tensor_scalar(out=self.neglam[:, l:l + 1], in0=self.lamw[:, 4:5], scalar1=float(-lam_init),
                                scalar2=None, op0=ALU.add))
            vop(V.tensor_reduce(out=self.gmax[:, 0:2], in_=self.gd[:, l, :, :], axis=AX.X, op=ALU.max,
                                apply_absolute_value=True))
            vop(V.tensor_reduce(out=self.gmax[:, 2:4], in_=self.gm[:, l, :, :], axis=AX.X, op=ALU.max,
                                apply_absolute_value=True))
            vop(V.tensor_tensor(out=self.gmax[:, 4:5], in0=self.gmax[:, 0:1], in1=self.gmax[:, 1:2], op=ALU.mult))
            vop(V.tensor_tensor(out=self.gmax[:, 5:6], in0=self.gmax[:, 2:3], in1=self.gmax[:, 3:4], op=ALU.mult))
            vop(V.tensor_scalar(out=self.negM[:, l, 0:1], in0=self.gmax[:, 4:5], scalar1=float(-math.sqrt(64.0)),
                                scalar2=None, op0=ALU.mult))
            vop(V.tensor_scalar(out=self.negM[:, l, 1:2], in0=self.gmax[:, 5:6], scalar1=float(-math.sqrt(192.0)),
                                scalar2=None, op0=ALU.mult))
        for c in range(8):
            vop(V.tensor_scalar(out=self.rw_s[:, c, :], in0=self.rw_s[:, c, :], scalar1=self.sc_ffn[:, 1, c:c + 1],
                                scalar2=None, op0=ALU.mult))
        vop(V.memset(self.comb[:], 1.0))

    def setup_wjobs(self):
        nc = self.nc
        NB = 3
        CM = 1408
        self.w_fst = [nc.alloc_sbuf_tensor("wf%d" % i, [128, CM], F32) for i in range(NB)]
        self.w_bst = [nc.alloc_sbuf_tensor("wb%d" % i, [128, CM], BF16) for i in range(NB)]
        self.w_fs = [Slot() for _ in range(NB)]
        self.w_bs = [Slot() for _ in range(NB)]
        self.w_fd = [self.dsem() for _ in range(NB)]
        self.w_bd = [self.dsem() for _ in range(NB)]
        self.w_nb = NB
        self.wjobs = []
        self.wnext = 0
        self.wpending = []
        self.wtags = {}

        def add(src, dst, R, C, sc):
            ncs = 1 if C <= CM else 2
            cw = C // ncs
            for c in range(R // 128):
                for j in range(ncs):
                    self.wjobs.append((src[c * 128:(c + 1) * 128, j * cw:(j + 1) * cw],
                                       dst[c * 128:(c + 1) * 128, j * cw:(j + 1) * cw], cw, sc(c)))

        one = lambda c: self.ones[:, 0:1]

        def attn(l):
            add(self.w_in[l], self.b_win[l], D, INC, lambda c, l=l: self.sc_attn[:, l, c:c + 1])
            add(self.w_uq[l], self.b_wuq[l], 256, 768, lambda c, l=l: self.sc_qln[:, l, c:c + 1])
            add(self.w_ukv[l], self.b_wukv[l], 128, 1024, lambda c, l=l: self.sc_kvln[:, l, c:c + 1])
            self.wtags["attn%d" % l] = len(self.wjobs)
            add(self.w_o[l], self.b_wo[l], D, D, lambda c, l=l: self.sc_wo[:, l, c:c + 1])
            self.wtags["wo%d" % l] = len(self.wjobs)

        attn(0)
        add(self.dwg, self.b_dwg, D, 2816, lambda c: self.sc_ffn[:, 0, c:c + 1])
        add(self.dwu, self.b_dwu, D, 2816, lambda c: self.sc_ffn[:, 0, c:c + 1])
        add(self.dwd, self.b_dwd, 2816, D, one)
        self.wtags["ffn0"] = len(self.wjobs)
        if self.depth > 1:
            attn(1)
            for e in range(8):
                add(self.mwg[e], self.b_mwg[e], D, FFE, lambda c: self.sc_ffn[:, 1, c:c + 1])
                add(self.mwu[e], self.b_mwu[e], D, FFE, lambda c: self.sc_ffn[:, 1, c:c + 1])
                add(self.mwd[e], self.b_mwd[e], FFE, D, one)
            self.wtags["ffn1"] = len(self.wjobs)

    def _wflush(self):
        for (i, dst, C) in self.wpending:
            self.dma("sp", dst, self.w_bst[i][:, 0:C], self.w_bd[i], reads=[self.w_bs[i]])
        self.wpending = []

    def pump(self, n, engs=("dve", "act")):
        if self.wnext >= len(self.wjobs):
            if self.wpending:
                self._wflush()
            return
        self._wflush()
        for _ in range(n):
            if self.wnext >= len(self.wjobs):
                break
            src, dst, C, scal = self.wjobs[self.wnext]
            i = self.wnext % self.w_nb
            eng = engs[self.wnext % len(engs)]
            self.wnext += 1
            fst, bst = self.w_fst[i], self.w_bst[i]
            self.dma("sp", fst[:, 0:C], src, self.w_fd[i], writes=[self.w_fs[i]])
            if eng == "act":
                self.op("act", lambda a: a.activation(out=bst[:, 0:C], in_=fst[:, 0:C], func=AF.Copy, scale=scal),
                        reads=[self.w_fs[i]], writes=[self.w_bs[i]])
            else:
                self.op(eng, lambda v: v.tensor_scalar(out=bst[:, 0:C], in0=fst[:, 0:C], scalar1=scal, scalar2=None,
                                                       op0=ALU.mult), reads=[self.w_fs[i]], writes=[self.w_bs[i]])
            self.wpending.append((i, dst, C))

    def pump_until(self, tag, engs=("dve", "act")):
        tgt = self.wtags[tag]
        while self.wnext < tgt:
            self.pump(min(2, tgt - self.wnext), engs)
        self._wflush()
        self.wait("sp", [(d.h, d.v) for d in self.w_bd if d.v > 0])

    def rstd(self, src_ap, src_slot, out_ap, out_slot, tmp_ap, tmp_slot, inv_w):
        self.op("act", lambda a: a.activation(out=tmp_ap, in_=src_ap, func=AF.Ln, scale=float(inv_w), bias=float(EPS)),
                reads=[src_slot], writes=[tmp_slot])
        self.op("act", lambda a: a.activation(out=out_ap, in_=tmp_ap, func=AF.Exp, scale=-0.5),
                reads=[tmp_slot], writes=[out_slot])

    def normrope(self, W_, src3, src_slots, G, W, gain3, R, rot_lo, cs_ap, cs_slot, dst_nonrot, dst_rot, dst_slot,
                 nonrot_lo, nonrot_hi):
        sq, sq_s, ssg, ssg_s, lg, lg_s, rsg, rsg_s, tmp, tmp_s, xr_, xr_s, ac, ac_s, sw, sw_s = W_
        n = G * W
        h = R // 2
        sq3 = sq[:, 0:n].rearrange("p (g w) -> p g w", w=W)
        tmp3 = tmp[:, 0:n].rearrange("p (g w) -> p g w", w=W)
        xr3 = xr_[:, 0:G * R].rearrange("p (g r) -> p g r", r=R)
        ac3 = ac[:, 0:G * R].rearrange("p (g r) -> p g r", r=R)
        sw3 = sw[:, 0:G * R].rearrange("p (g r) -> p g r", r=R)
        self.op("act", lambda a: a.activation(out=sq3, in_=src3, func=AF.Square), reads=src_slots, writes=[sq_s])
        self.op("dve", lambda v: v.tensor_reduce(out=ssg[:, 0:G], in_=sq3, axis=AX.X, op=ALU.add),
                reads=[sq_s], writes=[ssg_s])
        self.rstd(ssg[:, 0:G], ssg_s, rsg[:, 0:G], rsg_s, lg[:, 0:G], lg_s, 1.0 / W)
        self.op("dve", lambda v: v.tensor_tensor(out=tmp3, in0=src3, in1=rsg[:, 0:G].unsqueeze(2).broadcast_to([128, G, W]),
                                                 op=ALU.mult),
                reads=list(src_slots) + [rsg_s], writes=[tmp_s])
        self.op("pool", lambda p: p.tensor_tensor(out=dst_nonrot, in0=tmp3[:, :, nonrot_lo:nonrot_hi],
                                                  in1=gain3[:, :, nonrot_lo:nonrot_hi], op=ALU.mult),
                reads=[tmp_s], writes=[dst_slot])
        self.op("pool", lambda p: p.tensor_tensor(out=xr3, in0=tmp3[:, :, rot_lo:rot_lo + R],
                                                  in1=gain3[:, :, rot_lo:rot_lo + R], op=ALU.mult),
                reads=[tmp_s], writes=[xr_s])
        cc = cs_ap[:, 0:R].unsqueeze(1).broadcast_to([128, G, R])
        ns = cs_ap[:, R:R + h].unsqueeze(1).broadcast_to([128, G, h])
        ps_ = cs_ap[:, R + h:2 * R].unsqueeze(1).broadcast_to([128, G, h])
        self.op("pool", lambda p: p.tensor_tensor(out=ac3, in0=xr3, in1=cc, op=ALU.mult),
                reads=[xr_s, cs_slot], writes=[ac_s])
        self.op("pool", lambda p: p.tensor_tensor(out=sw3[:, :, 0:h], in0=xr3[:, :, h:R], in1=ns, op=ALU.mult),
                reads=[xr_s, cs_slot], writes=[sw_s])
        self.op("pool", lambda p: p.tensor_tensor(out=sw3[:, :, h:R], in0=xr3[:, :, 0:h], in1=ps_, op=ALU.mult),
                reads=[xr_s, cs_slot], writes=[sw_s])
        tok = self.op("pool", lambda p: p.tensor_tensor(out=dst_rot, in0=ac3, in1=sw3, op=ALU.add),
                      reads=[ac_s, sw_s], writes=[dst_slot])
        return tok

    def phase1(self, l):
        self.begin_phase()
        self.pump_until("attn%d" % l)
        if l == 0:
            self.wait("sp", [(self.xinit.h, self.xinit.v)])
        nc = self.nc
        sb = self.sb
        win = sb("win", [128, 8, INC], BF16)
        wuq = sb("wuq", [128, 2, 768], BF16)
        wukv = sb("wukv", [128, 1024], BF16)
        w_s = Slot()
        d0 = self.dsem()
        self.dma("sp", win[:], self.b_win[l].rearrange("(c p) n -> p c n", p=128), d0, writes=[])
        self.dma("sp", wuq[:], self.b_wuq[l].rearrange("(c p) n -> p c n", p=128), d0, writes=[])
        tokw = self.dma("sp", wukv[:], self.b_wukv[l], d0, writes=[])
        w_s.wr = [tokw]

        def bufs(name, shape, dt, n):
            return [sb(name, shape, dt) for _ in range(n)], [Slot() for _ in range(n)]

        xt, xt_s = bufs("xt", [128, D], F32, 2)
        hb, hb_s = bufs("hb", [128, D], BF16, 2)
        hT, hT_s = bufs("hT", [128, D], BF16, 2)
        pj = [sb("pj", [128, INC], F32) for _ in range(2)]
        pj_s = [[Slot() for _ in range(4)] for _ in range(2)]
        smA = [sb("smA", [128, 4], F32) for _ in range(2)]
        ss_s, lg_s, rs_s = [Slot(), Slot()], [Slot(), Slot()], [Slot(), Slot()]
        smB = [sb("smB", [128, 8], F32) for _ in range(2)]
        ssc_s, lgc_s, rsc_s = [Slot(), Slot()], [Slot(), Slot()], [Slot(), Slot()]
        cn, cn_s = bufs("cn", [128, 384], BF16, 2)
        cT, cT_s = bufs("cT", [128, 384], BF16, 2)
        qm = [sb("qm", [128, 768], F32) for _ in range(2)]
        qm_s = [[Slot(), Slot()] for _ in range(2)]
        kvm = [sb("kvm", [128, 1024], F32) for _ in range(2)]
        kvm_s = [[Slot(), Slot()] for _ in range(2)]
        kf, kf_s = bufs("kf", [128, 768], F32, 2)
        dA, dA_s = bufs("dA", [128, 1024], BF16, 2)
        mQ, mQ_s = bufs("mQ", [128, 768], BF16, 2)
        mK, mK_s = bufs("mK", [128, 768], BF16, 2)
        vb, vb_s = bufs("vb", [128, 512], BF16, 3)
        vmb, vmb_s = bufs("vmb", [128, 512], BF16, 3)
        csd, csd_s = bufs("csd", [128, 32], F32, 3)
        csm, csm_s = bufs("csm", [128, 128], F32, 3)
        stage = [sb("stage", [128, 20, 256], BF16) for _ in range(2)]
        stage_s = [Slot(), Slot()]
        xt_d = [self.dsem() for _ in range(2)]
        cs_d_ = [self.dsem() for _ in range(3)]
        cs_m_ = [self.dsem() for _ in range(3)]
        vb_d = [self.dsem() for _ in range(3)]
        vmb_d = [self.dsem() for _ in range(3)]
        st_d = [self.dsem() for _ in range(2)]

        def mkscratch(n, gr):
            return (sb("sq", [128, n], F32), Slot(), sb("ssg", [128, 16], F32), Slot(), sb("lg", [128, 16], F32), Slot(),
                    sb("rsg", [128, 16], F32), Slot(), sb("tmp", [128, n], F32), Slot(), sb("xr_", [128, gr], F32), Slot(),
                    sb("ac", [128, gr], F32), Slot(), sb("sw", [128, gr], F32), Slot())

        W_d = mkscratch(1024, 256)
        W_q = mkscratch(768, 256)
        W_k = mkscratch(768, 256)

        gdf = sb("gdf", [128, 16, 64], F32)
        gdf_s = Slot()
        self.op("dve", lambda v: v.tensor_copy(out=gdf[:, 0:8, :], in_=self.gd[:, l, 0, :].unsqueeze(1).broadcast_to([128, 8, 64])),
                writes=[gdf_s])
        self.op("dve", lambda v: v.tensor_copy(out=gdf[:, 8:16, :], in_=self.gd[:, l, 1, :].unsqueeze(1).broadcast_to([128, 8, 64])),
                writes=[gdf_s])
        self.wait("pool", gdf_s.wr)
        gd3 = gdf[:]
        gq3 = self.gm[:, l, 0, :].unsqueeze(1).broadcast_to([128, 4, 192])
        gk3 = self.gm[:, l, 1, :].unsqueeze(1).broadcast_to([128, 4, 192])
        TPB = [0, 1]
        MMB = [2, 3, 4, 5, 6, 7]
        self.mmi = 0
        self.tpi = 0
        self.evi = 0

        def evac(bank_i, src_ap, dst_ap, writes):
            eng = "act" if self.evi % 2 == 0 else "dve"
            self.evi += 1
            if eng == "act":
                return self.op("act", lambda a: a.copy(out=dst_ap, in_=src_ap), reads=[self.bank_s[bank_i]], writes=writes)
            return self.op("dve", lambda v: v.tensor_copy(out=dst_ap, in_=src_ap), reads=[self.bank_s[bank_i]], writes=writes)

        def project(lhs_chunks, rhs_fn, ncols, dst, dst_slots, reads):
            n0 = 0
            i = 0
            while n0 < ncols:
                w = min(512, ncols - n0)
                bi = MMB[self.mmi % len(MMB)]
                self.mmi += 1
                K = len(lhs_chunks)
                mms = [(self.banks[bi][:, 0:w], lhs_chunks[k], rhs_fn(k, n0, w), k == 0, k == K - 1) for k in range(K)]
                self.mm_group(bi, mms, reads=reads)
                evac(bi, self.banks[bi][:, 0:w], dst[:, n0:n0 + w], [dst_slots[i]])
                n0 += w
                i += 1

        def transposes(src_ap, ncol_blocks, src_slots, dst_ap, dst_writes):
            bi = TPB[self.tpi % 2]
            self.tpi += 1
            bv = self.bank_bf(bi)
            trs = [(bv[:, k * 128:(k + 1) * 128], src_ap[:, k * 128:(k + 1) * 128]) for k in range(ncol_blocks)]
            self.tr_group(bi, trs, self.ident_b[:], reads=src_slots)
            src = bv[:, 0:ncol_blocks * 128]
            if len(dst_ap.shape) == 3:
                src = src.rearrange("p (k n) -> p k n", n=128)
            return evac(bi, src, dst_ap, dst_writes)

        def stA(t):
            b = t % 2
            b3 = t % 3
            rows = slice(t * 128, (t + 1) * 128)
            sm = smA[b]
            self.dma("sp", xt[b][:], self.xr[rows, :], xt_d[b], writes=[xt_s[b]])
            self.dma("sp", csd[b3][:], self.cs_d[rows, :], cs_d_[b3], writes=[csd_s[b3]])
            self.dma("sp", csm[b3][:], self.cs_m[rows, :], cs_m_[b3], writes=[csm_s[b3]])
            self.op("act", lambda a: a.activation(out=self.junk[:], in_=xt[b][:], func=AF.Square, accum_out=sm[:, 0:1]),
                    reads=[xt_s[b]], writes=[ss_s[b]])
            self.rstd(sm[:, 0:1], ss_s[b], sm[:, 2:3], rs_s[b], sm[:, 1:2], lg_s[b], 1.0 / D)
            self.op("dve", lambda v: v.tensor_scalar(out=hb[b][:], in0=xt[b][:], scalar1=sm[:, 2:3], scalar2=None,
                                                     op0=ALU.mult), reads=[xt_s[b], rs_s[b]], writes=[hb_s[b]])
            transposes(hb[b], 8, [hb_s[b]], hT[b][:], [hT_s[b]])
            project([hT[b][:, k * 128:(k + 1) * 128] for k in range(8)],
                    lambda k, n0, w: win[:, k, n0:n0 + w], INC, pj[b], pj_s[b], [hT_s[b], w_s])
            self.op("act", lambda a: a.copy(out=vb[b3][:], in_=pj[b][:, 1024:1536]), reads=[pj_s[b][2]], writes=[vb_s[b3]])

        def stB(t):
            b = t % 2
            b3 = t % 3
            rows = slice(t * 128, (t + 1) * 128)
            sm = smB[b]
            self.dma("sp", self.vd[rows, :], vb[b3][:], vb_d[b3], reads=[vb_s[b3]])
            src3 = pj[b][:, 0:1024].rearrange("p (g w) -> p g w", w=64)
            dA3 = dA[b][:].rearrange("p (g w) -> p g w", w=64)
            self.normrope(W_d, src3, [pj_s[b][0], pj_s[b][1]], 16, 64, gd3, 16, 0, csd[b3], csd_s[b3],
                          dA3[:, :, 16:64], dA3[:, :, 0:16], dA_s[b], 16, 64)
            self.op("act", lambda a: a.activation(out=self.junk[:, 0:256], in_=pj[b][:, 1536:1792], func=AF.Square,
                                                  accum_out=sm[:, 0:1]), reads=[pj_s[b][3]], writes=[ssc_s[b]])
            self.op("act", lambda a: a.activation(out=self.junk[:, 0:128], in_=pj[b][:, 1792:1920], func=AF.Square,
                                                  accum_out=sm[:, 1:2]), reads=[pj_s[b][3]], writes=[ssc_s[b]])
            self.op("act", lambda a: a.activation(out=sm[:, 2:3], in_=sm[:, 0:1], func=AF.Ln, scale=1.0 / 256, bias=float(EPS)),
                    reads=[ssc_s[b]], writes=[lgc_s[b]])
            self.op("act", lambda a: a.activation(out=sm[:, 3:4], in_=sm[:, 1:2], func=AF.Ln, scale=1.0 / 128, bias=float(EPS)),
                    reads=[ssc_s[b]], writes=[lgc_s[b]])
            self.op("act", lambda a: a.activation(out=sm[:, 4:6], in_=sm[:, 2:4], func=AF.Exp, scale=-0.5),
                    reads=[lgc_s[b]], writes=[rsc_s[b]])
            self.op("dve", lambda v: v.tensor_scalar(out=cn[b][:, 0:256], in0=pj[b][:, 1536:1792], scalar1=sm[:, 4:5],
                                                     scalar2=None, op0=ALU.mult), reads=[pj_s[b][3], rsc_s[b]], writes=[cn_s[b]])
            self.op("dve", lambda v: v.tensor_scalar(out=cn[b][:, 256:384], in0=pj[b][:, 1792:1920], scalar1=sm[:, 5:6],
                                                     scalar2=None, op0=ALU.mult), reads=[pj_s[b][3], rsc_s[b]],
                    writes=[cn_s[b]])
            transposes(cn[b], 3, [cn_s[b]], cT[b][:], [cT_s[b]])
            project([cT[b][:, 0:128], cT[b][:, 128:256]], lambda k, n0, w: wuq[:, k, n0:n0 + w], 768, qm[b], qm_s[b],
                    [cT_s[b], w_s])
            project([cT[b][:, 256:384]], lambda k, n0, w: wukv[:, n0:n0 + w], 1024, kvm[b], kvm_s[b], [cT_s[b], w_s])
            kvm4 = kvm[b][:].rearrange("p (h c) -> p h c", c=256)
            kf3 = kf[b][:].rearrange("p (h c) -> p h c", c=192)
            self.op("pool", lambda p: p.tensor_copy(out=kf3[:, :, 0:128], in_=kvm4[:, :, 0:128]), reads=kvm_s[b],
                    writes=[kf_s[b]])
            self.op("pool", lambda p: p.tensor_copy(out=kf3[:, :, 128:192],
                                                    in_=pj[b][:, 1920:1984].unsqueeze(1).broadcast_to([128, 4, 64])),
                    reads=[pj_s[b][3]], writes=[kf_s[b]])
            self.op("act", lambda a: a.copy(out=vmb[b3][:].rearrange("p (h c) -> p h c", c=128), in_=kvm4[:, :, 128:256]),
                    reads=kvm_s[b], writes=[vmb_s[b3]])

        def stC(t):
            b = t % 2
            b3 = t % 3
            rows = slice(t * 128, (t + 1) * 128)
            self.dma("sp", self.vm[rows, :], vmb[b3][:], vmb_d[b3], reads=[vmb_s[b3]])
            qm3 = qm[b][:].rearrange("p (h c) -> p h c", c=192)
            kf3 = kf[b][:].rearrange("p (h c) -> p h c", c=192)
            mQn = mQ[b][:, 0:512].rearrange("p (h c) -> p h c", c=128)
            mQr = mQ[b][:, 512:768].rearrange("p (h c) -> p h c", c=64)
            self.normrope(W_q, qm3, qm_s[b], 4, 192, gq3, 64, 128, csm[b3], csm_s[b3], mQn, mQr, mQ_s[b], 0, 128)
            mKn = mK[b][:, 0:512].rearrange("p (h c) -> p h c", c=128)
            mKr = mK[b][:, 512:768].rearrange("p (h c) -> p h c", c=64)
            self.normrope(W_k, kf3, [kf_s[b]], 4, 192, gk3, 64, 128, csm[b3], csm_s[b3], mKn, mKr, mK_s[b], 0, 128)
            pr = t // 2
            sg = pr % 2
            tl = t % 2
            stg = stage[sg]
            cols = slice(tl * 128, (tl + 1) * 128)
            if tl == 0:
                old = stage_s[sg].rd + stage_s[sg].wr
                self.wait("act", old)
                self.wait("dve", old)
                stage_s[sg].wr = []
                stage_s[sg].rd = []
            toks = []
            toks.append(transposes(dA[b], 8, [dA_s[b]], stg[:, 0:8, cols], []))
            toks.append(transposes(mQ[b], 6, [mQ_s[b]], stg[:, 8:14, cols], []))
            toks.append(transposes(mK[b], 6, [mK_s[b]], stg[:, 14:20, cols], []))
            stage_s[sg].wr.extend(toks)
            if tl == 1:
                self.dma("sp", self.qkt[:, :, pr * 256:(pr + 1) * 256].rearrange("k p n -> p k n"), stg[:], st_d[sg],
                         reads=[stage_s[sg]])

        for i in range(NT + 2):
            if i < NT:
                stA(i)
            if 0 <= i - 1 < NT:
                stB(i - 1)
            if 0 <= i - 2 < NT:
                stC(i - 2)
            self.pump(2)
        self.end_phase()

    def phase2(self, l):
        self.begin_phase()
        nc = self.nc
        sb = self.sb
        qt = [sb("qt", [128, S], BF16) for _ in range(2)]
        kt = [sb("kt", [128, S], BF16) for _ in range(2)]
        qr = [sb("qr", [128, S], BF16) for _ in range(2)]
        kr = [sb("kr", [128, S], BF16) for _ in range(2)]
        vs = [sb("vs", [128, NT, 129], BF16) for _ in range(2)]
        ld_s = [Slot() for _ in range(2)]
        ld_d = [self.dsem() for _ in range(2)]
        NP = 4
        pt = [sb("pt", [128, 512], BF16) for _ in range(NP)]
        pt_s = [Slot() for _ in range(NP)]
        o1 = [sb("o1", [128, 4, 128], F32) for _ in range(2)]
        o1_s = [Slot() for _ in range(2)]
        rc = [sb("rc", [128, 8], F32) for _ in range(4)]
        rc_s = [Slot() for _ in range(4)]
        ocs = [sb("ocs", [128, 4, 128], BF16) for _ in range(2)]
        ocs_s = [Slot() for _ in range(2)]
        ocs_d = [self.dsem() for _ in range(2)]
        for i in range(2):
            self.op("dve", lambda v: v.memset(vs[i][:, :, 128:129], 1.0), writes=[ld_s[i]])
        SB_ = [0, 1, 2]
        OB_ = [4, 5, 6, 7]
        qkt = self.qkt
        heads = [("d", h) for h in range(4)] + [("m", h) for h in range(4)]
        self.gi = 0
        self.oci = 0
        self.rci = 0

        def load(hi):
            typ, h = heads[hi]
            i = hi % 2
            s = ld_s[i]
            self._deps("sp", [], [s])
            if typ == "d":
                self.dma("sp", qt[i][:], qkt[h], ld_d[i])
                self.dma("sp", kt[i][:], qkt[4 + h], ld_d[i])
                vsrc = self.vd
            else:
                o = (h % 2) * 64
                self.dma("sp", qt[i][:], qkt[8 + h], ld_d[i])
                self.dma("sp", kt[i][:], qkt[14 + h], ld_d[i])
                self.dma("sp", qr[i][o:o + 64, :], qkt[12 + h // 2, o:o + 64, :], ld_d[i])
                self.dma("sp", kr[i][o:o + 64, :], qkt[18 + h // 2, o:o + 64, :], ld_d[i])
                vsrc = self.vm
            tok = self.dma("sp", vs[i][:, :, 0:128],
                           vsrc.rearrange("(j p) c -> p j c", p=128)[:, :, h * 128:(h + 1) * 128], ld_d[i])
            s.wr = [tok]
            s.rd = []

        load(0)
        for hi, (typ, h) in enumerate(heads):
            if hi + 1 < len(heads):
                load(hi + 1)
            i = hi % 2
            lds = ld_s[i]
            if typ == "d":
                maps = [[(kt[i][0:64, :], qt[i][0:64, :])], [(kt[i][64:128, :], qt[i][64:128, :])]]
                scale = 64.0 ** -0.5
                negM = self.negM[:, l, 0:1]
                col = h
            else:
                o = (h % 2) * 64
                maps = [[(kt[i][:, :], qt[i][:, :]), (kr[i][o:o + 64, :], qr[i][o:o + 64, :])]]
                scale = 192.0 ** -0.5
                negM = self.negM[:, l, 1:2]
                col = 4 + h
            steps = []
            for Qb in range(8):
                for m in range(len(maps)):
                    for j in range(4 * Qb + 4):
                        steps.append((Qb, m, j))
            n = len(steps)

            def qk(si):
                Qb, m, j = steps[si]
                r = max(0, j - 4 * Qb)
                N = (4 - r) * 128
                bi = SB_[si % 3]
                parts = maps[m]
                q0 = Qb * 512 + r * 128
                mms = [(self.banks[bi][:, 0:N], kp[:, j * 128:(j + 1) * 128], qp[:, q0:q0 + N], pi == 0, pi == len(parts) - 1)
                       for pi, (kp, qp) in enumerate(parts)]
                self.mm_group(bi, mms, reads=[lds])

            qk(0)
            if n > 1:
                qk(1)
            for si in range(n):
                Qb, m, j = steps[si]
                if si % 6 == 0:
                    self.pump(1, engs=("dve", "pool"))
                if si + 2 < n:
                    qk(si + 2)
                r = max(0, j - 4 * Qb)
                N = (4 - r) * 128
                bi = SB_[si % 3]
                pi_ = si % NP
                first_of_group = (j == 0)
                if first_of_group:
                    g = self.gi
                    self.gi += 1
                    self.cur_ob = OB_[2 * (g % 2)], OB_[2 * (g % 2) + 1]
                ob = self.cur_ob
                self.op("act", lambda a: a.activation(out=pt[pi_][:, 0:N], in_=self.banks[bi][:, 0:N], func=AF.Exp,
                                                      scale=float(scale), bias=negM),
                        reads=[self.bank_s[bi]], writes=[pt_s[pi_]])
                if j >= 4 * Qb:
                    self.op("pool", lambda p: p.affine_select(out=pt[pi_][:, 0:128], in_=pt[pi_][:, 0:128], pattern=[[1, 128]],
                                                              compare_op=ALU.is_ge, fill=0.0, base=0, channel_multiplier=-1),
                            reads=[], writes=[pt_s[pi_]])
                self._deps("pe", [pt_s[pi_], lds], [])
                last_tok = None
                for tq in range(r, 4):
                    gt = 4 * Qb + tq
                    obk = ob[tq // 2]
                    oc0 = (tq % 2) * 256
                    if j == 0:
                        self._deps("pe", [], [self.bank_s[obk]])
                    ins = nc.tensor.matmul(self.banks[obk][:, oc0:oc0 + 129],
                                           lhsT=pt[pi_][:, (tq - r) * 128:(tq - r + 1) * 128], rhs=vs[i][:, j, :],
                                           start=(j == 0 and tq % 2 == 0), stop=(j == gt), skip_group_check=True)
                tok = self.sig("pe", ins)
                pt_s[pi_].rd.append(tok)
                lds.rd.append(tok)
                if j == 0:
                    for bk in ob:
                        self.bank_s[bk].wr = [tok]
                        self.bank_s[bk].rd = []
                else:
                    for bk in ob:
                        self.bank_s[bk].wr = [tok]
                if j == 4 * Qb + 3:
                    ri = self.rci % 4
                    self.rci += 1
                    for k2 in range(2):
                        bview = self.banks[ob[k2]][:].rearrange("p (a b) -> p a b", b=256)
                        self.op("dve", lambda v: v.reciprocal(out=rc[ri][:, 2 * k2:2 * k2 + 2].unsqueeze(2), in_=bview[:, :, 128:129]),
                                reads=[self.bank_s[ob[k2]]], writes=[rc_s[ri]])
                    if typ == "d" and m == 0:
                        oi = Qb % 2
                        for tq in range(4):
                            self.op("dve", lambda v: v.tensor_scalar(
                                out=o1[oi][:, tq, :], in0=self.banks[ob[tq // 2]][:, (tq % 2) * 256:(tq % 2) * 256 + 128],
                                scalar1=rc[ri][:, tq:tq + 1], scalar2=None, op0=ALU.mult),
                                reads=[self.bank_s[ob[tq // 2]], rc_s[ri]], writes=[o1_s[oi]] if tq == 0 else [])
                        o1_s[oi].wr = [(self.csem["dve"], self.ccnt["dve"])]
                    else:
                        ci = self.oci % 2
                        self.oci += 1
                        if typ == "d":
                            oi = Qb % 2
                            self.op("dve", lambda v: v.tensor_scalar(out=rc[ri][:, 4:8], in0=rc[ri][:, 0:4],
                                                                     scalar1=self.neglam[:, l:l + 1], scalar2=None,
                                                                     op0=ALU.mult), reads=[rc_s[ri]], writes=[rc_s[ri]])
                        for tq in range(4):
                            src = self.banks[ob[tq // 2]][:, (tq % 2) * 256:(tq % 2) * 256 + 128]
                            if typ == "d":
                                self.op("dve", lambda v: v.scalar_tensor_tensor(
                                    out=ocs[ci][:, tq, :], in0=src, scalar=rc[ri][:, 4 + tq:5 + tq], in1=o1[oi][:, tq, :],
                                    op0=ALU.mult, op1=ALU.add),
                                    reads=[self.bank_s[ob[tq // 2]], rc_s[ri], o1_s[oi]], writes=[ocs_s[ci]] if tq == 0 else [])
                            else:
                                self.op("dve", lambda v: v.tensor_scalar(out=ocs[ci][:, tq, :], in0=src,
                                                                         scalar1=rc[ri][:, tq:tq + 1], scalar2=None,
                                                                         op0=ALU.mult),
                                        reads=[self.bank_s[ob[tq // 2]], rc_s[ri]], writes=[ocs_s[ci]] if tq == 0 else [])
                        ocs_s[ci].wr = [(self.csem["dve"], self.ccnt["dve"])]
                        self.dma("sp", self.ocat.rearrange("(j p) c -> p j c", p=128)[:, Qb * 4:Qb * 4 + 4,
                                                                                      col * 128:(col + 1) * 128],
                                 ocs[ci][:], ocs_d[ci], reads=[ocs_s[ci]])
        self.end_phase()

    def phase3a(self, l):
        self.begin_phase()
        self.pump_until("wo%d" % l)
        nc = self.nc
        sb = self.sb
        moe = (l % 2 == 1)
        wo = sb("wo", [128, 8, D], BF16)
        w_s = Slot()
        d0 = self.dsem()
        w_s.wr = [self.dma("sp", wo[:], self.b_wo[l].rearrange("(c p) n -> p c n", p=128), d0)]

        def bufs(name, shape, dt, n):
            return [sb(name, shape, dt) for _ in range(n)], [Slot() for _ in range(n)]

        oc, oc_s = bufs("oc", [128, D], BF16, 2)
        xt, xt_s = bufs("xt", [128, D], F32, 2)
        sq, sq_s = bufs("sq", [128, 512], F32, 2)
        smA = [sb("smA", [128, 12], F32) for _ in range(2)]
        ss_s, lg_s, rs_s = [Slot(), Slot()], [Slot(), Slot()], [Slot(), Slot()]
        ocn, ocn_s = bufs("ocn", [128, 512], BF16, 2)
        ocT, ocT_s = bufs("ocT", [128, D], BF16, 2)
        x1 = [sb("x1", [128, D], F32) for _ in range(3)]
        x1_s = [[Slot(), Slot()] for _ in range(3)]
        smB = [sb("smB", [128, 4], F32) for _ in range(2)]
        ss2_s, lg2_s, rs2_s = [Slot(), Slot()], [Slot(), Slot()], [Slot(), Slot()]
        h2, h2_s = bufs("h2", [128, D], BF16, 2)
        stage = [sb("stage", [128, 8, 512], BF16) for _ in range(2)]
        stage_s = [Slot(), Slot()]
        if moe:
            h2f, h2f_s = bufs("h2f", [128, D], F32, 2)
            h2fT = [sb("h2fT", [128, D], F32) for _ in range(2)]
            h2fT_s = [[Slot(), Slot()] for _ in range(2)]
            rt = [sb("rt", [128, 48], F32) for _ in range(2)]
            rt_s = [[Slot() for _ in range(8)] for _ in range(2)]
        oc_d = [self.dsem() for _ in range(2)]
        xt_d = [self.dsem() for _ in range(2)]
        x1_d = [self.dsem() for _ in range(3)]
        st_d = [self.dsem() for _ in range(2)]
        TPB = [0, 1]
        MMB = [2, 3]
        FTB = [4, 5]
        LGB = [6, 7]
        self.tpi = 0
        self.mmi = 0
        self.lgi = 0

        def stA(t):
            b = t % 2
            b3 = t % 3
            rows = slice(t * 128, (t + 1) * 128)
            self.dma("sp", oc[b][:], self.ocat[rows, :], oc_d[b], writes=[oc_s[b]])
            self.dma("sp", xt[b][:], self.xr[rows, :], xt_d[b], writes=[xt_s[b]])
            oc3 = oc[b][:, 0:512].rearrange("p (g w) -> p g w", w=128)
            sq3 = sq[b][:].rearrange("p (g w) -> p g w", w=128)
            sm = smA[b]
            self.op("act", lambda a: a.activation(out=sq[b][:], in_=oc[b][:, 0:512], func=AF.Square), reads=[oc_s[b]],
                    writes=[sq_s[b]])
            self.op("dve", lambda v: v.tensor_reduce(out=sm[:, 0:4], in_=sq3, axis=AX.X, op=ALU.add), reads=[sq_s[b]],
                    writes=[ss_s[b]])
            self.rstd(sm[:, 0:4], ss_s[b], sm[:, 8:12], rs_s[b], sm[:, 4:8], lg_s[b], 1.0 / 128)
            self.op("dve", lambda v: v.tensor_tensor(out=ocn[b][:].rearrange("p (g w) -> p g w", w=128), in0=oc3,
                                                     in1=sm[:, 8:12].unsqueeze(2).broadcast_to([128, 4, 128]), op=ALU.mult),
                    reads=[oc_s[b], rs_s[b]], writes=[ocn_s[b]])
            bi = TPB[self.tpi % 2]
            self.tpi += 1
            bv = self.bank_bf(bi)
            trs = [(bv[:, k * 128:(k + 1) * 128], ocn[b][:, k * 128:(k + 1) * 128]) for k in range(4)]
            trs += [(bv[:, k * 128:(k + 1) * 128], oc[b][:, k * 128:(k + 1) * 128]) for k in range(4, 8)]
            self.tr_group(bi, trs, self.ident_b[:], reads=[ocn_s[b], oc_s[b]])
            self.op("act", lambda a: a.copy(out=ocT[b][:], in_=bv[:, :]), reads=[self.bank_s[bi]], writes=[ocT_s[b]])
            for hf in range(2):
                mb = MMB[self.mmi % 2]
                self.mmi += 1
                mms = [(self.banks[mb][:, :], ocT[b][:, k * 128:(k + 1) * 128], wo[:, k, hf * 512:(hf + 1) * 512], k == 0, k == 7)
                       for k in range(8)]
                self.mm_group(mb, mms, reads=[ocT_s[b], w_s])
                self.op("dve", lambda v: v.tensor_tensor(out=x1[b3][:, hf * 512:(hf + 1) * 512], in0=self.banks[mb][:, :],
                                                         in1=xt[b][:, hf * 512:(hf + 1) * 512], op=ALU.add),
                        reads=[self.bank_s[mb], xt_s[b]], writes=[x1_s[b3][hf]])

        def stB(t):
            b = t % 2
            b3 = t % 3
            blk, tl = t // 4, t % 4
            sg = blk % 2
            rows = slice(t * 128, (t + 1) * 128)
            sm = smB[b]
            self.dma("sp", self.xr[rows, :], x1[b3][:], x1_d[b3], reads=x1_s[b3])
            self.op("act", lambda a: a.activation(out=self.junk[:], in_=x1[b3][:], func=AF.Square, accum_out=sm[:, 0:1]),
                    reads=x1_s[b3], writes=[ss2_s[b]])
            self.rstd(sm[:, 0:1], ss2_s[b], sm[:, 2:3], rs2_s[b], sm[:, 1:2], lg2_s[b], 1.0 / D)
            self.op("dve", lambda v: v.tensor_scalar(out=h2[b][:], in0=x1[b3][:], scalar1=sm[:, 2:3], scalar2=None,
                                                     op0=ALU.mult), reads=x1_s[b3] + [rs2_s[b]], writes=[h2_s[b]])
            bi = TPB[self.tpi % 2]
            self.tpi += 1
            bv = self.bank_bf(bi)
            self.tr_group(bi, [(bv[:, k * 128:(k + 1) * 128], h2[b][:, k * 128:(k + 1) * 128]) for k in range(8)],
                          self.ident_b[:], reads=[h2_s[b]])
            stg = stage[sg]
            if tl == 0:
                self.wait("act", stage_s[sg].rd + stage_s[sg].wr)
                stage_s[sg].wr = []
                stage_s[sg].rd = []
            tok = self.op("act", lambda a: a.copy(out=stg[:, :, tl * 128:(tl + 1) * 128],
                                                  in_=bv[:, :].rearrange("p (k n) -> p k n", n=128)),
                          reads=[self.bank_s[bi]], writes=[])
            stage_s[sg].wr.append(tok)
            if tl == 3:
                self.dma("sp", self.h2t[:, :, blk * 512:(blk + 1) * 512].rearrange("k p n -> p k n"), stg[:], st_d[sg],
                         reads=[stage_s[sg]])
            if moe:
                self.op("pool", lambda p: p.tensor_scalar(out=h2f[b][:], in0=x1[b3][:], scalar1=sm[:, 2:3], scalar2=None,
                                                          op0=ALU.mult), reads=x1_s[b3] + [rs2_s[b]], writes=[h2f_s[b]])
                for hf in range(2):
                    fb = FTB[hf]
                    self.tr_group(fb, [(self.banks[fb][:, k * 128:(k + 1) * 128],
                                        h2f[b][:, (hf * 4 + k) * 128:(hf * 4 + k + 1) * 128]) for k in range(4)],
                                  self.ident_f[:], reads=[h2f_s[b]])
                    self.op("act", lambda a: a.copy(out=h2fT[b][:, hf * 512:(hf + 1) * 512], in_=self.banks[fb][:, :]),
                            reads=[self.bank_s[fb]], writes=[h2fT_s[b][hf]])
                lb = LGB[self.lgi % 2]
                self.lgi += 1
                mms = [(self.banks[lb][:, 0:8], h2fT[b][:, k * 128:(k + 1) * 128], self.rw_s[:, k, :], k == 0, k == 7)
                       for k in range(8)]
                self.mm_group(lb, mms, reads=h2fT_s[b])
                r = rt[b]
                R = rt_s[b]
                self.op("dve", lambda v: v.tensor_copy(out=r[:, 0:8], in_=self.banks[lb][:, 0:8]), reads=[self.bank_s[lb]],
                        writes=[R[0]])
                self.op("dve", lambda v: v.max(out=r[:, 8:16], in_=r[:, 0:8]), reads=[R[0]], writes=[R[1]])
                self.op("dve", lambda v: v.tensor_scalar(out=r[:, 16:24], in0=r[:, 0:8], scalar1=r[:, 8:9], scalar2=None,
                                                         op0=ALU.subtract), reads=[R[0], R[1]], writes=[R[2]])
                self.op("act", lambda a: a.activation(out=r[:, 24:32], in_=r[:, 16:24], func=AF.Exp), reads=[R[2]], writes=[R[3]])
                self.op("dve", lambda v: v.scalar_tensor_tensor(out=r[:, 32:40], in0=r[:, 0:8], scalar=r[:, 9:10], in1=r[:, 24:32],
                                                                op0=ALU.is_ge, op1=ALU.mult), reads=[R[0], R[1], R[3]],
                        writes=[R[4]])
                self.op("dve", lambda v: v.tensor_reduce(out=r[:, 40:41], in_=r[:, 32:40], axis=AX.X, op=ALU.add), reads=[R[4]],
                        writes=[R[5]])
                self.op("dve", lambda v: v.reciprocal(out=r[:, 41:42], in_=r[:, 40:41]), reads=[R[5]], writes=[R[6]])
                self.op("dve", lambda v: v.tensor_scalar(out=self.comb[:, t, :], in0=r[:, 32:40], scalar1=r[:, 41:42], scalar2=None,
                                                         op0=ALU.mult), reads=[R[4], R[6]], writes=[R[7]])

        for i in range(NT + 1):
            if i < NT:
                stA(i)
            if 0 <= i - 1 < NT:
                stB(i - 1)
            self.pump(2)
        self.end_phase()

    def phase3b(self, l):
        self.begin_phase()
        self.pump_until("ffn%d" % l)
        nc = self.nc
        sb = self.sb
        moe = (l % 2 == 1)
        NE = 8 if moe else 2
        wg = [sb("wg", [128, 8, FFE], BF16) for _ in range(2)]
        wu = [sb("wu", [128, 8, FFE], BF16) for _ in range(2)]
        wd = sb("wd", [128, NCH, D], BF16)
        wgu_s = [Slot() for _ in range(2)]
        wd_s = Slot()
        wgu_d = [self.dsem() for _ in range(2)]
        wd_d = self.dsem()
        hTb = [sb("hTb", [128, 8, 512], BF16) for _ in range(2)]
        hTb_s = [Slot() for _ in range(2)]
        hTb_d = [self.dsem() for _ in range(2)]
        aT = [sb("aT", [128, NCH, 512], BF16) for _ in range(2)]
        aT_s = [Slot() for _ in range(2)]
        NSG = 3
        sgb = [sb("sgb", [128, 512], F32) for _ in range(NSG)]
        sgb_s = [Slot() for _ in range(NSG)]
        NOS = 2
        ost = [sb("ost", [128, D], F32) for _ in range(NOS)]
        ost_s = [Slot() for _ in range(NOS)]
        ost_d = [self.dsem() for _ in range(NOS)]
        xrow_s = [Slot() for _ in range(NT)]
        GB = [0, 1]
        UB = [2, 3]
        DB = [4, 5]
        self.gi = 0
        self.di = 0
        self.oi = 0
        self.si = 0
        self.evi = 0

        def load_gu(e):
            i = e % 2
            self._deps("sp", [], [wgu_s[i]])
            if moe:
                gsrc = self.b_mwg[e].rearrange("(c p) n -> p c n", p=128)
                usrc = self.b_mwu[e].rearrange("(c p) n -> p c n", p=128)
            else:
                gsrc = self.b_dwg[:, e * FFE:(e + 1) * FFE].rearrange("(c p) n -> p c n", p=128)
                usrc = self.b_dwu[:, e * FFE:(e + 1) * FFE].rearrange("(c p) n -> p c n", p=128)
            self.dma("sp", wg[i][:], gsrc, wgu_d[i])
            tok = self.dma("sp", wu[i][:], usrc, wgu_d[i])
            wgu_s[i].wr = [tok]
            wgu_s[i].rd = []

        def load_d(e):
            if moe:
                dsrc = self.b_mwd[e].rearrange("(c p) n -> p c n", p=128)
            else:
                dsrc = self.b_dwd[e * FFE:(e + 1) * FFE, :].rearrange("(c p) n -> p c n", p=128)
            self.dma("sp", wd[:], dsrc, wd_d, writes=[wd_s])

        def load_h(bi_):
            i = bi_ % 2
            blk = bi_ % 8
            self.dma("sp", hTb[i][:], self.h2t[:, :, blk * 512:(blk + 1) * 512].rearrange("k p n -> p k n"), hTb_d[i],
                     writes=[hTb_s[i]])

        def gate_up(e, blk, bi_):
            i = e % 2
            hi = bi_ % 2
            ai = bi_ % 2
            for c in range(NCH):
                gb = GB[self.gi % 2]
                ub = UB[self.gi % 2]
                self.gi += 1
                mms = [(self.banks[gb][:, :], wg[i][:, k, c * 128:(c + 1) * 128], hTb[hi][:, k, :], k == 0, k == 7) for k in range(8)]
                self.mm_group(gb, mms, reads=[wgu_s[i], hTb_s[hi]])
                mms = [(self.banks[ub][:, :], wu[i][:, k, c * 128:(c + 1) * 128], hTb[hi][:, k, :], k == 0, k == 7) for k in range(8)]
                self.mm_group(ub, mms, reads=[wgu_s[i], hTb_s[hi]])
                si_ = self.si % NSG
                self.si += 1
                self.op("act", lambda a: a.activation(out=sgb[si_][:], in_=self.banks[gb][:, :], func=AF.Silu),
                        reads=[self.bank_s[gb]], writes=[sgb_s[si_]])
                tok = self.op("dve", lambda v: v.tensor_tensor(out=aT[ai][:, c, :], in0=sgb[si_][:], in1=self.banks[ub][:, :],
                                                               op=ALU.mult),
                              reads=[sgb_s[si_], self.bank_s[ub]], writes=[aT_s[ai]] if c == 0 else [])
                if c > 0:
                    aT_s[ai].wr.append(tok)

        def down(e, blk, bi_):
            ai = bi_ % 2
            for tq in range(4):
                t = blk * 4 + tq
                oi_ = self.oi % NOS
                self.oi += 1
                rdt = ost_s[oi_].rd + ost_s[oi_].wr
                self.wait("act", rdt)
                self.wait("dve", rdt)
                ost_s[oi_].rd = []
                ost_s[oi_].wr = []
                for hf in range(2):
                    db = DB[self.di % 2]
                    self.di += 1
                    mms = [(self.banks[db][:, :], aT[ai][:, c, tq * 128:(tq + 1) * 128], wd[:, c, hf * 512:(hf + 1) * 512],
                            c == 0, c == NCH - 1) for c in range(NCH)]
                    self.mm_group(db, mms, reads=[aT_s[ai], wd_s])
                    eng = "act" if self.evi % 2 == 0 else "dve"
                    self.evi += 1
                    dst = ost[oi_][:, hf * 512:(hf + 1) * 512]
                    wr = []
                    if moe:
                        sc = self.comb[:, t, e:e + 1]
                        if eng == "act":
                            tok = self.op("act", lambda a: a.activation(out=dst, in_=self.banks[db][:, :], func=AF.Copy, scale=sc),
                                          reads=[self.bank_s[db]], writes=wr)
                        else:
                            tok = self.op("dve", lambda v: v.tensor_scalar(out=dst, in0=self.banks[db][:, :], scalar1=sc,
                                                                           scalar2=None, op0=ALU.mult),
                                          reads=[self.bank_s[db]], writes=wr)
                    else:
                        if eng == "act":
                            tok = self.op("act", lambda a: a.copy(out=dst, in_=self.banks[db][:, :]), reads=[self.bank_s[db]],
                                          writes=wr)
                        else:
                            tok = self.op("dve", lambda v: v.tensor_copy(out=dst, in_=self.banks[db][:, :]),
                                          reads=[self.bank_s[db]], writes=wr)
                    ost_s[oi_].wr.append(tok)
                self.dma("pool", self.xr[t * 128:(t + 1) * 128, :], ost[oi_][:], ost_d[oi_], reads=[ost_s[oi_]],
                         writes=[xrow_s[t]], accum_op=ALU.add)

        seq = [(e, blk) for e in range(NE) for blk in range(8)]
        load_gu(0)
        load_d(0)
        load_h(0)
        cur_wd = 0
        for bi_, (e, blk) in enumerate(seq):
            if bi_ + 1 < len(seq):
                load_h(bi_ + 1)
            if blk == 0 and e + 1 < NE:
                load_gu(e + 1)
            if bi_ == 0:
                gate_up(e, blk, bi_)
            if bi_ + 1 < len(seq):
                gate_up(seq[bi_ + 1][0], seq[bi_ + 1][1], bi_ + 1)
            if e != cur_wd:
                load_d(e)
                cur_wd = e
            down(e, blk, bi_)
        self.end_phase()


def _rope_table(theta, rot):
    half = rot // 2
    inv_freq = (1.0 / (np.float32(theta) ** (np.arange(half, dtype=np.float32) * np.float32(2.0 / rot)))).astype(np.float32)
    ang = (np.arange(S, dtype=np.float32)[:, None] * inv_freq[None, :]).astype(np.float32)
    c = np.cos(ang.astype(np.float64)).astype(np.float32)
    s = np.sin(ang.astype(np.float64)).astype(np.float32)
    return np.ascontiguousarray(np.concatenate([c, c, -s, s], axis=1))


_CACHE = {}


def _get_nc(**kw):
    key = tuple(sorted(kw.items()))
    if key not in _CACHE:
        kb = KB(**kw)
        kb.build()
        _CACHE[key] = kb
    return _CACHE[key]


def make_in_maps(inputs, ncores=8):
    f = lambda a: np.ascontiguousarray(np.asarray(a, dtype=np.float32))
    shared = {
        "attn_norm_g": f(inputs["attn_norm_g"]),
        "w_in": f(inputs["w_in"]),
        "diff_q_norm_g": f(inputs["diff_q_norm_g"]),
        "diff_k_norm_g": f(inputs["diff_k_norm_g"]),
        "diff_lambda": f(inputs["diff_lambda"]).reshape(DEPTH, 256),
        "diff_subln_g": f(inputs["diff_subln_g"]),
        "mla_q_ln_g": f(inputs["mla_q_ln_g"]),
        "w_uq": f(inputs["w_uq"]),
        "mla_kv_ln_g": f(inputs["mla_kv_ln_g"]),
        "w_ukv": f(inputs["w_ukv"]),
        "mla_qk_norm_g": f(inputs["mla_qk_norm_g"]).reshape(DEPTH, 384),
        "w_o": f(inputs["w_o"]),
        "ffn_norm_g": f(inputs["ffn_norm_g"]),
        "dense_w_gate": f(inputs["dense_w_gate"])[0],
        "dense_w_up": f(inputs["dense_w_up"])[0],
        "dense_w_down": f(inputs["dense_w_down"])[0],
        "router_w": f(inputs["router_w"])[0],
        "moe_w_gate": f(inputs["moe_w_gate"])[0],
        "moe_w_up": f(inputs["moe_w_up"])[0],
        "moe_w_down": f(inputs["moe_w_down"])[0],
        "cs_d": _rope_table(ROPE_THETA, 16),
        "cs_m": _rope_table(MLA_ROPE_THETA, 64),
        "ident": np.eye(128, dtype=np.float32),
    }
    x = f(inputs["x"])
    maps = []
    for c in range(ncores):
        m = dict(shared)
        m["x"] = np.ascontiguousarray(x[c])
        maps.append(m)
    return maps


def kernel(**inputs):
    kb = _get_nc()
    in_maps = make_in_maps(inputs)
    res = run_bass_kernel_spmd(kb.nc, in_maps, core_ids=list(range(8)))
    out = np.stack([np.asarray(r["y"], dtype=np.float32) for r in res.results], axis=0)
    return out
```

```python
import math
from contextlib import ExitStack
import numpy as np
import ml_dtypes
import concourse.bass as bass
import concourse.mybir as mybir
from concourse.bass_utils import run_bass_kernel_spmd

F32 = mybir.dt.float32
BF16 = mybir.dt.bfloat16
ALU = mybir.AluOpType
AF = mybir.ActivationFunctionType
AX = mybir.AxisListType

S = 4096
D = 1024
NT = S // 128
DEPTH = 2
EPS = 1e-6
INC = 1984
FFE = 1408
NCH = FFE // 128
ROPE_THETA = 500000.0
MLA_ROPE_THETA = 10000.0


class DSem:
    def __init__(self, h):
        self.h = h
        self.v = 0


class Slot:
    __slots__ = ("wr", "rd")

    def __init__(self):
        self.wr = []
        self.rd = []


class KB:
    def __init__(self, depth=DEPTH, stop_after=None, debug=False):
        self.depth = depth
        self.stop_after = stop_after
        self.debug = debug
        nc = self.nc = bass.Bass("TRN2", target_bir_lowering=False)
        self.E = dict(pe=nc.tensor, dve=nc.vector, act=nc.scalar, pool=nc.gpsimd, sp=nc.sync)
        self.csem = {k: nc.alloc_semaphore("c_" + k) for k in ("pe", "dve", "act", "pool")}
        self.ccnt = dict.fromkeys(self.csem, 0)
        self.waited = {}
        self.dsems = [DSem(nc.alloc_semaphore("d%d" % i)) for i in range(72)]
        self.dptr = 0
        self.swsems = [DSem(nc.alloc_semaphore("sw%d" % i)) for i in range(3)]
        self.banks = [nc.alloc_psum_tensor("bank%d" % i, [128, 512], F32) for i in range(8)]
        self.bank_s = [Slot() for _ in range(8)]
        self.stack = None
        self.uid = 0
        self.const_s = None

    def sig(self, e, ins):
        self.ccnt[e] += 1
        ins.then_inc(self.csem[e], 1)
        return (self.csem[e], self.ccnt[e])

    def wait(self, e, toks):
        for tok in toks:
            sem, v = tok
            key = (e, sem.num)
            if self.waited.get(key, 0) >= v:
                continue
            self.waited[key] = v
            self.E[e].wait_ge(sem, v)

    def _deps(self, e, reads, writes):
        toks = []
        for s in reads:
            toks.extend(s.wr)
        for s in writes:
            toks.extend(s.rd)
            toks.extend(s.wr)
        self.wait(e, toks)

    def _commit(self, tok, reads, writes):
        for s in reads:
            if s is not self.const_s:
                s.rd.append(tok)
        for s in writes:
            s.wr = [tok]
            s.rd = []

    def op(self, e, fn, reads=(), writes=()):
        self._deps(e, reads, writes)
        ins = fn(self.E[e])
        tok = self.sig(e, ins)
        self._commit(tok, reads, writes)
        return tok

    def dsem(self):
        d = self.dsems[self.dptr]
        self.dptr += 1
        return d

    def dma(self, q, out, in_, ds, reads=(), writes=(), **kw):
        self._deps(q, reads, writes)
        ins = self.E[q].dma_start(out=out, in_=in_, **kw)
        ins.then_inc(ds.h, 16)
        ds.v += 16
        tok = (ds.h, ds.v)
        self._commit(tok, reads, writes)
        return tok

    def mm_group(self, bank_i, mms, reads=()):
        bs = self.bank_s[bank_i]
        self._deps("pe", reads, [bs])
        ins = None
        for (o, l, r, st, sp) in mms:
            ins = self.nc.tensor.matmul(o, lhsT=l, rhs=r, start=st, stop=sp)
        tok = self.sig("pe", ins)
        self._commit(tok, reads, [bs])
        return tok

    def tr_group(self, bank_i, trs, ident, reads=()):
        bs = self.bank_s[bank_i]
        self._deps("pe", reads, [bs])
        ins = None
        for (o, i) in trs:
            ins = self.nc.tensor.transpose(out=o, in_=i, identity=ident)
        tok = self.sig("pe", ins)
        self._commit(tok, reads, [bs])
        return tok

    def barrier(self):
        toks = [(self.csem[c], self.ccnt[c]) for c in self.csem if self.ccnt[c] > 0]
        toks += [(d.h, d.v) for d in self.dsems + self.swsems if d.v > 0]
        for e in self.E:
            self.wait(e, toks)

    def begin_phase(self):
        self.barrier()
        self.stack = ExitStack()
        self.dptr = self.dkeep
        for b in self.bank_s:
            b.wr = []
            b.rd = []

    def end_phase(self):
        self.barrier()
        self.stack.close()
        self.stack = None

    def sb(self, name, shape, dt):
        self.uid += 1
        return self.stack.enter_context(self.nc.sbuf_tensor("%s_%d" % (name, self.uid), shape, dt))

    def interleave(self, gens):
        gens = list(gens)
        while gens:
            nxt = []
            for g in gens:
                try:
                    next(g)
                    nxt.append(g)
                except StopIteration:
                    pass
            gens = nxt

    def bank_bf(self, i):
        return self.banks[i][:].bitcast(BF16)

    def build(self):
        nc = self.nc

        def din(name, shape):
            return nc.dram_tensor(name, shape, F32, kind="ExternalInput").ap()

        self.x = din("x", [S, D])
        self.attn_g = din("attn_norm_g", [DEPTH, D])
        self.w_in = din("w_in", [DEPTH, D, INC])
        self.dq_g = din("diff_q_norm_g", [DEPTH, 64])
        self.dk_g = din("diff_k_norm_g", [DEPTH, 64])
        self.dlam = din("diff_lambda", [DEPTH, 256])
        self.subln_g = din("diff_subln_g", [DEPTH, 128])
        self.qln_g = din("mla_q_ln_g", [DEPTH, 256])
        self.w_uq = din("w_uq", [DEPTH, 256, 768])
        self.kvln_g = din("mla_kv_ln_g", [DEPTH, 128])
        self.w_ukv = din("w_ukv", [DEPTH, 128, 1024])
        self.mqk_g = din("mla_qk_norm_g", [DEPTH, 2 * 192])
        self.w_o = din("w_o", [DEPTH, D, D])
        self.ffn_g = din("ffn_norm_g", [DEPTH, D])
        self.dwg = din("dense_w_gate", [D, 2816])
        self.dwu = din("dense_w_up", [D, 2816])
        self.dwd = din("dense_w_down", [2816, D])
        self.rw = din("router_w", [D, 8])
        self.mwg = din("moe_w_gate", [8, D, FFE])
        self.mwu = din("moe_w_up", [8, D, FFE])
        self.mwd = din("moe_w_down", [8, FFE, D])
        self.cs_d = din("cs_d", [S, 32])
        self.cs_m = din("cs_m", [S, 128])
        self.ident_in = din("ident", [128, 128])
        self.y = nc.dram_tensor("y", [S, D], F32, kind="ExternalOutput").ap()
        xr = self.xr = self.y

        kind = "ExternalOutput" if self.debug else "Internal"

        def dscr(name, shape, dt):
            return nc.dram_tensor(name, shape, dt, kind=kind).ap()

        self.qkt = dscr("qkt", [20, 128, S], BF16)
        self.vd = dscr("vd", [S, 512], BF16)
        self.vm = dscr("vm", [S, 512], BF16)
        self.ocat = dscr("ocat", [S, D], BF16)
        self.h2t = dscr("h2t", [8, 128, S], BF16)
        self.b_win = dscr("b_win", [DEPTH, D, INC], BF16)
        self.b_wuq = dscr("b_wuq", [DEPTH, 256, 768], BF16)
        self.b_wukv = dscr("b_wukv", [DEPTH, 128, 1024], BF16)
        self.b_wo = dscr("b_wo", [DEPTH, D, D], BF16)
        self.b_dwg = dscr("b_dwg", [D, 2816], BF16)
        self.b_dwu = dscr("b_dwu", [D, 2816], BF16)
        self.b_dwd = dscr("b_dwd", [2816, D], BF16)
        self.b_mwg = dscr("b_mwg", [8, D, FFE], BF16)
        self.b_mwu = dscr("b_mwu", [8, D, FFE], BF16)
        self.b_mwd = dscr("b_mwd", [8, FFE, D], BF16)

        A = nc.alloc_sbuf_tensor
        self.ident_f = A("ident_f", [128, 128], F32)
        self.ident_b = A("ident_b", [128, 128], BF16)
        self.sc_attn = A("sc_attn", [128, DEPTH, 8], F32)
        self.sc_ffn = A("sc_ffn", [128, DEPTH, 8], F32)
        self.sc_qln = A("sc_qln", [128, DEPTH, 2], F32)
        self.sc_kvln = A("sc_kvln", [128, DEPTH, 1], F32)
        self.sc_wo = A("sc_wo", [128, DEPTH, 8], F32)
        self.ones = A("ones", [128, 24], F32)
        self.gd = A("gd", [128, DEPTH, 2, 64], F32)
        self.gm = A("gm", [128, DEPTH, 2, 192], F32)
        self.lamt = A("lamt", [128, DEPTH, 256], F32)
        self.lamw = A("lamw", [128, 8], F32)
        self.neglam = A("neglam", [128, DEPTH], F32)
        self.negM = A("negM", [128, DEPTH, 2], F32)
        self.gmax = A("gmax", [128, 8], F32)
        self.rw_s = A("rw_s", [128, 8, 8], F32)
        self.comb = A("comb", [128, NT, 8], F32)
        self.junk = A("junk", [128, 1024], BF16)
        self.const_s = Slot()

        self.setup_consts()
        self.setup_wjobs()
        self.dkeep = self.dptr
        for l in range(self.depth):
            self.phase1(l)
            if self.stop_after == "p1_%d" % l:
                return self.finish()
            self.phase2(l)
            if self.stop_after == "p2_%d" % l:
                return self.finish()
            self.phase3a(l)
            if self.stop_after == "p3a_%d" % l:
                return self.finish()
            self.phase3b(l)
            if self.stop_after == "p3b_%d" % l:
                return self.finish()
        return self.finish()

    def finish(self):
        self.barrier()
        return self.nc

    def setup_consts(self):
        nc = self.nc
        cs = self.const_s
        ds = self.dsem()
        toks = []

        def ld(out, in_, **kw):
            ins = nc.sync.dma_start(out=out, in_=in_, **kw)
            ins.then_inc(ds.h, 16)
            ds.v += 16

        self.xinit = self.dsem()
        for i in range(4):
            ins = nc.sync.dma_start(out=self.y[i * 1024:(i + 1) * 1024, :], in_=self.x[i * 1024:(i + 1) * 1024, :])
            ins.then_inc(self.xinit.h, 16)
            self.xinit.v += 16

        ld(self.ident_f[:], self.ident_in[:, :])
        for l in range(DEPTH):
            ld(self.sc_attn[:, l, :], self.attn_g[l].rearrange("(c p) -> p c", p=128), allow_slow_non_contiguous=True)
            ld(self.sc_ffn[:, l, :], self.ffn_g[l].rearrange("(c p) -> p c", p=128), allow_slow_non_contiguous=True)
            ld(self.sc_qln[:, l, :], self.qln_g[l].rearrange("(c p) -> p c", p=128), allow_slow_non_contiguous=True)
            ld(self.sc_kvln[:, l, :], self.kvln_g[l].rearrange("(c p) -> p c", p=128), allow_slow_non_contiguous=True)
            ld(self.sc_wo[:, l, 0:1], self.subln_g[l].rearrange("(c p) -> p c", p=128), allow_slow_non_contiguous=True)
            ld(self.gd[:, l, 0, :], self.dq_g[l].partition_broadcast(128))
            ld(self.gd[:, l, 1, :], self.dk_g[l].partition_broadcast(128))
            ld(self.gm[:, l, :, :].rearrange("p a b -> p (a b)"), self.mqk_g[l].partition_broadcast(128))
            ld(self.lamt[:, l, :], self.dlam[l].partition_broadcast(128))
        ld(self.rw_s[:], self.rw.rearrange("(c p) n -> p c n", p=128))
        nc.vector.wait_ge(ds.h, ds.v)
        nc.gpsimd.wait_ge(ds.h, ds.v)
        nc.scalar.wait_ge(ds.h, ds.v)
        V = nc.vector
        sv = self.csem["dve"]

        def vop(ins):
            self.ccnt["dve"] += 1
            ins.then_inc(sv, 1)
            V.wait_ge(sv, self.ccnt["dve"])

        def aop(ins):
            self.ccnt["act"] += 1
            ins.then_inc(self.csem["act"], 1)
            nc.scalar.wait_ge(self.csem["act"], self.ccnt["act"])
            V.wait_ge(self.csem["act"], self.ccnt["act"])

        vop(V.tensor_copy(out=self.ident_b[:], in_=self.ident_f[:]))
        vop(V.memset(self.ones[:], 1.0))
        for l in range(DEPTH):
            lam_init = 0.8 - 0.6 * math.exp(-0.3 * l)
            vop(V.tensor_scalar(out=self.sc_wo[:, l, 0:1], in0=self.sc_wo[:, l, 0:1], scalar1=float(1.0 - lam_init),
                                scalar2=None, op0=ALU.mult))
            for c in range(1, 4):
                vop(V.tensor_copy(out=self.sc_wo[:, l, c:c + 1], in_=self.sc_wo[:, l, 0:1]))
            vop(V.memset(self.sc_wo[:, l, 4:8], 1.0))
        for l in range(DEPTH):
            lam_init = 0.8 - 0.6 * math.exp(-0.3 * l)
            vop(V.tensor_tensor(out=self.lamt[:, l, 0:64], in0=self.lamt[:, l, 0:64], in1=self.lamt[:, l, 64:128], op=ALU.mult))
            vop(V.tensor_tensor(out=self.lamt[:, l, 128:192], in0=self.lamt[:, l, 128:192], in1=self.lamt[:, l, 192:256], op=ALU.mult))
            vop(V.tensor_reduce(out=self.lamw[:, 0:1], in_=self.lamt[:, l, 0:64], axis=AX.X, op=ALU.add))
            vop(V.tensor_reduce(out=self.lamw[:, 1:2], in_=self.lamt[:, l, 128:192], axis=AX.X, op=ALU.add))
            nc.scalar.wait_ge(sv, self.ccnt["dve"])
            aop(nc.scalar.activation(out=self.lamw[:, 2:4], in_=self.lamw[:, 0:2], func=AF.Exp))
            vop(V.tensor_tensor(out=self.lamw[:, 4:5], in0=self.lamw[:, 3:4], in1=self.lamw[:, 2:3], op=ALU.subtract))
            vop(V.tensor_scalar(out=self.neglam[:, l:l + 1], in0=self.lamw[:, 4:5], scalar1=float(-lam_init),
                                scalar2=None, op0=ALU.add))
            vop(V.tensor_reduce(out=self.gmax[:, 0:2], in_=self.gd[:, l, :, :], axis=AX.X, op=ALU.max,
                                apply_absolute_value=True))
            vop(V.tensor_reduce(out=self.gmax[:, 2:4], in_=self.gm[:, l, :, :], axis=AX.X, op=ALU.max,
                                apply_absolute_value=True))
            vop(V.tensor_tensor(out=self.gmax[:, 4:5], in0=self.gmax[:, 0:1], in1=self.gmax[:, 1:2], op=ALU.mult))
            vop(V.tensor_tensor(out=self.gmax[:, 5:6], in0=self.gmax[:, 2:3], in1=self.gmax[:, 3:4], op=ALU.mult))
            vop(V.tensor_scalar(out=self.negM[:, l, 0:1], in0=self.gmax[:, 4:5], scalar1=float(-math.sqrt(64.0)),
                                scalar2=None, op0=ALU.mult))
            vop(V.tensor_scalar(out=self.negM[:, l, 1:2], in0=self.gmax[:, 5:6], scalar1=float(-math.sqrt(192.0)),
                                scalar2=None, op0=ALU.mult))
        for c in range(8):
            vop(V.tensor_scalar(out=self.rw_s[:, c, :], in0=self.rw_s[:, c, :], scalar1=self.sc_ffn[:, 1, c:c + 1],
                                scalar2=None, op0=ALU.mult))
        vop(V.memset(self.comb[:], 1.0))

    def setup_wjobs(self):
        nc = self.nc
        NB = 3
        CM = 1408
        self.w_fst = [nc.alloc_sbuf_tensor("wf%d" % i, [128, CM], F32) for i in range(NB)]
        self.w_bst = [nc.alloc_sbuf_tensor("wb%d" % i, [128, CM], BF16) for i in range(NB)]
        self.w_fs = [Slot() for _ in range(NB)]
        self.w_bs = [Slot() for _ in range(NB)]
        self.w_fd = [self.dsem() for _ in range(NB)]
        self.w_bd = [self.dsem() for _ in range(NB)]
        self.w_nb = NB
        self.wjobs = []
        self.wnext = 0
        self.wpending = []
        self.wtags = {}

        def add(src, dst, R, C, sc):
            ncs = 1 if C <= CM else 2
            cw = C // ncs
            for c in range(R // 128):
                for j in range(ncs):
                    self.wjobs.append((src[c * 128:(c + 1) * 128, j * cw:(j + 1) * cw],
                                       dst[c * 128:(c + 1) * 128, j * cw:(j + 1) * cw], cw, sc(c)))

        one = lambda c: self.ones[:, 0:1]

        def attn(l):
            add(self.w_in[l], self.b_win[l], D, INC, lambda c, l=l: self.sc_attn[:, l, c:c + 1])
            add(self.w_uq[l], self.b_wuq[l], 256, 768, lambda c, l=l: self.sc_qln[:, l, c:c + 1])
            add(self.w_ukv[l], self.b_wukv[l], 128, 1024, lambda c, l=l: self.sc_kvln[:, l, c:c + 1])
            self.wtags["attn%d" % l] = len(self.wjobs)
            add(self.w_o[l], self.b_wo[l], D, D, lambda c, l=l: self.sc_wo[:, l, c:c + 1])
            self.wtags["wo%d" % l] = len(self.wjobs)

        attn(0)
        add(self.dwg, self.b_dwg, D, 2816, lambda c: self.sc_ffn[:, 0, c:c + 1])
        add(self.dwu, self.b_dwu, D, 2816, lambda c: self.sc_ffn[:, 0, c:c + 1])
        add(self.dwd, self.b_dwd, 2816, D, one)
        self.wtags["ffn0"] = len(self.wjobs)
        if self.depth > 1:
            attn(1)
            for e in range(8):
                add(self.mwg[e], self.b_mwg[e], D, FFE, lambda c: self.sc_ffn[:, 1, c:c + 1])
                add(self.mwu[e], self.b_mwu[e], D, FFE, lambda c: self.sc_ffn[:, 1, c:c + 1])
                add(self.mwd[e], self.b_mwd[e], FFE, D, one)
            self.wtags["ffn1"] = len(self.wjobs)

    def _wflush(self):
        for (i, dst, C) in self.wpending:
            self.dma("sp", dst, self.w_bst[i][:, 0:C], self.w_bd[i], reads=[self.w_bs[i]])
        self.wpending = []

    def pump(self, n, engs=("dve", "act")):
        if self.wnext >= len(self.wjobs):
            if self.wpending:
                self._wflush()
            return
        self._wflush()
        for _ in range(n):
            if self.wnext >= len(self.wjobs):
                break
            src, dst, C, scal = self.wjobs[self.wnext]
            i = self.wnext % self.w_nb
            eng = engs[self.wnext % len(engs)]
            self.wnext += 1
            fst, bst = self.w_fst[i], self.w_bst[i]
            self.dma("sp", fst[:, 0:C], src, self.w_fd[i], writes=[self.w_fs[i]])
            if eng == "act":
                self.op("act", lambda a: a.activation(out=bst[:, 0:C], in_=fst[:, 0:C], func=AF.Copy, scale=scal),
                        reads=[self.w_fs[i]], writes=[self.w_bs[i]])
            else:
                self.op(eng, lambda v: v.tensor_scalar(out=bst[:, 0:C], in0=fst[:, 0:C], scalar1=scal, scalar2=None,
                                                       op0=ALU.mult), reads=[self.w_fs[i]], writes=[self.w_bs[i]])
            self.wpending.append((i, dst, C))

    def pump_until(self, tag, engs=("dve", "act")):
        tgt = self.wtags[tag]
        while self.wnext < tgt:
            self.pump(min(2, tgt - self.wnext), engs)
        self._wflush()
        self.wait("sp", [(d.h, d.v) for d in self.w_bd if d.v > 0])

    def rstd(self, src_ap, src_slot, out_ap, out_slot, tmp_ap, tmp_slot, inv_w):
        self.op("act", lambda a: a.activation(out=tmp_ap, in_=src_ap, func=AF.Ln, scale=float(inv_w), bias=float(EPS)),
                reads=[src_slot], writes=[tmp_slot])
        self.op("act", lambda a: a.activation(out=out_ap, in_=tmp_ap, func=AF.Exp, scale=-0.5),
                reads=[tmp_slot], writes=[out_slot])

    def normrope(self, W_, src3, src_slots, G, W, gain3, R, rot_lo, cs_ap, cs_slot, dst_nonrot, dst_rot, dst_slot,
                 nonrot_lo, nonrot_hi):
        sq, sq_s, ssg, ssg_s, lg, lg_s, rsg, rsg_s, tmp, tmp_s, xr_, xr_s, ac, ac_s, sw, sw_s = W_
        n = G * W
        h = R // 2
        sq3 = sq[:, 0:n].rearrange("p (g w) -> p g w", w=W)
        tmp3 = tmp[:, 0:n].rearrange("p (g w) -> p g w", w=W)
        xr3 = xr_[:, 0:G * R].rearrange("p (g r) -> p g r", r=R)
        ac3 = ac[:, 0:G * R].rearrange("p (g r) -> p g r", r=R)
        sw3 = sw[:, 0:G * R].rearrange("p (g r) -> p g r", r=R)
        self.op("act", lambda a: a.activation(out=sq3, in_=src3, func=AF.Square), reads=src_slots, writes=[sq_s])
        yield
        self.op("dve", lambda v: v.tensor_reduce(out=ssg[:, 0:G], in_=sq3, axis=AX.X, op=ALU.add),
                reads=[sq_s], writes=[ssg_s])
        yield
        self.rstd(ssg[:, 0:G], ssg_s, rsg[:, 0:G], rsg_s, lg[:, 0:G], lg_s, 1.0 / W)
        yield
        self.op("dve", lambda v: v.tensor_tensor(out=tmp3, in0=src3, in1=rsg[:, 0:G].unsqueeze(2).broadcast_to([128, G, W]),
                                                 op=ALU.mult),
                reads=list(src_slots) + [rsg_s], writes=[tmp_s])
        yield
        self.op("pool", lambda p: p.tensor_tensor(out=dst_nonrot, in0=tmp3[:, :, nonrot_lo:nonrot_hi],
                                                  in1=gain3[:, :, nonrot_lo:nonrot_hi], op=ALU.mult),
                reads=[tmp_s], writes=[dst_slot])
        yield
        self.op("pool", lambda p: p.tensor_tensor(out=xr3, in0=tmp3[:, :, rot_lo:rot_lo + R],
                                                  in1=gain3[:, :, rot_lo:rot_lo + R], op=ALU.mult),
                reads=[tmp_s], writes=[xr_s])
        yield
        cc = cs_ap[:, 0:R].unsqueeze(1).broadcast_to([128, G, R])
        ns = cs_ap[:, R:R + h].unsqueeze(1).broadcast_to([128, G, h])
        ps_ = cs_ap[:, R + h:2 * R].unsqueeze(1).broadcast_to([128, G, h])
        self.op("pool", lambda p: p.tensor_tensor(out=ac3, in0=xr3, in1=cc, op=ALU.mult),
                reads=[xr_s, cs_slot], writes=[ac_s])
        yield
        self.op("pool", lambda p: p.tensor_tensor(out=sw3[:, :, 0:h], in0=xr3[:, :, h:R], in1=ns, op=ALU.mult),
                reads=[xr_s, cs_slot], writes=[sw_s])
        yield
        self.op("pool", lambda p: p.tensor_tensor(out=sw3[:, :, h:R], in0=xr3[:, :, 0:h], in1=ps_, op=ALU.mult),
                reads=[xr_s, cs_slot], writes=[sw_s])
        yield
        tok = self.op("pool", lambda p: p.tensor_tensor(out=dst_rot, in0=ac3, in1=sw3, op=ALU.add),
                      reads=[ac_s, sw_s], writes=[dst_slot])
        yield
        return tok

    def phase1(self, l):
        self.begin_phase()
        self.pump_until("attn%d" % l)
        if l == 0:
            self.wait("sp", [(self.xinit.h, self.xinit.v)])
        nc = self.nc
        sb = self.sb
        win = sb("win", [128, 8, INC], BF16)
        wuq = sb("wuq", [128, 2, 768], BF16)
        wukv = sb("wukv", [128, 1024], BF16)
        w_s = Slot()
        d0 = self.dsem()
        self.dma("sp", win[:], self.b_win[l].rearrange("(c p) n -> p c n", p=128), d0, writes=[])
        self.dma("sp", wuq[:], self.b_wuq[l].rearrange("(c p) n -> p c n", p=128), d0, writes=[])
        tokw = self.dma("sp", wukv[:], self.b_wukv[l], d0, writes=[])
        w_s.wr = [tokw]

        def bufs(name, shape, dt, n):
            return [sb(name, shape, dt) for _ in range(n)], [Slot() for _ in range(n)]

        xt, xt_s = bufs("xt", [128, D], F32, 2)
        hb, hb_s = bufs("hb", [128, D], BF16, 2)
        hT, hT_s = bufs("hT", [128, D], BF16, 2)
        pj = [sb("pj", [128, INC], F32) for _ in range(2)]
        pj_s = [[Slot() for _ in range(4)] for _ in range(2)]
        smA = [sb("smA", [128, 4], F32) for _ in range(2)]
        ss_s, lg_s, rs_s = [Slot(), Slot()], [Slot(), Slot()], [Slot(), Slot()]
        smB = [sb("smB", [128, 8], F32) for _ in range(2)]
        ssc_s, lgc_s, rsc_s = [Slot(), Slot()], [Slot(), Slot()], [Slot(), Slot()]
        cn, cn_s = bufs("cn", [128, 384], BF16, 2)
        cT, cT_s = bufs("cT", [128, 384], BF16, 2)
        qm = [sb("qm", [128, 768], F32) for _ in range(2)]
        qm_s = [[Slot(), Slot()] for _ in range(2)]
        kvm = [sb("kvm", [128, 1024], F32) for _ in range(2)]
        kvm_s = [[Slot(), Slot()] for _ in range(2)]
        kf, kf_s = bufs("kf", [128, 768], F32, 2)
        dA, dA_s = bufs("dA", [128, 1024], BF16, 2)
        mQ, mQ_s = bufs("mQ", [128, 768], BF16, 2)
        mK, mK_s = bufs("mK", [128, 768], BF16, 2)
        vb, vb_s = bufs("vb", [128, 512], BF16, 3)
        vmb, vmb_s = bufs("vmb", [128, 512], BF16, 3)
        csd, csd_s = bufs("csd", [128, 32], F32, 3)
        csm, csm_s = bufs("csm", [128, 128], F32, 3)
        stage = [sb("stage", [128, 20, 256], BF16) for _ in range(2)]
        stage_s = [Slot(), Slot()]
        xt_d = [self.dsem() for _ in range(2)]
        cs_d_ = [self.dsem() for _ in range(3)]
        cs_m_ = [self.dsem() for _ in range(3)]
        vb_d = [self.dsem() for _ in range(3)]
        vmb_d = [self.dsem() for _ in range(3)]
        st_d = [self.dsem() for _ in range(2)]

        def mkscratch(n, gr):
            return (sb("sq", [128, n], F32), Slot(), sb("ssg", [128, 16], F32), Slot(), sb("lg", [128, 16], F32), Slot(),
                    sb("rsg", [128, 16], F32), Slot(), sb("tmp", [128, n], F32), Slot(), sb("xr_", [128, gr], F32), Slot(),
                    sb("ac", [128, gr], F32), Slot(), sb("sw", [128, gr], F32), Slot())

        W_d = mkscratch(1024, 256)
        W_q = mkscratch(768, 256)
        W_k = mkscratch(768, 256)

        gdf = sb("gdf", [128, 16, 64], F32)
        gdf_s = Slot()
        self.op("dve", lambda v: v.tensor_copy(out=gdf[:, 0:8, :], in_=self.gd[:, l, 0, :].unsqueeze(1).broadcast_to([128, 8, 64])),
                writes=[gdf_s])
        self.op("dve", lambda v: v.tensor_copy(out=gdf[:, 8:16, :], in_=self.gd[:, l, 1, :].unsqueeze(1).broadcast_to([128, 8, 64])),
                writes=[gdf_s])
        self.wait("pool", gdf_s.wr)
        gd3 = gdf[:]
        gq3 = self.gm[:, l, 0, :].unsqueeze(1).broadcast_to([128, 4, 192])
        gk3 = self.gm[:, l, 1, :].unsqueeze(1).broadcast_to([128, 4, 192])
        TPB = [0, 1]
        MMB = [2, 3, 4, 5, 6, 7]
        self.mmi = 0
        self.tpi = 0
        self.evi = 0

        def evac(bank_i, src_ap, dst_ap, writes):
            eng = "act" if self.evi % 2 == 0 else "dve"
            self.evi += 1
            if eng == "act":
                return self.op("act", lambda a: a.copy(out=dst_ap, in_=src_ap), reads=[self.bank_s[bank_i]], writes=writes)
            return self.op("dve", lambda v: v.tensor_copy(out=dst_ap, in_=src_ap), reads=[self.bank_s[bank_i]], writes=writes)

        def project(lhs_chunks, rhs_fn, ncols, dst, dst_slots, reads):
            n0 = 0
            i = 0
            while n0 < ncols:
                w = min(512, ncols - n0)
                bi = MMB[self.mmi % len(MMB)]
                self.mmi += 1
                K = len(lhs_chunks)
                mms = [(self.banks[bi][:, 0:w], lhs_chunks[k], rhs_fn(k, n0, w), k == 0, k == K - 1) for k in range(K)]
                self.mm_group(bi, mms, reads=reads)
                evac(bi, self.banks[bi][:, 0:w], dst[:, n0:n0 + w], [dst_slots[i]])
                n0 += w
                i += 1

        def transposes(src_ap, ncol_blocks, src_slots, dst_ap, dst_writes):
            bi = TPB[self.tpi % 2]
            self.tpi += 1
            bv = self.bank_bf(bi)
            trs = [(bv[:, k * 128:(k + 1) * 128], src_ap[:, k * 128:(k + 1) * 128]) for k in range(ncol_blocks)]
            self.tr_group(bi, trs, self.ident_b[:], reads=src_slots)
            src = bv[:, 0:ncol_blocks * 128]
            if len(dst_ap.shape) == 3:
                src = src.rearrange("p (k n) -> p k n", n=128)
            return evac(bi, src, dst_ap, dst_writes)

        def stA(t):
            b = t % 2
            b3 = t % 3
            rows = slice(t * 128, (t + 1) * 128)
            sm = smA[b]
            self.dma("sp", xt[b][:], self.xr[rows, :], xt_d[b], writes=[xt_s[b]])
            yield
            self.dma("sp", csd[b3][:], self.cs_d[rows, :], cs_d_[b3], writes=[csd_s[b3]])
            yield
            self.dma("sp", csm[b3][:], self.cs_m[rows, :], cs_m_[b3], writes=[csm_s[b3]])
            yield
            self.op("act", lambda a: a.activation(out=self.junk[:], in_=xt[b][:], func=AF.Square, accum_out=sm[:, 0:1]),
                    reads=[xt_s[b]], writes=[ss_s[b]])
            yield
            self.rstd(sm[:, 0:1], ss_s[b], sm[:, 2:3], rs_s[b], sm[:, 1:2], lg_s[b], 1.0 / D)
            yield
            self.op("dve", lambda v: v.tensor_scalar(out=hb[b][:], in0=xt[b][:], scalar1=sm[:, 2:3], scalar2=None,
                                                     op0=ALU.mult), reads=[xt_s[b], rs_s[b]], writes=[hb_s[b]])
            yield
            transposes(hb[b], 8, [hb_s[b]], hT[b][:], [hT_s[b]])
            yield
            project([hT[b][:, k * 128:(k + 1) * 128] for k in range(8)],
                    lambda k, n0, w: win[:, k, n0:n0 + w], INC, pj[b], pj_s[b], [hT_s[b], w_s])
            yield
            self.op("act", lambda a: a.copy(out=vb[b3][:], in_=pj[b][:, 1024:1536]), reads=[pj_s[b][2]], writes=[vb_s[b3]])
            yield

        def stB(t):
            b = t % 2
            b3 = t % 3
            rows = slice(t * 128, (t + 1) * 128)
            sm = smB[b]
            self.dma("sp", self.vd[rows, :], vb[b3][:], vb_d[b3], reads=[vb_s[b3]])
            yield
            src3 = pj[b][:, 0:1024].rearrange("p (g w) -> p g w", w=64)
            dA3 = dA[b][:].rearrange("p (g w) -> p g w", w=64)
            yield from self.normrope(W_d, src3, [pj_s[b][0], pj_s[b][1]], 16, 64, gd3, 16, 0, csd[b3], csd_s[b3],
                          dA3[:, :, 16:64], dA3[:, :, 0:16], dA_s[b], 16, 64)
            self.op("act", lambda a: a.activation(out=self.junk[:, 0:256], in_=pj[b][:, 1536:1792], func=AF.Square,
                                                  accum_out=sm[:, 0:1]), reads=[pj_s[b][3]], writes=[ssc_s[b]])
            yield
            self.op("act", lambda a: a.activation(out=self.junk[:, 0:128], in_=pj[b][:, 1792:1920], func=AF.Square,
                                                  accum_out=sm[:, 1:2]), reads=[pj_s[b][3]], writes=[ssc_s[b]])
            yield
            self.op("act", lambda a: a.activation(out=sm[:, 2:3], in_=sm[:, 0:1], func=AF.Ln, scale=1.0 / 256, bias=float(EPS)),
                    reads=[ssc_s[b]], writes=[lgc_s[b]])
            yield
            self.op("act", lambda a: a.activation(out=sm[:, 3:4], in_=sm[:, 1:2], func=AF.Ln, scale=1.0 / 128, bias=float(EPS)),
                    reads=[ssc_s[b]], writes=[lgc_s[b]])
            yield
            self.op("act", lambda a: a.activation(out=sm[:, 4:6], in_=sm[:, 2:4], func=AF.Exp, scale=-0.5),
                    reads=[lgc_s[b]], writes=[rsc_s[b]])
            yield
            self.op("dve", lambda v: v.tensor_scalar(out=cn[b][:, 0:256], in0=pj[b][:, 1536:1792], scalar1=sm[:, 4:5],
                                                     scalar2=None, op0=ALU.mult), reads=[pj_s[b][3], rsc_s[b]], writes=[cn_s[b]])
            yield
            self.op("dve", lambda v: v.tensor_scalar(out=cn[b][:, 256:384], in0=pj[b][:, 1792:1920], scalar1=sm[:, 5:6],
                                                     scalar2=None, op0=ALU.mult), reads=[pj_s[b][3], rsc_s[b]],
                    writes=[cn_s[b]])
            yield
            transposes(cn[b], 3, [cn_s[b]], cT[b][:], [cT_s[b]])
            yield
            project([cT[b][:, 0:128], cT[b][:, 128:256]], lambda k, n0, w: wuq[:, k, n0:n0 + w], 768, qm[b], qm_s[b],
                    [cT_s[b], w_s])
            yield
            project([cT[b][:, 256:384]], lambda k, n0, w: wukv[:, n0:n0 + w], 1024, kvm[b], kvm_s[b], [cT_s[b], w_s])
            yield
            kvm4 = kvm[b][:].rearrange("p (h c) -> p h c", c=256)
            kf3 = kf[b][:].rearrange("p (h c) -> p h c", c=192)
            self.op("pool", lambda p: p.tensor_copy(out=kf3[:, :, 0:128], in_=kvm4[:, :, 0:128]), reads=kvm_s[b],
                    writes=[kf_s[b]])
            yield
            self.op("pool", lambda p: p.tensor_copy(out=kf3[:, :, 128:192],
                                                    in_=pj[b][:, 1920:1984].unsqueeze(1).broadcast_to([128, 4, 64])),
                    reads=[pj_s[b][3]], writes=[kf_s[b]])
            yield
            self.op("act", lambda a: a.copy(out=vmb[b3][:].rearrange("p (h c) -> p h c", c=128), in_=kvm4[:, :, 128:256]),
                    reads=kvm_s[b], writes=[vmb_s[b3]])
            yield

        def stCq(t):
            b = t % 2
            b3 = t % 3
            rows = slice(t * 128, (t + 1) * 128)
            self.dma("sp", self.vm[rows, :], vmb[b3][:], vmb_d[b3], reads=[vmb_s[b3]])
            yield
            qm3 = qm[b][:].rearrange("p (h c) -> p h c", c=192)
            mQn = mQ[b][:, 0:512].rearrange("p (h c) -> p h c", c=128)
            mQr = mQ[b][:, 512:768].rearrange("p (h c) -> p h c", c=64)
            yield from self.normrope(W_q, qm3, qm_s[b], 4, 192, gq3, 64, 128, csm[b3], csm_s[b3], mQn, mQr, mQ_s[b], 0, 128)

        def stCk(t):
            b = t % 2
            b3 = t % 3
            kf3 = kf[b][:].rearrange("p (h c) -> p h c", c=192)
            mKn = mK[b][:, 0:512].rearrange("p (h c) -> p h c", c=128)
            mKr = mK[b][:, 512:768].rearrange("p (h c) -> p h c", c=64)
            yield from self.normrope(W_k, kf3, [kf_s[b]], 4, 192, gk3, 64, 128, csm[b3], csm_s[b3], mKn, mKr, mK_s[b], 0, 128)

        def stCt(t):
            b = t % 2
            pr = t // 2
            sg = pr % 2
            tl = t % 2
            stg = stage[sg]
            cols = slice(tl * 128, (tl + 1) * 128)
            if tl == 0:
                old = stage_s[sg].rd + stage_s[sg].wr
                self.wait("act", old)
                self.wait("dve", old)
                stage_s[sg].wr = []
                stage_s[sg].rd = []
            toks = []
            toks.append(transposes(dA[b], 8, [dA_s[b]], stg[:, 0:8, cols], []))
            toks.append(transposes(mQ[b], 6, [mQ_s[b]], stg[:, 8:14, cols], []))
            toks.append(transposes(mK[b], 6, [mK_s[b]], stg[:, 14:20, cols], []))
            stage_s[sg].wr.extend(toks)
            if tl == 1:
                self.dma("sp", self.qkt[:, :, pr * 256:(pr + 1) * 256].rearrange("k p n -> p k n"), stg[:], st_d[sg],
                         reads=[stage_s[sg]])

        for i in range(NT + 2):
            gens = []
            if i < NT:
                gens.append(stA(i))
            if 0 <= i - 1 < NT:
                gens.append(stB(i - 1))
            if 0 <= i - 2 < NT:
                gens.append(stCq(i - 2))
                gens.append(stCk(i - 2))
            self.interleave(gens)
            if 0 <= i - 2 < NT:
                stCt(i - 2)
            self.pump(2)
        self.end_phase()

    def phase2(self, l):
        self.begin_phase()
        nc = self.nc
        sb = self.sb
        qt = [sb("qt", [128, S], BF16) for _ in range(2)]
        kt = [sb("kt", [128, S], BF16) for _ in range(2)]
        qr = [sb("qr", [128, S], BF16) for _ in range(2)]
        kr = [sb("kr", [128, S], BF16) for _ in range(2)]
        vs = [sb("vs", [128, NT, 129], BF16) for _ in range(2)]
        ld_s = [Slot() for _ in range(2)]
        ld_d = [self.dsem() for _ in range(2)]
        NP = 4
        pt = [sb("pt", [128, 512], BF16) for _ in range(NP)]
        pt_s = [Slot() for _ in range(NP)]
        o1 = [sb("o1", [128, 4, 128], F32) for _ in range(2)]
        o1_s = [Slot() for _ in range(2)]
        rc = [sb("rc", [128, 8], F32) for _ in range(4)]
        rc_s = [Slot() for _ in range(4)]
        ocs = [sb("ocs", [128, 4, 128], BF16) for _ in range(2)]
        ocs_s = [Slot() for _ in range(2)]
        ocs_d = [self.dsem() for _ in range(2)]
        for i in range(2):
            self.op("dve", lambda v: v.memset(vs[i][:, :, 128:129], 1.0), writes=[ld_s[i]])
        SB_ = [0, 1, 2]
        OB_ = [4, 5, 6, 7]
        qkt = self.qkt
        heads = [("d", h) for h in range(4)] + [("m", h) for h in range(4)]
        self.gi = 0
        self.oci = 0
        self.rci = 0

        def load(hi):
            typ, h = heads[hi]
            i = hi % 2
            s = ld_s[i]
            self._deps("sp", [], [s])
            if typ == "d":
                self.dma("sp", qt[i][:], qkt[h], ld_d[i])
                self.dma("sp", kt[i][:], qkt[4 + h], ld_d[i])
                vsrc = self.vd
            else:
                o = (h % 2) * 64
                self.dma("sp", qt[i][:], qkt[8 + h], ld_d[i])
                self.dma("sp", kt[i][:], qkt[14 + h], ld_d[i])
                self.dma("sp", qr[i][o:o + 64, :], qkt[12 + h // 2, o:o + 64, :], ld_d[i])
                self.dma("sp", kr[i][o:o + 64, :], qkt[18 + h // 2, o:o + 64, :], ld_d[i])
                vsrc = self.vm
            tok = self.dma("sp", vs[i][:, :, 0:128],
                           vsrc.rearrange("(j p) c -> p j c", p=128)[:, :, h * 128:(h + 1) * 128], ld_d[i])
            s.wr = [tok]
            s.rd = []

        load(0)
        for hi, (typ, h) in enumerate(heads):
            if hi + 1 < len(heads):
                load(hi + 1)
            i = hi % 2
            lds = ld_s[i]
            if typ == "d":
                maps = [[(kt[i][0:64, :], qt[i][0:64, :])], [(kt[i][64:128, :], qt[i][64:128, :])]]
                scale = 64.0 ** -0.5
                negM = self.negM[:, l, 0:1]
                col = h
            else:
                o = (h % 2) * 64
                maps = [[(kt[i][:, :], qt[i][:, :]), (kr[i][o:o + 64, :], qr[i][o:o + 64, :])]]
                scale = 192.0 ** -0.5
                negM = self.negM[:, l, 1:2]
                col = 4 + h
            steps = []
            for Qb in range(8):
                for m in range(len(maps)):
                    for j in range(4 * Qb + 4):
                        steps.append((Qb, m, j))
            n = len(steps)

            def qk(si):
                Qb, m, j = steps[si]
                r = max(0, j - 4 * Qb)
                N = (4 - r) * 128
                bi = SB_[si % 3]
                parts = maps[m]
                q0 = Qb * 512 + r * 128
                mms = [(self.banks[bi][:, 0:N], kp[:, j * 128:(j + 1) * 128], qp[:, q0:q0 + N], pi == 0, pi == len(parts) - 1)
                       for pi, (kp, qp) in enumerate(parts)]
                self.mm_group(bi, mms, reads=[lds])

            qk(0)
            if n > 1:
                qk(1)
            for si in range(n):
                Qb, m, j = steps[si]
                if si % 6 == 0:
                    self.pump(1, engs=("dve",))
                if si + 2 < n:
                    qk(si + 2)
                r = max(0, j - 4 * Qb)
                N = (4 - r) * 128
                bi = SB_[si % 3]
                pi_ = si % NP
                first_of_group = (j == 0)
                if first_of_group:
                    g = self.gi
                    self.gi += 1
                    self.cur_ob = OB_[2 * (g % 2)], OB_[2 * (g % 2) + 1]
                ob = self.cur_ob
                self.op("act", lambda a: a.activation(out=pt[pi_][:, 0:N], in_=self.banks[bi][:, 0:N], func=AF.Exp,
                                                      scale=float(scale), bias=negM),
                        reads=[self.bank_s[bi]], writes=[pt_s[pi_]])
                if j >= 4 * Qb:
                    self.op("pool", lambda p: p.affine_select(out=pt[pi_][:, 0:128], in_=pt[pi_][:, 0:128], pattern=[[1, 128]],
                                                              compare_op=ALU.is_ge, fill=0.0, base=0, channel_multiplier=-1),
                            reads=[], writes=[pt_s[pi_]])
                self._deps("pe", [pt_s[pi_], lds], [])
                last_tok = None
                for tq in range(r, 4):
                    gt = 4 * Qb + tq
                    obk = ob[tq // 2]
                    oc0 = (tq % 2) * 256
                    if j == 0:
                        self._deps("pe", [], [self.bank_s[obk]])
                    ins = nc.tensor.matmul(self.banks[obk][:, oc0:oc0 + 129],
                                           lhsT=pt[pi_][:, (tq - r) * 128:(tq - r + 1) * 128], rhs=vs[i][:, j, :],
                                           start=(j == 0 and tq % 2 == 0), stop=(j == gt), skip_group_check=True)
                tok = self.sig("pe", ins)
                pt_s[pi_].rd.append(tok)
                lds.rd.append(tok)
                if j == 0:
                    for bk in ob:
                        self.bank_s[bk].wr = [tok]
                        self.bank_s[bk].rd = []
                else:
                    for bk in ob:
                        self.bank_s[bk].wr = [tok]
                if j == 4 * Qb + 3:
                    ri = self.rci % 4
                    self.rci += 1
                    for k2 in range(2):
                        bview = self.banks[ob[k2]][:].rearrange("p (a b) -> p a b", b=256)
                        self.op("dve", lambda v: v.reciprocal(out=rc[ri][:, 2 * k2:2 * k2 + 2].unsqueeze(2), in_=bview[:, :, 128:129]),
                                reads=[self.bank_s[ob[k2]]], writes=[rc_s[ri]])
                    if typ == "d" and m == 0:
                        oi = Qb % 2
                        for tq in range(4):
                            self.op("dve", lambda v: v.tensor_scalar(
                                out=o1[oi][:, tq, :], in0=self.banks[ob[tq // 2]][:, (tq % 2) * 256:(tq % 2) * 256 + 128],
                                scalar1=rc[ri][:, tq:tq + 1], scalar2=None, op0=ALU.mult),
                                reads=[self.bank_s[ob[tq // 2]], rc_s[ri]], writes=[o1_s[oi]] if tq == 0 else [])
                        o1_s[oi].wr = [(self.csem["dve"], self.ccnt["dve"])]
                    else:
                        ci = self.oci % 2
                        self.oci += 1
                        if typ == "d":
                            oi = Qb % 2
                            self.op("dve", lambda v: v.tensor_scalar(out=rc[ri][:, 4:8], in0=rc[ri][:, 0:4],
                                                                     scalar1=self.neglam[:, l:l + 1], scalar2=None,
                                                                     op0=ALU.mult), reads=[rc_s[ri]], writes=[rc_s[ri]])
                        for tq in range(4):
                            src = self.banks[ob[tq // 2]][:, (tq % 2) * 256:(tq % 2) * 256 + 128]
                            if typ == "d":
                                self.op("dve", lambda v: v.scalar_tensor_tensor(
                                    out=ocs[ci][:, tq, :], in0=src, scalar=rc[ri][:, 4 + tq:5 + tq], in1=o1[oi][:, tq, :],
                                    op0=ALU.mult, op1=ALU.add),
                                    reads=[self.bank_s[ob[tq // 2]], rc_s[ri], o1_s[oi]], writes=[ocs_s[ci]] if tq == 0 else [])
                            else:
                                self.op("dve", lambda v: v.tensor_scalar(out=ocs[ci][:, tq, :], in0=src,
                                                                         scalar1=rc[ri][:, tq:tq + 1], scalar2=None,
                                                                         op0=ALU.mult),
                                        reads=[self.bank_s[ob[tq // 2]], rc_s[ri]], writes=[ocs_s[ci]] if tq == 0 else [])
                        ocs_s[ci].wr = [(self.csem["dve"], self.ccnt["dve"])]
                        self.dma("sp", self.ocat.rearrange("(j p) c -> p j c", p=128)[:, Qb * 4:Qb * 4 + 4,
                                                                                      col * 128:(col + 1) * 128],
                                 ocs[ci][:], ocs_d[ci], reads=[ocs_s[ci]])
        self.end_phase()

    def phase3a(self, l):
        self.begin_phase()
        self.pump_until("wo%d" % l)
        nc = self.nc
        sb = self.sb
        moe = (l % 2 == 1)
        wo = sb("wo", [128, 8, D], BF16)
        w_s = Slot()
        d0 = self.dsem()
        w_s.wr = [self.dma("sp", wo[:], self.b_wo[l].rearrange("(c p) n -> p c n", p=128), d0)]

        def bufs(name, shape, dt, n):
            return [sb(name, shape, dt) for _ in range(n)], [Slot() for _ in range(n)]

        oc, oc_s = bufs("oc", [128, D], BF16, 2)
        xt, xt_s = bufs("xt", [128, D], F32, 2)
        sq, sq_s = bufs("sq", [128, 512], F32, 2)
        smA = [sb("smA", [128, 12], F32) for _ in range(2)]
        ss_s, lg_s, rs_s = [Slot(), Slot()], [Slot(), Slot()], [Slot(), Slot()]
        ocn, ocn_s = bufs("ocn", [128, 512], BF16, 2)
        ocT, ocT_s = bufs("ocT", [128, D], BF16, 2)
        x1 = [sb("x1", [128, D], F32) for _ in range(3)]
        x1_s = [[Slot(), Slot()] for _ in range(3)]
        smB = [sb("smB", [128, 4], F32) for _ in range(2)]
        ss2_s, lg2_s, rs2_s = [Slot(), Slot()], [Slot(), Slot()], [Slot(), Slot()]
        h2, h2_s = bufs("h2", [128, D], BF16, 2)
        stage = [sb("stage", [128, 8, 512], BF16) for _ in range(2)]
        stage_s = [Slot(), Slot()]
        if moe:
            h2f, h2f_s = bufs("h2f", [128, D], F32, 2)
            h2fT = [sb("h2fT", [128, D], F32) for _ in range(2)]
            h2fT_s = [[Slot(), Slot()] for _ in range(2)]
            rt = [sb("rt", [128, 48], F32) for _ in range(2)]
            rt_s = [[Slot() for _ in range(8)] for _ in range(2)]
        oc_d = [self.dsem() for _ in range(2)]
        xt_d = [self.dsem() for _ in range(2)]
        x1_d = [self.dsem() for _ in range(3)]
        st_d = [self.dsem() for _ in range(2)]
        TPB = [0, 1]
        MMB = [2, 3]
        FTB = [4, 5]
        LGB = [6, 7]
        self.tpi = 0
        self.mmi = 0
        self.lgi = 0

        def stA(t):
            b = t % 2
            b3 = t % 3
            rows = slice(t * 128, (t + 1) * 128)
            self.dma("sp", oc[b][:], self.ocat[rows, :], oc_d[b], writes=[oc_s[b]])
            self.dma("sp", xt[b][:], self.xr[rows, :], xt_d[b], writes=[xt_s[b]])
            oc3 = oc[b][:, 0:512].rearrange("p (g w) -> p g w", w=128)
            sq3 = sq[b][:].rearrange("p (g w) -> p g w", w=128)
            sm = smA[b]
            self.op("act", lambda a: a.activation(out=sq[b][:], in_=oc[b][:, 0:512], func=AF.Square), reads=[oc_s[b]],
                    writes=[sq_s[b]])
            self.op("dve", lambda v: v.tensor_reduce(out=sm[:, 0:4], in_=sq3, axis=AX.X, op=ALU.add), reads=[sq_s[b]],
                    writes=[ss_s[b]])
            self.rstd(sm[:, 0:4], ss_s[b], sm[:, 8:12], rs_s[b], sm[:, 4:8], lg_s[b], 1.0 / 128)
            self.op("dve", lambda v: v.tensor_tensor(out=ocn[b][:].rearrange("p (g w) -> p g w", w=128), in0=oc3,
                                                     in1=sm[:, 8:12].unsqueeze(2).broadcast_to([128, 4, 128]), op=ALU.mult),
                    reads=[oc_s[b], rs_s[b]], writes=[ocn_s[b]])
            bi = TPB[self.tpi % 2]
            self.tpi += 1
            bv = self.bank_bf(bi)
            trs = [(bv[:, k * 128:(k + 1) * 128], ocn[b][:, k * 128:(k + 1) * 128]) for k in range(4)]
            trs += [(bv[:, k * 128:(k + 1) * 128], oc[b][:, k * 128:(k + 1) * 128]) for k in range(4, 8)]
            self.tr_group(bi, trs, self.ident_b[:], reads=[ocn_s[b], oc_s[b]])
            self.op("act", lambda a: a.copy(out=ocT[b][:], in_=bv[:, :]), reads=[self.bank_s[bi]], writes=[ocT_s[b]])
            for hf in range(2):
                mb = MMB[self.mmi % 2]
                self.mmi += 1
                mms = [(self.banks[mb][:, :], ocT[b][:, k * 128:(k + 1) * 128], wo[:, k, hf * 512:(hf + 1) * 512], k == 0, k == 7)
                       for k in range(8)]
                self.mm_group(mb, mms, reads=[ocT_s[b], w_s])
                self.op("dve", lambda v: v.tensor_tensor(out=x1[b3][:, hf * 512:(hf + 1) * 512], in0=self.banks[mb][:, :],
                                                         in1=xt[b][:, hf * 512:(hf + 1) * 512], op=ALU.add),
                        reads=[self.bank_s[mb], xt_s[b]], writes=[x1_s[b3][hf]])

        def stB(t):
            b = t % 2
            b3 = t % 3
            blk, tl = t // 4, t % 4
            sg = blk % 2
            rows = slice(t * 128, (t + 1) * 128)
            sm = smB[b]
            self.dma("sp", self.xr[rows, :], x1[b3][:], x1_d[b3], reads=x1_s[b3])
            self.op("act", lambda a: a.activation(out=self.junk[:], in_=x1[b3][:], func=AF.Square, accum_out=sm[:, 0:1]),
                    reads=x1_s[b3], writes=[ss2_s[b]])
            self.rstd(sm[:, 0:1], ss2_s[b], sm[:, 2:3], rs2_s[b], sm[:, 1:2], lg2_s[b], 1.0 / D)
            self.op("dve", lambda v: v.tensor_scalar(out=h2[b][:], in0=x1[b3][:], scalar1=sm[:, 2:3], scalar2=None,
                                                     op0=ALU.mult), reads=x1_s[b3] + [rs2_s[b]], writes=[h2_s[b]])
            bi = TPB[self.tpi % 2]
            self.tpi += 1
            bv = self.bank_bf(bi)
            self.tr_group(bi, [(bv[:, k * 128:(k + 1) * 128], h2[b][:, k * 128:(k + 1) * 128]) for k in range(8)],
                          self.ident_b[:], reads=[h2_s[b]])
            stg = stage[sg]
            if tl == 0:
                self.wait("act", stage_s[sg].rd + stage_s[sg].wr)
                stage_s[sg].wr = []
                stage_s[sg].rd = []
            tok = self.op("act", lambda a: a.copy(out=stg[:, :, tl * 128:(tl + 1) * 128],
                                                  in_=bv[:, :].rearrange("p (k n) -> p k n", n=128)),
                          reads=[self.bank_s[bi]], writes=[])
            stage_s[sg].wr.append(tok)
            if tl == 3:
                self.dma("sp", self.h2t[:, :, blk * 512:(blk + 1) * 512].rearrange("k p n -> p k n"), stg[:], st_d[sg],
                         reads=[stage_s[sg]])
            if moe:
                self.op("pool", lambda p: p.tensor_scalar(out=h2f[b][:], in0=x1[b3][:], scalar1=sm[:, 2:3], scalar2=None,
                                                          op0=ALU.mult), reads=x1_s[b3] + [rs2_s[b]], writes=[h2f_s[b]])
                for hf in range(2):
                    fb = FTB[hf]
                    self.tr_group(fb, [(self.banks[fb][:, k * 128:(k + 1) * 128],
                                        h2f[b][:, (hf * 4 + k) * 128:(hf * 4 + k + 1) * 128]) for k in range(4)],
                                  self.ident_f[:], reads=[h2f_s[b]])
                    self.op("act", lambda a: a.copy(out=h2fT[b][:, hf * 512:(hf + 1) * 512], in_=self.banks[fb][:, :]),
                            reads=[self.bank_s[fb]], writes=[h2fT_s[b][hf]])
                lb = LGB[self.lgi % 2]
                self.lgi += 1
                mms = [(self.banks[lb][:, 0:8], h2fT[b][:, k * 128:(k + 1) * 128], self.rw_s[:, k, :], k == 0, k == 7)
                       for k in range(8)]
                self.mm_group(lb, mms, reads=h2fT_s[b])
                r = rt[b]
                R = rt_s[b]
                self.op("dve", lambda v: v.tensor_copy(out=r[:, 0:8], in_=self.banks[lb][:, 0:8]), reads=[self.bank_s[lb]],
                        writes=[R[0]])
                self.op("dve", lambda v: v.max(out=r[:, 8:16], in_=r[:, 0:8]), reads=[R[0]], writes=[R[1]])
                self.op("dve", lambda v: v.tensor_scalar(out=r[:, 16:24], in0=r[:, 0:8], scalar1=r[:, 8:9], scalar2=None,
                                                         op0=ALU.subtract), reads=[R[0], R[1]], writes=[R[2]])
                self.op("act", lambda a: a.activation(out=r[:, 24:32], in_=r[:, 16:24], func=AF.Exp), reads=[R[2]], writes=[R[3]])
                self.op("dve", lambda v: v.scalar_tensor_tensor(out=r[:, 32:40], in0=r[:, 0:8], scalar=r[:, 9:10], in1=r[:, 24:32],
                                                                op0=ALU.is_ge, op1=ALU.mult), reads=[R[0], R[1], R[3]],
                        writes=[R[4]])
                self.op("dve", lambda v: v.tensor_reduce(out=r[:, 40:41], in_=r[:, 32:40], axis=AX.X, op=ALU.add), reads=[R[4]],
                        writes=[R[5]])
                self.op("dve", lambda v: v.reciprocal(out=r[:, 41:42], in_=r[:, 40:41]), reads=[R[5]], writes=[R[6]])
                self.op("dve", lambda v: v.tensor_scalar(out=self.comb[:, t, :], in0=r[:, 32:40], scalar1=r[:, 41:42], scalar2=None,
                                                         op0=ALU.mult), reads=[R[4], R[6]], writes=[R[7]])

        for i in range(NT + 1):
            if i < NT:
                stA(i)
            if 0 <= i - 1 < NT:
                stB(i - 1)
            self.pump(2)
        self.end_phase()

    def phase3b(self, l):
        self.begin_phase()
        self.pump_until("ffn%d" % l)
        nc = self.nc
        sb = self.sb
        moe = (l % 2 == 1)
        NE = 8 if moe else 2
        wg = [sb("wg", [128, 8, FFE], BF16) for _ in range(2)]
        wu = [sb("wu", [128, 8, FFE], BF16) for _ in range(2)]
        wd = sb("wd", [128, NCH, D], BF16)
        wgu_s = [Slot() for _ in range(2)]
        wd_s = Slot()
        wgu_d = [self.dsem() for _ in range(2)]
        wd_d = self.dsem()
        hTb = [sb("hTb", [128, 8, 512], BF16) for _ in range(2)]
        hTb_s = [Slot() for _ in range(2)]
        hTb_d = [self.dsem() for _ in range(2)]
        aT = [sb("aT", [128, NCH, 512], BF16) for _ in range(2)]
        aT_s = [Slot() for _ in range(2)]
        NSG = 3
        sgb = [sb("sgb", [128, 512], F32) for _ in range(NSG)]
        sgb_s = [Slot() for _ in range(NSG)]
        NOS = 2
        ost = [sb("ost", [128, D], F32) for _ in range(NOS)]
        ost_s = [Slot() for _ in range(NOS)]
        ost_d = self.swsems[:NOS]
        xrow_s = [Slot() for _ in range(NT)]
        GB = [0, 1]
        UB = [2, 3]
        DB = [4, 5]
        self.gi = 0
        self.di = 0
        self.oi = 0
        self.si = 0
        self.evi = 0

        def load_gu(e):
            i = e % 2
            self._deps("sp", [], [wgu_s[i]])
            if moe:
                gsrc = self.b_mwg[e].rearrange("(c p) n -> p c n", p=128)
                usrc = self.b_mwu[e].rearrange("(c p) n -> p c n", p=128)
            else:
                gsrc = self.b_dwg[:, e * FFE:(e + 1) * FFE].rearrange("(c p) n -> p c n", p=128)
                usrc = self.b_dwu[:, e * FFE:(e + 1) * FFE].rearrange("(c p) n -> p c n", p=128)
            self.dma("sp", wg[i][:], gsrc, wgu_d[i])
            tok = self.dma("sp", wu[i][:], usrc, wgu_d[i])
            wgu_s[i].wr = [tok]
            wgu_s[i].rd = []

        def load_d(e):
            if moe:
                dsrc = self.b_mwd[e].rearrange("(c p) n -> p c n", p=128)
            else:
                dsrc = self.b_dwd[e * FFE:(e + 1) * FFE, :].rearrange("(c p) n -> p c n", p=128)
            self.dma("sp", wd[:], dsrc, wd_d, writes=[wd_s])

        def load_h(bi_):
            i = bi_ % 2
            blk = bi_ % 8
            self.dma("sp", hTb[i][:], self.h2t[:, :, blk * 512:(blk + 1) * 512].rearrange("k p n -> p k n"), hTb_d[i],
                     writes=[hTb_s[i]])

        def gate_up(e, blk, bi_):
            i = e % 2
            hi = bi_ % 2
            ai = bi_ % 2
            for c in range(NCH):
                gb = GB[self.gi % 2]
                ub = UB[self.gi % 2]
                self.gi += 1
                mms = [(self.banks[gb][:, :], wg[i][:, k, c * 128:(c + 1) * 128], hTb[hi][:, k, :], k == 0, k == 7) for k in range(8)]
                self.mm_group(gb, mms, reads=[wgu_s[i], hTb_s[hi]])
                mms = [(self.banks[ub][:, :], wu[i][:, k, c * 128:(c + 1) * 128], hTb[hi][:, k, :], k == 0, k == 7) for k in range(8)]
                self.mm_group(ub, mms, reads=[wgu_s[i], hTb_s[hi]])
                si_ = self.si % NSG
                self.si += 1
                self.op("act", lambda a: a.activation(out=sgb[si_][:], in_=self.banks[gb][:, :], func=AF.Silu),
                        reads=[self.bank_s[gb]], writes=[sgb_s[si_]])
                tok = self.op("dve", lambda v: v.tensor_tensor(out=aT[ai][:, c, :], in0=sgb[si_][:], in1=self.banks[ub][:, :],
                                                               op=ALU.mult),
                              reads=[sgb_s[si_], self.bank_s[ub]], writes=[aT_s[ai]] if c == 0 else [])
                if c > 0:
                    aT_s[ai].wr.append(tok)

        def down(e, blk, bi_):
            ai = bi_ % 2
            for tq in range(4):
                t = blk * 4 + tq
                oi_ = self.oi % NOS
                self.oi += 1
                rdt = ost_s[oi_].rd + ost_s[oi_].wr
                self.wait("act", rdt)
                self.wait("dve", rdt)
                ost_s[oi_].rd = []
                ost_s[oi_].wr = []
                for hf in range(2):
                    db = DB[self.di % 2]
                    self.di += 1
                    mms = [(self.banks[db][:, :], aT[ai][:, c, tq * 128:(tq + 1) * 128], wd[:, c, hf * 512:(hf + 1) * 512],
                            c == 0, c == NCH - 1) for c in range(NCH)]
                    self.mm_group(db, mms, reads=[aT_s[ai], wd_s])
                    eng = "act" if self.evi % 2 == 0 else "dve"
                    self.evi += 1
                    dst = ost[oi_][:, hf * 512:(hf + 1) * 512]
                    wr = []
                    if moe:
                        sc = self.comb[:, t, e:e + 1]
                        if eng == "act":
                            tok = self.op("act", lambda a: a.activation(out=dst, in_=self.banks[db][:, :], func=AF.Copy, scale=sc),
                                          reads=[self.bank_s[db]], writes=wr)
                        else:
                            tok = self.op("dve", lambda v: v.tensor_scalar(out=dst, in0=self.banks[db][:, :], scalar1=sc,
                                                                           scalar2=None, op0=ALU.mult),
                                          reads=[self.bank_s[db]], writes=wr)
                    else:
                        if eng == "act":
                            tok = self.op("act", lambda a: a.copy(out=dst, in_=self.banks[db][:, :]), reads=[self.bank_s[db]],
                                          writes=wr)
                        else:
                            tok = self.op("dve", lambda v: v.tensor_copy(out=dst, in_=self.banks[db][:, :]),
                                          reads=[self.bank_s[db]], writes=wr)
                    ost_s[oi_].wr.append(tok)
                self.dma("pool", self.xr[t * 128:(t + 1) * 128, :], ost[oi_][:], ost_d[oi_], reads=[ost_s[oi_]],
                         writes=[xrow_s[t]], accum_op=ALU.add)

        seq = [(e, blk) for e in range(NE) for blk in range(8)]
        load_gu(0)
        load_d(0)
        load_h(0)
        cur_wd = 0
        for bi_, (e, blk) in enumerate(seq):
            if bi_ + 1 < len(seq):
                load_h(bi_ + 1)
            if blk == 0 and e + 1 < NE:
                load_gu(e + 1)
            if bi_ == 0:
                gate_up(e, blk, bi_)
            if bi_ + 1 < len(seq):
                gate_up(seq[bi_ + 1][0], seq[bi_ + 1][1], bi_ + 1)
            if e != cur_wd:
                load_d(e)
                cur_wd = e
            down(e, blk, bi_)
        self.end_phase()


def _rope_table(theta, rot):
    half = rot // 2
    inv_freq = (1.0 / (np.float32(theta) ** (np.arange(half, dtype=np.float32) * np.float32(2.0 / rot)))).astype(np.float32)
    ang = (np.arange(S, dtype=np.float32)[:, None] * inv_freq[None, :]).astype(np.float32)
    c = np.cos(ang.astype(np.float64)).astype(np.float32)
    s = np.sin(ang.astype(np.float64)).astype(np.float32)
    return np.ascontiguousarray(np.concatenate([c, c, -s, s], axis=1))


_CACHE = {}


def _get_nc(**kw):
    key = tuple(sorted(kw.items()))
    if key not in _CACHE:
        kb = KB(**kw)
        kb.build()
        _CACHE[key] = kb
    return _CACHE[key]


def make_in_maps(inputs, ncores=8):
    f = lambda a: np.ascontiguousarray(np.asarray(a, dtype=np.float32))
    shared = {
        "attn_norm_g": f(inputs["attn_norm_g"]),
        "w_in": f(inputs["w_in"]),
        "diff_q_norm_g": f(inputs["diff_q_norm_g"]),
        "diff_k_norm_g": f(inputs["diff_k_norm_g"]),
        "diff_lambda": f(inputs["diff_lambda"]).reshape(DEPTH, 256),
        "diff_subln_g": f(inputs["diff_subln_g"]),
        "mla_q_ln_g": f(inputs["mla_q_ln_g"]),
        "w_uq": f(inputs["w_uq"]),
        "mla_kv_ln_g": f(inputs["mla_kv_ln_g"]),
        "w_ukv": f(inputs["w_ukv"]),
        "mla_qk_norm_g": f(inputs["mla_qk_norm_g"]).reshape(DEPTH, 384),
        "w_o": f(inputs["w_o"]),
        "ffn_norm_g": f(inputs["ffn_norm_g"]),
        "dense_w_gate": f(inputs["dense_w_gate"])[0],
        "dense_w_up": f(inputs["dense_w_up"])[0],
        "dense_w_down": f(inputs["dense_w_down"])[0],
        "router_w": f(inputs["router_w"])[0],
        "moe_w_gate": f(inputs["moe_w_gate"])[0],
        "moe_w_up": f(inputs["moe_w_up"])[0],
        "moe_w_down": f(inputs["moe_w_down"])[0],
        "cs_d": _rope_table(ROPE_THETA, 16),
        "cs_m": _rope_table(MLA_ROPE_THETA, 64),
        "ident": np.eye(128, dtype=np.float32),
    }
    x = f(inputs["x"])
    maps = []
    for c in range(ncores):
        m = dict(shared)
        m["x"] = np.ascontiguousarray(x[c])
        maps.append(m)
    return maps


def kernel(**inputs):
    kb = _get_nc()
    in_maps = make_in_maps(inputs)
    res = run_bass_kernel_spmd(kb.nc, in_maps, core_ids=list(range(8)))
    out = np.stack([np.asarray(r["y"], dtype=np.float32) for r in res.results], axis=0)
    return out
```

```python
import math
from contextlib import ExitStack
import numpy as np
import ml_dtypes
import concourse.bass as bass
import concourse.mybir as mybir
from concourse.bass_utils import run_bass_kernel_spmd

F32 = mybir.dt.float32
BF16 = mybir.dt.bfloat16
ALU = mybir.AluOpType
AF = mybir.ActivationFunctionType
AX = mybir.AxisListType

S = 4096
D = 1024
NT = S // 128
DEPTH = 2
EPS = 1e-6
INC = 1984
FFE = 1408
NCH = FFE // 128
ROPE_THETA = 500000.0
MLA_ROPE_THETA = 10000.0


class DSem:
    def __init__(self, h):
        self.h = h
        self.v = 0


class Slot:
    __slots__ = ("wr", "rd")

    def __init__(self):
        self.wr = []
        self.rd = []


class KB:
    def __init__(self, depth=DEPTH, stop_after=None, debug=False):
        self.depth = depth
        self.stop_after = stop_after
        self.debug = debug
        nc = self.nc = bass.Bass("TRN2", target_bir_lowering=False)
        self.E = dict(pe=nc.tensor, dve=nc.vector, act=nc.scalar, pool=nc.gpsimd, sp=nc.sync)
        self.csem = {k: nc.alloc_semaphore("c_" + k) for k in ("pe", "dve", "act", "pool")}
        self.ccnt = dict.fromkeys(self.csem, 0)
        self.waited = {}
        self.dsems = [DSem(nc.alloc_semaphore("d%d" % i)) for i in range(72)]
        self.dptr = 0
        self.swsems = [DSem(nc.alloc_semaphore("sw%d" % i)) for i in range(3)]
        self.banks = [nc.alloc_psum_tensor("bank%d" % i, [128, 512], F32) for i in range(8)]
        self.bank_s = [Slot() for _ in range(8)]
        self.stack = None
        self.uid = 0
        self.const_s = None
        self.log = [] if debug else None

    def sig(self, e, ins):
        self.ccnt[e] += 1
        ins.then_inc(self.csem[e], 1)
        if self.log is not None:
            self.log.append(("inc", e, self.csem[e].num, 1))
        return (self.csem[e], self.ccnt[e])

    def wait(self, e, toks):
        for tok in toks:
            sem, v = tok
            key = (e, sem.num)
            if self.waited.get(key, 0) >= v:
                continue
            self.waited[key] = v
            self.E[e].wait_ge(sem, v)
            if self.log is not None:
                self.log.append(("wait", e, sem.num, v))

    def _deps(self, e, reads, writes):
        toks = []
        for s in reads:
            toks.extend(s.wr)
        for s in writes:
            toks.extend(s.rd)
            toks.extend(s.wr)
        self.wait(e, toks)

    def _commit(self, tok, reads, writes):
        for s in reads:
            if s is not self.const_s:
                s.rd.append(tok)
        for s in writes:
            s.wr = [tok]
            s.rd = []

    def op(self, e, fn, reads=(), writes=()):
        self._deps(e, reads, writes)
        ins = fn(self.E[e])
        tok = self.sig(e, ins)
        self._commit(tok, reads, writes)
        return tok

    def dsem(self):
        d = self.dsems[self.dptr]
        self.dptr += 1
        return d

    def dma(self, q, out, in_, ds, reads=(), writes=(), **kw):
        self._deps(q, reads, writes)
        ins = self.E[q].dma_start(out=out, in_=in_, **kw)
        ins.then_inc(ds.h, 16)
        ds.v += 16
        if self.log is not None:
            self.log.append(("dma", q, ds.h.num, 16))
        tok = (ds.h, ds.v)
        self._commit(tok, reads, writes)
        return tok

    def mm_group(self, bank_i, mms, reads=()):
        bs = self.bank_s[bank_i]
        self._deps("pe", reads, [bs])
        ins = None
        for (o, l, r, st, sp) in mms:
            ins = self.nc.tensor.matmul(o, lhsT=l, rhs=r, start=st, stop=sp)
        tok = self.sig("pe", ins)
        self._commit(tok, reads, [bs])
        return tok

    def tr_group(self, bank_i, trs, ident, reads=()):
        bs = self.bank_s[bank_i]
        self._deps("pe", reads, [bs])
        ins = None
        for (o, i) in trs:
            ins = self.nc.tensor.transpose(out=o, in_=i, identity=ident)
        tok = self.sig("pe", ins)
        self._commit(tok, reads, [bs])
        return tok

    def barrier(self):
        toks = [(self.csem[c], self.ccnt[c]) for c in self.csem if self.ccnt[c] > 0]
        toks += [(d.h, d.v) for d in self.dsems + self.swsems if d.v > 0]
        for e in self.E:
            self.wait(e, toks)

    def begin_phase(self):
        self.barrier()
        self.stack = ExitStack()
        self.dptr = self.dkeep
        for b in self.bank_s:
            b.wr = []
            b.rd = []

    def end_phase(self):
        self.barrier()
        self.stack.close()
        self.stack = None

    def sb(self, name, shape, dt):
        self.uid += 1
        return self.stack.enter_context(self.nc.sbuf_tensor("%s_%d" % (name, self.uid), shape, dt))

    def interleave(self, gens):
        gens = list(gens)
        while gens:
            nxt = []
            for g in gens:
                try:
                    next(g)
                    nxt.append(g)
                except StopIteration:
                    pass
            gens = nxt

    def bank_bf(self, i):
        return self.banks[i][:].bitcast(BF16)

    def build(self):
        nc = self.nc

        def din(name, shape):
            return nc.dram_tensor(name, shape, F32, kind="ExternalInput").ap()

        self.x = din("x", [S, D])
        self.attn_g = din("attn_norm_g", [DEPTH, D])
        self.w_in = din("w_in", [DEPTH, D, INC])
        self.dq_g = din("diff_q_norm_g", [DEPTH, 64])
        self.dk_g = din("diff_k_norm_g", [DEPTH, 64])
        self.dlam = din("diff_lambda", [DEPTH, 256])
        self.subln_g = din("diff_subln_g", [DEPTH, 128])
        self.qln_g = din("mla_q_ln_g", [DEPTH, 256])
        self.w_uq = din("w_uq", [DEPTH, 256, 768])
        self.kvln_g = din("mla_kv_ln_g", [DEPTH, 128])
        self.w_ukv = din("w_ukv", [DEPTH, 128, 1024])
        self.mqk_g = din("mla_qk_norm_g", [DEPTH, 2 * 192])
        self.w_o = din("w_o", [DEPTH, D, D])
        self.ffn_g = din("ffn_norm_g", [DEPTH, D])
        self.dwg = din("dense_w_gate", [D, 2816])
        self.dwu = din("dense_w_up", [D, 2816])
        self.dwd = din("dense_w_down", [2816, D])
        self.rw = din("router_w", [D, 8])
        self.mwg = din("moe_w_gate", [8, D, FFE])
        self.mwu = din("moe_w_up", [8, D, FFE])
        self.mwd = din("moe_w_down", [8, FFE, D])
        self.cs_d = din("cs_d", [S, 32])
        self.cs_m = din("cs_m", [S, 128])
        self.ident_in = din("ident", [128, 128])
        self.y = nc.dram_tensor("y", [S, D], F32, kind="ExternalOutput").ap()
        xr = self.xr = self.y

        kind = "ExternalOutput" if self.debug else "Internal"

        def dscr(name, shape, dt):
            return nc.dram_tensor(name, shape, dt, kind=kind).ap()

        self.qkt = dscr("qkt", [20, 128, S], BF16)
        self.vd = dscr("vd", [S, 512], BF16)
        self.vm = dscr("vm", [S, 512], BF16)
        self.oct_d = dscr("oct_d", [8, 128, S], BF16)
        self.h2t = dscr("h2t", [8, 128, S], BF16)
        self.b_win = dscr("b_win", [DEPTH, D, INC], BF16)
        self.b_wuq = dscr("b_wuq", [DEPTH, 256, 768], BF16)
        self.b_wukv = dscr("b_wukv", [DEPTH, 128, 1024], BF16)
        self.b_wo = dscr("b_wo", [DEPTH, D, D], BF16)
        self.b_dwg = dscr("b_dwg", [D, 2816], BF16)
        self.b_dwu = dscr("b_dwu", [D, 2816], BF16)
        self.b_dwd = dscr("b_dwd", [2816, D], BF16)
        self.b_mwg = dscr("b_mwg", [8, D, FFE], BF16)
        self.b_mwu = dscr("b_mwu", [8, D, FFE], BF16)
        self.b_mwd = dscr("b_mwd", [8, FFE, D], BF16)

        A = nc.alloc_sbuf_tensor
        self.ident_f = A("ident_f", [128, 128], F32)
        self.ident_b = A("ident_b", [128, 128], BF16)
        self.ones_b = A("ones_b", [128, 128], BF16)
        self.sc_attn = A("sc_attn", [128, DEPTH, 8], F32)
        self.sc_ffn = A("sc_ffn", [128, DEPTH, 8], F32)
        self.sc_qln = A("sc_qln", [128, DEPTH, 2], F32)
        self.sc_kvln = A("sc_kvln", [128, DEPTH, 1], F32)
        self.sc_wo = A("sc_wo", [128, DEPTH, 8], F32)
        self.ones = A("ones", [128, 24], F32)
        self.gd = A("gd", [128, DEPTH, 2, 64], F32)
        self.gm = A("gm", [128, DEPTH, 2, 192], F32)
        self.lamt = A("lamt", [128, DEPTH, 256], F32)
        self.lamw = A("lamw", [128, 8], F32)
        self.neglam = A("neglam", [128, DEPTH], F32)
        self.negM = A("negM", [128, DEPTH, 2], F32)
        self.gmax = A("gmax", [128, 8], F32)
        self.rw_s = A("rw_s", [128, 8, 8], F32)
        self.comb = A("comb", [128, NT, 8], F32)
        self.junk = A("junk", [128, 1024], BF16)
        self.const_s = Slot()

        self.setup_consts()
        self.setup_wjobs()
        self.dkeep = self.dptr
        if self.log is not None:
            snap = {self.csem[c].num: self.ccnt[c] for c in self.csem}
            snap.update({d.h.num: d.v for d in self.dsems + self.swsems + [self.xinit]})
            self.log.append(("snapshot", snap))
        for l in range(self.depth):
            self.phase1(l)
            if self.stop_after == "p1_%d" % l:
                return self.finish()
            self.phase2(l)
            if self.stop_after == "p2_%d" % l:
                return self.finish()
            self.phase3a(l)
            if self.stop_after == "p3a_%d" % l:
                return self.finish()
            self.phase3b(l)
            if self.stop_after == "p3b_%d" % l:
                return self.finish()
        return self.finish()

    def finish(self):
        self.barrier()
        return self.nc

    def setup_consts(self):
        nc = self.nc
        cs = self.const_s
        ds = self.dsem()
        toks = []

        def ld(out, in_, **kw):
            ins = nc.sync.dma_start(out=out, in_=in_, **kw)
            ins.then_inc(ds.h, 16)
            ds.v += 16

        self.xinit = self.dsem()

        ld(self.ident_f[:], self.ident_in[:, :])
        for l in range(DEPTH):
            ld(self.sc_attn[:, l, :], self.attn_g[l].rearrange("(c p) -> p c", p=128), allow_slow_non_contiguous=True)
            ld(self.sc_ffn[:, l, :], self.ffn_g[l].rearrange("(c p) -> p c", p=128), allow_slow_non_contiguous=True)
            ld(self.sc_qln[:, l, :], self.qln_g[l].rearrange("(c p) -> p c", p=128), allow_slow_non_contiguous=True)
            ld(self.sc_kvln[:, l, :], self.kvln_g[l].rearrange("(c p) -> p c", p=128), allow_slow_non_contiguous=True)
            ld(self.sc_wo[:, l, 0:1], self.subln_g[l].rearrange("(c p) -> p c", p=128), allow_slow_non_contiguous=True)
            ld(self.gd[:, l, 0, :], self.dq_g[l].partition_broadcast(128))
            ld(self.gd[:, l, 1, :], self.dk_g[l].partition_broadcast(128))
            ld(self.gm[:, l, :, :].rearrange("p a b -> p (a b)"), self.mqk_g[l].partition_broadcast(128))
            ld(self.lamt[:, l, :], self.dlam[l].partition_broadcast(128))
        ld(self.rw_s[:], self.rw.rearrange("(c p) n -> p c n", p=128))
        nc.vector.wait_ge(ds.h, ds.v)
        nc.gpsimd.wait_ge(ds.h, ds.v)
        nc.scalar.wait_ge(ds.h, ds.v)
        V = nc.vector
        sv = self.csem["dve"]

        def vop(ins):
            self.ccnt["dve"] += 1
            ins.then_inc(sv, 1)
            V.wait_ge(sv, self.ccnt["dve"])

        def aop(ins):
            self.ccnt["act"] += 1
            ins.then_inc(self.csem["act"], 1)
            nc.scalar.wait_ge(self.csem["act"], self.ccnt["act"])
            V.wait_ge(self.csem["act"], self.ccnt["act"])

        vop(V.tensor_copy(out=self.ident_b[:], in_=self.ident_f[:]))
        vop(V.memset(self.ones[:], 1.0))
        vop(V.memset(self.ones_b[:], 1.0))
        for l in range(DEPTH):
            lam_init = 0.8 - 0.6 * math.exp(-0.3 * l)
            vop(V.tensor_scalar(out=self.sc_wo[:, l, 0:1], in0=self.sc_wo[:, l, 0:1], scalar1=float(1.0 - lam_init),
                                scalar2=None, op0=ALU.mult))
            for c in range(1, 4):
                vop(V.tensor_copy(out=self.sc_wo[:, l, c:c + 1], in_=self.sc_wo[:, l, 0:1]))
            vop(V.memset(self.sc_wo[:, l, 4:8], 1.0))
        for l in range(DEPTH):
            lam_init = 0.8 - 0.6 * math.exp(-0.3 * l)
            vop(V.tensor_tensor(out=self.lamt[:, l, 0:64], in0=self.lamt[:, l, 0:64], in1=self.lamt[:, l, 64:128], op=ALU.mult))
            vop(V.tensor_tensor(out=self.lamt[:, l, 128:192], in0=self.lamt[:, l, 128:192], in1=self.lamt[:, l, 192:256], op=ALU.mult))
            vop(V.tensor_reduce(out=self.lamw[:, 0:1], in_=self.lamt[:, l, 0:64], axis=AX.X, op=ALU.add))
            vop(V.tensor_reduce(out=self.lamw[:, 1:2], in_=self.lamt[:, l, 128:192], axis=AX.X, op=ALU.add))
            nc.scalar.wait_ge(sv, self.ccnt["dve"])
            aop(nc.scalar.activation(out=self.lamw[:, 2:4], in_=self.lamw[:, 0:2], func=AF.Exp))
            vop(V.tensor_tensor(out=self.lamw[:, 4:5], in0=self.lamw[:, 3:4], in1=self.lamw[:, 2:3], op=ALU.subtract))
            vop(V.tensor_scalar(out=self.neglam[:, l:l + 1], in0=self.lamw[:, 4:5], scalar1=float(-lam_init),
                                scalar2=None, op0=ALU.add))
            vop(V.tensor_reduce(out=self.gmax[:, 0:2], in_=self.gd[:, l, :, :], axis=AX.X, op=ALU.max,
                                apply_absolute_value=True))
            vop(V.tensor_reduce(out=self.gmax[:, 2:4], in_=self.gm[:, l, :, :], axis=AX.X, op=ALU.max,
                                apply_absolute_value=True))
            vop(V.tensor_tensor(out=self.gmax[:, 4:5], in0=self.gmax[:, 0:1], in1=self.gmax[:, 1:2], op=ALU.mult))
            vop(V.tensor_tensor(out=self.gmax[:, 5:6], in0=self.gmax[:, 2:3], in1=self.gmax[:, 3:4], op=ALU.mult))
            vop(V.tensor_scalar(out=self.negM[:, l, 0:1], in0=self.gmax[:, 4:5], scalar1=float(-math.sqrt(64.0)),
                                scalar2=None, op0=ALU.mult))
            vop(V.tensor_scalar(out=self.negM[:, l, 1:2], in0=self.gmax[:, 5:6], scalar1=float(-math.sqrt(192.0)),
                                scalar2=None, op0=ALU.mult))
        for c in range(8):
            vop(V.tensor_scalar(out=self.rw_s[:, c, :], in0=self.rw_s[:, c, :], scalar1=self.sc_ffn[:, 1, c:c + 1],
                                scalar2=None, op0=ALU.mult))
        vop(V.memset(self.comb[:], 1.0))

    def setup_wjobs(self):
        nc = self.nc
        NB = 3
        CM = 1408
        self.w_fst = [nc.alloc_sbuf_tensor("wf%d" % i, [128, CM], F32) for i in range(NB)]
        self.w_bst = [nc.alloc_sbuf_tensor("wb%d" % i, [128, CM], BF16) for i in range(NB)]
        self.w_fs = [Slot() for _ in range(NB)]
        self.w_bs = [Slot() for _ in range(NB)]
        self.w_fd = [self.dsem() for _ in range(NB)]
        self.w_bd = [self.dsem() for _ in range(NB)]
        self.w_nb = NB
        self.wjobs = []
        self.wnext = 0
        self.wpending = []
        self.wtags = {}

        def add(src, dst, R, C, sc):
            ncs = 1 if C <= CM else 2
            cw = C // ncs
            for c in range(R // 128):
                for j in range(ncs):
                    self.wjobs.append((src[c * 128:(c + 1) * 128, j * cw:(j + 1) * cw],
                                       dst[c * 128:(c + 1) * 128, j * cw:(j + 1) * cw], cw, sc(c)))

        one = lambda c: self.ones[:, 0:1]

        def attn(l):
            add(self.w_in[l], self.b_win[l], D, INC, lambda c, l=l: self.sc_attn[:, l, c:c + 1])
            add(self.w_uq[l], self.b_wuq[l], 256, 768, lambda c, l=l: self.sc_qln[:, l, c:c + 1])
            add(self.w_ukv[l], self.b_wukv[l], 128, 1024, lambda c, l=l: self.sc_kvln[:, l, c:c + 1])
            self.wtags["attn%d" % l] = len(self.wjobs)
            add(self.w_o[l], self.b_wo[l], D, D, lambda c, l=l: self.sc_wo[:, l, c:c + 1])
            self.wtags["wo%d" % l] = len(self.wjobs)

        attn(0)
        add(self.dwg, self.b_dwg, D, 2816, lambda c: self.sc_ffn[:, 0, c:c + 1])
        add(self.dwu, self.b_dwu, D, 2816, lambda c: self.sc_ffn[:, 0, c:c + 1])
        add(self.dwd, self.b_dwd, 2816, D, one)
        self.wtags["ffn0"] = len(self.wjobs)
        if self.depth > 1:
            attn(1)
            for e in range(8):
                add(self.mwg[e], self.b_mwg[e], D, FFE, lambda c: self.sc_ffn[:, 1, c:c + 1])
                add(self.mwu[e], self.b_mwu[e], D, FFE, lambda c: self.sc_ffn[:, 1, c:c + 1])
                add(self.mwd[e], self.b_mwd[e], FFE, D, one)
            self.wtags["ffn1"] = len(self.wjobs)

    def _wflush(self):
        for (i, dst, C) in self.wpending:
            self.dma("sp", dst, self.w_bst[i][:, 0:C], self.w_bd[i], reads=[self.w_bs[i]])
        self.wpending = []

    def pump(self, n, engs=("dve", "act")):
        if self.wnext >= len(self.wjobs):
            if self.wpending:
                self._wflush()
            return
        self._wflush()
        for _ in range(n):
            if self.wnext >= len(self.wjobs):
                break
            src, dst, C, scal = self.wjobs[self.wnext]
            i = self.wnext % self.w_nb
            eng = engs[self.wnext % len(engs)]
            self.wnext += 1
            fst, bst = self.w_fst[i], self.w_bst[i]
            self.dma("sp", fst[:, 0:C], src, self.w_fd[i], writes=[self.w_fs[i]])
            if eng == "act":
                self.op("act", lambda a: a.activation(out=bst[:, 0:C], in_=fst[:, 0:C], func=AF.Copy, scale=scal),
                        reads=[self.w_fs[i]], writes=[self.w_bs[i]])
            else:
                self.op(eng, lambda v: v.tensor_scalar(out=bst[:, 0:C], in0=fst[:, 0:C], scalar1=scal, scalar2=None,
                                                       op0=ALU.mult), reads=[self.w_fs[i]], writes=[self.w_bs[i]])
            self.wpending.append((i, dst, C))

    def pump_until(self, tag, engs=("dve", "act")):
        tgt = self.wtags[tag]
        while self.wnext < tgt:
            self.pump(min(2, tgt - self.wnext), engs)
        self._wflush()
        self.wait("sp", [(d.h, d.v) for d in self.w_bd if d.v > 0])

    def rstd(self, src_ap, src_slot, out_ap, out_slot, tmp_ap, tmp_slot, inv_w):
        self.op("act", lambda a: a.activation(out=tmp_ap, in_=src_ap, func=AF.Ln, scale=float(inv_w), bias=float(EPS)),
                reads=[src_slot], writes=[tmp_slot])
        self.op("act", lambda a: a.activation(out=out_ap, in_=tmp_ap, func=AF.Exp, scale=-0.5),
                reads=[tmp_slot], writes=[out_slot])

    def normrope(self, W_, src3, src_slots, G, W, gain3, R, rot_lo, cs_ap, cs_slot, dst_nonrot, dst_rot, dst_slot,
                 nonrot_lo, nonrot_hi):
        sq, sq_s, ssg, ssg_s, lg, lg_s, rsg, rsg_s, tmp, tmp_s, xr_, xr_s, ac, ac_s, sw, sw_s = W_
        n = G * W
        h = R // 2
        sq3 = sq[:, 0:n].rearrange("p (g w) -> p g w", w=W)
        tmp3 = tmp[:, 0:n].rearrange("p (g w) -> p g w", w=W)
        xr3 = xr_[:, 0:G * R].rearrange("p (g r) -> p g r", r=R)
        ac3 = ac[:, 0:G * R].rearrange("p (g r) -> p g r", r=R)
        sw3 = sw[:, 0:G * R].rearrange("p (g r) -> p g r", r=R)
        self.op("act", lambda a: a.activation(out=sq3, in_=src3, func=AF.Square), reads=src_slots, writes=[sq_s])
        yield
        self.op("dve", lambda v: v.tensor_reduce(out=ssg[:, 0:G], in_=sq3, axis=AX.X, op=ALU.add),
                reads=[sq_s], writes=[ssg_s])
        yield
        self.rstd(ssg[:, 0:G], ssg_s, rsg[:, 0:G], rsg_s, lg[:, 0:G], lg_s, 1.0 / W)
        yield
        self.op("dve", lambda v: v.tensor_tensor(out=tmp3, in0=src3, in1=rsg[:, 0:G].unsqueeze(2).broadcast_to([128, G, W]),
                                                 op=ALU.mult),
                reads=list(src_slots) + [rsg_s], writes=[tmp_s])
        yield
        self.op("pool", lambda p: p.tensor_tensor(out=dst_nonrot, in0=tmp3[:, :, nonrot_lo:nonrot_hi],
                                                  in1=gain3[:, :, nonrot_lo:nonrot_hi], op=ALU.mult),
                reads=[tmp_s], writes=[dst_slot])
        yield
        self.op("pool", lambda p: p.tensor_tensor(out=xr3, in0=tmp3[:, :, rot_lo:rot_lo + R],
                                                  in1=gain3[:, :, rot_lo:rot_lo + R], op=ALU.mult),
                reads=[tmp_s], writes=[xr_s])
        yield
        cc = cs_ap[:, 0:R].unsqueeze(1).broadcast_to([128, G, R])
        ns = cs_ap[:, R:R + h].unsqueeze(1).broadcast_to([128, G, h])
        ps_ = cs_ap[:, R + h:2 * R].unsqueeze(1).broadcast_to([128, G, h])
        self.op("pool", lambda p: p.tensor_tensor(out=ac3, in0=xr3, in1=cc, op=ALU.mult),
                reads=[xr_s, cs_slot], writes=[ac_s])
        yield
        self.op("pool", lambda p: p.tensor_tensor(out=sw3[:, :, 0:h], in0=xr3[:, :, h:R], in1=ns, op=ALU.mult),
                reads=[xr_s, cs_slot], writes=[sw_s])
        yield
        self.op("pool", lambda p: p.tensor_tensor(out=sw3[:, :, h:R], in0=xr3[:, :, 0:h], in1=ps_, op=ALU.mult),
                reads=[xr_s, cs_slot], writes=[sw_s])
        yield
        tok = self.op("pool", lambda p: p.tensor_tensor(out=dst_rot, in0=ac3, in1=sw3, op=ALU.add),
                      reads=[ac_s, sw_s], writes=[dst_slot])
        yield
        return tok

    def phase1(self, l):
        self.begin_phase()
        self.pump_until("attn%d" % l)
        xsrc = self.x if l == 0 else self.xr
        nc = self.nc
        sb = self.sb
        win = sb("win", [128, 8, INC], BF16)
        wuq = sb("wuq", [128, 2, 768], BF16)
        wukv = sb("wukv", [128, 1024], BF16)
        w_s = Slot()
        d0 = self.dsem()
        self.dma("sp", win[:], self.b_win[l].rearrange("(c p) n -> p c n", p=128), d0, writes=[])
        self.dma("sp", wuq[:], self.b_wuq[l].rearrange("(c p) n -> p c n", p=128), d0, writes=[])
        tokw = self.dma("sp", wukv[:], self.b_wukv[l], d0, writes=[])
        w_s.wr = [tokw]

        def bufs(name, shape, dt, n):
            return [sb(name, shape, dt) for _ in range(n)], [Slot() for _ in range(n)]

        xt, xt_s = bufs("xt", [128, D], F32, 2)
        hb, hb_s = bufs("hb", [128, D], BF16, 2)
        hT, hT_s = bufs("hT", [128, D], BF16, 2)
        pj = [sb("pj", [128, INC], F32) for _ in range(2)]
        pj_s = [[Slot() for _ in range(4)] for _ in range(2)]
        smA = [sb("smA", [128, 4], F32) for _ in range(2)]
        ss_s, lg_s, rs_s = [Slot(), Slot()], [Slot(), Slot()], [Slot(), Slot()]
        smB = [sb("smB", [128, 8], F32) for _ in range(2)]
        ssc_s, lgc_s, rsc_s = [Slot(), Slot()], [Slot(), Slot()], [Slot(), Slot()]
        cn, cn_s = bufs("cn", [128, 384], BF16, 2)
        cT, cT_s = bufs("cT", [128, 384], BF16, 2)
        qm = [sb("qm", [128, 768], F32) for _ in range(2)]
        qm_s = [[Slot(), Slot()] for _ in range(2)]
        kvm = [sb("kvm", [128, 1024], F32) for _ in range(2)]
        kvm_s = [[Slot(), Slot()] for _ in range(2)]
        kf, kf_s = bufs("kf", [128, 768], F32, 2)
        dA, dA_s = bufs("dA", [128, 1024], BF16, 2)
        mQ, mQ_s = bufs("mQ", [128, 768], BF16, 2)
        mK, mK_s = bufs("mK", [128, 768], BF16, 2)
        vb, vb_s = bufs("vb", [128, 512], BF16, 3)
        vmb, vmb_s = bufs("vmb", [128, 512], BF16, 3)
        csd, csd_s = bufs("csd", [128, 32], F32, 3)
        csm, csm_s = bufs("csm", [128, 128], F32, 3)
        stage = [sb("stage", [128, 20, 256], BF16) for _ in range(2)]
        stage_s = [Slot(), Slot()]
        xt_d = [self.dsem() for _ in range(2)]
        cs_d_ = [self.dsem() for _ in range(3)]
        cs_m_ = [self.dsem() for _ in range(3)]
        vb_d = [self.dsem() for _ in range(3)]
        vmb_d = [self.dsem() for _ in range(3)]
        st_d = [self.dsem() for _ in range(2)]

        def mkscratch(n, gr):
            return (sb("sq", [128, n], F32), Slot(), sb("ssg", [128, 16], F32), Slot(), sb("lg", [128, 16], F32), Slot(),
                    sb("rsg", [128, 16], F32), Slot(), sb("tmp", [128, n], F32), Slot(), sb("xr_", [128, gr], F32), Slot(),
                    sb("ac", [128, gr], F32), Slot(), sb("sw", [128, gr], F32), Slot())

        W_d = mkscratch(1024, 256)
        W_q = mkscratch(768, 256)
        W_k = mkscratch(768, 256)

        gdf = sb("gdf", [128, 16, 64], F32)
        gdf_s = Slot()
        self.op("dve", lambda v: v.tensor_copy(out=gdf[:, 0:8, :], in_=self.gd[:, l, 0, :].unsqueeze(1).broadcast_to([128, 8, 64])),
                writes=[gdf_s])
        self.op("dve", lambda v: v.tensor_copy(out=gdf[:, 8:16, :], in_=self.gd[:, l, 1, :].unsqueeze(1).broadcast_to([128, 8, 64])),
                writes=[gdf_s])
        self.wait("pool", gdf_s.wr)
        gd3 = gdf[:]
        gq3 = self.gm[:, l, 0, :].unsqueeze(1).broadcast_to([128, 4, 192])
        gk3 = self.gm[:, l, 1, :].unsqueeze(1).broadcast_to([128, 4, 192])
        TPB = [0, 1]
        MMB = [2, 3, 4, 5, 6, 7]
        self.mmi = 0
        self.tpi = 0
        self.evi = 0

        def evac(bank_i, src_ap, dst_ap, writes):
            eng = "act" if self.evi % 2 == 0 else "dve"
            self.evi += 1
            if eng == "act":
                return self.op("act", lambda a: a.copy(out=dst_ap, in_=src_ap), reads=[self.bank_s[bank_i]], writes=writes)
            return self.op("dve", lambda v: v.tensor_copy(out=dst_ap, in_=src_ap), reads=[self.bank_s[bank_i]], writes=writes)

        def project(lhs_chunks, rhs_fn, ncols, dst, dst_slots, reads):
            n0 = 0
            i = 0
            while n0 < ncols:
                w = min(512, ncols - n0)
                bi = MMB[self.mmi % len(MMB)]
                self.mmi += 1
                K = len(lhs_chunks)
                mms = [(self.banks[bi][:, 0:w], lhs_chunks[k], rhs_fn(k, n0, w), k == 0, k == K - 1) for k in range(K)]
                self.mm_group(bi, mms, reads=reads)
                evac(bi, self.banks[bi][:, 0:w], dst[:, n0:n0 + w], [dst_slots[i]])
                n0 += w
                i += 1

        def transposes(src_ap, ncol_blocks, src_slots, dst_ap, dst_writes):
            bi = TPB[self.tpi % 2]
            self.tpi += 1
            bv = self.bank_bf(bi)
            trs = [(bv[:, k * 128:(k + 1) * 128], src_ap[:, k * 128:(k + 1) * 128]) for k in range(ncol_blocks)]
            self.tr_group(bi, trs, self.ident_b[:], reads=src_slots)
            src = bv[:, 0:ncol_blocks * 128]
            if len(dst_ap.shape) == 3:
                src = src.rearrange("p (k n) -> p k n", n=128)
            return evac(bi, src, dst_ap, dst_writes)

        def stA(t):
            b = t % 2
            b3 = t % 3
            rows = slice(t * 128, (t + 1) * 128)
            sm = smA[b]
            self.dma("sp", xt[b][:], xsrc[rows, :], xt_d[b], writes=[xt_s[b]])
            yield
            self.dma("sp", csd[b3][:], self.cs_d[rows, :], cs_d_[b3], writes=[csd_s[b3]])
            yield
            self.dma("sp", csm[b3][:], self.cs_m[rows, :], cs_m_[b3], writes=[csm_s[b3]])
            yield
            self.op("act", lambda a: a.activation(out=self.junk[:], in_=xt[b][:], func=AF.Square, accum_out=sm[:, 0:1]),
                    reads=[xt_s[b]], writes=[ss_s[b]])
            yield
            self.rstd(sm[:, 0:1], ss_s[b], sm[:, 2:3], rs_s[b], sm[:, 1:2], lg_s[b], 1.0 / D)
            yield
            self.op("dve", lambda v: v.tensor_scalar(out=hb[b][:], in0=xt[b][:], scalar1=sm[:, 2:3], scalar2=None,
                                                     op0=ALU.mult), reads=[xt_s[b], rs_s[b]], writes=[hb_s[b]])
            yield
            transposes(hb[b], 8, [hb_s[b]], hT[b][:], [hT_s[b]])
            yield
            project([hT[b][:, k * 128:(k + 1) * 128] for k in range(8)],
                    lambda k, n0, w: win[:, k, n0:n0 + w], INC, pj[b], pj_s[b], [hT_s[b], w_s])
            yield
            self.op("act", lambda a: a.copy(out=vb[b3][:], in_=pj[b][:, 1024:1536]), reads=[pj_s[b][2]], writes=[vb_s[b3]])
            yield

        def stB(t):
            b = t % 2
            b3 = t % 3
            rows = slice(t * 128, (t + 1) * 128)
            sm = smB[b]
            self.dma("sp", self.vd[rows, :], vb[b3][:], vb_d[b3], reads=[vb_s[b3]])
            yield
            src3 = pj[b][:, 0:1024].rearrange("p (g w) -> p g w", w=64)
            dA3 = dA[b][:].rearrange("p (g w) -> p g w", w=64)
            yield from self.normrope(W_d, src3, [pj_s[b][0], pj_s[b][1]], 16, 64, gd3, 16, 0, csd[b3], csd_s[b3],
                          dA3[:, :, 16:64], dA3[:, :, 0:16], dA_s[b], 16, 64)
            self.op("act", lambda a: a.activation(out=self.junk[:, 0:256], in_=pj[b][:, 1536:1792], func=AF.Square,
                                                  accum_out=sm[:, 0:1]), reads=[pj_s[b][3]], writes=[ssc_s[b]])
            yield
            self.op("act", lambda a: a.activation(out=self.junk[:, 0:128], in_=pj[b][:, 1792:1920], func=AF.Square,
                                                  accum_out=sm[:, 1:2]), reads=[pj_s[b][3]], writes=[ssc_s[b]])
            yield
            self.op("act", lambda a: a.activation(out=sm[:, 2:3], in_=sm[:, 0:1], func=AF.Ln, scale=1.0 / 256, bias=float(EPS)),
                    reads=[ssc_s[b]], writes=[lgc_s[b]])
            yield
            self.op("act", lambda a: a.activation(out=sm[:, 3:4], in_=sm[:, 1:2], func=AF.Ln, scale=1.0 / 128, bias=float(EPS)),
                    reads=[ssc_s[b]], writes=[lgc_s[b]])
            yield
            self.op("act", lambda a: a.activation(out=sm[:, 4:6], in_=sm[:, 2:4], func=AF.Exp, scale=-0.5),
                    reads=[lgc_s[b]], writes=[rsc_s[b]])
            yield
            self.op("dve", lambda v: v.tensor_scalar(out=cn[b][:, 0:256], in0=pj[b][:, 1536:1792], scalar1=sm[:, 4:5],
                                                     scalar2=None, op0=ALU.mult), reads=[pj_s[b][3], rsc_s[b]], writes=[cn_s[b]])
            yield
            self.op("dve", lambda v: v.tensor_scalar(out=cn[b][:, 256:384], in0=pj[b][:, 1792:1920], scalar1=sm[:, 5:6],
                                                     scalar2=None, op0=ALU.mult), reads=[pj_s[b][3], rsc_s[b]],
                    writes=[cn_s[b]])
            yield
            transposes(cn[b], 3, [cn_s[b]], cT[b][:], [cT_s[b]])
            yield
            project([cT[b][:, 0:128], cT[b][:, 128:256]], lambda k, n0, w: wuq[:, k, n0:n0 + w], 768, qm[b], qm_s[b],
                    [cT_s[b], w_s])
            yield
            project([cT[b][:, 256:384]], lambda k, n0, w: wukv[:, n0:n0 + w], 1024, kvm[b], kvm_s[b], [cT_s[b], w_s])
            yield
            kvm4 = kvm[b][:].rearrange("p (h c) -> p h c", c=256)
            kf3 = kf[b][:].rearrange("p (h c) -> p h c", c=192)
            self.op("pool", lambda p: p.tensor_copy(out=kf3[:, :, 0:128], in_=kvm4[:, :, 0:128]), reads=kvm_s[b],
                    writes=[kf_s[b]])
            yield
            self.op("pool", lambda p: p.tensor_copy(out=kf3[:, :, 128:192],
                                                    in_=pj[b][:, 1920:1984].unsqueeze(1).broadcast_to([128, 4, 64])),
                    reads=[pj_s[b][3]], writes=[kf_s[b]])
            yield
            self.op("act", lambda a: a.copy(out=vmb[b3][:].rearrange("p (h c) -> p h c", c=128), in_=kvm4[:, :, 128:256]),
                    reads=kvm_s[b], writes=[vmb_s[b3]])
            yield

        def stCq(t):
            b = t % 2
            b3 = t % 3
            rows = slice(t * 128, (t + 1) * 128)
            self.dma("sp", self.vm[rows, :], vmb[b3][:], vmb_d[b3], reads=[vmb_s[b3]])
            yield
            qm3 = qm[b][:].rearrange("p (h c) -> p h c", c=192)
            mQn = mQ[b][:, 0:512].rearrange("p (h c) -> p h c", c=128)
            mQr = mQ[b][:, 512:768].rearrange("p (h c) -> p h c", c=64)
            yield from self.normrope(W_q, qm3, qm_s[b], 4, 192, gq3, 64, 128, csm[b3], csm_s[b3], mQn, mQr, mQ_s[b], 0, 128)

        def stCk(t):
            b = t % 2
            b3 = t % 3
            kf3 = kf[b][:].rearrange("p (h c) -> p h c", c=192)
            mKn = mK[b][:, 0:512].rearrange("p (h c) -> p h c", c=128)
            mKr = mK[b][:, 512:768].rearrange("p (h c) -> p h c", c=64)
            yield from self.normrope(W_k, kf3, [kf_s[b]], 4, 192, gk3, 64, 128, csm[b3], csm_s[b3], mKn, mKr, mK_s[b], 0, 128)

        def stCt(t):
            b = t % 2
            pr = t // 2
            sg = pr % 2
            tl = t % 2
            stg = stage[sg]
            cols = slice(tl * 128, (tl + 1) * 128)
            if tl == 0:
                old = stage_s[sg].rd + stage_s[sg].wr
                self.wait("act", old)
                self.wait("dve", old)
                stage_s[sg].wr = []
                stage_s[sg].rd = []
            toks = []
            toks.append(transposes(dA[b], 8, [dA_s[b]], stg[:, 0:8, cols], []))
            toks.append(transposes(mQ[b], 6, [mQ_s[b]], stg[:, 8:14, cols], []))
            toks.append(transposes(mK[b], 6, [mK_s[b]], stg[:, 14:20, cols], []))
            stage_s[sg].wr.extend(toks)
            if tl == 1:
                self.dma("sp", self.qkt[:, :, pr * 256:(pr + 1) * 256].rearrange("k p n -> p k n"), stg[:], st_d[sg],
                         reads=[stage_s[sg]])

        for i in range(NT + 2):
            gens = []
            if i < NT:
                gens.append(stA(i))
            if 0 <= i - 1 < NT:
                gens.append(stB(i - 1))
            if 0 <= i - 2 < NT:
                gens.append(stCq(i - 2))
                gens.append(stCk(i - 2))
            self.interleave(gens)
            if 0 <= i - 2 < NT:
                stCt(i - 2)
            self.pump(2)
        self.end_phase()

    def phase2(self, l):
        self.begin_phase()
        nc = self.nc
        sb = self.sb
        qt = [sb("qt", [128, S], BF16) for _ in range(2)]
        kt = [sb("kt", [128, S], BF16) for _ in range(2)]
        qr = [sb("qr", [128, S], BF16) for _ in range(2)]
        kr = [sb("kr", [128, S], BF16) for _ in range(2)]
        vs = [sb("vs", [128, NT, 128], BF16) for _ in range(2)]
        ld_s = [Slot() for _ in range(2)]
        ld_d = [self.dsem() for _ in range(2)]
        NP = 4
        pt = [sb("pt", [128, 512], BF16) for _ in range(NP)]
        pt_s = [Slot() for _ in range(NP)]
        o1 = [sb("o1", [128, 512], F32) for _ in range(2)]
        o1_s = [Slot() for _ in range(2)]
        rcb = [sb("rcb", [128, 512], F32) for _ in range(2)]
        rcb_s = [Slot() for _ in range(2)]
        tb = [sb("tb", [128, 512], F32) for _ in range(2)]
        tb_s = [Slot() for _ in range(2)]
        sqb = [sb("sqb", [128, 512], BF16) for _ in range(2)]
        sqb_s = [Slot() for _ in range(2)]
        rs2 = [sb("rs2", [128, 512], F32) for _ in range(2)]
        rs2_s = [Slot() for _ in range(2)]
        ocs = [sb("ocs", [128, 512], BF16) for _ in range(2)]
        ocs_s = [Slot() for _ in range(2)]
        ocs_d = [self.dsem() for _ in range(2)]
        mk = sb("mk", [128, 128], BF16)
        mk_s = Slot()
        self.op("dve", lambda v: v.memset(mk[:], 0.0), writes=[mk_s])
        self.op("pool", lambda p: p.affine_select(out=mk[:], in_=mk[:], pattern=[[1, 128]], compare_op=ALU.is_ge,
                                                  fill=-30000.0, base=0, channel_multiplier=-1), writes=[mk_s])
        for i in range(2):
            self.op("dve", lambda v: v.memset(kt[i][64:128, :], 0.0), writes=[ld_s[i]])
            self.op("dve", lambda v: v.memset(kr[i][0:64, :], 0.0), writes=[ld_s[i]])
        SB_ = [0, 1, 2]
        SSB = 3
        OB_ = [(4, 5), (6, 7)]
        qkt = self.qkt
        heads = [("d", h) for h in range(4)] + [("m", h) for h in range(4)]
        self.gi = 0
        self.oci = 0
        self.rci = 0
        self.tbi = 0
        self.gstep = 0
        deferred = []

        def defer(delay, fn, tag):
            deferred.append((self.gstep + delay, fn, tag))

        def run_due(force=False, tag=None):
            while True:
                due = [d for d in deferred if (force or d[0] <= self.gstep) and (tag is None or d[2] == tag)]
                if not due:
                    break
                d = min(due, key=lambda x: x[0])
                deferred.remove(d)
                d[1]()

        def load(hi):
            typ, h = heads[hi]
            i = hi % 2
            s = ld_s[i]
            self._deps("sp", [], [s])
            if typ == "d":
                self.dma("sp", qt[i][:], qkt[h], ld_d[i])
                self.dma("sp", kt[i][0:64, :], qkt[4 + h, 0:64, :], ld_d[i])
                self.dma("sp", kr[i][64:128, :], qkt[4 + h, 64:128, :], ld_d[i])
                vsrc = self.vd
            else:
                o = (h % 2) * 64
                self.dma("sp", qt[i][:], qkt[8 + h], ld_d[i])
                self.dma("sp", kt[i][:], qkt[14 + h], ld_d[i])
                self.dma("sp", qr[i][:], qkt[12 + h // 2], ld_d[i])
                self.dma("sp", kr[i][o:o + 64, :], qkt[18 + h // 2, o:o + 64, :], ld_d[i])
                vsrc = self.vm
            tok = self.dma("sp", vs[i][:], vsrc.rearrange("(j p) c -> p j c", p=128)[:, :, h * 128:(h + 1) * 128], ld_d[i])
            s.wr = [tok]
            s.rd = []

        def prep_mla(hi):
            typ, h = heads[hi]
            i = hi % 2
            o = (h % 2) * 64
            z = 64 - o
            self.op("dve", lambda v: v.memset(kr[i][z:z + 64, :], 0.0), writes=[ld_s[i]])

        load(0)
        for hi, (typ, h) in enumerate(heads):
            if hi + 1 < len(heads):
                if heads[hi + 1][0] == "m":
                    prep_mla(hi + 1)
                load(hi + 1)
            i = hi % 2
            lds = ld_s[i]
            if typ == "d":
                maps = [[(kt[i], qt[i])], [(kr[i], qt[i])]]
                scale = 64.0 ** -0.5
                negM = self.negM[:, l, 0:1]
                col = h
            else:
                maps = [[(kt[i], qt[i]), (kr[i], qr[i])]]
                scale = 192.0 ** -0.5
                negM = self.negM[:, l, 1:2]
                col = 4 + h
            steps = []
            for Qb in range(8):
                for m in range(len(maps)):
                    for j in range(4 * Qb + 4):
                        steps.append((Qb, m, j))
            n = len(steps)

            def qk(si):
                Qb, m, j = steps[si]
                r = max(0, j - 4 * Qb)
                N = (4 - r) * 128
                bi = SB_[si % 3]
                parts = maps[m]
                q0 = Qb * 512 + r * 128
                if j < 4 * Qb:
                    mms = [(self.banks[bi][:, 0:N], kp[:, j * 128:(j + 1) * 128], qp[:, q0:q0 + N], pi == 0, pi == len(parts) - 1)
                           for pi, (kp, qp) in enumerate(parts)]
                    self.mm_group(bi, mms, reads=[lds])
                    return
                bs = self.bank_s[bi]
                self._deps("pe", [lds, mk_s], [bs])
                for pi, (kp, qp) in enumerate(parts):
                    nc.tensor.matmul(self.banks[bi][:, 0:N], lhsT=kp[:, j * 128:(j + 1) * 128], rhs=qp[:, q0:q0 + N],
                                     start=(pi == 0), stop=False, skip_group_check=True)
                ins = nc.tensor.matmul(self.banks[bi][:, 0:128], lhsT=self.ident_b[:], rhs=mk[:], start=False, stop=True,
                                       skip_group_check=True)
                tok = self.sig("pe", ins)
                self._commit(tok, [lds], [bs])

            def epilogue(Qb, m, ot, sm, typ=typ, col=col):
                ri = self.rci % 2
                self.rci += 1
                cols = slice(Qb * 512, (Qb + 1) * 512)
                self.op("dve", lambda v: v.reciprocal(out=rcb[ri][:], in_=self.banks[sm][:, :]), reads=[self.bank_s[sm]],
                        writes=[rcb_s[ri]])
                if typ == "m":
                    ci = self.oci % 2
                    self.oci += 1
                    self.op("dve", lambda v: v.tensor_tensor(out=ocs[ci][:], in0=self.banks[ot][:, :], in1=rcb[ri][:], op=ALU.mult),
                            reads=[self.bank_s[ot], rcb_s[ri]], writes=[ocs_s[ci]])
                    self.dma("sp", self.oct_d[col][:, cols], ocs[ci][:], ocs_d[ci], reads=[ocs_s[ci]])
                elif m == 0:
                    oi = Qb % 2
                    self.op("dve", lambda v: v.tensor_tensor(out=o1[oi][:], in0=self.banks[ot][:, :], in1=rcb[ri][:], op=ALU.mult),
                            reads=[self.bank_s[ot], rcb_s[ri]], writes=[o1_s[oi]])
                else:
                    oi = Qb % 2
                    ti = self.tbi % 2
                    self.tbi += 1
                    run_due(force=True, tag=ti)
                    self.op("dve", lambda v: v.scalar_tensor_tensor(out=tb[ti][:], in0=self.banks[ot][:, :],
                                                                    scalar=self.neglam[:, l:l + 1], in1=rcb[ri][:],
                                                                    op0=ALU.mult, op1=ALU.mult),
                            reads=[self.bank_s[ot], rcb_s[ri]], writes=[tb_s[ti]])
                    self.op("dve", lambda v: v.tensor_tensor(out=tb[ti][:], in0=tb[ti][:], in1=o1[oi][:], op=ALU.add),
                            reads=[o1_s[oi]], writes=[tb_s[ti]])

                    def partB():
                        self.op("act", lambda a: a.activation(out=sqb[ti][:], in_=tb[ti][:], func=AF.Square), reads=[tb_s[ti]],
                                writes=[sqb_s[ti]])

                    def partC():
                        self.mm_group(SSB, [(self.banks[SSB][:, :], self.ones_b[:], sqb[ti][:], True, True)], reads=[sqb_s[ti]])

                    def partD():
                        self.op("act", lambda a: a.activation(out=rs2[ti][:], in_=self.banks[SSB][:, :], func=AF.Ln,
                                                              scale=1.0 / 128, bias=float(EPS)),
                                reads=[self.bank_s[SSB]], writes=[rs2_s[ti]])
                        self.op("act", lambda a: a.activation(out=rs2[ti][:], in_=rs2[ti][:], func=AF.Exp, scale=-0.5),
                                reads=[], writes=[rs2_s[ti]])

                    def partE():
                        ci = self.oci % 2
                        self.oci += 1
                        self.op("dve", lambda v: v.tensor_tensor(out=ocs[ci][:], in0=tb[ti][:], in1=rs2[ti][:], op=ALU.mult),
                                reads=[tb_s[ti], rs2_s[ti]], writes=[ocs_s[ci]])
                        self.dma("sp", self.oct_d[col][:, cols], ocs[ci][:], ocs_d[ci], reads=[ocs_s[ci]])

                    defer(8, partB, ti)
                    defer(10, partC, ti)
                    defer(12, partD, ti)
                    defer(15, partE, ti)

            qk(0)
            if n > 1:
                qk(1)
            for si in range(n):
                Qb, m, j = steps[si]
                self.gstep += 1
                run_due()
                if si % 8 == 0:
                    self.pump(1, engs=("pool",))
                if si + 2 < n:
                    qk(si + 2)
                r = max(0, j - 4 * Qb)
                N = (4 - r) * 128
                c0 = r * 128
                bi = SB_[si % 3]
                pi_ = si % NP
                if j == 0:
                    g = self.gi
                    self.gi += 1
                    self.cur_ob = OB_[g % 2]
                ot, sm = self.cur_ob
                self.op("act", lambda a: a.activation(out=pt[pi_][:, 0:N], in_=self.banks[bi][:, 0:N], func=AF.Exp,
                                                      scale=float(scale), bias=negM),
                        reads=[self.bank_s[bi]], writes=[pt_s[pi_]])
                self._deps("pe", [pt_s[pi_], lds], [])
                if j == 0:
                    self._deps("pe", [], [self.bank_s[ot], self.bank_s[sm]])
                last = (j == 4 * Qb + 3)
                nc.tensor.matmul(self.banks[ot][:, c0:512], lhsT=vs[i][:, j, :], rhs=pt[pi_][:, 0:N], start=(j == 0), stop=last,
                                 skip_group_check=True)
                ins = nc.tensor.matmul(self.banks[sm][:, c0:512], lhsT=self.ones_b[:], rhs=pt[pi_][:, 0:N], start=(j == 0),
                                       stop=last, skip_group_check=True)
                tok = self.sig("pe", ins)
                pt_s[pi_].rd.append(tok)
                lds.rd.append(tok)
                for bk in (ot, sm):
                    self.bank_s[bk].wr = [tok]
                    if j == 0:
                        self.bank_s[bk].rd = []
                if last:
                    epilogue(Qb, m, ot, sm)
        run_due(force=True)
        self.end_phase()

    def phase3a(self, l):
        self.begin_phase()
        self.pump_until("wo%d" % l)
        nc = self.nc
        sb = self.sb
        moe = (l % 2 == 1)
        xsrc = self.x if l == 0 else self.xr
        wo = sb("wo", [128, 8, D], BF16)
        w_s = Slot()
        d0 = self.dsem()
        w_s.wr = [self.dma("sp", wo[:], self.b_wo[l].rearrange("(c p) n -> p c n", p=128), d0)]

        def bufs(name, shape, dt, n):
            return [sb(name, shape, dt) for _ in range(n)], [Slot() for _ in range(n)]

        oc, oc_s = bufs("oc", [128, 8, 512], BF16, 2)
        xt, xt_s = bufs("xt", [128, D], F32, 2)
        x1 = [sb("x1", [128, D], F32) for _ in range(3)]
        x1_s = [[Slot(), Slot()] for _ in range(3)]
        smB = [sb("smB", [128, 4], F32) for _ in range(2)]
        ss2_s, lg2_s, rs2_s = [Slot(), Slot()], [Slot(), Slot()], [Slot(), Slot()]
        h2, h2_s = bufs("h2", [128, D], BF16, 2)
        stage = [sb("stage", [128, 8, 512], BF16) for _ in range(2)]
        stage_s = [Slot(), Slot()]
        if moe:
            h2f, h2f_s = bufs("h2f", [128, D], F32, 2)
            h2fT = [sb("h2fT", [128, D], F32) for _ in range(2)]
            h2fT_s = [[Slot(), Slot()] for _ in range(2)]
            rt = [sb("rt", [128, 48], F32) for _ in range(2)]
            rt_s = [[Slot() for _ in range(8)] for _ in range(2)]
        oc_d = [self.dsem() for _ in range(2)]
        xt_d = [self.dsem() for _ in range(2)]
        x1_d = [self.dsem() for _ in range(3)]
        st_d = [self.dsem() for _ in range(2)]
        TPB = [0, 1]
        MMB = [2, 3]
        FTB = [4, 5]
        LGB = [6, 7]
        self.tpi = 0
        self.mmi = 0
        self.lgi = 0

        def stA(t):
            b = t % 2
            b3 = t % 3
            blk, tl = t // 4, t % 4
            ob = blk % 2
            rows = slice(t * 128, (t + 1) * 128)
            if tl == 0:
                self.dma("sp", oc[ob][:], self.oct_d[:, :, blk * 512:(blk + 1) * 512].rearrange("k p n -> p k n"), oc_d[ob],
                         writes=[oc_s[ob]])
            self.dma("sp", xt[b][:], xsrc[rows, :], xt_d[b], writes=[xt_s[b]])
            yield
            for hf in range(2):
                mb = MMB[self.mmi % 2]
                self.mmi += 1
                mms = [(self.banks[mb][:, :], oc[ob][:, k, tl * 128:(tl + 1) * 128], wo[:, k, hf * 512:(hf + 1) * 512], k == 0, k == 7)
                       for k in range(8)]
                self.mm_group(mb, mms, reads=[oc_s[ob], w_s])
                yield
                self.op("dve", lambda v: v.tensor_tensor(out=x1[b3][:, hf * 512:(hf + 1) * 512], in0=self.banks[mb][:, :],
                                                         in1=xt[b][:, hf * 512:(hf + 1) * 512], op=ALU.add),
                        reads=[self.bank_s[mb], xt_s[b]], writes=[x1_s[b3][hf]])
                yield

        def stB(t):
            b = t % 2
            b3 = t % 3
            blk, tl = t // 4, t % 4
            sg = blk % 2
            rows = slice(t * 128, (t + 1) * 128)
            sm = smB[b]
            self.dma("sp", self.xr[rows, :], x1[b3][:], x1_d[b3], reads=x1_s[b3])
            yield
            self.op("act", lambda a: a.activation(out=self.junk[:], in_=x1[b3][:], func=AF.Square, accum_out=sm[:, 0:1]),
                    reads=x1_s[b3], writes=[ss2_s[b]])
            yield
            self.rstd(sm[:, 0:1], ss2_s[b], sm[:, 2:3], rs2_s[b], sm[:, 1:2], lg2_s[b], 1.0 / D)
            yield
            self.op("dve", lambda v: v.tensor_scalar(out=h2[b][:], in0=x1[b3][:], scalar1=sm[:, 2:3], scalar2=None,
                                                     op0=ALU.mult), reads=x1_s[b3] + [rs2_s[b]], writes=[h2_s[b]])
            yield
            bi = TPB[self.tpi % 2]
            self.tpi += 1
            bv = self.bank_bf(bi)
            self.tr_group(bi, [(bv[:, k * 128:(k + 1) * 128], h2[b][:, k * 128:(k + 1) * 128]) for k in range(8)],
                          self.ident_b[:], reads=[h2_s[b]])
            yield
            stg = stage[sg]
            if tl == 0:
                self.wait("act", stage_s[sg].rd + stage_s[sg].wr)
                stage_s[sg].wr = []
                stage_s[sg].rd = []
            tok = self.op("act", lambda a: a.copy(out=stg[:, :, tl * 128:(tl + 1) * 128],
                                                  in_=bv[:, :].rearrange("p (k n) -> p k n", n=128)),
                          reads=[self.bank_s[bi]], writes=[])
            yield
            stage_s[sg].wr.append(tok)
            if tl == 3:
                self.dma("sp", self.h2t[:, :, blk * 512:(blk + 1) * 512].rearrange("k p n -> p k n"), stg[:], st_d[sg],
                         reads=[stage_s[sg]])
                yield

        def stR(t):
            b = t % 2
            b3 = t % 3
            sm = smB[b]
            self.op("pool", lambda p: p.tensor_scalar(out=h2f[b][:], in0=x1[b3][:], scalar1=sm[:, 2:3], scalar2=None,
                                                      op0=ALU.mult), reads=x1_s[b3] + [rs2_s[b]], writes=[h2f_s[b]])
            yield
            for hf in range(2):
                fb = FTB[hf]
                self.tr_group(fb, [(self.banks[fb][:, k * 128:(k + 1) * 128],
                                    h2f[b][:, (hf * 4 + k) * 128:(hf * 4 + k + 1) * 128]) for k in range(4)],
                              self.ident_f[:], reads=[h2f_s[b]])
                yield
                self.op("act", lambda a: a.copy(out=h2fT[b][:, hf * 512:(hf + 1) * 512], in_=self.banks[fb][:, :]),
                        reads=[self.bank_s[fb]], writes=[h2fT_s[b][hf]])
                yield
            lb = LGB[self.lgi % 2]
            self.lgi += 1
            mms = [(self.banks[lb][:, 0:8], h2fT[b][:, k * 128:(k + 1) * 128], self.rw_s[:, k, :], k == 0, k == 7)
                   for k in range(8)]
            self.mm_group(lb, mms, reads=h2fT_s[b])
            yield
            r = rt[b]
            R = rt_s[b]
            self.op("dve", lambda v: v.tensor_copy(out=r[:, 0:8], in_=self.banks[lb][:, 0:8]), reads=[self.bank_s[lb]],
                    writes=[R[0]])
            yield
            self.op("dve", lambda v: v.max(out=r[:, 8:16], in_=r[:, 0:8]), reads=[R[0]], writes=[R[1]])
            yield
            self.op("dve", lambda v: v.tensor_scalar(out=r[:, 16:24], in0=r[:, 0:8], scalar1=r[:, 8:9], scalar2=None,
                                                     op0=ALU.subtract), reads=[R[0], R[1]], writes=[R[2]])
            yield
            self.op("act", lambda a: a.activation(out=r[:, 24:32], in_=r[:, 16:24], func=AF.Exp), reads=[R[2]], writes=[R[3]])
            yield
            self.op("dve", lambda v: v.scalar_tensor_tensor(out=r[:, 32:40], in0=r[:, 0:8], scalar=r[:, 9:10], in1=r[:, 24:32],
                                                            op0=ALU.is_ge, op1=ALU.mult), reads=[R[0], R[1], R[3]],
                    writes=[R[4]])
            yield
            self.op("dve", lambda v: v.tensor_reduce(out=r[:, 40:41], in_=r[:, 32:40], axis=AX.X, op=ALU.add), reads=[R[4]],
                    writes=[R[5]])
            yield
            self.op("dve", lambda v: v.reciprocal(out=r[:, 41:42], in_=r[:, 40:41]), reads=[R[5]], writes=[R[6]])
            yield
            self.op("dve", lambda v: v.tensor_scalar(out=self.comb[:, t, :], in0=r[:, 32:40], scalar1=r[:, 41:42], scalar2=None,
                                                     op0=ALU.mult), reads=[R[4], R[6]], writes=[R[7]])
            yield


        for i in range(NT + 2):
            gens = []
            if i < NT:
                gens.append(stA(i))
            if 0 <= i - 1 < NT:
                gens.append(stB(i - 1))
            if moe and 0 <= i - 2 < NT:
                gens.append(stR(i - 2))
            self.interleave(gens)
            self.pump(2)
        self.end_phase()

    def phase3b(self, l):
        self.begin_phase()
        self.pump_until("ffn%d" % l)
        nc = self.nc
        sb = self.sb
        moe = (l % 2 == 1)
        NE = 8 if moe else 2
        wg = [sb("wg", [128, 8, FFE], BF16) for _ in range(2)]
        wu = [sb("wu", [128, 8, FFE], BF16) for _ in range(2)]
        wd = sb("wd", [128, NCH, D], BF16)
        wgu_s = [Slot() for _ in range(2)]
        wd_s = Slot()
        wgu_d = [self.dsem() for _ in range(2)]
        wd_d = self.dsem()
        hTb = [sb("hTb", [128, 8, 512], BF16) for _ in range(2)]
        hTb_s = [Slot() for _ in range(2)]
        hTb_d = [self.dsem() for _ in range(2)]
        aT = [sb("aT", [128, NCH, 512], BF16) for _ in range(2)]
        aT_s = [Slot() for _ in range(2)]
        NSG = 3
        sgb = [sb("sgb", [128, 512], F32) for _ in range(NSG)]
        sgb_s = [Slot() for _ in range(NSG)]
        NOS = 2
        ost = [sb("ost", [128, D], F32) for _ in range(NOS)]
        ost_s = [Slot() for _ in range(NOS)]
        ost_d = self.swsems[:NOS]
        xrow_s = [Slot() for _ in range(NT)]
        GB = [0, 1]
        UB = [2, 3]
        DB = [4, 5]
        self.gi = 0
        self.di = 0
        self.oi = 0
        self.si = 0
        self.evi = 0

        def load_gu(e):
            i = e % 2
            self._deps("sp", [], [wgu_s[i]])
            if moe:
                gsrc = self.b_mwg[e].rearrange("(c p) n -> p c n", p=128)
                usrc = self.b_mwu[e].rearrange("(c p) n -> p c n", p=128)
            else:
                gsrc = self.b_dwg[:, e * FFE:(e + 1) * FFE].rearrange("(c p) n -> p c n", p=128)
                usrc = self.b_dwu[:, e * FFE:(e + 1) * FFE].rearrange("(c p) n -> p c n", p=128)
            self.dma("sp", wg[i][:], gsrc, wgu_d[i])
            tok = self.dma("sp", wu[i][:], usrc, wgu_d[i])
            wgu_s[i].wr = [tok]
            wgu_s[i].rd = []

        def load_d(e):
            if moe:
                dsrc = self.b_mwd[e].rearrange("(c p) n -> p c n", p=128)
            else:
                dsrc = self.b_dwd[e * FFE:(e + 1) * FFE, :].rearrange("(c p) n -> p c n", p=128)
            self.dma("sp", wd[:], dsrc, wd_d, writes=[wd_s])

        def load_h(bi_):
            i = bi_ % 2
            blk = bi_ % 8
            self.dma("sp", hTb[i][:], self.h2t[:, :, blk * 512:(blk + 1) * 512].rearrange("k p n -> p k n"), hTb_d[i],
                     writes=[hTb_s[i]])

        def gate_up(e, blk, bi_):
            i = e % 2
            hi = bi_ % 2
            ai = bi_ % 2
            for c in range(NCH):
                gb = GB[self.gi % 2]
                ub = UB[self.gi % 2]
                self.gi += 1
                mms = [(self.banks[gb][:, :], wg[i][:, k, c * 128:(c + 1) * 128], hTb[hi][:, k, :], k == 0, k == 7) for k in range(8)]
                self.mm_group(gb, mms, reads=[wgu_s[i], hTb_s[hi]])
                mms = [(self.banks[ub][:, :], wu[i][:, k, c * 128:(c + 1) * 128], hTb[hi][:, k, :], k == 0, k == 7) for k in range(8)]
                self.mm_group(ub, mms, reads=[wgu_s[i], hTb_s[hi]])
                si_ = self.si % NSG
                self.si += 1
                self.op("act", lambda a: a.activation(out=sgb[si_][:], in_=self.banks[gb][:, :], func=AF.Silu),
                        reads=[self.bank_s[gb]], writes=[sgb_s[si_]])
                tok = self.op("dve", lambda v: v.tensor_tensor(out=aT[ai][:, c, :], in0=sgb[si_][:], in1=self.banks[ub][:, :],
                                                               op=ALU.mult),
                              reads=[sgb_s[si_], self.bank_s[ub]], writes=[aT_s[ai]] if c == 0 else [])
                if c > 0:
                    aT_s[ai].wr.append(tok)

        def down(e, blk, bi_):
            ai = bi_ % 2
            for tq in range(4):
                t = blk * 4 + tq
                oi_ = self.oi % NOS
                self.oi += 1
                rdt = ost_s[oi_].rd + ost_s[oi_].wr
                self.wait("act", rdt)
                self.wait("dve", rdt)
                ost_s[oi_].rd = []
                ost_s[oi_].wr = []
                for hf in range(2):
                    db = DB[self.di % 2]
                    self.di += 1
                    mms = [(self.banks[db][:, :], aT[ai][:, c, tq * 128:(tq + 1) * 128], wd[:, c, hf * 512:(hf + 1) * 512],
                            c == 0, c == NCH - 1) for c in range(NCH)]
                    self.mm_group(db, mms, reads=[aT_s[ai], wd_s])
                    eng = "act" if self.evi % 2 == 0 else "dve"
                    self.evi += 1
                    dst = ost[oi_][:, hf * 512:(hf + 1) * 512]
                    wr = []
                    if moe:
                        sc = self.comb[:, t, e:e + 1]
                        if eng == "act":
                            tok = self.op("act", lambda a: a.activation(out=dst, in_=self.banks[db][:, :], func=AF.Copy, scale=sc),
                                          reads=[self.bank_s[db]], writes=wr)
                        else:
                            tok = self.op("dve", lambda v: v.tensor_scalar(out=dst, in0=self.banks[db][:, :], scalar1=sc,
                                                                           scalar2=None, op0=ALU.mult),
                                          reads=[self.bank_s[db]], writes=wr)
                    else:
                        if eng == "act":
                            tok = self.op("act", lambda a: a.copy(out=dst, in_=self.banks[db][:, :]), reads=[self.bank_s[db]],
                                          writes=wr)
                        else:
                            tok = self.op("dve", lambda v: v.tensor_copy(out=dst, in_=self.banks[db][:, :]),
                                          reads=[self.bank_s[db]], writes=wr)
                    ost_s[oi_].wr.append(tok)
                self.dma("pool", self.xr[t * 128:(t + 1) * 128, :], ost[oi_][:], ost_d[oi_], reads=[ost_s[oi_]],
                         writes=[xrow_s[t]], accum_op=ALU.add)

        seq = [(e, blk) for e in range(NE) for blk in range(8)]
        load_gu(0)
        load_d(0)
        load_h(0)
        cur_wd = 0
        for bi_, (e, blk) in enumerate(seq):
            if bi_ + 1 < len(seq):
                load_h(bi_ + 1)
            if blk == 0 and e + 1 < NE:
                load_gu(e + 1)
            if bi_ == 0:
                gate_up(e, blk, bi_)
            if bi_ + 1 < len(seq):
                gate_up(seq[bi_ + 1][0], seq[bi_ + 1][1], bi_ + 1)
            if e != cur_wd:
                load_d(e)
                cur_wd = e
            down(e, blk, bi_)
        self.end_phase()


def _rope_table(theta, rot):
    half = rot // 2
    inv_freq = (1.0 / (np.float32(theta) ** (np.arange(half, dtype=np.float32) * np.float32(2.0 / rot)))).astype(np.float32)
    ang = (np.arange(S, dtype=np.float32)[:, None] * inv_freq[None, :]).astype(np.float32)
    c = np.cos(ang.astype(np.float64)).astype(np.float32)
    s = np.sin(ang.astype(np.float64)).astype(np.float32)
    return np.ascontiguousarray(np.concatenate([c, c, -s, s], axis=1))


_CACHE = {}


def _get_nc(**kw):
    key = tuple(sorted(kw.items()))
    if key not in _CACHE:
        kb = KB(**kw)
        kb.build()
        _CACHE[key] = kb
    return _CACHE[key]


def make_in_maps(inputs, ncores=8):
    f = lambda a: np.ascontiguousarray(np.asarray(a, dtype=np.float32))
    shared = {
        "attn_norm_g": f(inputs["attn_norm_g"]),
        "w_in": f(inputs["w_in"]),
        "diff_q_norm_g": f(inputs["diff_q_norm_g"]),
        "diff_k_norm_g": f(inputs["diff_k_norm_g"]),
        "diff_lambda": f(inputs["diff_lambda"]).reshape(DEPTH, 256),
        "diff_subln_g": f(inputs["diff_subln_g"]),
        "mla_q_ln_g": f(inputs["mla_q_ln_g"]),
        "w_uq": f(inputs["w_uq"]),
        "mla_kv_ln_g": f(inputs["mla_kv_ln_g"]),
        "w_ukv": f(inputs["w_ukv"]),
        "mla_qk_norm_g": f(inputs["mla_qk_norm_g"]).reshape(DEPTH, 384),
        "w_o": f(inputs["w_o"]),
        "ffn_norm_g": f(inputs["ffn_norm_g"]),
        "dense_w_gate": f(inputs["dense_w_gate"])[0],
        "dense_w_up": f(inputs["dense_w_up"])[0],
        "dense_w_down": f(inputs["dense_w_down"])[0],
        "router_w": f(inputs["router_w"])[0],
        "moe_w_gate": f(inputs["moe_w_gate"])[0],
        "moe_w_up": f(inputs["moe_w_up"])[0],
        "moe_w_down": f(inputs["moe_w_down"])[0],
        "cs_d": _rope_table(ROPE_THETA, 16),
        "cs_m": _rope_table(MLA_ROPE_THETA, 64),
        "ident": np.eye(128, dtype=np.float32),
    }
    x = f(inputs["x"])
    maps = []
    for c in range(ncores):
        m = dict(shared)
        m["x"] = np.ascontiguousarray(x[c])
        maps.append(m)
    return maps


def kernel(**inputs):
    kb = _get_nc()
    in_maps = make_in_maps(inputs)
    res = run_bass_kernel_spmd(kb.nc, in_maps, core_ids=list(range(8)))
    out = np.stack([np.asarray(r["y"], dtype=np.float32) for r in res.results], axis=0)
    return out
```

```python
import math
from contextlib import ExitStack
import numpy as np
import ml_dtypes
import concourse.bass as bass
import concourse.mybir as mybir
from concourse.bass_utils import run_bass_kernel_spmd

F32 = mybir.dt.float32
BF16 = mybir.dt.bfloat16
ALU = mybir.AluOpType
AF = mybir.ActivationFunctionType
AX = mybir.AxisListType

S = 4096
D = 1024
NT = S // 128
DEPTH = 2
EPS = 1e-6
INC = 1984
FFE = 1408
NCH = FFE // 128
ROPE_THETA = 500000.0
MLA_ROPE_THETA = 10000.0


class DSem:
    def __init__(self, h):
        self.h = h
        self.v = 0


class Slot:
    __slots__ = ("wr", "rd")

    def __init__(self):
        self.wr = []
        self.rd = []


class KB:
    def __init__(self, depth=DEPTH, stop_after=None, debug=False):
        self.depth = depth
        self.stop_after = stop_after
        self.debug = debug
        nc = self.nc = bass.Bass("TRN2", target_bir_lowering=False)
        self.E = dict(pe=nc.tensor, dve=nc.vector, act=nc.scalar, pool=nc.gpsimd, sp=nc.sync)
        self.csem = {k: nc.alloc_semaphore("c_" + k) for k in ("pe", "dve", "act", "pool")}
        self.ccnt = dict.fromkeys(self.csem, 0)
        self.waited = {}
        self.dsems = [DSem(nc.alloc_semaphore("d%d" % i)) for i in range(72)]
        self.dptr = 0
        self.swsems = [DSem(nc.alloc_semaphore("sw%d" % i)) for i in range(3)]
        self.banks = [nc.alloc_psum_tensor("bank%d" % i, [128, 512], F32) for i in range(8)]
        self.bank_s = [Slot() for _ in range(8)]
        self.stack = None
        self.uid = 0
        self.const_s = None
        self.log = [] if debug else None

    def sig(self, e, ins):
        self.ccnt[e] += 1
        ins.then_inc(self.csem[e], 1)
        if self.log is not None:
            self.log.append(("inc", e, self.csem[e].num, 1))
        return (self.csem[e], self.ccnt[e])

    def wait(self, e, toks):
        for tok in toks:
            sem, v = tok
            key = (e, sem.num)
            if self.waited.get(key, 0) >= v:
                continue
            self.waited[key] = v
            self.E[e].wait_ge(sem, v)
            if self.log is not None:
                self.log.append(("wait", e, sem.num, v))

    def _deps(self, e, reads, writes):
        toks = []
        for s in reads:
            toks.extend(s.wr)
        for s in writes:
            toks.extend(s.rd)
            toks.extend(s.wr)
        self.wait(e, toks)

    def _commit(self, tok, reads, writes):
        for s in reads:
            if s is not self.const_s:
                s.rd.append(tok)
        for s in writes:
            s.wr = [tok]
            s.rd = []

    def op(self, e, fn, reads=(), writes=()):
        self._deps(e, reads, writes)
        ins = fn(self.E[e])
        tok = self.sig(e, ins)
        self._commit(tok, reads, writes)
        return tok

    def dsem(self):
        d = self.dsems[self.dptr]
        self.dptr += 1
        return d

    def dma(self, q, out, in_, ds, reads=(), writes=(), **kw):
        self._deps(q, reads, writes)
        ins = self.E[q].dma_start(out=out, in_=in_, **kw)
        ins.then_inc(ds.h, 16)
        ds.v += 16
        if self.log is not None:
            self.log.append(("dma", q, ds.h.num, 16))
        tok = (ds.h, ds.v)
        self._commit(tok, reads, writes)
        return tok

    def mm_group(self, bank_i, mms, reads=()):
        bs = self.bank_s[bank_i]
        self._deps("pe", reads, [bs])
        ins = None
        for (o, l, r, st, sp) in mms:
            ins = self.nc.tensor.matmul(o, lhsT=l, rhs=r, start=st, stop=sp)
        tok = self.sig("pe", ins)
        self._commit(tok, reads, [bs])
        return tok

    def tr_group(self, bank_i, trs, ident, reads=()):
        bs = self.bank_s[bank_i]
        self._deps("pe", reads, [bs])
        ins = None
        for (o, i) in trs:
            ins = self.nc.tensor.transpose(out=o, in_=i, identity=ident)
        tok = self.sig("pe", ins)
        self._commit(tok, reads, [bs])
        return tok

    def barrier(self):
        toks = [(self.csem[c], self.ccnt[c]) for c in self.csem if self.ccnt[c] > 0]
        toks += [(d.h, d.v) for d in self.dsems + self.swsems if d.v > 0]
        for e in self.E:
            self.wait(e, toks)

    def begin_phase(self):
        self.barrier()
        self.stack = ExitStack()
        self.dptr = self.dkeep
        for b in self.bank_s:
            b.wr = []
            b.rd = []

    def end_phase(self):
        self.barrier()
        self.stack.close()
        self.stack = None

    def sb(self, name, shape, dt):
        self.uid += 1
        return self.stack.enter_context(self.nc.sbuf_tensor("%s_%d" % (name, self.uid), shape, dt))

    def interleave(self, gens):
        gens = list(gens)
        while gens:
            nxt = []
            for g in gens:
                try:
                    next(g)
                    nxt.append(g)
                except StopIteration:
                    pass
            gens = nxt

    def bank_bf(self, i):
        return self.banks[i][:].bitcast(BF16)

    def build(self):
        nc = self.nc

        def din(name, shape):
            return nc.dram_tensor(name, shape, F32, kind="ExternalInput").ap()

        self.x = din("x", [S, D])
        self.attn_g = din("attn_norm_g", [DEPTH, D])
        self.w_in = din("w_in", [DEPTH, D, INC])
        self.dq_g = din("diff_q_norm_g", [DEPTH, 64])
        self.dk_g = din("diff_k_norm_g", [DEPTH, 64])
        self.dlam = din("diff_lambda", [DEPTH, 256])
        self.subln_g = din("diff_subln_g", [DEPTH, 128])
        self.qln_g = din("mla_q_ln_g", [DEPTH, 256])
        self.w_uq = din("w_uq", [DEPTH, 256, 768])
        self.kvln_g = din("mla_kv_ln_g", [DEPTH, 128])
        self.w_ukv = din("w_ukv", [DEPTH, 128, 1024])
        self.mqk_g = din("mla_qk_norm_g", [DEPTH, 2 * 192])
        self.w_o = din("w_o", [DEPTH, D, D])
        self.ffn_g = din("ffn_norm_g", [DEPTH, D])
        self.dwg = din("dense_w_gate", [D, 2816])
        self.dwu = din("dense_w_up", [D, 2816])
        self.dwd = din("dense_w_down", [2816, D])
        self.rw = din("router_w", [D, 8])
        self.mwg = din("moe_w_gate", [8, D, FFE])
        self.mwu = din("moe_w_up", [8, D, FFE])
        self.mwd = din("moe_w_down", [8, FFE, D])
        self.cs_d = din("cs_d", [S, 32])
        self.cs_m = din("cs_m", [S, 128])
        self.ident_in = din("ident", [128, 128])
        self.y = nc.dram_tensor("y", [S, D], F32, kind="ExternalOutput").ap()
        xr = self.xr = self.y

        kind = "ExternalOutput" if self.debug else "Internal"

        def dscr(name, shape, dt):
            return nc.dram_tensor(name, shape, dt, kind=kind).ap()

        self.qkt = dscr("qkt", [20, 128, S], BF16)
        self.vd = dscr("vd", [S, 512], BF16)
        self.vm = dscr("vm", [S, 512], BF16)
        self.oct_d = dscr("oct_d", [8, 128, S], BF16)
        self.h2t = dscr("h2t", [8, 128, S], BF16)
        self.b_win = dscr("b_win", [DEPTH, D, INC], BF16)
        self.b_wuq = dscr("b_wuq", [DEPTH, 256, 768], BF16)
        self.b_wukv = dscr("b_wukv", [DEPTH, 128, 1024], BF16)
        self.b_wo = dscr("b_wo", [DEPTH, D, D], BF16)
        self.b_dwg = dscr("b_dwg", [D, 2816], BF16)
        self.b_dwu = dscr("b_dwu", [D, 2816], BF16)
        self.b_dwd = dscr("b_dwd", [2816, D], BF16)
        self.b_mwg = dscr("b_mwg", [8, D, FFE], BF16)
        self.b_mwu = dscr("b_mwu", [8, D, FFE], BF16)
        self.b_mwd = dscr("b_mwd", [8, FFE, D], BF16)

        A = nc.alloc_sbuf_tensor
        self.ident_f = A("ident_f", [128, 128], F32)
        self.ident_b = A("ident_b", [128, 128], BF16)
        self.ones_b = A("ones_b", [128, 128], BF16)
        self.sc_attn = A("sc_attn", [128, DEPTH, 8], F32)
        self.sc_ffn = A("sc_ffn", [128, DEPTH, 8], F32)
        self.sc_qln = A("sc_qln", [128, DEPTH, 2], F32)
        self.sc_kvln = A("sc_kvln", [128, DEPTH, 1], F32)
        self.sc_wo = A("sc_wo", [128, DEPTH, 8], F32)
        self.ones = A("ones", [128, 24], F32)
        self.gd = A("gd", [128, DEPTH, 2, 64], F32)
        self.gm = A("gm", [128, DEPTH, 2, 192], F32)
        self.lamt = A("lamt", [128, DEPTH, 256], F32)
        self.lamw = A("lamw", [128, 8], F32)
        self.neglam = A("neglam", [128, DEPTH], F32)
        self.negM = A("negM", [128, DEPTH, 2], F32)
        self.gmax = A("gmax", [128, 8], F32)
        self.rw_s = A("rw_s", [128, 8, 8], F32)
        self.comb = A("comb", [128, NT, 8], F32)
        self.junk = A("junk", [128, 1024], BF16)
        self.const_s = Slot()

        self.setup_consts()
        self.setup_wjobs()
        self.dkeep = self.dptr
        if self.log is not None:
            snap = {self.csem[c].num: self.ccnt[c] for c in self.csem}
            snap.update({d.h.num: d.v for d in self.dsems + self.swsems + [self.xinit]})
            self.log.append(("snapshot", snap))
        for l in range(self.depth):
            self.phase1(l)
            if self.stop_after == "p1_%d" % l:
                return self.finish()
            self.phase2(l)
            if self.stop_after == "p2_%d" % l:
                return self.finish()
            self.phase3a(l)
            if self.stop_after == "p3a_%d" % l:
                return self.finish()
            self.phase3b(l)
            if self.stop_after == "p3b_%d" % l:
                return self.finish()
        return self.finish()

    def finish(self):
        self.barrier()
        return self.nc

    def setup_consts(self):
        nc = self.nc
        cs = self.const_s
        ds = self.dsem()
        toks = []

        def ld(out, in_, **kw):
            ins = nc.sync.dma_start(out=out, in_=in_, **kw)
            ins.then_inc(ds.h, 16)
            ds.v += 16

        self.xinit = self.dsem()

        ld(self.ident_f[:], self.ident_in[:, :])
        for l in range(DEPTH):
            ld(self.sc_attn[:, l, :], self.attn_g[l].rearrange("(c p) -> p c", p=128), allow_slow_non_contiguous=True)
            ld(self.sc_ffn[:, l, :], self.ffn_g[l].rearrange("(c p) -> p c", p=128), allow_slow_non_contiguous=True)
            ld(self.sc_qln[:, l, :], self.qln_g[l].rearrange("(c p) -> p c", p=128), allow_slow_non_contiguous=True)
            ld(self.sc_kvln[:, l, :], self.kvln_g[l].rearrange("(c p) -> p c", p=128), allow_slow_non_contiguous=True)
            ld(self.sc_wo[:, l, 0:1], self.subln_g[l].rearrange("(c p) -> p c", p=128), allow_slow_non_contiguous=True)
            ld(self.gd[:, l, 0, :], self.dq_g[l].partition_broadcast(128))
            ld(self.gd[:, l, 1, :], self.dk_g[l].partition_broadcast(128))
            ld(self.gm[:, l, :, :].rearrange("p a b -> p (a b)"), self.mqk_g[l].partition_broadcast(128))
            ld(self.lamt[:, l, :], self.dlam[l].partition_broadcast(128))
        ld(self.rw_s[:], self.rw.rearrange("(c p) n -> p c n", p=128))
        nc.vector.wait_ge(ds.h, ds.v)
        nc.gpsimd.wait_ge(ds.h, ds.v)
        nc.scalar.wait_ge(ds.h, ds.v)
        V = nc.vector
        sv = self.csem["dve"]

        def vop(ins):
            self.ccnt["dve"] += 1
            ins.then_inc(sv, 1)
            V.wait_ge(sv, self.ccnt["dve"])

        def aop(ins):
            self.ccnt["act"] += 1
            ins.then_inc(self.csem["act"], 1)
            nc.scalar.wait_ge(self.csem["act"], self.ccnt["act"])
            V.wait_ge(self.csem["act"], self.ccnt["act"])

        vop(V.tensor_copy(out=self.ident_b[:], in_=self.ident_f[:]))
        vop(V.memset(self.ones[:], 1.0))
        vop(V.memset(self.ones_b[:], 1.0))
        for l in range(DEPTH):
            lam_init = 0.8 - 0.6 * math.exp(-0.3 * l)
            vop(V.tensor_scalar(out=self.sc_wo[:, l, 0:1], in0=self.sc_wo[:, l, 0:1], scalar1=float(1.0 - lam_init),
                                scalar2=None, op0=ALU.mult))
            for c in range(1, 4):
                vop(V.tensor_copy(out=self.sc_wo[:, l, c:c + 1], in_=self.sc_wo[:, l, 0:1]))
            vop(V.memset(self.sc_wo[:, l, 4:8], 1.0))
        for l in range(DEPTH):
            lam_init = 0.8 - 0.6 * math.exp(-0.3 * l)
            vop(V.tensor_tensor(out=self.lamt[:, l, 0:64], in0=self.lamt[:, l, 0:64], in1=self.lamt[:, l, 64:128], op=ALU.mult))
            vop(V.tensor_tensor(out=self.lamt[:, l, 128:192], in0=self.lamt[:, l, 128:192], in1=self.lamt[:, l, 192:256], op=ALU.mult))
            vop(V.tensor_reduce(out=self.lamw[:, 0:1], in_=self.lamt[:, l, 0:64], axis=AX.X, op=ALU.add))
            vop(V.tensor_reduce(out=self.lamw[:, 1:2], in_=self.lamt[:, l, 128:192], axis=AX.X, op=ALU.add))
            nc.scalar.wait_ge(sv, self.ccnt["dve"])
            aop(nc.scalar.activation(out=self.lamw[:, 2:4], in_=self.lamw[:, 0:2], func=AF.Exp))
            vop(V.tensor_tensor(out=self.lamw[:, 4:5], in0=self.lamw[:, 3:4], in1=self.lamw[:, 2:3], op=ALU.subtract))
            vop(V.tensor_scalar(out=self.neglam[:, l:l + 1], in0=self.lamw[:, 4:5], scalar1=float(-lam_init),
                                scalar2=None, op0=ALU.add))
            vop(V.tensor_reduce(out=self.gmax[:, 0:2], in_=self.gd[:, l, :, :], axis=AX.X, op=ALU.max,
                                apply_absolute_value=True))
            vop(V.tensor_reduce(out=self.gmax[:, 2:4], in_=self.gm[:, l, :, :], axis=AX.X, op=ALU.max,
                                apply_absolute_value=True))
            vop(V.tensor_tensor(out=self.gmax[:, 4:5], in0=self.gmax[:, 0:1], in1=self.gmax[:, 1:2], op=ALU.mult))
            vop(V.tensor_tensor(out=self.gmax[:, 5:6], in0=self.gmax[:, 2:3], in1=self.gmax[:, 3:4], op=ALU.mult))
            vop(V.tensor_scalar(out=self.negM[:, l, 0:1], in0=self.gmax[:, 4:5], scalar1=float(-math.sqrt(64.0)),
                                scalar2=None, op0=ALU.mult))
            vop(V.tensor_scalar(out=self.negM[:, l, 1:2], in0=self.gmax[:, 5:6], scalar1=float(-math.sqrt(192.0)),
                                scalar2=None, op0=ALU.mult))
        for c in range(8):
            vop(V.tensor_scalar(out=self.rw_s[:, c, :], in0=self.rw_s[:, c, :], scalar1=self.sc_ffn[:, 1, c:c + 1],
                                scalar2=None, op0=ALU.mult))
        vop(V.memset(self.comb[:], 1.0))

    def setup_wjobs(self):
        nc = self.nc
        NB = 3
        CM = 1408
        self.w_fst = [nc.alloc_sbuf_tensor("wf%d" % i, [128, CM], F32) for i in range(NB)]
        self.w_bst = [nc.alloc_sbuf_tensor("wb%d" % i, [128, CM], BF16) for i in range(NB)]
        self.w_fs = [Slot() for _ in range(NB)]
        self.w_bs = [Slot() for _ in range(NB)]
        self.w_fd = [self.dsem() for _ in range(NB)]
        self.w_bd = [self.dsem() for _ in range(NB)]
        self.w_nb = NB
        self.wjobs = []
        self.wnext = 0
        self.wloadq = []
        self.wcastq = []
        self.wcall = 0
        self.wtags = {}

        def add(src, dst, R, C, sc):
            ncs = 1 if C <= CM else 2
            cw = C // ncs
            for c in range(R // 128):
                for j in range(ncs):
                    self.wjobs.append((src[c * 128:(c + 1) * 128, j * cw:(j + 1) * cw],
                                       dst[c * 128:(c + 1) * 128, j * cw:(j + 1) * cw], cw, sc(c)))

        one = lambda c: self.ones[:, 0:1]

        def attn(l):
            add(self.w_in[l], self.b_win[l], D, INC, lambda c, l=l: self.sc_attn[:, l, c:c + 1])
            add(self.w_uq[l], self.b_wuq[l], 256, 768, lambda c, l=l: self.sc_qln[:, l, c:c + 1])
            add(self.w_ukv[l], self.b_wukv[l], 128, 1024, lambda c, l=l: self.sc_kvln[:, l, c:c + 1])
            self.wtags["attn%d" % l] = len(self.wjobs)
            add(self.w_o[l], self.b_wo[l], D, D, lambda c, l=l: self.sc_wo[:, l, c:c + 1])
            self.wtags["wo%d" % l] = len(self.wjobs)

        attn(0)
        add(self.dwg, self.b_dwg, D, 2816, lambda c: self.sc_ffn[:, 0, c:c + 1])
        add(self.dwu, self.b_dwu, D, 2816, lambda c: self.sc_ffn[:, 0, c:c + 1])
        add(self.dwd, self.b_dwd, 2816, D, one)
        self.wtags["ffn0"] = len(self.wjobs)
        if self.depth > 1:
            attn(1)
            for e in range(8):
                add(self.mwg[e], self.b_mwg[e], D, FFE, lambda c: self.sc_ffn[:, 1, c:c + 1])
                add(self.mwu[e], self.b_mwu[e], D, FFE, lambda c: self.sc_ffn[:, 1, c:c + 1])
                add(self.mwd[e], self.b_mwd[e], FFE, D, one)
            self.wtags["ffn1"] = len(self.wjobs)

    def _wstage_store(self, upto):
        keep = []
        for ent in self.wcastq:
            (i, dst, C, call) = ent
            if call > upto:
                keep.append(ent)
                continue
            self.dma("sp", dst, self.w_bst[i][:, 0:C], self.w_bd[i], reads=[self.w_bs[i]])
        self.wcastq = keep

    def _wstage_cast(self, upto, engs):
        keep = []
        for ent in self.wloadq:
            (i, dst, C, scal, call, k) = ent
            if call > upto:
                keep.append(ent)
                continue
            eng = engs[k % len(engs)]
            fst, bst = self.w_fst[i], self.w_bst[i]
            if eng == "act":
                self.op("act", lambda a: a.activation(out=bst[:, 0:C], in_=fst[:, 0:C], func=AF.Copy, scale=scal),
                        reads=[self.w_fs[i]], writes=[self.w_bs[i]])
            else:
                self.op(eng, lambda v: v.tensor_scalar(out=bst[:, 0:C], in0=fst[:, 0:C], scalar1=scal, scalar2=None,
                                                       op0=ALU.mult), reads=[self.w_fs[i]], writes=[self.w_bs[i]])
            self.wcastq.append((i, dst, C, self.wcall))
        self.wloadq = keep

    def pump(self, n, engs=("dve", "act")):
        self.wcall += 1
        c = self.wcall
        self._wstage_store(c - 1)
        self._wstage_cast(c - 1, engs)
        for _ in range(n):
            if self.wnext >= len(self.wjobs):
                break
            src, dst, C, scal = self.wjobs[self.wnext]
            k = self.wnext
            i = k % self.w_nb
            self.wnext += 1
            self.dma("sp", self.w_fst[i][:, 0:C], src, self.w_fd[i], writes=[self.w_fs[i]])
            self.wloadq.append((i, dst, C, scal, c, k))

    def pump_until(self, tag, engs=("dve", "act")):
        tgt = self.wtags[tag]
        while self.wnext < tgt:
            self.pump(1, engs)
        self.wcall += 1
        self._wstage_store(self.wcall)
        self._wstage_cast(self.wcall, engs)
        self._wstage_store(self.wcall)
        self.wait("sp", [(d.h, d.v) for d in self.w_bd if d.v > 0])

    def gpump(self, rounds, engs=("dve", "act")):
        for _ in range(rounds):
            yield
        self.pump(1, engs)

    def rstd(self, src_ap, src_slot, out_ap, out_slot, tmp_ap, tmp_slot, inv_w):
        self.op("act", lambda a: a.activation(out=tmp_ap, in_=src_ap, func=AF.Ln, scale=float(inv_w), bias=float(EPS)),
                reads=[src_slot], writes=[tmp_slot])
        self.op("act", lambda a: a.activation(out=out_ap, in_=tmp_ap, func=AF.Exp, scale=-0.5),
                reads=[tmp_slot], writes=[out_slot])

    def normrope(self, W_, src3, src_slots, G, W, gain3, R, rot_lo, cs_ap, cs_slot, dst_nonrot, dst_rot, dst_slot,
                 nonrot_lo, nonrot_hi):
        sq, sq_s, ssg, ssg_s, lg, lg_s, rsg, rsg_s, tmp, tmp_s, xr_, xr_s, ac, ac_s, sw, sw_s = W_
        n = G * W
        h = R // 2
        sq3 = sq[:, 0:n].rearrange("p (g w) -> p g w", w=W)
        tmp3 = tmp[:, 0:n].rearrange("p (g w) -> p g w", w=W)
        xr3 = xr_[:, 0:G * R].rearrange("p (g r) -> p g r", r=R)
        ac3 = ac[:, 0:G * R].rearrange("p (g r) -> p g r", r=R)
        sw3 = sw[:, 0:G * R].rearrange("p (g r) -> p g r", r=R)
        self.op("act", lambda a: a.activation(out=sq3, in_=src3, func=AF.Square), reads=src_slots, writes=[sq_s])
        yield
        self.op("dve", lambda v: v.tensor_reduce(out=ssg[:, 0:G], in_=sq3, axis=AX.X, op=ALU.add),
                reads=[sq_s], writes=[ssg_s])
        yield
        self.rstd(ssg[:, 0:G], ssg_s, rsg[:, 0:G], rsg_s, lg[:, 0:G], lg_s, 1.0 / W)
        yield
        self.op("dve", lambda v: v.tensor_tensor(out=tmp3, in0=src3, in1=rsg[:, 0:G].unsqueeze(2).broadcast_to([128, G, W]),
                                                 op=ALU.mult),
                reads=list(src_slots) + [rsg_s], writes=[tmp_s])
        yield
        self.op("pool", lambda p: p.tensor_tensor(out=dst_nonrot, in0=tmp3[:, :, nonrot_lo:nonrot_hi],
                                                  in1=gain3[:, :, nonrot_lo:nonrot_hi], op=ALU.mult),
                reads=[tmp_s], writes=[dst_slot])
        yield
        self.op("pool", lambda p: p.tensor_tensor(out=xr3, in0=tmp3[:, :, rot_lo:rot_lo + R],
                                                  in1=gain3[:, :, rot_lo:rot_lo + R], op=ALU.mult),
                reads=[tmp_s], writes=[xr_s])
        yield
        cc = cs_ap[:, 0:R].unsqueeze(1).broadcast_to([128, G, R])
        ns = cs_ap[:, R:R + h].unsqueeze(1).broadcast_to([128, G, h])
        ps_ = cs_ap[:, R + h:2 * R].unsqueeze(1).broadcast_to([128, G, h])
        self.op("pool", lambda p: p.tensor_tensor(out=ac3, in0=xr3, in1=cc, op=ALU.mult),
                reads=[xr_s, cs_slot], writes=[ac_s])
        yield
        self.op("pool", lambda p: p.tensor_tensor(out=sw3[:, :, 0:h], in0=xr3[:, :, h:R], in1=ns, op=ALU.mult),
                reads=[xr_s, cs_slot], writes=[sw_s])
        yield
        self.op("pool", lambda p: p.tensor_tensor(out=sw3[:, :, h:R], in0=xr3[:, :, 0:h], in1=ps_, op=ALU.mult),
                reads=[xr_s, cs_slot], writes=[sw_s])
        yield
        tok = self.op("pool", lambda p: p.tensor_tensor(out=dst_rot, in0=ac3, in1=sw3, op=ALU.add),
                      reads=[ac_s, sw_s], writes=[dst_slot])
        yield
        return tok

    def phase1(self, l):
        self.begin_phase()
        self.pump_until("attn%d" % l)
        xsrc = self.x if l == 0 else self.xr
        nc = self.nc
        sb = self.sb
        win = sb("win", [128, 8, INC], BF16)
        wuq = sb("wuq", [128, 2, 768], BF16)
        wukv = sb("wukv", [128, 1024], BF16)
        w_s = Slot()
        d0 = self.dsem()
        self.dma("sp", win[:], self.b_win[l].rearrange("(c p) n -> p c n", p=128), d0, writes=[])
        self.dma("sp", wuq[:], self.b_wuq[l].rearrange("(c p) n -> p c n", p=128), d0, writes=[])
        tokw = self.dma("sp", wukv[:], self.b_wukv[l], d0, writes=[])
        w_s.wr = [tokw]

        def bufs(name, shape, dt, n):
            return [sb(name, shape, dt) for _ in range(n)], [Slot() for _ in range(n)]

        xt, xt_s = bufs("xt", [128, D], F32, 2)
        hb, hb_s = bufs("hb", [128, D], BF16, 2)
        hT, hT_s = bufs("hT", [128, D], BF16, 2)
        pj = [sb("pj", [128, INC], F32) for _ in range(2)]
        pj_s = [[Slot() for _ in range(4)] for _ in range(2)]
        smA = [sb("smA", [128, 4], F32) for _ in range(2)]
        ss_s, lg_s, rs_s = [Slot(), Slot()], [Slot(), Slot()], [Slot(), Slot()]
        smB = [sb("smB", [128, 8], F32) for _ in range(2)]
        ssc_s, lgc_s, rsc_s = [Slot(), Slot()], [Slot(), Slot()], [Slot(), Slot()]
        cn, cn_s = bufs("cn", [128, 384], BF16, 2)
        cT, cT_s = bufs("cT", [128, 384], BF16, 2)
        qm = [sb("qm", [128, 768], F32) for _ in range(2)]
        qm_s = [[Slot(), Slot()] for _ in range(2)]
        kvm = [sb("kvm", [128, 1024], F32) for _ in range(2)]
        kvm_s = [[Slot(), Slot()] for _ in range(2)]
        kf, kf_s = bufs("kf", [128, 768], F32, 2)
        dA, dA_s = bufs("dA", [128, 1024], BF16, 3)
        mQ, mQ_s = bufs("mQ", [128, 768], BF16, 3)
        mK, mK_s = bufs("mK", [128, 768], BF16, 3)
        vb, vb_s = bufs("vb", [128, 512], BF16, 3)
        vmb, vmb_s = bufs("vmb", [128, 512], BF16, 3)
        csd, csd_s = bufs("csd", [128, 32], F32, 3)
        csm, csm_s = bufs("csm", [128, 128], F32, 3)
        stage = [sb("stage", [128, 20, 256], BF16) for _ in range(2)]
        stage_s = [Slot(), Slot()]
        xt_d = [self.dsem() for _ in range(2)]
        cs_d_ = [self.dsem() for _ in range(3)]
        cs_m_ = [self.dsem() for _ in range(3)]
        vb_d = [self.dsem() for _ in range(3)]
        vmb_d = [self.dsem() for _ in range(3)]
        st_d = [self.dsem() for _ in range(2)]

        def mkscratch(n, gr):
            return (sb("sq", [128, n], F32), Slot(), sb("ssg", [128, 16], F32), Slot(), sb("lg", [128, 16], F32), Slot(),
                    sb("rsg", [128, 16], F32), Slot(), sb("tmp", [128, n], F32), Slot(), sb("xr_", [128, gr], F32), Slot(),
                    sb("ac", [128, gr], F32), Slot(), sb("sw", [128, gr], F32), Slot())

        W_d = mkscratch(1024, 256)
        W_q = mkscratch(768, 256)
        W_k = mkscratch(768, 256)

        gdf = sb("gdf", [128, 16, 64], F32)
        gdf_s = Slot()
        self.op("dve", lambda v: v.tensor_copy(out=gdf[:, 0:8, :], in_=self.gd[:, l, 0, :].unsqueeze(1).broadcast_to([128, 8, 64])),
                writes=[gdf_s])
        self.op("dve", lambda v: v.tensor_copy(out=gdf[:, 8:16, :], in_=self.gd[:, l, 1, :].unsqueeze(1).broadcast_to([128, 8, 64])),
                writes=[gdf_s])
        self.wait("pool", gdf_s.wr)
        gd3 = gdf[:]
        gq3 = self.gm[:, l, 0, :].unsqueeze(1).broadcast_to([128, 4, 192])
        gk3 = self.gm[:, l, 1, :].unsqueeze(1).broadcast_to([128, 4, 192])
        TPB = [0, 1]
        MMB = [2, 3, 4, 5, 6, 7]
        self.mmi = 0
        self.tpi = 0
        self.evi = 0

        def evac(bank_i, src_ap, dst_ap, writes):
            eng = "act" if self.evi % 2 == 0 else "dve"
            self.evi += 1
            if eng == "act":
                return self.op("act", lambda a: a.copy(out=dst_ap, in_=src_ap), reads=[self.bank_s[bank_i]], writes=writes)
            return self.op("dve", lambda v: v.tensor_copy(out=dst_ap, in_=src_ap), reads=[self.bank_s[bank_i]], writes=writes)

        def project(lhs_chunks, rhs_fn, ncols, dst, dst_slots, reads):
            n0 = 0
            i = 0
            while n0 < ncols:
                w = min(512, ncols - n0)
                bi = MMB[self.mmi % len(MMB)]
                self.mmi += 1
                K = len(lhs_chunks)
                mms = [(self.banks[bi][:, 0:w], lhs_chunks[k], rhs_fn(k, n0, w), k == 0, k == K - 1) for k in range(K)]
                self.mm_group(bi, mms, reads=reads)
                evac(bi, self.banks[bi][:, 0:w], dst[:, n0:n0 + w], [dst_slots[i]])
                n0 += w
                i += 1

        def transposes(src_ap, ncol_blocks, src_slots, dst_ap, dst_writes):
            bi = TPB[self.tpi % 2]
            self.tpi += 1
            bv = self.bank_bf(bi)
            trs = [(bv[:, k * 128:(k + 1) * 128], src_ap[:, k * 128:(k + 1) * 128]) for k in range(ncol_blocks)]
            self.tr_group(bi, trs, self.ident_b[:], reads=src_slots)
            src = bv[:, 0:ncol_blocks * 128]
            if len(dst_ap.shape) == 3:
                src = src.rearrange("p (k n) -> p k n", n=128)
            return evac(bi, src, dst_ap, dst_writes)

        def stA(t):
            b = t % 2
            b3 = t % 3
            rows = slice(t * 128, (t + 1) * 128)
            sm = smA[b]
            self.dma("sp", xt[b][:], xsrc[rows, :], xt_d[b], writes=[xt_s[b]])
            yield
            self.dma("sp", csd[b3][:], self.cs_d[rows, :], cs_d_[b3], writes=[csd_s[b3]])
            yield
            self.dma("sp", csm[b3][:], self.cs_m[rows, :], cs_m_[b3], writes=[csm_s[b3]])
            yield
            self.op("act", lambda a: a.activation(out=self.junk[:], in_=xt[b][:], func=AF.Square, accum_out=sm[:, 0:1]),
                    reads=[xt_s[b]], writes=[ss_s[b]])
            yield
            self.rstd(sm[:, 0:1], ss_s[b], sm[:, 2:3], rs_s[b], sm[:, 1:2], lg_s[b], 1.0 / D)
            yield
            self.op("dve", lambda v: v.tensor_scalar(out=hb[b][:], in0=xt[b][:], scalar1=sm[:, 2:3], scalar2=None,
                                                     op0=ALU.mult), reads=[xt_s[b], rs_s[b]], writes=[hb_s[b]])
            yield
            transposes(hb[b], 8, [hb_s[b]], hT[b][:], [hT_s[b]])
            yield
            project([hT[b][:, k * 128:(k + 1) * 128] for k in range(8)],
                    lambda k, n0, w: win[:, k, n0:n0 + w], INC, pj[b], pj_s[b], [hT_s[b], w_s])
            yield
            self.op("act", lambda a: a.copy(out=vb[b3][:], in_=pj[b][:, 1024:1536]), reads=[pj_s[b][2]], writes=[vb_s[b3]])
            yield

        def stB(t):
            b = t % 2
            b3 = t % 3
            rows = slice(t * 128, (t + 1) * 128)
            sm = smB[b]
            self.dma("sp", self.vd[rows, :], vb[b3][:], vb_d[b3], reads=[vb_s[b3]])
            yield
            src3 = pj[b][:, 0:1024].rearrange("p (g w) -> p g w", w=64)
            dA3 = dA[b3][:].rearrange("p (g w) -> p g w", w=64)
            yield from self.normrope(W_d, src3, [pj_s[b][0], pj_s[b][1]], 16, 64, gd3, 16, 0, csd[b3], csd_s[b3],
                          dA3[:, :, 16:64], dA3[:, :, 0:16], dA_s[b3], 16, 64)
            self.op("act", lambda a: a.activation(out=self.junk[:, 0:256], in_=pj[b][:, 1536:1792], func=AF.Square,
                                                  accum_out=sm[:, 0:1]), reads=[pj_s[b][3]], writes=[ssc_s[b]])
            yield
            self.op("act", lambda a: a.activation(out=self.junk[:, 0:128], in_=pj[b][:, 1792:1920], func=AF.Square,
                                                  accum_out=sm[:, 1:2]), reads=[pj_s[b][3]], writes=[ssc_s[b]])
            yield
            self.op("act", lambda a: a.activation(out=sm[:, 2:3], in_=sm[:, 0:1], func=AF.Ln, scale=1.0 / 256, bias=float(EPS)),
                    reads=[ssc_s[b]], writes=[lgc_s[b]])
            yield
            self.op("act", lambda a: a.activation(out=sm[:, 3:4], in_=sm[:, 1:2], func=AF.Ln, scale=1.0 / 128, bias=float(EPS)),
                    reads=[ssc_s[b]], writes=[lgc_s[b]])
            yield
            self.op("act", lambda a: a.activation(out=sm[:, 4:6], in_=sm[:, 2:4], func=AF.Exp, scale=-0.5),
                    reads=[lgc_s[b]], writes=[rsc_s[b]])
            yield
            self.op("dve", lambda v: v.tensor_scalar(out=cn[b][:, 0:256], in0=pj[b][:, 1536:1792], scalar1=sm[:, 4:5],
                                                     scalar2=None, op0=ALU.mult), reads=[pj_s[b][3], rsc_s[b]], writes=[cn_s[b]])
            yield
            self.op("dve", lambda v: v.tensor_scalar(out=cn[b][:, 256:384], in0=pj[b][:, 1792:1920], scalar1=sm[:, 5:6],
                                                     scalar2=None, op0=ALU.mult), reads=[pj_s[b][3], rsc_s[b]],
                    writes=[cn_s[b]])
            yield
            transposes(cn[b], 3, [cn_s[b]], cT[b][:], [cT_s[b]])
            yield
            project([cT[b][:, 0:128], cT[b][:, 128:256]], lambda k, n0, w: wuq[:, k, n0:n0 + w], 768, qm[b], qm_s[b],
                    [cT_s[b], w_s])
            yield
            project([cT[b][:, 256:384]], lambda k, n0, w: wukv[:, n0:n0 + w], 1024, kvm[b], kvm_s[b], [cT_s[b], w_s])
            yield
            kvm4 = kvm[b][:].rearrange("p (h c) -> p h c", c=256)
            kf3 = kf[b][:].rearrange("p (h c) -> p h c", c=192)
            self.op("pool", lambda p: p.tensor_copy(out=kf3[:, :, 0:128], in_=kvm4[:, :, 0:128]), reads=kvm_s[b],
                    writes=[kf_s[b]])
            yield
            self.op("pool", lambda p: p.tensor_copy(out=kf3[:, :, 128:192],
                                                    in_=pj[b][:, 1920:1984].unsqueeze(1).broadcast_to([128, 4, 64])),
                    reads=[pj_s[b][3]], writes=[kf_s[b]])
            yield
            self.op("act", lambda a: a.copy(out=vmb[b3][:].rearrange("p (h c) -> p h c", c=128), in_=kvm4[:, :, 128:256]),
                    reads=kvm_s[b], writes=[vmb_s[b3]])
            yield

        def stCq(t):
            b = t % 2
            b3 = t % 3
            rows = slice(t * 128, (t + 1) * 128)
            self.dma("sp", self.vm[rows, :], vmb[b3][:], vmb_d[b3], reads=[vmb_s[b3]])
            yield
            qm3 = qm[b][:].rearrange("p (h c) -> p h c", c=192)
            mQn = mQ[b3][:, 0:512].rearrange("p (h c) -> p h c", c=128)
            mQr = mQ[b3][:, 512:768].rearrange("p (h c) -> p h c", c=64)
            yield from self.normrope(W_q, qm3, qm_s[b], 4, 192, gq3, 64, 128, csm[b3], csm_s[b3], mQn, mQr, mQ_s[b3], 0, 128)

        def stCk(t):
            b = t % 2
            b3 = t % 3
            kf3 = kf[b][:].rearrange("p (h c) -> p h c", c=192)
            mKn = mK[b3][:, 0:512].rearrange("p (h c) -> p h c", c=128)
            mKr = mK[b3][:, 512:768].rearrange("p (h c) -> p h c", c=64)
            yield from self.normrope(W_k, kf3, [kf_s[b]], 4, 192, gk3, 64, 128, csm[b3], csm_s[b3], mKn, mKr, mK_s[b3], 0, 128)

        def stCt(t):
            b = t % 2
            b3 = t % 3
            pr = t // 2
            sg = pr % 2
            tl = t % 2
            stg = stage[sg]
            cols = slice(tl * 128, (tl + 1) * 128)
            if tl == 0:
                old = stage_s[sg].rd + stage_s[sg].wr
                self.wait("act", old)
                self.wait("dve", old)
                stage_s[sg].wr = []
                stage_s[sg].rd = []
            toks = []
            toks.append(transposes(dA[b3], 8, [dA_s[b3]], stg[:, 0:8, cols], []))
            toks.append(transposes(mQ[b3], 6, [mQ_s[b3]], stg[:, 8:14, cols], []))
            toks.append(transposes(mK[b3], 6, [mK_s[b3]], stg[:, 14:20, cols], []))
            stage_s[sg].wr.extend(toks)
            if tl == 1:
                self.dma("sp", self.qkt[:, :, pr * 256:(pr + 1) * 256].rearrange("k p n -> p k n"), stg[:], st_d[sg],
                         reads=[stage_s[sg]])

        def gCt(t):
            for _ in range(8):
                yield
            stCt(t)

        for i in range(NT + 3):
            gens = []
            if i < NT:
                gens.append(stA(i))
            if 0 <= i - 1 < NT:
                gens.append(stB(i - 1))
            if 0 <= i - 2 < NT:
                gens.append(stCq(i - 2))
                gens.append(stCk(i - 2))
            gens.append(self.gpump(6))
            if 0 <= i - 3 < NT:
                gens.append(gCt(i - 3))
            self.interleave(gens)
        self.end_phase()

    def phase2(self, l):
        self.begin_phase()
        nc = self.nc
        sb = self.sb
        qt = [sb("qt", [128, S], BF16) for _ in range(2)]
        kt = [sb("kt", [128, S], BF16) for _ in range(2)]
        qr = [sb("qr", [128, S], BF16) for _ in range(2)]
        kr = [sb("kr", [128, S], BF16) for _ in range(2)]
        vs = [sb("vs", [128, NT, 128], BF16) for _ in range(2)]
        ld_s = [Slot() for _ in range(2)]
        ld_d = [self.dsem() for _ in range(2)]
        NP = 4
        pt = [sb("pt", [128, 512], BF16) for _ in range(NP)]
        pt_s = [Slot() for _ in range(NP)]
        o1 = [sb("o1", [128, 512], F32) for _ in range(2)]
        o1_s = [Slot() for _ in range(2)]
        rcb = [sb("rcb", [128, 512], F32) for _ in range(2)]
        rcb_s = [Slot() for _ in range(2)]
        tb = [sb("tb", [128, 512], F32) for _ in range(2)]
        tb_s = [Slot() for _ in range(2)]
        sqb = [sb("sqb", [128, 512], BF16) for _ in range(2)]
        sqb_s = [Slot() for _ in range(2)]
        rs2 = [sb("rs2", [128, 512], F32) for _ in range(2)]
        rs2_s = [Slot() for _ in range(2)]
        ocs = [sb("ocs", [128, 512], BF16) for _ in range(2)]
        ocs_s = [Slot() for _ in range(2)]
        ocs_d = [self.dsem() for _ in range(2)]
        mk = sb("mk", [128, 128], BF16)
        mk_s = Slot()
        self.op("dve", lambda v: v.memset(mk[:], 0.0), writes=[mk_s])
        self.op("pool", lambda p: p.affine_select(out=mk[:], in_=mk[:], pattern=[[1, 128]], compare_op=ALU.is_ge,
                                                  fill=-30000.0, base=0, channel_multiplier=-1), writes=[mk_s])
        for i in range(2):
            self.op("dve", lambda v: v.memset(kt[i][64:128, :], 0.0), writes=[ld_s[i]])
            self.op("dve", lambda v: v.memset(kr[i][0:64, :], 0.0), writes=[ld_s[i]])
        SB_ = [0, 1, 2]
        SSB = 3
        OB_ = [(4, 5), (6, 7)]
        qkt = self.qkt
        heads = [("d", h) for h in range(4)] + [("m", h) for h in range(4)]
        self.gi = 0
        self.oci = 0
        self.rci = 0
        self.tbi = 0
        self.gstep = 0
        deferred = []

        def defer(delay, fn, tag):
            deferred.append((self.gstep + delay, fn, tag))

        def run_due(force=False, tag=None):
            while True:
                due = [d for d in deferred if (force or d[0] <= self.gstep) and (tag is None or d[2] == tag)]
                if not due:
                    break
                d = min(due, key=lambda x: x[0])
                deferred.remove(d)
                d[1]()

        def load(hi):
            typ, h = heads[hi]
            i = hi % 2
            s = ld_s[i]
            self._deps("sp", [], [s])
            if typ == "d":
                self.dma("sp", qt[i][:], qkt[h], ld_d[i])
                self.dma("sp", kt[i][0:64, :], qkt[4 + h, 0:64, :], ld_d[i])
                self.dma("sp", kr[i][64:128, :], qkt[4 + h, 64:128, :], ld_d[i])
                vsrc = self.vd
            else:
                o = (h % 2) * 64
                self.dma("sp", qt[i][:], qkt[8 + h], ld_d[i])
                self.dma("sp", kt[i][:], qkt[14 + h], ld_d[i])
                self.dma("sp", qr[i][:], qkt[12 + h // 2], ld_d[i])
                self.dma("sp", kr[i][o:o + 64, :], qkt[18 + h // 2, o:o + 64, :], ld_d[i])
                vsrc = self.vm
            tok = self.dma("sp", vs[i][:], vsrc.rearrange("(j p) c -> p j c", p=128)[:, :, h * 128:(h + 1) * 128], ld_d[i])
            s.wr = [tok]
            s.rd = []

        def prep_mla(hi):
            typ, h = heads[hi]
            i = hi % 2
            o = (h % 2) * 64
            z = 64 - o
            self.op("dve", lambda v: v.memset(kr[i][z:z + 64, :], 0.0), writes=[ld_s[i]])

        load(0)
        for hi, (typ, h) in enumerate(heads):
            if hi + 1 < len(heads):
                if heads[hi + 1][0] == "m":
                    prep_mla(hi + 1)
                load(hi + 1)
            i = hi % 2
            lds = ld_s[i]
            if typ == "d":
                maps = [[(kt[i], qt[i])], [(kr[i], qt[i])]]
                scale = 64.0 ** -0.5
                negM = self.negM[:, l, 0:1]
                col = h
            else:
                maps = [[(kt[i], qt[i]), (kr[i], qr[i])]]
                scale = 192.0 ** -0.5
                negM = self.negM[:, l, 1:2]
                col = 4 + h
            steps = []
            for Qb in range(8):
                for m in range(len(maps)):
                    for j in range(4 * Qb + 4):
                        steps.append((Qb, m, j))
            n = len(steps)

            def qk(si):
                Qb, m, j = steps[si]
                r = max(0, j - 4 * Qb)
                N = (4 - r) * 128
                bi = SB_[si % 3]
                parts = maps[m]
                q0 = Qb * 512 + r * 128
                if j < 4 * Qb:
                    mms = [(self.banks[bi][:, 0:N], kp[:, j * 128:(j + 1) * 128], qp[:, q0:q0 + N], pi == 0, pi == len(parts) - 1)
                           for pi, (kp, qp) in enumerate(parts)]
                    self.mm_group(bi, mms, reads=[lds])
                    return
                bs = self.bank_s[bi]
                self._deps("pe", [lds, mk_s], [bs])
                for pi, (kp, qp) in enumerate(parts):
                    nc.tensor.matmul(self.banks[bi][:, 0:N], lhsT=kp[:, j * 128:(j + 1) * 128], rhs=qp[:, q0:q0 + N],
                                     start=(pi == 0), stop=False, skip_group_check=True)
                ins = nc.tensor.matmul(self.banks[bi][:, 0:128], lhsT=self.ident_b[:], rhs=mk[:], start=False, stop=True,
                                       skip_group_check=True)
                tok = self.sig("pe", ins)
                self._commit(tok, [lds], [bs])

            def epilogue(Qb, m, ot, sm, typ=typ, col=col):
                ri = self.rci % 2
                self.rci += 1
                cols = slice(Qb * 512, (Qb + 1) * 512)
                self.op("dve", lambda v: v.reciprocal(out=rcb[ri][:], in_=self.banks[sm][:, :]), reads=[self.bank_s[sm]],
                        writes=[rcb_s[ri]])
                if typ == "m":
                    ci = self.oci % 2
                    self.oci += 1
                    self.op("dve", lambda v: v.tensor_tensor(out=ocs[ci][:], in0=self.banks[ot][:, :], in1=rcb[ri][:], op=ALU.mult),
                            reads=[self.bank_s[ot], rcb_s[ri]], writes=[ocs_s[ci]])
                    self.dma("sp", self.oct_d[col][:, cols], ocs[ci][:], ocs_d[ci], reads=[ocs_s[ci]])
                elif m == 0:
                    oi = Qb % 2
                    self.op("dve", lambda v: v.tensor_tensor(out=o1[oi][:], in0=self.banks[ot][:, :], in1=rcb[ri][:], op=ALU.mult),
                            reads=[self.bank_s[ot], rcb_s[ri]], writes=[o1_s[oi]])
                else:
                    oi = Qb % 2
                    ti = self.tbi % 2
                    self.tbi += 1
                    run_due(force=True, tag=ti)
                    self.op("dve", lambda v: v.scalar_tensor_tensor(out=tb[ti][:], in0=self.banks[ot][:, :],
                                                                    scalar=self.neglam[:, l:l + 1], in1=rcb[ri][:],
                                                                    op0=ALU.mult, op1=ALU.mult),
                            reads=[self.bank_s[ot], rcb_s[ri]], writes=[tb_s[ti]])
                    self.op("dve", lambda v: v.tensor_tensor(out=tb[ti][:], in0=tb[ti][:], in1=o1[oi][:], op=ALU.add),
                            reads=[o1_s[oi]], writes=[tb_s[ti]])

                    def partB():
                        self.op("act", lambda a: a.activation(out=sqb[ti][:], in_=tb[ti][:], func=AF.Square), reads=[tb_s[ti]],
                                writes=[sqb_s[ti]])

                    def partC():
                        self.mm_group(SSB, [(self.banks[SSB][:, :], self.ones_b[:], sqb[ti][:], True, True)], reads=[sqb_s[ti]])

                    def partD():
                        self.op("act", lambda a: a.activation(out=rs2[ti][:], in_=self.banks[SSB][:, :], func=AF.Ln,
                                                              scale=1.0 / 128, bias=float(EPS)),
                                reads=[self.bank_s[SSB]], writes=[rs2_s[ti]])
                        self.op("act", lambda a: a.activation(out=rs2[ti][:], in_=rs2[ti][:], func=AF.Exp, scale=-0.5),
                                reads=[], writes=[rs2_s[ti]])

                    def partE():
                        ci = self.oci % 2
                        self.oci += 1
                        self.op("dve", lambda v: v.tensor_tensor(out=ocs[ci][:], in0=tb[ti][:], in1=rs2[ti][:], op=ALU.mult),
                                reads=[tb_s[ti], rs2_s[ti]], writes=[ocs_s[ci]])
                        self.dma("sp", self.oct_d[col][:, cols], ocs[ci][:], ocs_d[ci], reads=[ocs_s[ci]])

                    defer(8, partB, ti)
                    defer(10, partC, ti)
                    defer(12, partD, ti)
                    defer(15, partE, ti)

            qk(0)
            if n > 1:
                qk(1)
            for si in range(n):
                Qb, m, j = steps[si]
                self.gstep += 1
                run_due()
                if si % 10 == 0:
                    self.pump(1, engs=("dve",))
                if si + 2 < n:
                    qk(si + 2)
                r = max(0, j - 4 * Qb)
                N = (4 - r) * 128
                c0 = r * 128
                bi = SB_[si % 3]
                pi_ = si % NP
                if j == 0:
                    g = self.gi
                    self.gi += 1
                    self.cur_ob = OB_[g % 2]
                ot, sm = self.cur_ob
                self.op("act", lambda a: a.activation(out=pt[pi_][:, 0:N], in_=self.banks[bi][:, 0:N], func=AF.Exp,
                                                      scale=float(scale), bias=negM),
                        reads=[self.bank_s[bi]], writes=[pt_s[pi_]])
                self._deps("pe", [pt_s[pi_], lds], [])
                if j == 0:
                    self._deps("pe", [], [self.bank_s[ot], self.bank_s[sm]])
                last = (j == 4 * Qb + 3)
                nc.tensor.matmul(self.banks[ot][:, c0:512], lhsT=vs[i][:, j, :], rhs=pt[pi_][:, 0:N], start=(j == 0), stop=last,
                                 skip_group_check=True)
                ins = nc.tensor.matmul(self.banks[sm][:, c0:512], lhsT=self.ones_b[:], rhs=pt[pi_][:, 0:N], start=(j == 0),
                                       stop=last, skip_group_check=True)
                tok = self.sig("pe", ins)
                pt_s[pi_].rd.append(tok)
                lds.rd.append(tok)
                for bk in (ot, sm):
                    self.bank_s[bk].wr = [tok]
                    if j == 0:
                        self.bank_s[bk].rd = []
                if last:
                    epilogue(Qb, m, ot, sm)
        run_due(force=True)
        self.end_phase()

    def phase3a(self, l):
        self.begin_phase()
        self.pump_until("wo%d" % l)
        nc = self.nc
        sb = self.sb
        moe = (l % 2 == 1)
        xsrc = self.x if l == 0 else self.xr
        wo = sb("wo", [128, 8, D], BF16)
        w_s = Slot()
        d0 = self.dsem()
        w_s.wr = [self.dma("sp", wo[:], self.b_wo[l].rearrange("(c p) n -> p c n", p=128), d0)]

        def bufs(name, shape, dt, n):
            return [sb(name, shape, dt) for _ in range(n)], [Slot() for _ in range(n)]

        oc, oc_s = bufs("oc", [128, 8, 512], BF16, 2)
        xt, xt_s = bufs("xt", [128, D], F32, 2)
        x1 = [sb("x1", [128, D], F32) for _ in range(3)]
        x1_s = [[Slot(), Slot()] for _ in range(3)]
        smB = [sb("smB", [128, 4], F32) for _ in range(2)]
        ss2_s, lg2_s, rs2_s = [Slot(), Slot()], [Slot(), Slot()], [Slot(), Slot()]
        h2, h2_s = bufs("h2", [128, D], BF16, 2)
        stage = [sb("stage", [128, 8, 512], BF16) for _ in range(2)]
        stage_s = [Slot(), Slot()]
        if moe:
            h2f, h2f_s = bufs("h2f", [128, D], F32, 2)
            h2fT = [sb("h2fT", [128, D], F32) for _ in range(2)]
            h2fT_s = [[Slot(), Slot()] for _ in range(2)]
            rt = [sb("rt", [128, 48], F32) for _ in range(2)]
            rt_s = [[Slot() for _ in range(8)] for _ in range(2)]
        oc_d = [self.dsem() for _ in range(2)]
        xt_d = [self.dsem() for _ in range(2)]
        x1_d = [self.dsem() for _ in range(3)]
        st_d = [self.dsem() for _ in range(2)]
        TPB = [0, 1]
        MMB = [2, 3]
        FTB = [4, 5]
        LGB = [6, 7]
        self.tpi = 0
        self.mmi = 0
        self.lgi = 0

        def stA(t):
            b = t % 2
            b3 = t % 3
            blk, tl = t // 4, t % 4
            ob = blk % 2
            rows = slice(t * 128, (t + 1) * 128)
            if tl == 0:
                self.dma("sp", oc[ob][:], self.oct_d[:, :, blk * 512:(blk + 1) * 512].rearrange("k p n -> p k n"), oc_d[ob],
                         writes=[oc_s[ob]])
            self.dma("sp", xt[b][:], xsrc[rows, :], xt_d[b], writes=[xt_s[b]])
            yield
            for hf in range(2):
                mb = MMB[self.mmi % 2]
                self.mmi += 1
                mms = [(self.banks[mb][:, :], oc[ob][:, k, tl * 128:(tl + 1) * 128], wo[:, k, hf * 512:(hf + 1) * 512], k == 0, k == 7)
                       for k in range(8)]
                self.mm_group(mb, mms, reads=[oc_s[ob], w_s])
                yield
                self.op("dve", lambda v: v.tensor_tensor(out=x1[b3][:, hf * 512:(hf + 1) * 512], in0=self.banks[mb][:, :],
                                                         in1=xt[b][:, hf * 512:(hf + 1) * 512], op=ALU.add),
                        reads=[self.bank_s[mb], xt_s[b]], writes=[x1_s[b3][hf]])
                yield

        def stB(t):
            b = t % 2
            b3 = t % 3
            blk, tl = t // 4, t % 4
            sg = blk % 2
            rows = slice(t * 128, (t + 1) * 128)
            sm = smB[b]
            self.dma("sp", self.xr[rows, :], x1[b3][:], x1_d[b3], reads=x1_s[b3])
            yield
            self.op("act", lambda a: a.activation(out=self.junk[:], in_=x1[b3][:], func=AF.Square, accum_out=sm[:, 0:1]),
                    reads=x1_s[b3], writes=[ss2_s[b]])
            yield
            self.rstd(sm[:, 0:1], ss2_s[b], sm[:, 2:3], rs2_s[b], sm[:, 1:2], lg2_s[b], 1.0 / D)
            yield
            self.op("dve", lambda v: v.tensor_scalar(out=h2[b][:], in0=x1[b3][:], scalar1=sm[:, 2:3], scalar2=None,
                                                     op0=ALU.mult), reads=x1_s[b3] + [rs2_s[b]], writes=[h2_s[b]])
            yield
            bi = TPB[self.tpi % 2]
            self.tpi += 1
            bv = self.bank_bf(bi)
            self.tr_group(bi, [(bv[:, k * 128:(k + 1) * 128], h2[b][:, k * 128:(k + 1) * 128]) for k in range(8)],
                          self.ident_b[:], reads=[h2_s[b]])
            yield
            stg = stage[sg]
            if tl == 0:
                self.wait("act", stage_s[sg].rd + stage_s[sg].wr)
                stage_s[sg].wr = []
                stage_s[sg].rd = []
            tok = self.op("act", lambda a: a.copy(out=stg[:, :, tl * 128:(tl + 1) * 128],
                                                  in_=bv[:, :].rearrange("p (k n) -> p k n", n=128)),
                          reads=[self.bank_s[bi]], writes=[])
            yield
            stage_s[sg].wr.append(tok)
            if tl == 3:
                self.dma("sp", self.h2t[:, :, blk * 512:(blk + 1) * 512].rearrange("k p n -> p k n"), stg[:], st_d[sg],
                         reads=[stage_s[sg]])
                yield

        def stR(t):
            b = t % 2
            b3 = t % 3
            sm = smB[b]
            self.op("pool", lambda p: p.tensor_scalar(out=h2f[b][:], in0=x1[b3][:], scalar1=sm[:, 2:3], scalar2=None,
                                                      op0=ALU.mult), reads=x1_s[b3] + [rs2_s[b]], writes=[h2f_s[b]])
            yield
            for hf in range(2):
                fb = FTB[hf]
                self.tr_group(fb, [(self.banks[fb][:, k * 128:(k + 1) * 128],
                                    h2f[b][:, (hf * 4 + k) * 128:(hf * 4 + k + 1) * 128]) for k in range(4)],
                              self.ident_f[:], reads=[h2f_s[b]])
                yield
                self.op("act", lambda a: a.copy(out=h2fT[b][:, hf * 512:(hf + 1) * 512], in_=self.banks[fb][:, :]),
                        reads=[self.bank_s[fb]], writes=[h2fT_s[b][hf]])
                yield
            lb = LGB[self.lgi % 2]
            self.lgi += 1
            mms = [(self.banks[lb][:, 0:8], h2fT[b][:, k * 128:(k + 1) * 128], self.rw_s[:, k, :], k == 0, k == 7)
                   for k in range(8)]
            self.mm_group(lb, mms, reads=h2fT_s[b])
            yield
            r = rt[b]
            R = rt_s[b]
            self.op("dve", lambda v: v.tensor_copy(out=r[:, 0:8], in_=self.banks[lb][:, 0:8]), reads=[self.bank_s[lb]],
                    writes=[R[0]])
            yield
            self.op("dve", lambda v: v.max(out=r[:, 8:16], in_=r[:, 0:8]), reads=[R[0]], writes=[R[1]])
            yield
            self.op("dve", lambda v: v.tensor_scalar(out=r[:, 16:24], in0=r[:, 0:8], scalar1=r[:, 8:9], scalar2=None,
                                                     op0=ALU.subtract), reads=[R[0], R[1]], writes=[R[2]])
            yield
            self.op("act", lambda a: a.activation(out=r[:, 24:32], in_=r[:, 16:24], func=AF.Exp), reads=[R[2]], writes=[R[3]])
            yield
            self.op("dve", lambda v: v.scalar_tensor_tensor(out=r[:, 32:40], in0=r[:, 0:8], scalar=r[:, 9:10], in1=r[:, 24:32],
                                                            op0=ALU.is_ge, op1=ALU.mult), reads=[R[0], R[1], R[3]],
                    writes=[R[4]])
            yield
            self.op("dve", lambda v: v.tensor_reduce(out=r[:, 40:41], in_=r[:, 32:40], axis=AX.X, op=ALU.add), reads=[R[4]],
                    writes=[R[5]])
            yield
            self.op("dve", lambda v: v.reciprocal(out=r[:, 41:42], in_=r[:, 40:41]), reads=[R[5]], writes=[R[6]])
            yield
            self.op("dve", lambda v: v.tensor_scalar(out=self.comb[:, t, :], in0=r[:, 32:40], scalar1=r[:, 41:42], scalar2=None,
                                                     op0=ALU.mult), reads=[R[4], R[6]], writes=[R[7]])
            yield


        for i in range(NT + 2):
            gens = []
            if i < NT:
                gens.append(stA(i))
            if 0 <= i - 1 < NT:
                gens.append(stB(i - 1))
            if moe and 0 <= i - 2 < NT:
                gens.append(stR(i - 2))
            self.interleave(gens)
        self.end_phase()

    def phase3b(self, l):
        self.begin_phase()
        self.pump_until("ffn%d" % l)
        nc = self.nc
        sb = self.sb
        moe = (l % 2 == 1)
        NE = 8 if moe else 2
        wg = [sb("wg", [128, 8, FFE], BF16) for _ in range(2)]
        wu = [sb("wu", [128, 8, FFE], BF16) for _ in range(2)]
        wd = sb("wd", [128, NCH, D], BF16)
        wgu_s = [Slot() for _ in range(2)]
        wd_s = Slot()
        wgu_d = [self.dsem() for _ in range(2)]
        wd_d = self.dsem()
        hTb = [sb("hTb", [128, 8, 512], BF16) for _ in range(2)]
        hTb_s = [Slot() for _ in range(2)]
        hTb_d = [self.dsem() for _ in range(2)]
        aT = [sb("aT", [128, NCH, 512], BF16) for _ in range(2)]
        aT_s = [Slot() for _ in range(2)]
        NSG = 3
        sgb = [sb("sgb", [128, 512], F32) for _ in range(NSG)]
        sgb_s = [Slot() for _ in range(NSG)]
        NOS = 2
        ost = [sb("ost", [128, D], F32) for _ in range(NOS)]
        ost_s = [Slot() for _ in range(NOS)]
        ost_d = self.swsems[:NOS]
        xrow_s = [Slot() for _ in range(NT)]
        GB = [0, 1]
        UB = [2, 3]
        DB = [4, 5]
        self.gi = 0
        self.di = 0
        self.oi = 0
        self.si = 0
        self.evi = 0

        def load_gu(e):
            i = e % 2
            self._deps("sp", [], [wgu_s[i]])
            if moe:
                gsrc = self.b_mwg[e].rearrange("(c p) n -> p c n", p=128)
                usrc = self.b_mwu[e].rearrange("(c p) n -> p c n", p=128)
            else:
                gsrc = self.b_dwg[:, e * FFE:(e + 1) * FFE].rearrange("(c p) n -> p c n", p=128)
                usrc = self.b_dwu[:, e * FFE:(e + 1) * FFE].rearrange("(c p) n -> p c n", p=128)
            self.dma("sp", wg[i][:], gsrc, wgu_d[i])
            tok = self.dma("sp", wu[i][:], usrc, wgu_d[i])
            wgu_s[i].wr = [tok]
            wgu_s[i].rd = []

        def load_d(e):
            if moe:
                dsrc = self.b_mwd[e].rearrange("(c p) n -> p c n", p=128)
            else:
                dsrc = self.b_dwd[e * FFE:(e + 1) * FFE, :].rearrange("(c p) n -> p c n", p=128)
            self.dma("sp", wd[:], dsrc, wd_d, writes=[wd_s])

        def load_h(bi_):
            i = bi_ % 2
            blk = bi_ % 8
            self.dma("sp", hTb[i][:], self.h2t[:, :, blk * 512:(blk + 1) * 512].rearrange("k p n -> p k n"), hTb_d[i],
                     writes=[hTb_s[i]])

        def gate_up(e, blk, bi_):
            i = e % 2
            hi = bi_ % 2
            ai = bi_ % 2
            for c in range(NCH):
                gb = GB[self.gi % 2]
                ub = UB[self.gi % 2]
                self.gi += 1
                mms = [(self.banks[gb][:, :], wg[i][:, k, c * 128:(c + 1) * 128], hTb[hi][:, k, :], k == 0, k == 7) for k in range(8)]
                self.mm_group(gb, mms, reads=[wgu_s[i], hTb_s[hi]])
                mms = [(self.banks[ub][:, :], wu[i][:, k, c * 128:(c + 1) * 128], hTb[hi][:, k, :], k == 0, k == 7) for k in range(8)]
                self.mm_group(ub, mms, reads=[wgu_s[i], hTb_s[hi]])
                si_ = self.si % NSG
                self.si += 1
                self.op("act", lambda a: a.activation(out=sgb[si_][:], in_=self.banks[gb][:, :], func=AF.Silu),
                        reads=[self.bank_s[gb]], writes=[sgb_s[si_]])
                tok = self.op("dve", lambda v: v.tensor_tensor(out=aT[ai][:, c, :], in0=sgb[si_][:], in1=self.banks[ub][:, :],
                                                               op=ALU.mult),
                              reads=[sgb_s[si_], self.bank_s[ub]], writes=[aT_s[ai]] if c == 0 else [])
                if c > 0:
                    aT_s[ai].wr.append(tok)
                if c % 2 == 1:
                    self.pump(1)

        def down(e, blk, bi_):
            ai = bi_ % 2
            for tq in range(4):
                t = blk * 4 + tq
                oi_ = self.oi % NOS
                self.oi += 1
                rdt = ost_s[oi_].rd + ost_s[oi_].wr
                self.wait("act", rdt)
                self.wait("dve", rdt)
                ost_s[oi_].rd = []
                ost_s[oi_].wr = []
                for hf in range(2):
                    db = DB[self.di % 2]
                    self.di += 1
                    mms = [(self.banks[db][:, :], aT[ai][:, c, tq * 128:(tq + 1) * 128], wd[:, c, hf * 512:(hf + 1) * 512],
                            c == 0, c == NCH - 1) for c in range(NCH)]
                    self.mm_group(db, mms, reads=[aT_s[ai], wd_s])
                    eng = "act" if self.evi % 2 == 0 else "dve"
                    self.evi += 1
                    dst = ost[oi_][:, hf * 512:(hf + 1) * 512]
                    wr = []
                    if moe:
                        sc = self.comb[:, t, e:e + 1]
                        if eng == "act":
                            tok = self.op("act", lambda a: a.activation(out=dst, in_=self.banks[db][:, :], func=AF.Copy, scale=sc),
                                          reads=[self.bank_s[db]], writes=wr)
                        else:
                            tok = self.op("dve", lambda v: v.tensor_scalar(out=dst, in0=self.banks[db][:, :], scalar1=sc,
                                                                           scalar2=None, op0=ALU.mult),
                                          reads=[self.bank_s[db]], writes=wr)
                    else:
                        if eng == "act":
                            tok = self.op("act", lambda a: a.copy(out=dst, in_=self.banks[db][:, :]), reads=[self.bank_s[db]],
                                          writes=wr)
                        else:
                            tok = self.op("dve", lambda v: v.tensor_copy(out=dst, in_=self.banks[db][:, :]),
                                          reads=[self.bank_s[db]], writes=wr)
                    ost_s[oi_].wr.append(tok)
                self.dma("pool", self.xr[t * 128:(t + 1) * 128, :], ost[oi_][:], ost_d[oi_], reads=[ost_s[oi_]],
                         writes=[xrow_s[t]], accum_op=ALU.add)

        seq = [(e, blk) for e in range(NE) for blk in range(8)]
        load_gu(0)
        load_d(0)
        load_h(0)
        cur_wd = 0
        for bi_, (e, blk) in enumerate(seq):
            if bi_ + 1 < len(seq):
                load_h(bi_ + 1)
            if blk == 0 and e + 1 < NE:
                load_gu(e + 1)
            if bi_ == 0:
                gate_up(e, blk, bi_)
            if bi_ + 1 < len(seq):
                gate_up(seq[bi_ + 1][0], seq[bi_ + 1][1], bi_ + 1)
            if e != cur_wd:
                load_d(e)
                cur_wd = e
            down(e, blk, bi_)
        self.end_phase()


def _rope_table(theta, rot):
    half = rot // 2
    inv_freq = (1.0 / (np.float32(theta) ** (np.arange(half, dtype=np.float32) * np.float32(2.0 / rot)))).astype(np.float32)
    ang = (np.arange(S, dtype=np.float32)[:, None] * inv_freq[None, :]).astype(np.float32)
    c = np.cos(ang.astype(np.float64)).astype(np.float32)
    s = np.sin(ang.astype(np.float64)).astype(np.float32)
    return np.ascontiguousarray(np.concatenate([c, c, -s, s], axis=1))


_CACHE = {}


def _get_nc(**kw):
    key = tuple(sorted(kw.items()))
    if key not in _CACHE:
        kb = KB(**kw)
        kb.build()
        _CACHE[key] = kb
    return _CACHE[key]


def make_in_maps(inputs, ncores=8):
    f = lambda a: np.ascontiguousarray(np.asarray(a, dtype=np.float32))
    shared = {
        "attn_norm_g": f(inputs["attn_norm_g"]),
        "w_in": f(inputs["w_in"]),
        "diff_q_norm_g": f(inputs["diff_q_norm_g"]),
        "diff_k_norm_g": f(inputs["diff_k_norm_g"]),
        "diff_lambda": f(inputs["diff_lambda"]).reshape(DEPTH, 256),
        "diff_subln_g": f(inputs["diff_subln_g"]),
        "mla_q_ln_g": f(inputs["mla_q_ln_g"]),
        "w_uq": f(inputs["w_uq"]),
        "mla_kv_ln_g": f(inputs["mla_kv_ln_g"]),
        "w_ukv": f(inputs["w_ukv"]),
        "mla_qk_norm_g": f(inputs["mla_qk_norm_g"]).reshape(DEPTH, 384),
        "w_o": f(inputs["w_o"]),
        "ffn_norm_g": f(inputs["ffn_norm_g"]),
        "dense_w_gate": f(inputs["dense_w_gate"])[0],
        "dense_w_up": f(inputs["dense_w_up"])[0],
        "dense_w_down": f(inputs["dense_w_down"])[0],
        "router_w": f(inputs["router_w"])[0],
        "moe_w_gate": f(inputs["moe_w_gate"])[0],
        "moe_w_up": f(inputs["moe_w_up"])[0],
        "moe_w_down": f(inputs["moe_w_down"])[0],
        "cs_d": _rope_table(ROPE_THETA, 16),
        "cs_m": _rope_table(MLA_ROPE_THETA, 64),
        "ident": np.eye(128, dtype=np.float32),
    }
    x = f(inputs["x"])
    maps = []
    for c in range(ncores):
        m = dict(shared)
        m["x"] = np.ascontiguousarray(x[c])
        maps.append(m)
    return maps


def kernel(**inputs):
    kb = _get_nc()
    in_maps = make_in_maps(inputs)
    res = run_bass_kernel_spmd(kb.nc, in_maps, core_ids=list(range(8)))
    out = np.stack([np.asarray(r["y"], dtype=np.float32) for r in res.results], axis=0)
    return out
```

```python
import math
from contextlib import ExitStack
import numpy as np
import ml_dtypes
import concourse.bass as bass
import concourse.mybir as mybir
from concourse.bass_utils import run_bass_kernel_spmd

F32 = mybir.dt.float32
BF16 = mybir.dt.bfloat16
ALU = mybir.AluOpType
AF = mybir.ActivationFunctionType
AX = mybir.AxisListType

S = 4096
D = 1024
NT = S // 128
DEPTH = 2
EPS = 1e-6
INC = 1984
FFE = 1408
NCH = FFE // 128
ROPE_THETA = 500000.0
MLA_ROPE_THETA = 10000.0


class DSem:
    def __init__(self, h):
        self.h = h
        self.v = 0


class Slot:
    __slots__ = ("wr", "rd")

    def __init__(self):
        self.wr = []
        self.rd = []


class KB:
    def __init__(self, depth=DEPTH, stop_after=None, debug=False):
        self.depth = depth
        self.stop_after = stop_after
        self.debug = debug
        nc = self.nc = bass.Bass("TRN2", target_bir_lowering=False)
        self.E = dict(pe=nc.tensor, dve=nc.vector, act=nc.scalar, pool=nc.gpsimd, sp=nc.sync)
        self.csem = {k: nc.alloc_semaphore("c_" + k) for k in ("pe", "dve", "act", "pool")}
        self.ccnt = dict.fromkeys(self.csem, 0)
        self.waited = {}
        self.dsems = [DSem(nc.alloc_semaphore("d%d" % i)) for i in range(72)]
        self.dptr = 0
        self.swsems = [DSem(nc.alloc_semaphore("sw%d" % i)) for i in range(3)]
        self.banks = [nc.alloc_psum_tensor("bank%d" % i, [128, 512], F32) for i in range(8)]
        self.bank_s = [Slot() for _ in range(8)]
        self.stack = None
        self.uid = 0
        self.const_s = None
        self.log = [] if debug else None

    def sig(self, e, ins):
        self.ccnt[e] += 1
        ins.then_inc(self.csem[e], 1)
        if self.log is not None:
            self.log.append(("inc", e, self.csem[e].num, 1))
        return (self.csem[e], self.ccnt[e])

    def wait(self, e, toks):
        for tok in toks:
            sem, v = tok
            key = (e, sem.num)
            if self.waited.get(key, 0) >= v:
                continue
            self.waited[key] = v
            self.E[e].wait_ge(sem, v)
            if self.log is not None:
                self.log.append(("wait", e, sem.num, v))

    def _deps(self, e, reads, writes):
        toks = []
        for s in reads:
            toks.extend(s.wr)
        for s in writes:
            toks.extend(s.rd)
            toks.extend(s.wr)
        self.wait(e, toks)

    def _commit(self, tok, reads, writes):
        for s in reads:
            if s is not self.const_s:
                s.rd.append(tok)
        for s in writes:
            s.wr = [tok]
            s.rd = []

    def op(self, e, fn, reads=(), writes=()):
        self._deps(e, reads, writes)
        ins = fn(self.E[e])
        tok = self.sig(e, ins)
        self._commit(tok, reads, writes)
        return tok

    def dsem(self):
        d = self.dsems[self.dptr]
        self.dptr += 1
        return d

    def dma(self, q, out, in_, ds, reads=(), writes=(), **kw):
        self._deps(q, reads, writes)
        ins = self.E[q].dma_start(out=out, in_=in_, **kw)
        ins.then_inc(ds.h, 16)
        ds.v += 16
        if self.log is not None:
            self.log.append(("dma", q, ds.h.num, 16))
        tok = (ds.h, ds.v)
        self._commit(tok, reads, writes)
        return tok

    def mm_group(self, bank_i, mms, reads=()):
        bs = self.bank_s[bank_i]
        self._deps("pe", reads, [bs])
        ins = None
        for (o, l, r, st, sp) in mms:
            ins = self.nc.tensor.matmul(o, lhsT=l, rhs=r, start=st, stop=sp)
        tok = self.sig("pe", ins)
        self._commit(tok, reads, [bs])
        return tok

    def tr_group(self, bank_i, trs, ident, reads=()):
        bs = self.bank_s[bank_i]
        self._deps("pe", reads, [bs])
        ins = None
        for (o, i) in trs:
            ins = self.nc.tensor.transpose(out=o, in_=i, identity=ident)
        tok = self.sig("pe", ins)
        self._commit(tok, reads, [bs])
        return tok

    def barrier(self):
        toks = [(self.csem[c], self.ccnt[c]) for c in self.csem if self.ccnt[c] > 0]
        toks += [(d.h, d.v) for d in self.dsems + self.swsems if d.v > 0]
        for e in self.E:
            self.wait(e, toks)

    def begin_phase(self):
        self.barrier()
        self.stack = ExitStack()
        self.dptr = self.dkeep
        for b in self.bank_s:
            b.wr = []
            b.rd = []

    def end_phase(self):
        self.barrier()
        self.stack.close()
        self.stack = None

    def sb(self, name, shape, dt):
        self.uid += 1
        return self.stack.enter_context(self.nc.sbuf_tensor("%s_%d" % (name, self.uid), shape, dt))

    def interleave(self, gens):
        gens = list(gens)
        while gens:
            nxt = []
            for g in gens:
                try:
                    next(g)
                    nxt.append(g)
                except StopIteration:
                    pass
            gens = nxt

    def bank_bf(self, i):
        return self.banks[i][:].bitcast(BF16)

    def build(self):
        nc = self.nc

        def din(name, shape):
            return nc.dram_tensor(name, shape, F32, kind="ExternalInput").ap()

        self.x = din("x", [S, D])
        self.attn_g = din("attn_norm_g", [DEPTH, D])
        self.w_in = din("w_in", [DEPTH, D, INC])
        self.dq_g = din("diff_q_norm_g", [DEPTH, 64])
        self.dk_g = din("diff_k_norm_g", [DEPTH, 64])
        self.dlam = din("diff_lambda", [DEPTH, 256])
        self.subln_g = din("diff_subln_g", [DEPTH, 128])
        self.qln_g = din("mla_q_ln_g", [DEPTH, 256])
        self.w_uq = din("w_uq", [DEPTH, 256, 768])
        self.kvln_g = din("mla_kv_ln_g", [DEPTH, 128])
        self.w_ukv = din("w_ukv", [DEPTH, 128, 1024])
        self.mqk_g = din("mla_qk_norm_g", [DEPTH, 2 * 192])
        self.w_o = din("w_o", [DEPTH, D, D])
        self.ffn_g = din("ffn_norm_g", [DEPTH, D])
        self.dwg = din("dense_w_gate", [D, 2816])
        self.dwu = din("dense_w_up", [D, 2816])
        self.dwd = din("dense_w_down", [2816, D])
        self.rw = din("router_w", [D, 8])
        self.mwg = din("moe_w_gate", [8, D, FFE])
        self.mwu = din("moe_w_up", [8, D, FFE])
        self.mwd = din("moe_w_down", [8, FFE, D])
        self.cs_d = din("cs_d", [S, 32])
        self.cs_m = din("cs_m", [S, 128])
        self.ident_in = din("ident", [128, 128])
        self.y = nc.dram_tensor("y", [S, D], F32, kind="ExternalOutput").ap()
        xr = self.xr = self.y

        kind = "ExternalOutput" if self.debug else "Internal"

        def dscr(name, shape, dt):
            return nc.dram_tensor(name, shape, dt, kind=kind).ap()

        self.qkt = dscr("qkt", [20, 128, S], BF16)
        self.vd = dscr("vd", [S, 512], BF16)
        self.vm = dscr("vm", [S, 512], BF16)
        self.oct_d = dscr("oct_d", [8, 128, S], BF16)
        self.h2t = dscr("h2t", [8, 128, S], BF16)
        self.b_win = dscr("b_win", [DEPTH, D, INC], BF16)
        self.b_wuq = dscr("b_wuq", [DEPTH, 256, 768], BF16)
        self.b_wukv = dscr("b_wukv", [DEPTH, 128, 1024], BF16)
        self.b_wo = dscr("b_wo", [DEPTH, D, D], BF16)
        self.b_dwg = dscr("b_dwg", [D, 2816], BF16)
        self.b_dwu = dscr("b_dwu", [D, 2816], BF16)
        self.b_dwd = dscr("b_dwd", [2816, D], BF16)
        self.b_mwg = dscr("b_mwg", [8, D, FFE], BF16)
        self.b_mwu = dscr("b_mwu", [8, D, FFE], BF16)
        self.b_mwd = dscr("b_mwd", [8, FFE, D], BF16)

        A = nc.alloc_sbuf_tensor
        self.ident_f = A("ident_f", [128, 128], F32)
        self.ident_b = A("ident_b", [128, 128], BF16)
        self.ones_b = A("ones_b", [128, 128], BF16)
        self.sc_attn = A("sc_attn", [128, DEPTH, 8], F32)
        self.sc_ffn = A("sc_ffn", [128, DEPTH, 8], F32)
        self.sc_qln = A("sc_qln", [128, DEPTH, 2], F32)
        self.sc_kvln = A("sc_kvln", [128, DEPTH, 1], F32)
        self.sc_wo = A("sc_wo", [128, DEPTH, 8], F32)
        self.ones = A("ones", [128, 24], F32)
        self.gd = A("gd", [128, DEPTH, 2, 64], F32)
        self.gm = A("gm", [128, DEPTH, 2, 192], F32)
        self.lamt = A("lamt", [128, DEPTH, 256], F32)
        self.lamw = A("lamw", [128, 8], F32)
        self.neglam = A("neglam", [128, DEPTH], F32)
        self.negM = A("negM", [128, DEPTH, 2], F32)
        self.gmax = A("gmax", [128, 8], F32)
        self.rw_s = A("rw_s", [128, 8, 8], F32)
        self.comb = A("comb", [128, NT, 8], F32)
        self.junk = A("junk", [128, 1024], BF16)
        self.const_s = Slot()

        self.setup_consts()
        self.setup_wjobs()
        self.dkeep = self.dptr
        if self.log is not None:
            snap = {self.csem[c].num: self.ccnt[c] for c in self.csem}
            snap.update({d.h.num: d.v for d in self.dsems + self.swsems + [self.xinit]})
            self.log.append(("snapshot", snap))
        for l in range(self.depth):
            self.phase1(l)
            if self.stop_after == "p1_%d" % l:
                return self.finish()
            self.phase2(l)
            if self.stop_after == "p2_%d" % l:
                return self.finish()
            self.phase3a(l)
            if self.stop_after == "p3a_%d" % l:
                return self.finish()
            self.phase3b(l)
            if self.stop_after == "p3b_%d" % l:
                return self.finish()
        return self.finish()

    def finish(self):
        self.barrier()
        return self.nc

    def setup_consts(self):
        nc = self.nc
        cs = self.const_s
        ds = self.dsem()
        toks = []

        def ld(out, in_, **kw):
            ins = nc.sync.dma_start(out=out, in_=in_, **kw)
            ins.then_inc(ds.h, 16)
            ds.v += 16

        self.xinit = self.dsem()

        ld(self.ident_f[:], self.ident_in[:, :])
        for l in range(DEPTH):
            ld(self.sc_attn[:, l, :], self.attn_g[l].rearrange("(c p) -> p c", p=128), allow_slow_non_contiguous=True)
            ld(self.sc_ffn[:, l, :], self.ffn_g[l].rearrange("(c p) -> p c", p=128), allow_slow_non_contiguous=True)
            ld(self.sc_qln[:, l, :], self.qln_g[l].rearrange("(c p) -> p c", p=128), allow_slow_non_contiguous=True)
            ld(self.sc_kvln[:, l, :], self.kvln_g[l].rearrange("(c p) -> p c", p=128), allow_slow_non_contiguous=True)
            ld(self.sc_wo[:, l, 0:1], self.subln_g[l].rearrange("(c p) -> p c", p=128), allow_slow_non_contiguous=True)
            ld(self.gd[:, l, 0, :], self.dq_g[l].partition_broadcast(128))
            ld(self.gd[:, l, 1, :], self.dk_g[l].partition_broadcast(128))
            ld(self.gm[:, l, :, :].rearrange("p a b -> p (a b)"), self.mqk_g[l].partition_broadcast(128))
            ld(self.lamt[:, l, :], self.dlam[l].partition_broadcast(128))
        ld(self.rw_s[:], self.rw.rearrange("(c p) n -> p c n", p=128))
        nc.vector.wait_ge(ds.h, ds.v)
        nc.gpsimd.wait_ge(ds.h, ds.v)
        nc.scalar.wait_ge(ds.h, ds.v)
        V = nc.vector
        sv = self.csem["dve"]

        def vop(ins):
            self.ccnt["dve"] += 1
            ins.then_inc(sv, 1)
            V.wait_ge(sv, self.ccnt["dve"])

        def aop(ins):
            self.ccnt["act"] += 1
            ins.then_inc(self.csem["act"], 1)
            nc.scalar.wait_ge(self.csem["act"], self.ccnt["act"])
            V.wait_ge(self.csem["act"], self.ccnt["act"])

        vop(V.tensor_copy(out=self.ident_b[:], in_=self.ident_f[:]))
        vop(V.memset(self.ones[:], 1.0))
        vop(V.memset(self.ones_b[:], 1.0))
        for l in range(DEPTH):
            lam_init = 0.8 - 0.6 * math.exp(-0.3 * l)
            vop(V.tensor_scalar(out=self.sc_wo[:, l, 0:1], in0=self.sc_wo[:, l, 0:1], scalar1=float(1.0 - lam_init),
                                scalar2=None, op0=ALU.mult))
            for c in range(1, 4):
                vop(V.tensor_copy(out=self.sc_wo[:, l, c:c + 1], in_=self.sc_wo[:, l, 0:1]))
            vop(V.memset(self.sc_wo[:, l, 4:8], 1.0))
        for l in range(DEPTH):
            lam_init = 0.8 - 0.6 * math.exp(-0.3 * l)
            vop(V.tensor_tensor(out=self.lamt[:, l, 0:64], in0=self.lamt[:, l, 0:64], in1=self.lamt[:, l, 64:128], op=ALU.mult))
            vop(V.tensor_tensor(out=self.lamt[:, l, 128:192], in0=self.lamt[:, l, 128:192], in1=self.lamt[:, l, 192:256], op=ALU.mult))
            vop(V.tensor_reduce(out=self.lamw[:, 0:1], in_=self.lamt[:, l, 0:64], axis=AX.X, op=ALU.add))
            vop(V.tensor_reduce(out=self.lamw[:, 1:2], in_=self.lamt[:, l, 128:192], axis=AX.X, op=ALU.add))
            nc.scalar.wait_ge(sv, self.ccnt["dve"])
            aop(nc.scalar.activation(out=self.lamw[:, 2:4], in_=self.lamw[:, 0:2], func=AF.Exp))
            vop(V.tensor_tensor(out=self.lamw[:, 4:5], in0=self.lamw[:, 3:4], in1=self.lamw[:, 2:3], op=ALU.subtract))
            vop(V.tensor_scalar(out=self.neglam[:, l:l + 1], in0=self.lamw[:, 4:5], scalar1=float(-lam_init),
                                scalar2=None, op0=ALU.add))
            vop(V.tensor_reduce(out=self.gmax[:, 0:2], in_=self.gd[:, l, :, :], axis=AX.X, op=ALU.max,
                                apply_absolute_value=True))
            vop(V.tensor_reduce(out=self.gmax[:, 2:4], in_=self.gm[:, l, :, :], axis=AX.X, op=ALU.max,
                                apply_absolute_value=True))
            vop(V.tensor_tensor(out=self.gmax[:, 4:5], in0=self.gmax[:, 0:1], in1=self.gmax[:, 1:2], op=ALU.mult))
            vop(V.tensor_tensor(out=self.gmax[:, 5:6], in0=self.gmax[:, 2:3], in1=self.gmax[:, 3:4], op=ALU.mult))
            vop(V.tensor_scalar(out=self.negM[:, l, 0:1], in0=self.gmax[:, 4:5], scalar1=float(-math.sqrt(64.0)),
                                scalar2=None, op0=ALU.mult))
            vop(V.tensor_scalar(out=self.negM[:, l, 1:2], in0=self.gmax[:, 5:6], scalar1=float(-math.sqrt(192.0)),
                                scalar2=None, op0=ALU.mult))
        for c in range(8):
            vop(V.tensor_scalar(out=self.rw_s[:, c, :], in0=self.rw_s[:, c, :], scalar1=self.sc_ffn[:, 1, c:c + 1],
                                scalar2=None, op0=ALU.mult))
        vop(V.memset(self.comb[:], 1.0))

    def setup_wjobs(self):
        nc = self.nc
        NB = 3
        CM = 1408
        self.w_fst = [nc.alloc_sbuf_tensor("wf%d" % i, [128, CM], F32) for i in range(NB)]
        self.w_bst = [nc.alloc_sbuf_tensor("wb%d" % i, [128, CM], BF16) for i in range(NB)]
        self.w_fs = [Slot() for _ in range(NB)]
        self.w_bs = [Slot() for _ in range(NB)]
        self.w_fd = [self.dsem() for _ in range(NB)]
        self.w_bd = [self.dsem() for _ in range(NB)]
        self.w_nb = NB
        self.wjobs = []
        self.wnext = 0
        self.wloadq = []
        self.wcastq = []
        self.wcall = 0
        self.wtags = {}

        def add(src, dst, R, C, sc):
            ncs = 1 if C <= CM else 2
            cw = C // ncs
            for c in range(R // 128):
                for j in range(ncs):
                    self.wjobs.append((src[c * 128:(c + 1) * 128, j * cw:(j + 1) * cw],
                                       dst[c * 128:(c + 1) * 128, j * cw:(j + 1) * cw], cw, sc(c)))

        one = lambda c: self.ones[:, 0:1]

        def attn(l):
            add(self.w_in[l], self.b_win[l], D, INC, lambda c, l=l: self.sc_attn[:, l, c:c + 1])
            add(self.w_uq[l], self.b_wuq[l], 256, 768, lambda c, l=l: self.sc_qln[:, l, c:c + 1])
            add(self.w_ukv[l], self.b_wukv[l], 128, 1024, lambda c, l=l: self.sc_kvln[:, l, c:c + 1])
            self.wtags["attn%d" % l] = len(self.wjobs)
            add(self.w_o[l], self.b_wo[l], D, D, lambda c, l=l: self.sc_wo[:, l, c:c + 1])
            self.wtags["wo%d" % l] = len(self.wjobs)

        attn(0)
        add(self.dwg, self.b_dwg, D, 2816, lambda c: self.sc_ffn[:, 0, c:c + 1])
        add(self.dwu, self.b_dwu, D, 2816, lambda c: self.sc_ffn[:, 0, c:c + 1])
        add(self.dwd, self.b_dwd, 2816, D, one)
        self.wtags["ffn0"] = len(self.wjobs)
        if self.depth > 1:
            attn(1)
            for e in range(8):
                add(self.mwg[e], self.b_mwg[e], D, FFE, lambda c: self.sc_ffn[:, 1, c:c + 1])
                add(self.mwu[e], self.b_mwu[e], D, FFE, lambda c: self.sc_ffn[:, 1, c:c + 1])
                add(self.mwd[e], self.b_mwd[e], FFE, D, one)
            self.wtags["ffn1"] = len(self.wjobs)

    def _wstage_store(self, upto):
        keep = []
        for ent in self.wcastq:
            (i, dst, C, call) = ent
            if call > upto:
                keep.append(ent)
                continue
            self.dma("sp", dst, self.w_bst[i][:, 0:C], self.w_bd[i], reads=[self.w_bs[i]])
        self.wcastq = keep

    def _wstage_cast(self, upto, engs):
        keep = []
        for ent in self.wloadq:
            (i, dst, C, scal, call, k) = ent
            if call > upto:
                keep.append(ent)
                continue
            eng = engs[k % len(engs)]
            fst, bst = self.w_fst[i], self.w_bst[i]
            if eng == "act":
                self.op("act", lambda a: a.activation(out=bst[:, 0:C], in_=fst[:, 0:C], func=AF.Copy, scale=scal),
                        reads=[self.w_fs[i]], writes=[self.w_bs[i]])
            else:
                self.op(eng, lambda v: v.tensor_scalar(out=bst[:, 0:C], in0=fst[:, 0:C], scalar1=scal, scalar2=None,
                                                       op0=ALU.mult), reads=[self.w_fs[i]], writes=[self.w_bs[i]])
            self.wcastq.append((i, dst, C, self.wcall))
        self.wloadq = keep

    def pump(self, n, engs=("dve", "act")):
        self.wcall += 1
        c = self.wcall
        self._wstage_store(c - 1)
        self._wstage_cast(c - 1, engs)
        for _ in range(n):
            if self.wnext >= len(self.wjobs):
                break
            src, dst, C, scal = self.wjobs[self.wnext]
            k = self.wnext
            i = k % self.w_nb
            self.wnext += 1
            self.dma("sp", self.w_fst[i][:, 0:C], src, self.w_fd[i], writes=[self.w_fs[i]])
            self.wloadq.append((i, dst, C, scal, c, k))

    def pump_until(self, tag, engs=("dve", "act")):
        tgt = self.wtags[tag]
        while self.wnext < tgt:
            self.pump(1, engs)
        self.wcall += 1
        self._wstage_store(self.wcall)
        self._wstage_cast(self.wcall, engs)
        self._wstage_store(self.wcall)
        self.wait("sp", [(d.h, d.v) for d in self.w_bd if d.v > 0])

    def gpump(self, rounds, engs=("dve", "act")):
        for _ in range(rounds):
            yield
        self.pump(1, engs)

    def rstd(self, src_ap, src_slot, out_ap, out_slot, tmp_ap, tmp_slot, inv_w):
        self.op("act", lambda a: a.activation(out=tmp_ap, in_=src_ap, func=AF.Ln, scale=float(inv_w), bias=float(EPS)),
                reads=[src_slot], writes=[tmp_slot])
        self.op("act", lambda a: a.activation(out=out_ap, in_=tmp_ap, func=AF.Exp, scale=-0.5),
                reads=[tmp_slot], writes=[out_slot])

    def normrope(self, W_, src3, src_slots, G, W, gain3, R, rot_lo, cs_ap, cs_slot, dst_nonrot, dst_rot, dst_slot,
                 nonrot_lo, nonrot_hi):
        sq, sq_s, ssg, ssg_s, lg, lg_s, rsg, rsg_s, tmp, tmp_s, xr_, xr_s, ac, ac_s, sw, sw_s = W_
        n = G * W
        h = R // 2
        sq3 = sq[:, 0:n].rearrange("p (g w) -> p g w", w=W)
        tmp3 = tmp[:, 0:n].rearrange("p (g w) -> p g w", w=W)
        xr3 = xr_[:, 0:G * R].rearrange("p (g r) -> p g r", r=R)
        ac3 = ac[:, 0:G * R].rearrange("p (g r) -> p g r", r=R)
        sw3 = sw[:, 0:G * R].rearrange("p (g r) -> p g r", r=R)
        self.op("act", lambda a: a.activation(out=sq3, in_=src3, func=AF.Square), reads=src_slots, writes=[sq_s])
        yield
        self.op("dve", lambda v: v.tensor_reduce(out=ssg[:, 0:G], in_=sq3, axis=AX.X, op=ALU.add),
                reads=[sq_s], writes=[ssg_s])
        yield
        self.rstd(ssg[:, 0:G], ssg_s, rsg[:, 0:G], rsg_s, lg[:, 0:G], lg_s, 1.0 / W)
        yield
        self.op("dve", lambda v: v.tensor_tensor(out=tmp3, in0=src3, in1=rsg[:, 0:G].unsqueeze(2).broadcast_to([128, G, W]),
                                                 op=ALU.mult),
                reads=list(src_slots) + [rsg_s], writes=[tmp_s])
        yield
        self.op("pool", lambda p: p.tensor_tensor(out=dst_nonrot, in0=tmp3[:, :, nonrot_lo:nonrot_hi],
                                                  in1=gain3[:, :, nonrot_lo:nonrot_hi], op=ALU.mult),
                reads=[tmp_s], writes=[dst_slot])
        yield
        self.op("pool", lambda p: p.tensor_tensor(out=xr3, in0=tmp3[:, :, rot_lo:rot_lo + R],
                                                  in1=gain3[:, :, rot_lo:rot_lo + R], op=ALU.mult),
                reads=[tmp_s], writes=[xr_s])
        yield
        cc = cs_ap[:, 0:R].unsqueeze(1).broadcast_to([128, G, R])
        ns = cs_ap[:, R:R + h].unsqueeze(1).broadcast_to([128, G, h])
        ps_ = cs_ap[:, R + h:2 * R].unsqueeze(1).broadcast_to([128, G, h])
        self.op("pool", lambda p: p.tensor_tensor(out=ac3, in0=xr3, in1=cc, op=ALU.mult),
                reads=[xr_s, cs_slot], writes=[ac_s])
        yield
        self.op("pool", lambda p: p.tensor_tensor(out=sw3[:, :, 0:h], in0=xr3[:, :, h:R], in1=ns, op=ALU.mult),
                reads=[xr_s, cs_slot], writes=[sw_s])
        yield
        self.op("pool", lambda p: p.tensor_tensor(out=sw3[:, :, h:R], in0=xr3[:, :, 0:h], in1=ps_, op=ALU.mult),
                reads=[xr_s, cs_slot], writes=[sw_s])
        yield
        tok = self.op("pool", lambda p: p.tensor_tensor(out=dst_rot, in0=ac3, in1=sw3, op=ALU.add),
                      reads=[ac_s, sw_s], writes=[dst_slot])
        yield
        return tok

    def phase1(self, l):
        self.begin_phase()
        self.pump_until("attn%d" % l)
        xsrc = self.x if l == 0 else self.xr
        nc = self.nc
        sb = self.sb
        win = sb("win", [128, 8, INC], BF16)
        wuq = sb("wuq", [128, 2, 768], BF16)
        wukv = sb("wukv", [128, 1024], BF16)
        w_s = Slot()
        d0 = self.dsem()
        self.dma("sp", win[:], self.b_win[l].rearrange("(c p) n -> p c n", p=128), d0, writes=[])
        self.dma("sp", wuq[:], self.b_wuq[l].rearrange("(c p) n -> p c n", p=128), d0, writes=[])
        tokw = self.dma("sp", wukv[:], self.b_wukv[l], d0, writes=[])
        w_s.wr = [tokw]

        def bufs(name, shape, dt, n):
            return [sb(name, shape, dt) for _ in range(n)], [Slot() for _ in range(n)]

        xt, xt_s = bufs("xt", [128, D], F32, 2)
        hb, hb_s = bufs("hb", [128, D], BF16, 2)
        hT, hT_s = bufs("hT", [128, D], BF16, 2)
        pj = [sb("pj", [128, INC], F32) for _ in range(2)]
        pj_s = [[Slot() for _ in range(4)] for _ in range(2)]
        smA = [sb("smA", [128, 4], F32) for _ in range(2)]
        ss_s, lg_s, rs_s = [Slot(), Slot()], [Slot(), Slot()], [Slot(), Slot()]
        smB = [sb("smB", [128, 8], F32) for _ in range(2)]
        ssc_s, lgc_s, rsc_s = [Slot(), Slot()], [Slot(), Slot()], [Slot(), Slot()]
        cn, cn_s = bufs("cn", [128, 384], BF16, 2)
        cT, cT_s = bufs("cT", [128, 384], BF16, 2)
        qm = [sb("qm", [128, 768], F32) for _ in range(2)]
        qm_s = [[Slot(), Slot()] for _ in range(2)]
        kvm = [sb("kvm", [128, 1024], F32) for _ in range(2)]
        kvm_s = [[Slot(), Slot()] for _ in range(2)]
        kf, kf_s = bufs("kf", [128, 768], F32, 2)
        dA, dA_s = bufs("dA", [128, 1024], BF16, 3)
        mQ, mQ_s = bufs("mQ", [128, 768], BF16, 3)
        mK, mK_s = bufs("mK", [128, 768], BF16, 3)
        vb, vb_s = bufs("vb", [128, 512], BF16, 3)
        vmb, vmb_s = bufs("vmb", [128, 512], BF16, 3)
        csd, csd_s = bufs("csd", [128, 32], F32, 3)
        csm, csm_s = bufs("csm", [128, 128], F32, 3)
        stage = [sb("stage", [128, 20, 256], BF16) for _ in range(2)]
        stage_s = [Slot(), Slot()]
        xt_d = [self.dsem() for _ in range(2)]
        cs_d_ = [self.dsem() for _ in range(3)]
        cs_m_ = [self.dsem() for _ in range(3)]
        vb_d = [self.dsem() for _ in range(3)]
        vmb_d = [self.dsem() for _ in range(3)]
        st_d = [self.dsem() for _ in range(2)]

        def mkscratch(n, gr):
            return (sb("sq", [128, n], F32), Slot(), sb("ssg", [128, 16], F32), Slot(), sb("lg", [128, 16], F32), Slot(),
                    sb("rsg", [128, 16], F32), Slot(), sb("tmp", [128, n], F32), Slot(), sb("xr_", [128, gr], F32), Slot(),
                    sb("ac", [128, gr], F32), Slot(), sb("sw", [128, gr], F32), Slot())

        W_d = mkscratch(1024, 256)
        W_q = mkscratch(768, 256)
        W_k = mkscratch(768, 256)

        gdf = sb("gdf", [128, 16, 64], F32)
        gdf_s = Slot()
        self.op("dve", lambda v: v.tensor_copy(out=gdf[:, 0:8, :], in_=self.gd[:, l, 0, :].unsqueeze(1).broadcast_to([128, 8, 64])),
                writes=[gdf_s])
        self.op("dve", lambda v: v.tensor_copy(out=gdf[:, 8:16, :], in_=self.gd[:, l, 1, :].unsqueeze(1).broadcast_to([128, 8, 64])),
                writes=[gdf_s])
        self.wait("pool", gdf_s.wr)
        gd3 = gdf[:]
        gq3 = self.gm[:, l, 0, :].unsqueeze(1).broadcast_to([128, 4, 192])
        gk3 = self.gm[:, l, 1, :].unsqueeze(1).broadcast_to([128, 4, 192])
        TPB = [0, 1]
        MMB = [2, 3, 4, 5, 6, 7]
        self.mmi = 0
        self.tpi = 0
        self.evi = 0

        def evac(bank_i, src_ap, dst_ap, writes):
            eng = "act" if self.evi % 2 == 0 else "dve"
            self.evi += 1
            if eng == "act":
                return self.op("act", lambda a: a.copy(out=dst_ap, in_=src_ap), reads=[self.bank_s[bank_i]], writes=writes)
            return self.op("dve", lambda v: v.tensor_copy(out=dst_ap, in_=src_ap), reads=[self.bank_s[bank_i]], writes=writes)

        def project(lhs_chunks, rhs_fn, ncols, dst, dst_slots, reads):
            n0 = 0
            i = 0
            while n0 < ncols:
                w = min(512, ncols - n0)
                bi = MMB[self.mmi % len(MMB)]
                self.mmi += 1
                K = len(lhs_chunks)
                mms = [(self.banks[bi][:, 0:w], lhs_chunks[k], rhs_fn(k, n0, w), k == 0, k == K - 1) for k in range(K)]
                self.mm_group(bi, mms, reads=reads)
                evac(bi, self.banks[bi][:, 0:w], dst[:, n0:n0 + w], [dst_slots[i]])
                n0 += w
                i += 1

        def transposes(src_ap, ncol_blocks, src_slots, dst_ap, dst_writes):
            bi = TPB[self.tpi % 2]
            self.tpi += 1
            bv = self.bank_bf(bi)
            trs = [(bv[:, k * 128:(k + 1) * 128], src_ap[:, k * 128:(k + 1) * 128]) for k in range(ncol_blocks)]
            self.tr_group(bi, trs, self.ident_b[:], reads=src_slots)
            src = bv[:, 0:ncol_blocks * 128]
            if len(dst_ap.shape) == 3:
                src = src.rearrange("p (k n) -> p k n", n=128)
            return evac(bi, src, dst_ap, dst_writes)

        def stA(t):
            b = t % 2
            b3 = t % 3
            rows = slice(t * 128, (t + 1) * 128)
            sm = smA[b]
            self.dma("sp", xt[b][:], xsrc[rows, :], xt_d[b], writes=[xt_s[b]])
            yield
            self.dma("sp", csd[b3][:], self.cs_d[rows, :], cs_d_[b3], writes=[csd_s[b3]])
            yield
            self.dma("sp", csm[b3][:], self.cs_m[rows, :], cs_m_[b3], writes=[csm_s[b3]])
            yield
            self.op("act", lambda a: a.activation(out=self.junk[:], in_=xt[b][:], func=AF.Square, accum_out=sm[:, 0:1]),
                    reads=[xt_s[b]], writes=[ss_s[b]])
            yield
            self.rstd(sm[:, 0:1], ss_s[b], sm[:, 2:3], rs_s[b], sm[:, 1:2], lg_s[b], 1.0 / D)
            yield
            self.op("dve", lambda v: v.tensor_scalar(out=hb[b][:], in0=xt[b][:], scalar1=sm[:, 2:3], scalar2=None,
                                                     op0=ALU.mult), reads=[xt_s[b], rs_s[b]], writes=[hb_s[b]])
            yield
            transposes(hb[b], 8, [hb_s[b]], hT[b][:], [hT_s[b]])
            yield
            project([hT[b][:, k * 128:(k + 1) * 128] for k in range(8)],
                    lambda k, n0, w: win[:, k, n0:n0 + w], INC, pj[b], pj_s[b], [hT_s[b], w_s])
            yield
            self.op("act", lambda a: a.copy(out=vb[b3][:], in_=pj[b][:, 1024:1536]), reads=[pj_s[b][2]], writes=[vb_s[b3]])
            yield

        def stB(t):
            b = t % 2
            b3 = t % 3
            rows = slice(t * 128, (t + 1) * 128)
            sm = smB[b]
            self.dma("sp", self.vd[rows, :], vb[b3][:], vb_d[b3], reads=[vb_s[b3]])
            yield
            src3 = pj[b][:, 0:1024].rearrange("p (g w) -> p g w", w=64)
            dA3 = dA[b3][:].rearrange("p (g w) -> p g w", w=64)
            yield from self.normrope(W_d, src3, [pj_s[b][0], pj_s[b][1]], 16, 64, gd3, 16, 0, csd[b3], csd_s[b3],
                          dA3[:, :, 16:64], dA3[:, :, 0:16], dA_s[b3], 16, 64)
            self.op("act", lambda a: a.activation(out=self.junk[:, 0:256], in_=pj[b][:, 1536:1792], func=AF.Square,
                                                  accum_out=sm[:, 0:1]), reads=[pj_s[b][3]], writes=[ssc_s[b]])
            yield
            self.op("act", lambda a: a.activation(out=self.junk[:, 0:128], in_=pj[b][:, 1792:1920], func=AF.Square,
                                                  accum_out=sm[:, 1:2]), reads=[pj_s[b][3]], writes=[ssc_s[b]])
            yield
            self.op("act", lambda a: a.activation(out=sm[:, 2:3], in_=sm[:, 0:1], func=AF.Ln, scale=1.0 / 256, bias=float(EPS)),
                    reads=[ssc_s[b]], writes=[lgc_s[b]])
            yield
            self.op("act", lambda a: a.activation(out=sm[:, 3:4], in_=sm[:, 1:2], func=AF.Ln, scale=1.0 / 128, bias=float(EPS)),
                    reads=[ssc_s[b]], writes=[lgc_s[b]])
            yield
            self.op("act", lambda a: a.activation(out=sm[:, 4:6], in_=sm[:, 2:4], func=AF.Exp, scale=-0.5),
                    reads=[lgc_s[b]], writes=[rsc_s[b]])
            yield
            self.op("dve", lambda v: v.tensor_scalar(out=cn[b][:, 0:256], in0=pj[b][:, 1536:1792], scalar1=sm[:, 4:5],
                                                     scalar2=None, op0=ALU.mult), reads=[pj_s[b][3], rsc_s[b]], writes=[cn_s[b]])
            yield
            self.op("dve", lambda v: v.tensor_scalar(out=cn[b][:, 256:384], in0=pj[b][:, 1792:1920], scalar1=sm[:, 5:6],
                                                     scalar2=None, op0=ALU.mult), reads=[pj_s[b][3], rsc_s[b]],
                    writes=[cn_s[b]])
            yield
            transposes(cn[b], 3, [cn_s[b]], cT[b][:], [cT_s[b]])
            yield
            project([cT[b][:, 0:128], cT[b][:, 128:256]], lambda k, n0, w: wuq[:, k, n0:n0 + w], 768, qm[b], qm_s[b],
                    [cT_s[b], w_s])
            yield
            project([cT[b][:, 256:384]], lambda k, n0, w: wukv[:, n0:n0 + w], 1024, kvm[b], kvm_s[b], [cT_s[b], w_s])
            yield
            kvm4 = kvm[b][:].rearrange("p (h c) -> p h c", c=256)
            kf3 = kf[b][:].rearrange("p (h c) -> p h c", c=192)
            self.op("pool", lambda p: p.tensor_copy(out=kf3[:, :, 0:128], in_=kvm4[:, :, 0:128]), reads=kvm_s[b],
                    writes=[kf_s[b]])
            yield
            self.op("pool", lambda p: p.tensor_copy(out=kf3[:, :, 128:192],
                                                    in_=pj[b][:, 1920:1984].unsqueeze(1).broadcast_to([128, 4, 64])),
                    reads=[pj_s[b][3]], writes=[kf_s[b]])
            yield
            self.op("act", lambda a: a.copy(out=vmb[b3][:].rearrange("p (h c) -> p h c", c=128), in_=kvm4[:, :, 128:256]),
                    reads=kvm_s[b], writes=[vmb_s[b3]])
            yield

        def stCq(t):
            b = t % 2
            b3 = t % 3
            rows = slice(t * 128, (t + 1) * 128)
            self.dma("sp", self.vm[rows, :], vmb[b3][:], vmb_d[b3], reads=[vmb_s[b3]])
            yield
            qm3 = qm[b][:].rearrange("p (h c) -> p h c", c=192)
            mQn = mQ[b3][:, 0:512].rearrange("p (h c) -> p h c", c=128)
            mQr = mQ[b3][:, 512:768].rearrange("p (h c) -> p h c", c=64)
            yield from self.normrope(W_q, qm3, qm_s[b], 4, 192, gq3, 64, 128, csm[b3], csm_s[b3], mQn, mQr, mQ_s[b3], 0, 128)

        def stCk(t):
            b = t % 2
            b3 = t % 3
            kf3 = kf[b][:].rearrange("p (h c) -> p h c", c=192)
            mKn = mK[b3][:, 0:512].rearrange("p (h c) -> p h c", c=128)
            mKr = mK[b3][:, 512:768].rearrange("p (h c) -> p h c", c=64)
            yield from self.normrope(W_k, kf3, [kf_s[b]], 4, 192, gk3, 64, 128, csm[b3], csm_s[b3], mKn, mKr, mK_s[b3], 0, 128)

        def stCt(t):
            b = t % 2
            b3 = t % 3
            pr = t // 2
            sg = pr % 2
            tl = t % 2
            stg = stage[sg]
            cols = slice(tl * 128, (tl + 1) * 128)
            if tl == 0:
                old = stage_s[sg].rd + stage_s[sg].wr
                self.wait("act", old)
                self.wait("dve", old)
                stage_s[sg].wr = []
                stage_s[sg].rd = []
            toks = []
            toks.append(transposes(dA[b3], 8, [dA_s[b3]], stg[:, 0:8, cols], []))
            toks.append(transposes(mQ[b3], 6, [mQ_s[b3]], stg[:, 8:14, cols], []))
            toks.append(transposes(mK[b3], 6, [mK_s[b3]], stg[:, 14:20, cols], []))
            stage_s[sg].wr.extend(toks)
            if tl == 1:
                self.dma("sp", self.qkt[:, :, pr * 256:(pr + 1) * 256].rearrange("k p n -> p k n"), stg[:], st_d[sg],
                         reads=[stage_s[sg]])

        def gCt(t):
            for _ in range(8):
                yield
            stCt(t)

        for i in range(NT + 3):
            gens = []
            if i < NT:
                gens.append(stA(i))
            if 0 <= i - 1 < NT:
                gens.append(stB(i - 1))
            if 0 <= i - 2 < NT:
                gens.append(stCq(i - 2))
                gens.append(stCk(i - 2))
            gens.append(self.gpump(6))
            if 0 <= i - 3 < NT:
                gens.append(gCt(i - 3))
            self.interleave(gens)
        self.end_phase()

    def phase2(self, l):
        self.begin_phase()
        nc = self.nc
        sb = self.sb
        qt = [sb("qt", [128, S], BF16) for _ in range(2)]
        kt = [sb("kt", [128, S], BF16) for _ in range(2)]
        qr = [sb("qr", [128, S], BF16) for _ in range(2)]
        kr = [sb("kr", [128, S], BF16) for _ in range(2)]
        vs = [sb("vs", [128, NT, 128], BF16) for _ in range(2)]
        ld_s = [Slot() for _ in range(2)]
        ld_d = [self.dsem() for _ in range(2)]
        NP = 4
        pt = [sb("pt", [128, 512], BF16) for _ in range(NP)]
        pt_s = [Slot() for _ in range(NP)]
        o1 = [sb("o1", [128, 512], F32) for _ in range(2)]
        o1_s = [Slot() for _ in range(2)]
        rcb = [sb("rcb", [128, 512], F32) for _ in range(2)]
        rcb_s = [Slot() for _ in range(2)]
        tb = [sb("tb", [128, 512], F32) for _ in range(2)]
        tb_s = [Slot() for _ in range(2)]
        sqb = [sb("sqb", [128, 512], BF16) for _ in range(2)]
        sqb_s = [Slot() for _ in range(2)]
        rs2 = [sb("rs2", [128, 512], F32) for _ in range(2)]
        rs2_s = [Slot() for _ in range(2)]
        ocs = [sb("ocs", [128, 512], BF16) for _ in range(2)]
        ocs_s = [Slot() for _ in range(2)]
        ocs_d = [self.dsem() for _ in range(2)]
        mk = sb("mk", [128, 128], BF16)
        mk_s = Slot()
        self.op("dve", lambda v: v.memset(mk[:], 0.0), writes=[mk_s])
        self.op("pool", lambda p: p.affine_select(out=mk[:], in_=mk[:], pattern=[[1, 128]], compare_op=ALU.is_ge,
                                                  fill=-30000.0, base=0, channel_multiplier=-1), writes=[mk_s])
        for i in range(2):
            self.op("dve", lambda v: v.memset(kt[i][64:128, :], 0.0), writes=[ld_s[i]])
            self.op("dve", lambda v: v.memset(kr[i][0:64, :], 0.0), writes=[ld_s[i]])
        SB_ = [0, 1, 2]
        SSB = 3
        OB_ = [(4, 5), (6, 7)]
        qkt = self.qkt
        heads = [("d", h) for h in range(4)] + [("m", h) for h in range(4)]
        self.gi = 0
        self.oci = 0
        self.rci = 0
        self.tbi = 0
        self.gstep = 0
        deferred = []

        def defer(delay, fn, tag):
            deferred.append((self.gstep + delay, fn, tag))

        def run_due(force=False, tag=None):
            while True:
                due = [d for d in deferred if (force or d[0] <= self.gstep) and (tag is None or d[2] == tag)]
                if not due:
                    break
                d = min(due, key=lambda x: x[0])
                deferred.remove(d)
                d[1]()

        def load(hi):
            typ, h = heads[hi]
            i = hi % 2
            s = ld_s[i]
            self._deps("sp", [], [s])
            if typ == "d":
                self.dma("sp", qt[i][:], qkt[h], ld_d[i])
                self.dma("sp", kt[i][0:64, :], qkt[4 + h, 0:64, :], ld_d[i])
                self.dma("sp", kr[i][64:128, :], qkt[4 + h, 64:128, :], ld_d[i])
                vsrc = self.vd
            else:
                o = (h % 2) * 64
                self.dma("sp", qt[i][:], qkt[8 + h], ld_d[i])
                self.dma("sp", kt[i][:], qkt[14 + h], ld_d[i])
                self.dma("sp", qr[i][:], qkt[12 + h // 2], ld_d[i])
                self.dma("sp", kr[i][o:o + 64, :], qkt[18 + h // 2, o:o + 64, :], ld_d[i])
                vsrc = self.vm
            tok = self.dma("sp", vs[i][:], vsrc.rearrange("(j p) c -> p j c", p=128)[:, :, h * 128:(h + 1) * 128], ld_d[i])
            s.wr = [tok]
            s.rd = []

        def prep_mla(hi):
            typ, h = heads[hi]
            i = hi % 2
            o = (h % 2) * 64
            z = 64 - o
            self.op("dve", lambda v: v.memset(kr[i][z:z + 64, :], 0.0), writes=[ld_s[i]])

        load(0)
        for hi, (typ, h) in enumerate(heads):
            if hi + 1 < len(heads):
                if heads[hi + 1][0] == "m":
                    prep_mla(hi + 1)
                load(hi + 1)
            i = hi % 2
            lds = ld_s[i]
            if typ == "d":
                maps = [[(kt[i], qt[i])], [(kr[i], qt[i])]]
                scale = 64.0 ** -0.5
                negM = self.negM[:, l, 0:1]
                col = h
            else:
                maps = [[(kt[i], qt[i]), (kr[i], qr[i])]]
                scale = 192.0 ** -0.5
                negM = self.negM[:, l, 1:2]
                col = 4 + h
            steps = []
            for Qb in range(8):
                for m in range(len(maps)):
                    for j in range(4 * Qb + 4):
                        steps.append((Qb, m, j))
            n = len(steps)

            def qk(si):
                Qb, m, j = steps[si]
                r = max(0, j - 4 * Qb)
                N = (4 - r) * 128
                bi = SB_[si % 3]
                parts = maps[m]
                q0 = Qb * 512 + r * 128
                if j < 4 * Qb:
                    mms = [(self.banks[bi][:, 0:N], kp[:, j * 128:(j + 1) * 128], qp[:, q0:q0 + N], pi == 0, pi == len(parts) - 1)
                           for pi, (kp, qp) in enumerate(parts)]
                    self.mm_group(bi, mms, reads=[lds])
                    return
                bs = self.bank_s[bi]
                self._deps("pe", [lds, mk_s], [bs])
                for pi, (kp, qp) in enumerate(parts):
                    nc.tensor.matmul(self.banks[bi][:, 0:N], lhsT=kp[:, j * 128:(j + 1) * 128], rhs=qp[:, q0:q0 + N],
                                     start=(pi == 0), stop=False, skip_group_check=True)
                ins = nc.tensor.matmul(self.banks[bi][:, 0:128], lhsT=self.ident_b[:], rhs=mk[:], start=False, stop=True,
                                       skip_group_check=True)
                tok = self.sig("pe", ins)
                self._commit(tok, [lds], [bs])

            def epilogue(Qb, m, ot, sm, typ=typ, col=col):
                ri = self.rci % 2
                self.rci += 1
                cols = slice(Qb * 512, (Qb + 1) * 512)
                self.op("dve", lambda v: v.reciprocal(out=rcb[ri][:], in_=self.banks[sm][:, :]), reads=[self.bank_s[sm]],
                        writes=[rcb_s[ri]])
                if typ == "m":
                    ci = self.oci % 2
                    self.oci += 1
                    self.op("dve", lambda v: v.tensor_tensor(out=ocs[ci][:], in0=self.banks[ot][:, :], in1=rcb[ri][:], op=ALU.mult),
                            reads=[self.bank_s[ot], rcb_s[ri]], writes=[ocs_s[ci]])
                    self.dma("sp", self.oct_d[col][:, cols], ocs[ci][:], ocs_d[ci], reads=[ocs_s[ci]])
                elif m == 0:
                    oi = Qb % 2
                    self.op("dve", lambda v: v.tensor_tensor(out=o1[oi][:], in0=self.banks[ot][:, :], in1=rcb[ri][:], op=ALU.mult),
                            reads=[self.bank_s[ot], rcb_s[ri]], writes=[o1_s[oi]])
                else:
                    oi = Qb % 2
                    ti = self.tbi % 2
                    self.tbi += 1
                    run_due(force=True, tag=ti)
                    self.op("dve", lambda v: v.scalar_tensor_tensor(out=tb[ti][:], in0=self.banks[ot][:, :],
                                                                    scalar=self.neglam[:, l:l + 1], in1=rcb[ri][:],
                                                                    op0=ALU.mult, op1=ALU.mult),
                            reads=[self.bank_s[ot], rcb_s[ri]], writes=[tb_s[ti]])
                    self.op("dve", lambda v: v.tensor_tensor(out=tb[ti][:], in0=tb[ti][:], in1=o1[oi][:], op=ALU.add),
                            reads=[o1_s[oi]], writes=[tb_s[ti]])

                    def partB():
                        self.op("act", lambda a: a.activation(out=sqb[ti][:], in_=tb[ti][:], func=AF.Square), reads=[tb_s[ti]],
                                writes=[sqb_s[ti]])

                    def partC():
                        self.mm_group(SSB, [(self.banks[SSB][:, :], self.ones_b[:], sqb[ti][:], True, True)], reads=[sqb_s[ti]])

                    def partD():
                        self.op("act", lambda a: a.activation(out=rs2[ti][:], in_=self.banks[SSB][:, :], func=AF.Ln,
                                                              scale=1.0 / 128, bias=float(EPS)),
                                reads=[self.bank_s[SSB]], writes=[rs2_s[ti]])
                        self.op("act", lambda a: a.activation(out=rs2[ti][:], in_=rs2[ti][:], func=AF.Exp, scale=-0.5),
                                reads=[], writes=[rs2_s[ti]])

                    def partE():
                        ci = self.oci % 2
                        self.oci += 1
                        self.op("dve", lambda v: v.tensor_tensor(out=ocs[ci][:], in0=tb[ti][:], in1=rs2[ti][:], op=ALU.mult),
                                reads=[tb_s[ti], rs2_s[ti]], writes=[ocs_s[ci]])
                        self.dma("sp", self.oct_d[col][:, cols], ocs[ci][:], ocs_d[ci], reads=[ocs_s[ci]])

                    defer(8, partB, ti)
                    defer(10, partC, ti)
                    defer(12, partD, ti)
                    defer(15, partE, ti)

            qk(0)
            if n > 1:
                qk(1)
            for si in range(n):
                Qb, m, j = steps[si]
                self.gstep += 1
                run_due()
                if si % 6 == 0:
                    self.pump(1, engs=("dve",))
                if si + 2 < n:
                    qk(si + 2)
                r = max(0, j - 4 * Qb)
                N = (4 - r) * 128
                c0 = r * 128
                bi = SB_[si % 3]
                pi_ = si % NP
                if j == 0:
                    g = self.gi
                    self.gi += 1
                    self.cur_ob = OB_[g % 2]
                ot, sm = self.cur_ob
                self.op("act", lambda a: a.activation(out=pt[pi_][:, 0:N], in_=self.banks[bi][:, 0:N], func=AF.Exp,
                                                      scale=float(scale), bias=negM),
                        reads=[self.bank_s[bi]], writes=[pt_s[pi_]])
                self._deps("pe", [pt_s[pi_], lds], [])
                if j == 0:
                    self._deps("pe", [], [self.bank_s[ot], self.bank_s[sm]])
                last = (j == 4 * Qb + 3)
                nc.tensor.matmul(self.banks[ot][:, c0:512], lhsT=vs[i][:, j, :], rhs=pt[pi_][:, 0:N], start=(j == 0), stop=last,
                                 skip_group_check=True)
                ins = nc.tensor.matmul(self.banks[sm][:, c0:512], lhsT=self.ones_b[:], rhs=pt[pi_][:, 0:N], start=(j == 0),
                                       stop=last, skip_group_check=True)
                tok = self.sig("pe", ins)
                pt_s[pi_].rd.append(tok)
                lds.rd.append(tok)
                for bk in (ot, sm):
                    self.bank_s[bk].wr = [tok]
                    if j == 0:
                        self.bank_s[bk].rd = []
                if last:
                    epilogue(Qb, m, ot, sm)
        run_due(force=True)
        self.end_phase()

    def phase3a(self, l):
        self.begin_phase()
        self.pump_until("wo%d" % l)
        nc = self.nc
        sb = self.sb
        moe = (l % 2 == 1)
        xsrc = self.x if l == 0 else self.xr
        wo = sb("wo", [128, 8, D], BF16)
        w_s = Slot()
        d0 = self.dsem()
        w_s.wr = [self.dma("sp", wo[:], self.b_wo[l].rearrange("(c p) n -> p c n", p=128), d0)]

        def bufs(name, shape, dt, n):
            return [sb(name, shape, dt) for _ in range(n)], [Slot() for _ in range(n)]

        oc, oc_s = bufs("oc", [128, 8, 512], BF16, 2)
        xt, xt_s = bufs("xt", [128, D], F32, 2)
        x1 = [sb("x1", [128, D], F32) for _ in range(3)]
        x1_s = [[Slot(), Slot()] for _ in range(3)]
        smB = [sb("smB", [128, 4], F32) for _ in range(2)]
        ss2_s, lg2_s, rs2_s = [Slot(), Slot()], [Slot(), Slot()], [Slot(), Slot()]
        h2, h2_s = bufs("h2", [128, D], BF16, 2)
        stage = [sb("stage", [128, 8, 512], BF16) for _ in range(2)]
        stage_s = [Slot(), Slot()]
        if moe:
            h2f, h2f_s = bufs("h2f", [128, D], F32, 2)
            h2fT = [sb("h2fT", [128, D], F32) for _ in range(2)]
            h2fT_s = [[Slot(), Slot()] for _ in range(2)]
            lgall = sb("lgall", [128, NT, 8], F32)
            lgall_s = Slot()
            rd = sb("rd", [128, NT, 8], F32)
            rd_s = Slot()
            rt1 = sb("rt1", [128, NT, 8], F32)
            rt1_s = Slot()
            rt2 = sb("rt2", [128, NT, 8], F32)
            rt2_s = Slot()
            rm = sb("rm", [128, 4, NT], F32)
            rm_s = [Slot() for _ in range(4)]
            comb_s = Slot()
        oc_d = [self.dsem() for _ in range(2)]
        xt_d = [self.dsem() for _ in range(2)]
        x1_d = [self.dsem() for _ in range(3)]
        st_d = [self.dsem() for _ in range(2)]
        TPB = [0, 1]
        MMB = [2, 3]
        FTB = [4, 5]
        LGB = [6, 7]
        self.tpi = 0
        self.mmi = 0
        self.lgi = 0

        def stA(t):
            b = t % 2
            b3 = t % 3
            blk, tl = t // 4, t % 4
            ob = blk % 2
            rows = slice(t * 128, (t + 1) * 128)
            if tl == 0:
                self.dma("sp", oc[ob][:], self.oct_d[:, :, blk * 512:(blk + 1) * 512].rearrange("k p n -> p k n"), oc_d[ob],
                         writes=[oc_s[ob]])
            self.dma("sp", xt[b][:], xsrc[rows, :], xt_d[b], writes=[xt_s[b]])
            yield
            for hf in range(2):
                mb = MMB[self.mmi % 2]
                self.mmi += 1
                mms = [(self.banks[mb][:, :], oc[ob][:, k, tl * 128:(tl + 1) * 128], wo[:, k, hf * 512:(hf + 1) * 512], k == 0, k == 7)
                       for k in range(8)]
                self.mm_group(mb, mms, reads=[oc_s[ob], w_s])
                yield
                self.op("dve", lambda v: v.tensor_tensor(out=x1[b3][:, hf * 512:(hf + 1) * 512], in0=self.banks[mb][:, :],
                                                         in1=xt[b][:, hf * 512:(hf + 1) * 512], op=ALU.add),
                        reads=[self.bank_s[mb], xt_s[b]], writes=[x1_s[b3][hf]])
                yield

        def stB(t):
            b = t % 2
            b3 = t % 3
            blk, tl = t // 4, t % 4
            sg = blk % 2
            rows = slice(t * 128, (t + 1) * 128)
            sm = smB[b]
            self.dma("sp", self.xr[rows, :], x1[b3][:], x1_d[b3], reads=x1_s[b3])
            yield
            self.op("act", lambda a: a.activation(out=self.junk[:], in_=x1[b3][:], func=AF.Square, accum_out=sm[:, 0:1]),
                    reads=x1_s[b3], writes=[ss2_s[b]])
            yield
            self.rstd(sm[:, 0:1], ss2_s[b], sm[:, 2:3], rs2_s[b], sm[:, 1:2], lg2_s[b], 1.0 / D)
            yield
            self.op("dve", lambda v: v.tensor_scalar(out=h2[b][:], in0=x1[b3][:], scalar1=sm[:, 2:3], scalar2=None,
                                                     op0=ALU.mult), reads=x1_s[b3] + [rs2_s[b]], writes=[h2_s[b]])
            yield
            bi = TPB[self.tpi % 2]
            self.tpi += 1
            bv = self.bank_bf(bi)
            self.tr_group(bi, [(bv[:, k * 128:(k + 1) * 128], h2[b][:, k * 128:(k + 1) * 128]) for k in range(8)],
                          self.ident_b[:], reads=[h2_s[b]])
            yield
            stg = stage[sg]
            if tl == 0:
                self.wait("act", stage_s[sg].rd + stage_s[sg].wr)
                stage_s[sg].wr = []
                stage_s[sg].rd = []
            tok = self.op("act", lambda a: a.copy(out=stg[:, :, tl * 128:(tl + 1) * 128],
                                                  in_=bv[:, :].rearrange("p (k n) -> p k n", n=128)),
                          reads=[self.bank_s[bi]], writes=[])
            yield
            stage_s[sg].wr.append(tok)
            if tl == 3:
                self.dma("sp", self.h2t[:, :, blk * 512:(blk + 1) * 512].rearrange("k p n -> p k n"), stg[:], st_d[sg],
                         reads=[stage_s[sg]])
                yield

        def stR(t):
            b = t % 2
            b3 = t % 3
            sm = smB[b]
            self.op("pool", lambda p: p.tensor_scalar(out=h2f[b][:], in0=x1[b3][:], scalar1=sm[:, 2:3], scalar2=None,
                                                      op0=ALU.mult), reads=x1_s[b3] + [rs2_s[b]], writes=[h2f_s[b]])
            yield
            for hf in range(2):
                fb = FTB[hf]
                self.tr_group(fb, [(self.banks[fb][:, k * 128:(k + 1) * 128],
                                    h2f[b][:, (hf * 4 + k) * 128:(hf * 4 + k + 1) * 128]) for k in range(4)],
                              self.ident_f[:], reads=[h2f_s[b]])
                yield
                self.op("act", lambda a: a.copy(out=h2fT[b][:, hf * 512:(hf + 1) * 512], in_=self.banks[fb][:, :]),
                        reads=[self.bank_s[fb]], writes=[h2fT_s[b][hf]])
                yield
            lb = LGB[self.lgi % 2]
            self.lgi += 1
            mms = [(self.banks[lb][:, 0:8], h2fT[b][:, k * 128:(k + 1) * 128], self.rw_s[:, k, :], k == 0, k == 7)
                   for k in range(8)]
            self.mm_group(lb, mms, reads=h2fT_s[b])
            yield
            tok = self.op("dve", lambda v: v.tensor_copy(out=lgall[:, t, :], in_=self.banks[lb][:, 0:8]),
                          reads=[self.bank_s[lb]], writes=[])
            lgall_s.wr.append(tok)
            yield

        def router_finish():
            def bc(ap2):
                return ap2.unsqueeze(2).broadcast_to([128, NT, 8])
            self.op("dve", lambda v: v.tensor_reduce(out=rm[:, 0, :], in_=lgall[:], axis=AX.X, op=ALU.max),
                    reads=[lgall_s], writes=[rm_s[0]])
            self.op("dve", lambda v: v.tensor_tensor(out=rd[:], in0=lgall[:], in1=bc(rm[:, 0, :]), op=ALU.subtract),
                    reads=[lgall_s, rm_s[0]], writes=[rd_s])
            self.op("dve", lambda v: v.tensor_scalar(out=rt1[:], in0=rd[:], scalar1=0.0, scalar2=-1e30, op0=ALU.is_ge,
                                                     op1=ALU.mult), reads=[rd_s], writes=[rt1_s])
            self.op("dve", lambda v: v.tensor_tensor(out=rt1[:], in0=rt1[:], in1=rd[:], op=ALU.add),
                    reads=[rd_s], writes=[rt1_s])
            self.op("dve", lambda v: v.tensor_reduce(out=rm[:, 1, :], in_=rt1[:], axis=AX.X, op=ALU.max),
                    reads=[rt1_s], writes=[rm_s[1]])
            self.op("dve", lambda v: v.tensor_tensor(out=rt1[:], in0=rd[:], in1=bc(rm[:, 1, :]), op=ALU.is_ge),
                    reads=[rd_s, rm_s[1]], writes=[rt1_s])
            self.op("act", lambda a: a.activation(out=rt2[:], in_=rd[:], func=AF.Exp), reads=[rd_s], writes=[rt2_s])
            self.op("dve", lambda v: v.tensor_tensor(out=rt1[:], in0=rt1[:], in1=rt2[:], op=ALU.mult),
                    reads=[rt2_s], writes=[rt1_s])
            self.op("dve", lambda v: v.tensor_reduce(out=rm[:, 2, :], in_=rt1[:], axis=AX.X, op=ALU.add),
                    reads=[rt1_s], writes=[rm_s[2]])
            self.op("dve", lambda v: v.reciprocal(out=rm[:, 3, :], in_=rm[:, 2, :]), reads=[rm_s[2]], writes=[rm_s[3]])
            self.op("dve", lambda v: v.tensor_tensor(out=self.comb[:], in0=rt1[:], in1=bc(rm[:, 3, :]), op=ALU.mult),
                    reads=[rt1_s, rm_s[3]], writes=[comb_s])

        for i in range(NT + 2):
            gens = []
            if i < NT:
                gens.append(stA(i))
            if 0 <= i - 1 < NT:
                gens.append(stB(i - 1))
            if moe and 0 <= i - 2 < NT:
                gens.append(stR(i - 2))
            self.interleave(gens)
        if moe:
            router_finish()
        self.end_phase()

    def phase3b(self, l):
        self.begin_phase()
        self.pump_until("ffn%d" % l)
        nc = self.nc
        sb = self.sb
        moe = (l % 2 == 1)
        NE = 8 if moe else 2
        wg = [sb("wg", [128, 8, FFE], BF16) for _ in range(2)]
        wu = [sb("wu", [128, 8, FFE], BF16) for _ in range(2)]
        wd = sb("wd", [128, NCH, D], BF16)
        wgu_s = [Slot() for _ in range(2)]
        wd_s = Slot()
        wgu_d = [self.dsem() for _ in range(2)]
        wd_d = self.dsem()
        hTb = [sb("hTb", [128, 8, 512], BF16) for _ in range(2)]
        hTb_s = [Slot() for _ in range(2)]
        hTb_d = [self.dsem() for _ in range(2)]
        aT = [sb("aT", [128, NCH, 512], BF16) for _ in range(2)]
        aT_s = [Slot() for _ in range(2)]
        NSG = 3
        sgb = [sb("sgb", [128, 512], F32) for _ in range(NSG)]
        sgb_s = [Slot() for _ in range(NSG)]
        NOS = 2
        ost = [sb("ost", [128, D], F32) for _ in range(NOS)]
        ost_s = [Slot() for _ in range(NOS)]
        ost_d = self.swsems[:NOS]
        xrow_s = [Slot() for _ in range(NT)]
        GB = [0, 1]
        UB = [2, 3]
        DB = [4, 5]
        self.gi = 0
        self.di = 0
        self.oi = 0
        self.si = 0
        self.evi = 0

        def load_gu(e):
            i = e % 2
            self._deps("sp", [], [wgu_s[i]])
            if moe:
                gsrc = self.b_mwg[e].rearrange("(c p) n -> p c n", p=128)
                usrc = self.b_mwu[e].rearrange("(c p) n -> p c n", p=128)
            else:
                gsrc = self.b_dwg[:, e * FFE:(e + 1) * FFE].rearrange("(c p) n -> p c n", p=128)
                usrc = self.b_dwu[:, e * FFE:(e + 1) * FFE].rearrange("(c p) n -> p c n", p=128)
            self.dma("sp", wg[i][:], gsrc, wgu_d[i])
            tok = self.dma("sp", wu[i][:], usrc, wgu_d[i])
            wgu_s[i].wr = [tok]
            wgu_s[i].rd = []

        def load_d(e):
            if moe:
                dsrc = self.b_mwd[e].rearrange("(c p) n -> p c n", p=128)
            else:
                dsrc = self.b_dwd[e * FFE:(e + 1) * FFE, :].rearrange("(c p) n -> p c n", p=128)
            self.dma("sp", wd[:], dsrc, wd_d, writes=[wd_s])

        def load_h(bi_):
            i = bi_ % 2
            blk = bi_ % 8
            self.dma("sp", hTb[i][:], self.h2t[:, :, blk * 512:(blk + 1) * 512].rearrange("k p n -> p k n"), hTb_d[i],
                     writes=[hTb_s[i]])

        def gate_up(e, blk, bi_):
            i = e % 2
            hi = bi_ % 2
            ai = bi_ % 2
            for c in range(NCH):
                gb = GB[self.gi % 2]
                ub = UB[self.gi % 2]
                self.gi += 1
                mms = [(self.banks[gb][:, :], wg[i][:, k, c * 128:(c + 1) * 128], hTb[hi][:, k, :], k == 0, k == 7) for k in range(8)]
                self.mm_group(gb, mms, reads=[wgu_s[i], hTb_s[hi]])
                mms = [(self.banks[ub][:, :], wu[i][:, k, c * 128:(c + 1) * 128], hTb[hi][:, k, :], k == 0, k == 7) for k in range(8)]
                self.mm_group(ub, mms, reads=[wgu_s[i], hTb_s[hi]])
                si_ = self.si % NSG
                self.si += 1
                self.op("act", lambda a: a.activation(out=sgb[si_][:], in_=self.banks[gb][:, :], func=AF.Silu),
                        reads=[self.bank_s[gb]], writes=[sgb_s[si_]])
                tok = self.op("dve", lambda v: v.tensor_tensor(out=aT[ai][:, c, :], in0=sgb[si_][:], in1=self.banks[ub][:, :],
                                                               op=ALU.mult),
                              reads=[sgb_s[si_], self.bank_s[ub]], writes=[aT_s[ai]] if c == 0 else [])
                if c > 0:
                    aT_s[ai].wr.append(tok)

        def down(e, blk, bi_):
            ai = bi_ % 2
            for tq in range(4):
                t = blk * 4 + tq
                oi_ = self.oi % NOS
                self.oi += 1
                rdt = ost_s[oi_].rd + ost_s[oi_].wr
                self.wait("act", rdt)
                self.wait("dve", rdt)
                ost_s[oi_].rd = []
                ost_s[oi_].wr = []
                for hf in range(2):
                    db = DB[self.di % 2]
                    self.di += 1
                    mms = [(self.banks[db][:, :], aT[ai][:, c, tq * 128:(tq + 1) * 128], wd[:, c, hf * 512:(hf + 1) * 512],
                            c == 0, c == NCH - 1) for c in range(NCH)]
                    self.mm_group(db, mms, reads=[aT_s[ai], wd_s])
                    eng = "act" if self.evi % 2 == 0 else "dve"
                    self.evi += 1
                    dst = ost[oi_][:, hf * 512:(hf + 1) * 512]
                    wr = []
                    if moe:
                        sc = self.comb[:, t, e:e + 1]
                        if eng == "act":
                            tok = self.op("act", lambda a: a.activation(out=dst, in_=self.banks[db][:, :], func=AF.Copy, scale=sc),
                                          reads=[self.bank_s[db]], writes=wr)
                        else:
                            tok = self.op("dve", lambda v: v.tensor_scalar(out=dst, in0=self.banks[db][:, :], scalar1=sc,
                                                                           scalar2=None, op0=ALU.mult),
                                          reads=[self.bank_s[db]], writes=wr)
                    else:
                        if eng == "act":
                            tok = self.op("act", lambda a: a.copy(out=dst, in_=self.banks[db][:, :]), reads=[self.bank_s[db]],
                                          writes=wr)
                        else:
                            tok = self.op("dve", lambda v: v.tensor_copy(out=dst, in_=self.banks[db][:, :]),
                                          reads=[self.bank_s[db]], writes=wr)
                    ost_s[oi_].wr.append(tok)
                self.dma("pool", self.xr[t * 128:(t + 1) * 128, :], ost[oi_][:], ost_d[oi_], reads=[ost_s[oi_]],
                         writes=[xrow_s[t]], accum_op=ALU.add)

        seq = [(e, blk) for e in range(NE) for blk in range(8)]
        load_gu(0)
        load_d(0)
        load_h(0)
        cur_wd = 0
        for bi_, (e, blk) in enumerate(seq):
            if bi_ + 1 < len(seq):
                load_h(bi_ + 1)
            if blk == 0 and e + 1 < NE:
                load_gu(e + 1)
            if bi_ == 0:
                gate_up(e, blk, bi_)
            if bi_ + 1 < len(seq):
                gate_up(seq[bi_ + 1][0], seq[bi_ + 1][1], bi_ + 1)
            if e != cur_wd:
                load_d(e)
                cur_wd = e
            down(e, blk, bi_)
        self.end_phase()


def _rope_table(theta, rot):
    half = rot // 2
    inv_freq = (1.0 / (np.float32(theta) ** (np.arange(half, dtype=np.float32) * np.float32(2.0 / rot)))).astype(np.float32)
    ang = (np.arange(S, dtype=np.float32)[:, None] * inv_freq[None, :]).astype(np.float32)
    c = np.cos(ang.astype(np.float64)).astype(np.float32)
    s = np.sin(ang.astype(np.float64)).astype(np.float32)
    return np.ascontiguousarray(np.concatenate([c, c, -s, s], axis=1))


_CACHE = {}


def _get_nc(**kw):
    key = tuple(sorted(kw.items()))
    if key not in _CACHE:
        kb = KB(**kw)
        kb.build()
        _CACHE[key] = kb
    return _CACHE[key]


def make_in_maps(inputs, ncores=8):
    f = lambda a: np.ascontiguousarray(np.asarray(a, dtype=np.float32))
    shared = {
        "attn_norm_g": f(inputs["attn_norm_g"]),
        "w_in": f(inputs["w_in"]),
        "diff_q_norm_g": f(inputs["diff_q_norm_g"]),
        "diff_k_norm_g": f(inputs["diff_k_norm_g"]),
        "diff_lambda": f(inputs["diff_lambda"]).reshape(DEPTH, 256),
        "diff_subln_g": f(inputs["diff_subln_g"]),
        "mla_q_ln_g": f(inputs["mla_q_ln_g"]),
        "w_uq": f(inputs["w_uq"]),
        "mla_kv_ln_g": f(inputs["mla_kv_ln_g"]),
        "w_ukv": f(inputs["w_ukv"]),
        "mla_qk_norm_g": f(inputs["mla_qk_norm_g"]).reshape(DEPTH, 384),
        "w_o": f(inputs["w_o"]),
        "ffn_norm_g": f(inputs["ffn_norm_g"]),
        "dense_w_gate": f(inputs["dense_w_gate"])[0],
        "dense_w_up": f(inputs["dense_w_up"])[0],
        "dense_w_down": f(inputs["dense_w_down"])[0],
        "router_w": f(inputs["router_w"])[0],
        "moe_w_gate": f(inputs["moe_w_gate"])[0],
        "moe_w_up": f(inputs["moe_w_up"])[0],
        "moe_w_down": f(inputs["moe_w_down"])[0],
        "cs_d": _rope_table(ROPE_THETA, 16),
        "cs_m": _rope_table(MLA_ROPE_THETA, 64),
        "ident": np.eye(128, dtype=np.float32),
    }
    x = f(inputs["x"])
    maps = []
    for c in range(ncores):
        m = dict(shared)
        m["x"] = np.ascontiguousarray(x[c])
        maps.append(m)
    return maps


def kernel(**inputs):
    kb = _get_nc()
    in_maps = make_in_maps(inputs)
    res = run_bass_kernel_spmd(kb.nc, in_maps, core_ids=list(range(8)))
    out = np.stack([np.asarray(r["y"], dtype=np.float32) for r in res.results], axis=0)
    return out
```

```python
import math
from contextlib import ExitStack
import numpy as np
import ml_dtypes
import concourse.bass as bass
import concourse.mybir as mybir
from concourse.bass_utils import run_bass_kernel_spmd

F32 = mybir.dt.float32
BF16 = mybir.dt.bfloat16
ALU = mybir.AluOpType
AF = mybir.ActivationFunctionType
AX = mybir.AxisListType

S = 4096
D = 1024
NT = S // 128
DEPTH = 2
EPS = 1e-6
INC = 1984
FFE = 1408
NCH = FFE // 128
ROPE_THETA = 500000.0
MLA_ROPE_THETA = 10000.0


class DSem:
    def __init__(self, h):
        self.h = h
        self.v = 0


class Slot:
    __slots__ = ("wr", "rd")

    def __init__(self):
        self.wr = []
        self.rd = []


class KB:
    def __init__(self, depth=DEPTH, stop_after=None, debug=False):
        self.depth = depth
        self.stop_after = stop_after
        self.debug = debug
        nc = self.nc = bass.Bass("TRN2", target_bir_lowering=False)
        self.E = dict(pe=nc.tensor, dve=nc.vector, act=nc.scalar, pool=nc.gpsimd, sp=nc.sync)
        self.csem = {k: nc.alloc_semaphore("c_" + k) for k in ("pe", "dve", "act", "pool")}
        self.ccnt = dict.fromkeys(self.csem, 0)
        self.waited = {}
        self.dsems = [DSem(nc.alloc_semaphore("d%d" % i)) for i in range(72)]
        self.dptr = 0
        self.swsems = [DSem(nc.alloc_semaphore("sw%d" % i)) for i in range(3)]
        self.banks = [nc.alloc_psum_tensor("bank%d" % i, [128, 512], F32) for i in range(8)]
        self.bank_s = [Slot() for _ in range(8)]
        self.stack = None
        self.uid = 0
        self.const_s = None
        self.log = [] if debug else None

    def sig(self, e, ins):
        self.ccnt[e] += 1
        ins.then_inc(self.csem[e], 1)
        if self.log is not None:
            self.log.append(("inc", e, self.csem[e].num, 1))
        return (self.csem[e], self.ccnt[e])

    def wait(self, e, toks):
        for tok in toks:
            sem, v = tok
            key = (e, sem.num)
            if self.waited.get(key, 0) >= v:
                continue
            self.waited[key] = v
            self.E[e].wait_ge(sem, v)
            if self.log is not None:
                self.log.append(("wait", e, sem.num, v))

    def _deps(self, e, reads, writes):
        toks = []
        for s in reads:
            toks.extend(s.wr)
        for s in writes:
            toks.extend(s.rd)
            toks.extend(s.wr)
        self.wait(e, toks)

    def _commit(self, tok, reads, writes):
        for s in reads:
            if s is not self.const_s:
                s.rd.append(tok)
        for s in writes:
            s.wr = [tok]
            s.rd = []

    def op(self, e, fn, reads=(), writes=()):
        self._deps(e, reads, writes)
        ins = fn(self.E[e])
        tok = self.sig(e, ins)
        self._commit(tok, reads, writes)
        return tok

    def dsem(self):
        d = self.dsems[self.dptr]
        self.dptr += 1
        return d

    def dma(self, q, out, in_, ds, reads=(), writes=(), **kw):
        self._deps(q, reads, writes)
        ins = self.E[q].dma_start(out=out, in_=in_, **kw)
        ins.then_inc(ds.h, 16)
        ds.v += 16
        if self.log is not None:
            self.log.append(("dma", q, ds.h.num, 16))
        tok = (ds.h, ds.v)
        self._commit(tok, reads, writes)
        return tok

    def mm_group(self, bank_i, mms, reads=()):
        bs = self.bank_s[bank_i]
        self._deps("pe", reads, [bs])
        ins = None
        for (o, l, r, st, sp) in mms:
            ins = self.nc.tensor.matmul(o, lhsT=l, rhs=r, start=st, stop=sp)
        tok = self.sig("pe", ins)
        self._commit(tok, reads, [bs])
        return tok

    def tr_group(self, bank_i, trs, ident, reads=()):
        bs = self.bank_s[bank_i]
        self._deps("pe", reads, [bs])
        ins = None
        for (o, i) in trs:
            ins = self.nc.tensor.transpose(out=o, in_=i, identity=ident)
        tok = self.sig("pe", ins)
        self._commit(tok, reads, [bs])
        return tok

    def barrier(self):
        toks = [(self.csem[c], self.ccnt[c]) for c in self.csem if self.ccnt[c] > 0]
        toks += [(d.h, d.v) for d in self.dsems + self.swsems if d.v > 0]
        for e in self.E:
            self.wait(e, toks)

    def begin_phase(self):
        self.barrier()
        self.stack = ExitStack()
        self.dptr = self.dkeep
        for b in self.bank_s:
            b.wr = []
            b.rd = []

    def end_phase(self):
        self.barrier()
        self.stack.close()
        self.stack = None

    def sb(self, name, shape, dt):
        self.uid += 1
        return self.stack.enter_context(self.nc.sbuf_tensor("%s_%d" % (name, self.uid), shape, dt))

    def interleave(self, gens):
        gens = list(gens)
        while gens:
            nxt = []
            for g in gens:
                try:
                    next(g)
                    nxt.append(g)
                except StopIteration:
                    pass
            gens = nxt

    def bank_bf(self, i):
        return self.banks[i][:].bitcast(BF16)

    def build(self):
        nc = self.nc

        def din(name, shape):
            return nc.dram_tensor(name, shape, F32, kind="ExternalInput").ap()

        self.x = din("x", [S, D])
        self.attn_g = din("attn_norm_g", [DEPTH, D])
        self.w_in = din("w_in", [DEPTH, D, INC])
        self.dq_g = din("diff_q_norm_g", [DEPTH, 64])
        self.dk_g = din("diff_k_norm_g", [DEPTH, 64])
        self.dlam = din("diff_lambda", [DEPTH, 256])
        self.subln_g = din("diff_subln_g", [DEPTH, 128])
        self.qln_g = din("mla_q_ln_g", [DEPTH, 256])
        self.w_uq = din("w_uq", [DEPTH, 256, 768])
        self.kvln_g = din("mla_kv_ln_g", [DEPTH, 128])
        self.w_ukv = din("w_ukv", [DEPTH, 128, 1024])
        self.mqk_g = din("mla_qk_norm_g", [DEPTH, 2 * 192])
        self.w_o = din("w_o", [DEPTH, D, D])
        self.ffn_g = din("ffn_norm_g", [DEPTH, D])
        self.dwg = din("dense_w_gate", [D, 2816])
        self.dwu = din("dense_w_up", [D, 2816])
        self.dwd = din("dense_w_down", [2816, D])
        self.rw = din("router_w", [D, 8])
        self.mwg = din("moe_w_gate", [8, D, FFE])
        self.mwu = din("moe_w_up", [8, D, FFE])
        self.mwd = din("moe_w_down", [8, FFE, D])
        self.cs_d = din("cs_d", [S, 32])
        self.cs_m = din("cs_m", [S, 128])
        self.ident_in = din("ident", [128, 128])
        self.y = nc.dram_tensor("y", [S, D], F32, kind="ExternalOutput").ap()
        xr = self.xr = self.y

        kind = "ExternalOutput" if self.debug else "Internal"

        def dscr(name, shape, dt):
            return nc.dram_tensor(name, shape, dt, kind=kind).ap()

        self.qkt = dscr("qkt", [20, 128, S], BF16)
        self.vd = dscr("vd", [S, 512], BF16)
        self.vm = dscr("vm", [S, 512], BF16)
        self.oct_d = dscr("oct_d", [8, 128, S], BF16)
        self.h2t = dscr("h2t", [8, 128, S], BF16)
        self.b_win = dscr("b_win", [DEPTH, D, INC], BF16)
        self.b_wuq = dscr("b_wuq", [DEPTH, 256, 768], BF16)
        self.b_wukv = dscr("b_wukv", [DEPTH, 128, 1024], BF16)
        self.b_wo = dscr("b_wo", [DEPTH, D, D], BF16)
        self.b_dwg = dscr("b_dwg", [D, 2816], BF16)
        self.b_dwu = dscr("b_dwu", [D, 2816], BF16)
        self.b_dwd = dscr("b_dwd", [2816, D], BF16)
        self.b_mwg = dscr("b_mwg", [8, D, FFE], BF16)
        self.b_mwu = dscr("b_mwu", [8, D, FFE], BF16)
        self.b_mwd = dscr("b_mwd", [8, FFE, D], BF16)

        A = nc.alloc_sbuf_tensor
        self.ident_f = A("ident_f", [128, 128], F32)
        self.ident_b = A("ident_b", [128, 128], BF16)
        self.ones_b = A("ones_b", [128, 128], BF16)
        self.sc_attn = A("sc_attn", [128, DEPTH, 8], F32)
        self.sc_ffn = A("sc_ffn", [128, DEPTH, 8], F32)
        self.sc_qln = A("sc_qln", [128, DEPTH, 2], F32)
        self.sc_kvln = A("sc_kvln", [128, DEPTH, 1], F32)
        self.sc_wo = A("sc_wo", [128, DEPTH, 8], F32)
        self.ones = A("ones", [128, 24], F32)
        self.gd = A("gd", [128, DEPTH, 2, 64], F32)
        self.gm = A("gm", [128, DEPTH, 2, 192], F32)
        self.lamt = A("lamt", [128, DEPTH, 256], F32)
        self.lamw = A("lamw", [128, 8], F32)
        self.neglam = A("neglam", [128, DEPTH], F32)
        self.negM = A("negM", [128, DEPTH, 2], F32)
        self.gmax = A("gmax", [128, 8], F32)
        self.rw_s = A("rw_s", [128, 8, 8], F32)
        self.comb = A("comb", [128, NT, 8], F32)
        self.junk = A("junk", [128, 1024], BF16)
        self.const_s = Slot()

        self.setup_consts()
        self.setup_wjobs()
        self.dkeep = self.dptr
        if self.log is not None:
            snap = {self.csem[c].num: self.ccnt[c] for c in self.csem}
            snap.update({d.h.num: d.v for d in self.dsems + self.swsems + [self.xinit]})
            self.log.append(("snapshot", snap))
        for l in range(self.depth):
            self.phase1(l)
            if self.stop_after == "p1_%d" % l:
                return self.finish()
            self.phase2(l)
            if self.stop_after == "p2_%d" % l:
                return self.finish()
            self.phase3a(l)
            if self.stop_after == "p3a_%d" % l:
                return self.finish()
            self.phase3b(l)
            if self.stop_after == "p3b_%d" % l:
                return self.finish()
        return self.finish()

    def finish(self):
        self.barrier()
        return self.nc

    def setup_consts(self):
        nc = self.nc
        cs = self.const_s
        ds = self.dsem()
        toks = []

        def ld(out, in_, **kw):
            ins = nc.sync.dma_start(out=out, in_=in_, **kw)
            ins.then_inc(ds.h, 16)
            ds.v += 16

        self.xinit = self.dsem()

        ld(self.ident_f[:], self.ident_in[:, :])
        for l in range(DEPTH):
            ld(self.sc_attn[:, l, :], self.attn_g[l].rearrange("(c p) -> p c", p=128), allow_slow_non_contiguous=True)
            ld(self.sc_ffn[:, l, :], self.ffn_g[l].rearrange("(c p) -> p c", p=128), allow_slow_non_contiguous=True)
            ld(self.sc_qln[:, l, :], self.qln_g[l].rearrange("(c p) -> p c", p=128), allow_slow_non_contiguous=True)
            ld(self.sc_kvln[:, l, :], self.kvln_g[l].rearrange("(c p) -> p c", p=128), allow_slow_non_contiguous=True)
            ld(self.sc_wo[:, l, 0:1], self.subln_g[l].rearrange("(c p) -> p c", p=128), allow_slow_non_contiguous=True)
            ld(self.gd[:, l, 0, :], self.dq_g[l].partition_broadcast(128))
            ld(self.gd[:, l, 1, :], self.dk_g[l].partition_broadcast(128))
            ld(self.gm[:, l, :, :].rearrange("p a b -> p (a b)"), self.mqk_g[l].partition_broadcast(128))
            ld(self.lamt[:, l, :], self.dlam[l].partition_broadcast(128))
        ld(self.rw_s[:], self.rw.rearrange("(c p) n -> p c n", p=128))
        nc.vector.wait_ge(ds.h, ds.v)
        nc.gpsimd.wait_ge(ds.h, ds.v)
        nc.scalar.wait_ge(ds.h, ds.v)
        V = nc.vector
        sv = self.csem["dve"]

        def vop(ins):
            self.ccnt["dve"] += 1
            ins.then_inc(sv, 1)
            V.wait_ge(sv, self.ccnt["dve"])

        def aop(ins):
            self.ccnt["act"] += 1
            ins.then_inc(self.csem["act"], 1)
            nc.scalar.wait_ge(self.csem["act"], self.ccnt["act"])
            V.wait_ge(self.csem["act"], self.ccnt["act"])

        vop(V.tensor_copy(out=self.ident_b[:], in_=self.ident_f[:]))
        vop(V.memset(self.ones[:], 1.0))
        vop(V.memset(self.ones_b[:], 1.0))
        for l in range(DEPTH):
            lam_init = 0.8 - 0.6 * math.exp(-0.3 * l)
            vop(V.tensor_scalar(out=self.sc_wo[:, l, 0:1], in0=self.sc_wo[:, l, 0:1], scalar1=float(1.0 - lam_init),
                                scalar2=None, op0=ALU.mult))
            for c in range(1, 4):
                vop(V.tensor_copy(out=self.sc_wo[:, l, c:c + 1], in_=self.sc_wo[:, l, 0:1]))
            vop(V.memset(self.sc_wo[:, l, 4:8], 1.0))
        for l in range(DEPTH):
            lam_init = 0.8 - 0.6 * math.exp(-0.3 * l)
            vop(V.tensor_tensor(out=self.lamt[:, l, 0:64], in0=self.lamt[:, l, 0:64], in1=self.lamt[:, l, 64:128], op=ALU.mult))
            vop(V.tensor_tensor(out=self.lamt[:, l, 128:192], in0=self.lamt[:, l, 128:192], in1=self.lamt[:, l, 192:256], op=ALU.mult))
            vop(V.tensor_reduce(out=self.lamw[:, 0:1], in_=self.lamt[:, l, 0:64], axis=AX.X, op=ALU.add))
            vop(V.tensor_reduce(out=self.lamw[:, 1:2], in_=self.lamt[:, l, 128:192], axis=AX.X, op=ALU.add))
            nc.scalar.wait_ge(sv, self.ccnt["dve"])
            aop(nc.scalar.activation(out=self.lamw[:, 2:4], in_=self.lamw[:, 0:2], func=AF.Exp))
            vop(V.tensor_tensor(out=self.lamw[:, 4:5], in0=self.lamw[:, 3:4], in1=self.lamw[:, 2:3], op=ALU.subtract))
            vop(V.tensor_scalar(out=self.neglam[:, l:l + 1], in0=self.lamw[:, 4:5], scalar1=float(-lam_init),
                                scalar2=None, op0=ALU.add))
            vop(V.tensor_reduce(out=self.gmax[:, 0:2], in_=self.gd[:, l, :, :], axis=AX.X, op=ALU.max,
                                apply_absolute_value=True))
            vop(V.tensor_reduce(out=self.gmax[:, 2:4], in_=self.gm[:, l, :, :], axis=AX.X, op=ALU.max,
                                apply_absolute_value=True))
            vop(V.tensor_tensor(out=self.gmax[:, 4:5], in0=self.gmax[:, 0:1], in1=self.gmax[:, 1:2], op=ALU.mult))
            vop(V.tensor_tensor(out=self.gmax[:, 5:6], in0=self.gmax[:, 2:3], in1=self.gmax[:, 3:4], op=ALU.mult))
            vop(V.tensor_scalar(out=self.negM[:, l, 0:1], in0=self.gmax[:, 4:5], scalar1=float(-math.sqrt(64.0)),
                                scalar2=None, op0=ALU.mult))
            vop(V.tensor_scalar(out=self.negM[:, l, 1:2], in0=self.gmax[:, 5:6], scalar1=float(-math.sqrt(192.0)),
                                scalar2=None, op0=ALU.mult))
        for c in range(8):
            vop(V.tensor_scalar(out=self.rw_s[:, c, :], in0=self.rw_s[:, c, :], scalar1=self.sc_ffn[:, 1, c:c + 1],
                                scalar2=None, op0=ALU.mult))
        vop(V.memset(self.comb[:], 1.0))

    def setup_wjobs(self):
        nc = self.nc
        NB = 3
        CM = 1408
        self.w_fst = [nc.alloc_sbuf_tensor("wf%d" % i, [128, CM], F32) for i in range(NB)]
        self.w_bst = [nc.alloc_sbuf_tensor("wb%d" % i, [128, CM], BF16) for i in range(NB)]
        self.w_fs = [Slot() for _ in range(NB)]
        self.w_bs = [Slot() for _ in range(NB)]
        self.w_fd = [self.dsem() for _ in range(NB)]
        self.w_bd = [self.dsem() for _ in range(NB)]
        self.w_nb = NB
        self.wjobs = []
        self.wnext = 0
        self.wloadq = []
        self.wcastq = []
        self.wcall = 0
        self.wtags = {}

        def add(src, dst, R, C, sc):
            ncs = 1 if C <= CM else 2
            cw = C // ncs
            for c in range(R // 128):
                for j in range(ncs):
                    self.wjobs.append((src[c * 128:(c + 1) * 128, j * cw:(j + 1) * cw],
                                       dst[c * 128:(c + 1) * 128, j * cw:(j + 1) * cw], cw, sc(c)))

        one = lambda c: self.ones[:, 0:1]

        def attn(l):
            add(self.w_in[l], self.b_win[l], D, INC, lambda c, l=l: self.sc_attn[:, l, c:c + 1])
            add(self.w_uq[l], self.b_wuq[l], 256, 768, lambda c, l=l: self.sc_qln[:, l, c:c + 1])
            add(self.w_ukv[l], self.b_wukv[l], 128, 1024, lambda c, l=l: self.sc_kvln[:, l, c:c + 1])
            self.wtags["attn%d" % l] = len(self.wjobs)
            add(self.w_o[l], self.b_wo[l], D, D, lambda c, l=l: self.sc_wo[:, l, c:c + 1])
            self.wtags["wo%d" % l] = len(self.wjobs)

        attn(0)
        add(self.dwg, self.b_dwg, D, 2816, lambda c: self.sc_ffn[:, 0, c:c + 1])
        add(self.dwu, self.b_dwu, D, 2816, lambda c: self.sc_ffn[:, 0, c:c + 1])
        add(self.dwd, self.b_dwd, 2816, D, one)
        self.wtags["ffn0"] = len(self.wjobs)
        if self.depth > 1:
            attn(1)
            for e in range(8):
                add(self.mwg[e], self.b_mwg[e], D, FFE, lambda c: self.sc_ffn[:, 1, c:c + 1])
                add(self.mwu[e], self.b_mwu[e], D, FFE, lambda c: self.sc_ffn[:, 1, c:c + 1])
                add(self.mwd[e], self.b_mwd[e], FFE, D, one)
            self.wtags["ffn1"] = len(self.wjobs)

    def _wstage_store(self, upto):
        keep = []
        for ent in self.wcastq:
            (i, dst, C, call) = ent
            if call > upto:
                keep.append(ent)
                continue
            self.dma("sp", dst, self.w_bst[i][:, 0:C], self.w_bd[i], reads=[self.w_bs[i]])
        self.wcastq = keep

    def _wstage_cast(self, upto, engs):
        keep = []
        for ent in self.wloadq:
            (i, dst, C, scal, call, k) = ent
            if call > upto:
                keep.append(ent)
                continue
            eng = engs[k % len(engs)]
            fst, bst = self.w_fst[i], self.w_bst[i]
            if eng == "act":
                self.op("act", lambda a: a.activation(out=bst[:, 0:C], in_=fst[:, 0:C], func=AF.Copy, scale=scal),
                        reads=[self.w_fs[i]], writes=[self.w_bs[i]])
            else:
                self.op(eng, lambda v: v.tensor_scalar(out=bst[:, 0:C], in0=fst[:, 0:C], scalar1=scal, scalar2=None,
                                                       op0=ALU.mult), reads=[self.w_fs[i]], writes=[self.w_bs[i]])
            self.wcastq.append((i, dst, C, self.wcall))
        self.wloadq = keep

    def pump(self, n, engs=("dve", "act")):
        self.wcall += 1
        c = self.wcall
        self._wstage_store(c - 1)
        self._wstage_cast(c - 1, engs)
        for _ in range(n):
            if self.wnext >= len(self.wjobs):
                break
            src, dst, C, scal = self.wjobs[self.wnext]
            k = self.wnext
            i = k % self.w_nb
            self.wnext += 1
            self.dma("sp", self.w_fst[i][:, 0:C], src, self.w_fd[i], writes=[self.w_fs[i]])
            self.wloadq.append((i, dst, C, scal, c, k))

    def pump_until(self, tag, engs=("dve", "act")):
        tgt = self.wtags[tag]
        while self.wnext < tgt:
            self.pump(1, engs)
        self.wcall += 1
        self._wstage_store(self.wcall)
        self._wstage_cast(self.wcall, engs)
        self._wstage_store(self.wcall)
        self.wait("sp", [(d.h, d.v) for d in self.w_bd if d.v > 0])

    def gpump(self, rounds, engs=("dve", "act")):
        for _ in range(rounds):
            yield
        self.pump(1, engs)

    def rstd(self, src_ap, src_slot, out_ap, out_slot, tmp_ap, tmp_slot, inv_w):
        self.op("act", lambda a: a.activation(out=tmp_ap, in_=src_ap, func=AF.Ln, scale=float(inv_w), bias=float(EPS)),
                reads=[src_slot], writes=[tmp_slot])
        self.op("act", lambda a: a.activation(out=out_ap, in_=tmp_ap, func=AF.Exp, scale=-0.5),
                reads=[tmp_slot], writes=[out_slot])

    def normrope(self, W_, src3, src_slots, G, W, gain3, R, rot_lo, cs_ap, cs_slot, dst_nonrot, dst_rot, dst_slot,
                 nonrot_lo, nonrot_hi):
        sq, sq_s, ssg, ssg_s, lg, lg_s, rsg, rsg_s, tmp, tmp_s, xr_, xr_s, ac, ac_s, sw, sw_s = W_
        n = G * W
        h = R // 2
        sq3 = sq[:, 0:n].rearrange("p (g w) -> p g w", w=W)
        tmp3 = tmp[:, 0:n].rearrange("p (g w) -> p g w", w=W)
        xr3 = xr_[:, 0:G * R].rearrange("p (g r) -> p g r", r=R)
        ac3 = ac[:, 0:G * R].rearrange("p (g r) -> p g r", r=R)
        sw3 = sw[:, 0:G * R].rearrange("p (g r) -> p g r", r=R)
        self.op("act", lambda a: a.activation(out=sq3, in_=src3, func=AF.Square), reads=src_slots, writes=[sq_s])
        yield
        self.op("dve", lambda v: v.tensor_reduce(out=ssg[:, 0:G], in_=sq3, axis=AX.X, op=ALU.add),
                reads=[sq_s], writes=[ssg_s])
        yield
        self.rstd(ssg[:, 0:G], ssg_s, rsg[:, 0:G], rsg_s, lg[:, 0:G], lg_s, 1.0 / W)
        yield
        self.op("dve", lambda v: v.tensor_tensor(out=tmp3, in0=src3, in1=rsg[:, 0:G].unsqueeze(2).broadcast_to([128, G, W]),
                                                 op=ALU.mult),
                reads=list(src_slots) + [rsg_s], writes=[tmp_s])
        yield
        self.op("pool", lambda p: p.tensor_tensor(out=dst_nonrot, in0=tmp3[:, :, nonrot_lo:nonrot_hi],
                                                  in1=gain3[:, :, nonrot_lo:nonrot_hi], op=ALU.mult),
                reads=[tmp_s], writes=[dst_slot])
        yield
        self.op("pool", lambda p: p.tensor_tensor(out=xr3, in0=tmp3[:, :, rot_lo:rot_lo + R],
                                                  in1=gain3[:, :, rot_lo:rot_lo + R], op=ALU.mult),
                reads=[tmp_s], writes=[xr_s])
        yield
        cc = cs_ap[:, 0:R].unsqueeze(1).broadcast_to([128, G, R])
        ns = cs_ap[:, R:R + h].unsqueeze(1).broadcast_to([128, G, h])
        ps_ = cs_ap[:, R + h:2 * R].unsqueeze(1).broadcast_to([128, G, h])
        self.op("pool", lambda p: p.tensor_tensor(out=ac3, in0=xr3, in1=cc, op=ALU.mult),
                reads=[xr_s, cs_slot], writes=[ac_s])
        yield
        self.op("pool", lambda p: p.tensor_tensor(out=sw3[:, :, 0:h], in0=xr3[:, :, h:R], in1=ns, op=ALU.mult),
                reads=[xr_s, cs_slot], writes=[sw_s])
        yield
        self.op("pool", lambda p: p.tensor_tensor(out=sw3[:, :, h:R], in0=xr3[:, :, 0:h], in1=ps_, op=ALU.mult),
                reads=[xr_s, cs_slot], writes=[sw_s])
        yield
        tok = self.op("pool", lambda p: p.tensor_tensor(out=dst_rot, in0=ac3, in1=sw3, op=ALU.add),
                      reads=[ac_s, sw_s], writes=[dst_slot])
        yield
        return tok

    def phase1(self, l):
        self.begin_phase()
        self.pump_until("attn%d" % l)
        xsrc = self.x if l == 0 else self.xr
        nc = self.nc
        sb = self.sb
        win = sb("win", [128, 8, INC], BF16)
        wuq = sb("wuq", [128, 2, 768], BF16)
        wukv = sb("wukv", [128, 1024], BF16)
        w_s = Slot()
        d0 = self.dsem()
        self.dma("sp", win[:], self.b_win[l].rearrange("(c p) n -> p c n", p=128), d0, writes=[])
        self.dma("sp", wuq[:], self.b_wuq[l].rearrange("(c p) n -> p c n", p=128), d0, writes=[])
        tokw = self.dma("sp", wukv[:], self.b_wukv[l], d0, writes=[])
        w_s.wr = [tokw]

        def bufs(name, shape, dt, n):
            return [sb(name, shape, dt) for _ in range(n)], [Slot() for _ in range(n)]

        xt, xt_s = bufs("xt", [128, D], F32, 2)
        hb, hb_s = bufs("hb", [128, D], BF16, 2)
        hT, hT_s = bufs("hT", [128, D], BF16, 2)
        pj = [sb("pj", [128, INC], F32) for _ in range(2)]
        pj_s = [[Slot() for _ in range(4)] for _ in range(2)]
        smA = [sb("smA", [128, 4], F32) for _ in range(2)]
        ss_s, lg_s, rs_s = [Slot(), Slot()], [Slot(), Slot()], [Slot(), Slot()]
        smB = [sb("smB", [128, 8], F32) for _ in range(2)]
        ssc_s, lgc_s, rsc_s = [Slot(), Slot()], [Slot(), Slot()], [Slot(), Slot()]
        cn, cn_s = bufs("cn", [128, 384], BF16, 2)
        cT, cT_s = bufs("cT", [128, 384], BF16, 2)
        qm = [sb("qm", [128, 768], F32) for _ in range(2)]
        qm_s = [[Slot(), Slot()] for _ in range(2)]
        kvm = [sb("kvm", [128, 1024], F32) for _ in range(2)]
        kvm_s = [[Slot(), Slot()] for _ in range(2)]
        kf, kf_s = bufs("kf", [128, 768], F32, 2)
        dA, dA_s = bufs("dA", [128, 1024], BF16, 3)
        mQ, mQ_s = bufs("mQ", [128, 768], BF16, 3)
        mK, mK_s = bufs("mK", [128, 768], BF16, 3)
        vb, vb_s = bufs("vb", [128, 512], BF16, 3)
        vmb, vmb_s = bufs("vmb", [128, 512], BF16, 3)
        csd, csd_s = bufs("csd", [128, 32], F32, 3)
        csm, csm_s = bufs("csm", [128, 128], F32, 3)
        stage = [sb("stage", [128, 20, 256], BF16) for _ in range(2)]
        stage_s = [Slot(), Slot()]
        xt_d = [self.dsem() for _ in range(2)]
        cs_d_ = [self.dsem() for _ in range(3)]
        cs_m_ = [self.dsem() for _ in range(3)]
        vb_d = [self.dsem() for _ in range(3)]
        vmb_d = [self.dsem() for _ in range(3)]
        st_d = [self.dsem() for _ in range(2)]

        def mkscratch(n, gr):
            return (sb("sq", [128, n], F32), Slot(), sb("ssg", [128, 16], F32), Slot(), sb("lg", [128, 16], F32), Slot(),
                    sb("rsg", [128, 16], F32), Slot(), sb("tmp", [128, n], F32), Slot(), sb("xr_", [128, gr], F32), Slot(),
                    sb("ac", [128, gr], F32), Slot(), sb("sw", [128, gr], F32), Slot())

        W_d = mkscratch(1024, 256)
        W_q = mkscratch(768, 256)
        W_k = mkscratch(768, 256)

        gdf = sb("gdf", [128, 16, 64], F32)
        gdf_s = Slot()
        self.op("dve", lambda v: v.tensor_copy(out=gdf[:, 0:8, :], in_=self.gd[:, l, 0, :].unsqueeze(1).broadcast_to([128, 8, 64])),
                writes=[gdf_s])
        self.op("dve", lambda v: v.tensor_copy(out=gdf[:, 8:16, :], in_=self.gd[:, l, 1, :].unsqueeze(1).broadcast_to([128, 8, 64])),
                writes=[gdf_s])
        self.wait("pool", gdf_s.wr)
        gd3 = gdf[:]
        gq3 = self.gm[:, l, 0, :].unsqueeze(1).broadcast_to([128, 4, 192])
        gk3 = self.gm[:, l, 1, :].unsqueeze(1).broadcast_to([128, 4, 192])
        TPB = [0, 1]
        MMB = [2, 3, 4, 5, 6, 7]
        self.mmi = 0
        self.tpi = 0
        self.evi = 0

        def evac(bank_i, src_ap, dst_ap, writes):
            eng = "act" if self.evi % 2 == 0 else "dve"
            self.evi += 1
            if eng == "act":
                return self.op("act", lambda a: a.copy(out=dst_ap, in_=src_ap), reads=[self.bank_s[bank_i]], writes=writes)
            return self.op("dve", lambda v: v.tensor_copy(out=dst_ap, in_=src_ap), reads=[self.bank_s[bank_i]], writes=writes)

        def project(lhs_chunks, rhs_fn, ncols, dst, dst_slots, reads):
            n0 = 0
            i = 0
            while n0 < ncols:
                w = min(512, ncols - n0)
                bi = MMB[self.mmi % len(MMB)]
                self.mmi += 1
                K = len(lhs_chunks)
                mms = [(self.banks[bi][:, 0:w], lhs_chunks[k], rhs_fn(k, n0, w), k == 0, k == K - 1) for k in range(K)]
                self.mm_group(bi, mms, reads=reads)
                evac(bi, self.banks[bi][:, 0:w], dst[:, n0:n0 + w], [dst_slots[i]])
                n0 += w
                i += 1

        def transposes(src_ap, ncol_blocks, src_slots, dst_ap, dst_writes):
            bi = TPB[self.tpi % 2]
            self.tpi += 1
            bv = self.bank_bf(bi)
            trs = [(bv[:, k * 128:(k + 1) * 128], src_ap[:, k * 128:(k + 1) * 128]) for k in range(ncol_blocks)]
            self.tr_group(bi, trs, self.ident_b[:], reads=src_slots)
            src = bv[:, 0:ncol_blocks * 128]
            if len(dst_ap.shape) == 3:
                src = src.rearrange("p (k n) -> p k n", n=128)
            return evac(bi, src, dst_ap, dst_writes)

        def stA(t):
            b = t % 2
            b3 = t % 3
            rows = slice(t * 128, (t + 1) * 128)
            sm = smA[b]
            self.dma("sp", xt[b][:], xsrc[rows, :], xt_d[b], writes=[xt_s[b]])
            yield
            self.dma("sp", csd[b3][:], self.cs_d[rows, :], cs_d_[b3], writes=[csd_s[b3]])
            yield
            self.dma("sp", csm[b3][:], self.cs_m[rows, :], cs_m_[b3], writes=[csm_s[b3]])
            yield
            self.op("act", lambda a: a.activation(out=self.junk[:], in_=xt[b][:], func=AF.Square, accum_out=sm[:, 0:1]),
                    reads=[xt_s[b]], writes=[ss_s[b]])
            yield
            self.rstd(sm[:, 0:1], ss_s[b], sm[:, 2:3], rs_s[b], sm[:, 1:2], lg_s[b], 1.0 / D)
            yield
            self.op("dve", lambda v: v.tensor_scalar(out=hb[b][:], in0=xt[b][:], scalar1=sm[:, 2:3], scalar2=None,
                                                     op0=ALU.mult), reads=[xt_s[b], rs_s[b]], writes=[hb_s[b]])
            yield
            transposes(hb[b], 8, [hb_s[b]], hT[b][:], [hT_s[b]])
            yield
            project([hT[b][:, k * 128:(k + 1) * 128] for k in range(8)],
                    lambda k, n0, w: win[:, k, n0:n0 + w], INC, pj[b], pj_s[b], [hT_s[b], w_s])
            yield
            self.op("act", lambda a: a.copy(out=vb[b3][:], in_=pj[b][:, 1024:1536]), reads=[pj_s[b][2]], writes=[vb_s[b3]])
            yield

        def stB(t):
            b = t % 2
            b3 = t % 3
            rows = slice(t * 128, (t + 1) * 128)
            sm = smB[b]
            self.dma("sp", self.vd[rows, :], vb[b3][:], vb_d[b3], reads=[vb_s[b3]])
            yield
            src3 = pj[b][:, 0:1024].rearrange("p (g w) -> p g w", w=64)
            dA3 = dA[b3][:].rearrange("p (g w) -> p g w", w=64)
            yield from self.normrope(W_d, src3, [pj_s[b][0], pj_s[b][1]], 16, 64, gd3, 16, 0, csd[b3], csd_s[b3],
                          dA3[:, :, 16:64], dA3[:, :, 0:16], dA_s[b3], 16, 64)
            self.op("act", lambda a: a.activation(out=self.junk[:, 0:256], in_=pj[b][:, 1536:1792], func=AF.Square,
                                                  accum_out=sm[:, 0:1]), reads=[pj_s[b][3]], writes=[ssc_s[b]])
            yield
            self.op("act", lambda a: a.activation(out=self.junk[:, 0:128], in_=pj[b][:, 1792:1920], func=AF.Square,
                                                  accum_out=sm[:, 1:2]), reads=[pj_s[b][3]], writes=[ssc_s[b]])
            yield
            self.op("act", lambda a: a.activation(out=sm[:, 2:3], in_=sm[:, 0:1], func=AF.Ln, scale=1.0 / 256, bias=float(EPS)),
                    reads=[ssc_s[b]], writes=[lgc_s[b]])
            yield
            self.op("act", lambda a: a.activation(out=sm[:, 3:4], in_=sm[:, 1:2], func=AF.Ln, scale=1.0 / 128, bias=float(EPS)),
                    reads=[ssc_s[b]], writes=[lgc_s[b]])
            yield
            self.op("act", lambda a: a.activation(out=sm[:, 4:6], in_=sm[:, 2:4], func=AF.Exp, scale=-0.5),
                    reads=[lgc_s[b]], writes=[rsc_s[b]])
            yield
            self.op("dve", lambda v: v.tensor_scalar(out=cn[b][:, 0:256], in0=pj[b][:, 1536:1792], scalar1=sm[:, 4:5],
                                                     scalar2=None, op0=ALU.mult), reads=[pj_s[b][3], rsc_s[b]], writes=[cn_s[b]])
            yield
            self.op("dve", lambda v: v.tensor_scalar(out=cn[b][:, 256:384], in0=pj[b][:, 1792:1920], scalar1=sm[:, 5:6],
                                                     scalar2=None, op0=ALU.mult), reads=[pj_s[b][3], rsc_s[b]],
                    writes=[cn_s[b]])
            yield
            transposes(cn[b], 3, [cn_s[b]], cT[b][:], [cT_s[b]])
            yield
            project([cT[b][:, 0:128], cT[b][:, 128:256]], lambda k, n0, w: wuq[:, k, n0:n0 + w], 768, qm[b], qm_s[b],
                    [cT_s[b], w_s])
            yield
            project([cT[b][:, 256:384]], lambda k, n0, w: wukv[:, n0:n0 + w], 1024, kvm[b], kvm_s[b], [cT_s[b], w_s])
            yield
            kvm4 = kvm[b][:].rearrange("p (h c) -> p h c", c=256)
            kf3 = kf[b][:].rearrange("p (h c) -> p h c", c=192)
            self.op("pool", lambda p: p.tensor_copy(out=kf3[:, :, 0:128], in_=kvm4[:, :, 0:128]), reads=kvm_s[b],
                    writes=[kf_s[b]])
            yield
            self.op("pool", lambda p: p.tensor_copy(out=kf3[:, :, 128:192],
                                                    in_=pj[b][:, 1920:1984].unsqueeze(1).broadcast_to([128, 4, 64])),
                    reads=[pj_s[b][3]], writes=[kf_s[b]])
            yield
            self.op("act", lambda a: a.copy(out=vmb[b3][:].rearrange("p (h c) -> p h c", c=128), in_=kvm4[:, :, 128:256]),
                    reads=kvm_s[b], writes=[vmb_s[b3]])
            yield

        def stCq(t):
            b = t % 2
            b3 = t % 3
            rows = slice(t * 128, (t + 1) * 128)
            self.dma("sp", self.vm[rows, :], vmb[b3][:], vmb_d[b3], reads=[vmb_s[b3]])
            yield
            qm3 = qm[b][:].rearrange("p (h c) -> p h c", c=192)
            mQn = mQ[b3][:, 0:512].rearrange("p (h c) -> p h c", c=128)
            mQr = mQ[b3][:, 512:768].rearrange("p (h c) -> p h c", c=64)
            yield from self.normrope(W_q, qm3, qm_s[b], 4, 192, gq3, 64, 128, csm[b3], csm_s[b3], mQn, mQr, mQ_s[b3], 0, 128)

        def stCk(t):
            b = t % 2
            b3 = t % 3
            kf3 = kf[b][:].rearrange("p (h c) -> p h c", c=192)
            mKn = mK[b3][:, 0:512].rearrange("p (h c) -> p h c", c=128)
            mKr = mK[b3][:, 512:768].rearrange("p (h c) -> p h c", c=64)
            yield from self.normrope(W_k, kf3, [kf_s[b]], 4, 192, gk3, 64, 128, csm[b3], csm_s[b3], mKn, mKr, mK_s[b3], 0, 128)

        def stCt(t):
            b = t % 2
            b3 = t % 3
            pr = t // 2
            sg = pr % 2
            tl = t % 2
            stg = stage[sg]
            cols = slice(tl * 128, (tl + 1) * 128)
            if tl == 0:
                old = stage_s[sg].rd + stage_s[sg].wr
                self.wait("act", old)
                self.wait("dve", old)
                stage_s[sg].wr = []
                stage_s[sg].rd = []
            toks = []
            toks.append(transposes(dA[b3], 8, [dA_s[b3]], stg[:, 0:8, cols], []))
            toks.append(transposes(mQ[b3], 6, [mQ_s[b3]], stg[:, 8:14, cols], []))
            toks.append(transposes(mK[b3], 6, [mK_s[b3]], stg[:, 14:20, cols], []))
            stage_s[sg].wr.extend(toks)
            if tl == 1:
                self.dma("sp", self.qkt[:, :, pr * 256:(pr + 1) * 256].rearrange("k p n -> p k n"), stg[:], st_d[sg],
                         reads=[stage_s[sg]])

        def gCt(t):
            for _ in range(8):
                yield
            stCt(t)

        for i in range(NT + 3):
            gens = []
            if i < NT:
                gens.append(stA(i))
            if 0 <= i - 1 < NT:
                gens.append(stB(i - 1))
            if 0 <= i - 2 < NT:
                gens.append(stCq(i - 2))
                gens.append(stCk(i - 2))
            gens.append(self.gpump(6))
            if 0 <= i - 3 < NT:
                gens.append(gCt(i - 3))
            self.interleave(gens)
        self.end_phase()

    def phase2(self, l):
        self.begin_phase()
        nc = self.nc
        sb = self.sb
        qt = [sb("qt", [128, S], BF16) for _ in range(2)]
        kt = [sb("kt", [128, S], BF16) for _ in range(2)]
        qr = [sb("qr", [128, S], BF16) for _ in range(2)]
        kr = [sb("kr", [128, S], BF16) for _ in range(2)]
        vs = [sb("vs", [128, NT, 128], BF16) for _ in range(2)]
        ld_s = [Slot() for _ in range(2)]
        ld_d = [self.dsem() for _ in range(2)]
        NP = 4
        pt = [sb("pt", [128, 512], BF16) for _ in range(NP)]
        pt_s = [Slot() for _ in range(NP)]
        o1 = [sb("o1", [128, 512], F32) for _ in range(2)]
        o1_s = [Slot() for _ in range(2)]
        rcb = [sb("rcb", [128, 512], F32) for _ in range(2)]
        rcb_s = [Slot() for _ in range(2)]
        tb = [sb("tb", [128, 512], F32) for _ in range(2)]
        tb_s = [Slot() for _ in range(2)]
        sqb = [sb("sqb", [128, 512], BF16) for _ in range(2)]
        sqb_s = [Slot() for _ in range(2)]
        rs2 = [sb("rs2", [128, 512], F32) for _ in range(2)]
        rs2_s = [Slot() for _ in range(2)]
        ocs = [sb("ocs", [128, 512], BF16) for _ in range(2)]
        ocs_s = [Slot() for _ in range(2)]
        ocs_d = [self.dsem() for _ in range(2)]
        mk = sb("mk", [128, 128], BF16)
        mk_s = Slot()
        self.op("dve", lambda v: v.memset(mk[:], 0.0), writes=[mk_s])
        self.op("pool", lambda p: p.affine_select(out=mk[:], in_=mk[:], pattern=[[1, 128]], compare_op=ALU.is_ge,
                                                  fill=-30000.0, base=0, channel_multiplier=-1), writes=[mk_s])
        for i in range(2):
            self.op("dve", lambda v: v.memset(kt[i][64:128, :], 0.0), writes=[ld_s[i]])
            self.op("dve", lambda v: v.memset(kr[i][0:64, :], 0.0), writes=[ld_s[i]])
        SB_ = [0, 1, 2]
        SSB = 3
        OB_ = [(4, 5), (6, 7)]
        qkt = self.qkt
        heads = [("d", h) for h in range(4)] + [("m", h) for h in range(4)]
        self.gi = 0
        self.oci = 0
        self.rci = 0
        self.tbi = 0
        self.gstep = 0
        deferred = []

        def defer(delay, fn, tag):
            deferred.append((self.gstep + delay, fn, tag))

        def run_due(force=False, tag=None):
            while True:
                due = [d for d in deferred if (force or d[0] <= self.gstep) and (tag is None or d[2] == tag)]
                if not due:
                    break
                d = min(due, key=lambda x: x[0])
                deferred.remove(d)
                d[1]()

        def load(hi):
            typ, h = heads[hi]
            i = hi % 2
            s = ld_s[i]
            self._deps("sp", [], [s])
            if typ == "d":
                self.dma("sp", qt[i][:], qkt[h], ld_d[i])
                self.dma("sp", kt[i][0:64, :], qkt[4 + h, 0:64, :], ld_d[i])
                self.dma("sp", kr[i][64:128, :], qkt[4 + h, 64:128, :], ld_d[i])
                vsrc = self.vd
            else:
                o = (h % 2) * 64
                self.dma("sp", qt[i][:], qkt[8 + h], ld_d[i])
                self.dma("sp", kt[i][:], qkt[14 + h], ld_d[i])
                self.dma("sp", qr[i][:], qkt[12 + h // 2], ld_d[i])
                self.dma("sp", kr[i][o:o + 64, :], qkt[18 + h // 2, o:o + 64, :], ld_d[i])
                vsrc = self.vm
            tok = self.dma("sp", vs[i][:], vsrc.rearrange("(j p) c -> p j c", p=128)[:, :, h * 128:(h + 1) * 128], ld_d[i])
            s.wr = [tok]
            s.rd = []

        def prep_mla(hi):
            typ, h = heads[hi]
            i = hi % 2
            o = (h % 2) * 64
            z = 64 - o
            self.op("dve", lambda v: v.memset(kr[i][z:z + 64, :], 0.0), writes=[ld_s[i]])

        load(0)
        for hi, (typ, h) in enumerate(heads):
            if hi + 1 < len(heads):
                if heads[hi + 1][0] == "m":
                    prep_mla(hi + 1)
                load(hi + 1)
            i = hi % 2
            lds = ld_s[i]
            if typ == "d":
                maps = [[(kt[i], qt[i])], [(kr[i], qt[i])]]
                scale = 64.0 ** -0.5
                negM = self.negM[:, l, 0:1]
                col = h
            else:
                maps = [[(kt[i], qt[i]), (kr[i], qr[i])]]
                scale = 192.0 ** -0.5
                negM = self.negM[:, l, 1:2]
                col = 4 + h
            steps = []
            for Qb in range(8):
                for m in range(len(maps)):
                    for j in range(4 * Qb + 4):
                        steps.append((Qb, m, j))
            n = len(steps)

            def qk(si):
                Qb, m, j = steps[si]
                r = max(0, j - 4 * Qb)
                N = (4 - r) * 128
                bi = SB_[si % 3]
                parts = maps[m]
                q0 = Qb * 512 + r * 128
                if j < 4 * Qb:
                    mms = [(self.banks[bi][:, 0:N], kp[:, j * 128:(j + 1) * 128], qp[:, q0:q0 + N], pi == 0, pi == len(parts) - 1)
                           for pi, (kp, qp) in enumerate(parts)]
                    self.mm_group(bi, mms, reads=[lds])
                    return
                bs = self.bank_s[bi]
                self._deps("pe", [lds, mk_s], [bs])
                for pi, (kp, qp) in enumerate(parts):
                    nc.tensor.matmul(self.banks[bi][:, 0:N], lhsT=kp[:, j * 128:(j + 1) * 128], rhs=qp[:, q0:q0 + N],
                                     start=(pi == 0), stop=False, skip_group_check=True)
                ins = nc.tensor.matmul(self.banks[bi][:, 0:128], lhsT=self.ident_b[:], rhs=mk[:], start=False, stop=True,
                                       skip_group_check=True)
                tok = self.sig("pe", ins)
                self._commit(tok, [lds], [bs])

            def epilogue(Qb, m, ot, sm, typ=typ, col=col):
                ri = self.rci % 2
                self.rci += 1
                cols = slice(Qb * 512, (Qb + 1) * 512)
                self.op("dve", lambda v: v.reciprocal(out=rcb[ri][:], in_=self.banks[sm][:, :]), reads=[self.bank_s[sm]],
                        writes=[rcb_s[ri]])
                if typ == "m":
                    ci = self.oci % 2
                    self.oci += 1
                    self.op("dve", lambda v: v.tensor_tensor(out=ocs[ci][:], in0=self.banks[ot][:, :], in1=rcb[ri][:], op=ALU.mult),
                            reads=[self.bank_s[ot], rcb_s[ri]], writes=[ocs_s[ci]])
                    self.dma("sp", self.oct_d[col][:, cols], ocs[ci][:], ocs_d[ci], reads=[ocs_s[ci]])
                elif m == 0:
                    oi = Qb % 2
                    self.op("dve", lambda v: v.tensor_tensor(out=o1[oi][:], in0=self.banks[ot][:, :], in1=rcb[ri][:], op=ALU.mult),
                            reads=[self.bank_s[ot], rcb_s[ri]], writes=[o1_s[oi]])
                else:
                    oi = Qb % 2
                    ti = self.tbi % 2
                    self.tbi += 1
                    run_due(force=True, tag=ti)
                    self.op("dve", lambda v: v.scalar_tensor_tensor(out=tb[ti][:], in0=self.banks[ot][:, :],
                                                                    scalar=self.neglam[:, l:l + 1], in1=rcb[ri][:],
                                                                    op0=ALU.mult, op1=ALU.mult),
                            reads=[self.bank_s[ot], rcb_s[ri]], writes=[tb_s[ti]])
                    self.op("dve", lambda v: v.tensor_tensor(out=tb[ti][:], in0=tb[ti][:], in1=o1[oi][:], op=ALU.add),
                            reads=[o1_s[oi]], writes=[tb_s[ti]])

                    def partB():
                        self.op("act", lambda a: a.activation(out=sqb[ti][:], in_=tb[ti][:], func=AF.Square), reads=[tb_s[ti]],
                                writes=[sqb_s[ti]])

                    def partC():
                        self.mm_group(SSB, [(self.banks[SSB][:, :], self.ones_b[:], sqb[ti][:], True, True)], reads=[sqb_s[ti]])

                    def partD():
                        self.op("act", lambda a: a.activation(out=rs2[ti][:], in_=self.banks[SSB][:, :], func=AF.Ln,
                                                              scale=1.0 / 128, bias=float(EPS)),
                                reads=[self.bank_s[SSB]], writes=[rs2_s[ti]])
                        self.op("act", lambda a: a.activation(out=rs2[ti][:], in_=rs2[ti][:], func=AF.Exp, scale=-0.5),
                                reads=[], writes=[rs2_s[ti]])

                    def partE():
                        ci = self.oci % 2
                        self.oci += 1
                        self.op("dve", lambda v: v.tensor_tensor(out=ocs[ci][:], in0=tb[ti][:], in1=rs2[ti][:], op=ALU.mult),
                                reads=[tb_s[ti], rs2_s[ti]], writes=[ocs_s[ci]])
                        self.dma("sp", self.oct_d[col][:, cols], ocs[ci][:], ocs_d[ci], reads=[ocs_s[ci]])

                    defer(8, partB, ti)
                    defer(10, partC, ti)
                    defer(12, partD, ti)
                    defer(15, partE, ti)

            qk(0)
            if n > 1:
                qk(1)
            for si in range(n):
                Qb, m, j = steps[si]
                self.gstep += 1
                run_due()
                if si % 6 == 0:
                    self.pump(1, engs=("dve",))
                if si + 2 < n:
                    qk(si + 2)
                r = max(0, j - 4 * Qb)
                N = (4 - r) * 128
                c0 = r * 128
                bi = SB_[si % 3]
                pi_ = si % NP
                if j == 0:
                    g = self.gi
                    self.gi += 1
                    self.cur_ob = OB_[g % 2]
                ot, sm = self.cur_ob
                self.op("act", lambda a: a.activation(out=pt[pi_][:, 0:N], in_=self.banks[bi][:, 0:N], func=AF.Exp,
                                                      scale=float(scale), bias=negM),
                        reads=[self.bank_s[bi]], writes=[pt_s[pi_]])
                self._deps("pe", [pt_s[pi_], lds], [])
                if j == 0:
                    self._deps("pe", [], [self.bank_s[ot], self.bank_s[sm]])
                last = (j == 4 * Qb + 3)
                nc.tensor.matmul(self.banks[ot][:, c0:512], lhsT=vs[i][:, j, :], rhs=pt[pi_][:, 0:N], start=(j == 0), stop=last,
                                 skip_group_check=True)
                ins = nc.tensor.matmul(self.banks[sm][:, c0:512], lhsT=self.ones_b[:], rhs=pt[pi_][:, 0:N], start=(j == 0),
                                       stop=last, skip_group_check=True)
                tok = self.sig("pe", ins)
                pt_s[pi_].rd.append(tok)
                lds.rd.append(tok)
                for bk in (ot, sm):
                    self.bank_s[bk].wr = [tok]
                    if j == 0:
                        self.bank_s[bk].rd = []
                if last:
                    epilogue(Qb, m, ot, sm)
        run_due(force=True)
        self.end_phase()

    def phase3a(self, l):
        self.begin_phase()
        self.pump_until("wo%d" % l)
        nc = self.nc
        sb = self.sb
        moe = (l % 2 == 1)
        xsrc = self.x if l == 0 else self.xr
        wo = sb("wo", [128, 8, D], BF16)
        w_s = Slot()
        d0 = self.dsem()
        w_s.wr = [self.dma("sp", wo[:], self.b_wo[l].rearrange("(c p) n -> p c n", p=128), d0)]

        def bufs(name, shape, dt, n):
            return [sb(name, shape, dt) for _ in range(n)], [Slot() for _ in range(n)]

        oc, oc_s = bufs("oc", [128, 8, 512], BF16, 2)
        xt, xt_s = bufs("xt", [128, D], F32, 2)
        x1 = [sb("x1", [128, D], F32) for _ in range(3)]
        x1_s = [[Slot(), Slot()] for _ in range(3)]
        smB = [sb("smB", [128, 4], F32) for _ in range(2)]
        ss2_s, lg2_s, rs2_s = [Slot(), Slot()], [Slot(), Slot()], [Slot(), Slot()]
        h2, h2_s = bufs("h2", [128, D], BF16, 2)
        stage = [sb("stage", [128, 8, 512], BF16) for _ in range(2)]
        stage_s = [Slot(), Slot()]
        if moe:
            h2f, h2f_s = bufs("h2f", [128, D], F32, 2)
            h2fT = [sb("h2fT", [128, D], F32) for _ in range(2)]
            h2fT_s = [[Slot(), Slot()] for _ in range(2)]
            lgall = sb("lgall", [128, NT, 8], F32)
            lgall_s = Slot()
            rd = sb("rd", [128, NT, 8], F32)
            rd_s = Slot()
            rt1 = sb("rt1", [128, NT, 8], F32)
            rt1_s = Slot()
            rt2 = sb("rt2", [128, NT, 8], F32)
            rt2_s = Slot()
            rm = sb("rm", [128, 4, NT], F32)
            rm_s = [Slot() for _ in range(4)]
            comb_s = Slot()
        oc_d = [self.dsem() for _ in range(2)]
        xt_d = [self.dsem() for _ in range(2)]
        x1_d = [self.dsem() for _ in range(3)]
        st_d = [self.dsem() for _ in range(2)]
        TPB = [0, 1]
        MMB = [2, 3]
        FTB = [4, 5]
        LGB = [6, 7]
        self.tpi = 0
        self.mmi = 0
        self.lgi = 0

        def stA(t):
            b = t % 2
            b3 = t % 3
            blk, tl = t // 4, t % 4
            ob = blk % 2
            rows = slice(t * 128, (t + 1) * 128)
            if tl == 0:
                self.dma("sp", oc[ob][:], self.oct_d[:, :, blk * 512:(blk + 1) * 512].rearrange("k p n -> p k n"), oc_d[ob],
                         writes=[oc_s[ob]])
            self.dma("sp", xt[b][:], xsrc[rows, :], xt_d[b], writes=[xt_s[b]])
            yield
            for hf in range(2):
                mb = MMB[self.mmi % 2]
                self.mmi += 1
                mms = [(self.banks[mb][:, :], oc[ob][:, k, tl * 128:(tl + 1) * 128], wo[:, k, hf * 512:(hf + 1) * 512], k == 0, k == 7)
                       for k in range(8)]
                self.mm_group(mb, mms, reads=[oc_s[ob], w_s])
                yield
                self.op("dve", lambda v: v.tensor_tensor(out=x1[b3][:, hf * 512:(hf + 1) * 512], in0=self.banks[mb][:, :],
                                                         in1=xt[b][:, hf * 512:(hf + 1) * 512], op=ALU.add),
                        reads=[self.bank_s[mb], xt_s[b]], writes=[x1_s[b3][hf]])
                yield

        def stB(t):
            b = t % 2
            b3 = t % 3
            blk, tl = t // 4, t % 4
            sg = blk % 2
            rows = slice(t * 128, (t + 1) * 128)
            sm = smB[b]
            self.dma("sp", self.xr[rows, :], x1[b3][:], x1_d[b3], reads=x1_s[b3])
            yield
            self.op("act", lambda a: a.activation(out=self.junk[:], in_=x1[b3][:], func=AF.Square, accum_out=sm[:, 0:1]),
                    reads=x1_s[b3], writes=[ss2_s[b]])
            yield
            self.rstd(sm[:, 0:1], ss2_s[b], sm[:, 2:3], rs2_s[b], sm[:, 1:2], lg2_s[b], 1.0 / D)
            yield
            self.op("dve", lambda v: v.tensor_scalar(out=h2[b][:], in0=x1[b3][:], scalar1=sm[:, 2:3], scalar2=None,
                                                     op0=ALU.mult), reads=x1_s[b3] + [rs2_s[b]], writes=[h2_s[b]])
            yield
            bi = TPB[self.tpi % 2]
            self.tpi += 1
            bv = self.bank_bf(bi)
            self.tr_group(bi, [(bv[:, k * 128:(k + 1) * 128], h2[b][:, k * 128:(k + 1) * 128]) for k in range(8)],
                          self.ident_b[:], reads=[h2_s[b]])
            yield
            stg = stage[sg]
            if tl == 0:
                self.wait("act", stage_s[sg].rd + stage_s[sg].wr)
                stage_s[sg].wr = []
                stage_s[sg].rd = []
            tok = self.op("act", lambda a: a.copy(out=stg[:, :, tl * 128:(tl + 1) * 128],
                                                  in_=bv[:, :].rearrange("p (k n) -> p k n", n=128)),
                          reads=[self.bank_s[bi]], writes=[])
            yield
            stage_s[sg].wr.append(tok)
            if tl == 3:
                self.dma("sp", self.h2t[:, :, blk * 512:(blk + 1) * 512].rearrange("k p n -> p k n"), stg[:], st_d[sg],
                         reads=[stage_s[sg]])
                yield

        def stR(t):
            b = t % 2
            b3 = t % 3
            sm = smB[b]
            self.op("dve", lambda v: v.tensor_scalar(out=h2f[b][:], in0=x1[b3][:], scalar1=sm[:, 2:3], scalar2=None,
                                                     op0=ALU.mult), reads=x1_s[b3] + [rs2_s[b]], writes=[h2f_s[b]])
            yield
            for hf in range(2):
                fb = FTB[hf]
                self.tr_group(fb, [(self.banks[fb][:, k * 128:(k + 1) * 128],
                                    h2f[b][:, (hf * 4 + k) * 128:(hf * 4 + k + 1) * 128]) for k in range(4)],
                              self.ident_f[:], reads=[h2f_s[b]])
                yield
                self.op("act", lambda a: a.copy(out=h2fT[b][:, hf * 512:(hf + 1) * 512], in_=self.banks[fb][:, :]),
                        reads=[self.bank_s[fb]], writes=[h2fT_s[b][hf]])
                yield
            lb = LGB[self.lgi % 2]
            self.lgi += 1
            mms = [(self.banks[lb][:, 0:8], h2fT[b][:, k * 128:(k + 1) * 128], self.rw_s[:, k, :], k == 0, k == 7)
                   for k in range(8)]
            self.mm_group(lb, mms, reads=h2fT_s[b])
            yield
            tok = self.op("dve", lambda v: v.tensor_copy(out=lgall[:, t, :], in_=self.banks[lb][:, 0:8]),
                          reads=[self.bank_s[lb]], writes=[])
            lgall_s.wr.append(tok)
            yield

        def router_finish():
            def bc(ap2):
                return ap2.unsqueeze(2).broadcast_to([128, NT, 8])
            self.op("dve", lambda v: v.tensor_reduce(out=rm[:, 0, :], in_=lgall[:], axis=AX.X, op=ALU.max),
                    reads=[lgall_s], writes=[rm_s[0]])
            self.op("dve", lambda v: v.tensor_tensor(out=rd[:], in0=lgall[:], in1=bc(rm[:, 0, :]), op=ALU.subtract),
                    reads=[lgall_s, rm_s[0]], writes=[rd_s])
            self.op("dve", lambda v: v.tensor_scalar(out=rt1[:], in0=rd[:], scalar1=0.0, scalar2=-1e30, op0=ALU.is_ge,
                                                     op1=ALU.mult), reads=[rd_s], writes=[rt1_s])
            self.op("dve", lambda v: v.tensor_tensor(out=rt1[:], in0=rt1[:], in1=rd[:], op=ALU.add),
                    reads=[rd_s], writes=[rt1_s])
            self.op("dve", lambda v: v.tensor_reduce(out=rm[:, 1, :], in_=rt1[:], axis=AX.X, op=ALU.max),
                    reads=[rt1_s], writes=[rm_s[1]])
            self.op("dve", lambda v: v.tensor_tensor(out=rt1[:], in0=rd[:], in1=bc(rm[:, 1, :]), op=ALU.is_ge),
                    reads=[rd_s, rm_s[1]], writes=[rt1_s])
            self.op("act", lambda a: a.activation(out=rt2[:], in_=rd[:], func=AF.Exp), reads=[rd_s], writes=[rt2_s])
            self.op("dve", lambda v: v.tensor_tensor(out=rt1[:], in0=rt1[:], in1=rt2[:], op=ALU.mult),
                    reads=[rt2_s], writes=[rt1_s])
            self.op("dve", lambda v: v.tensor_reduce(out=rm[:, 2, :], in_=rt1[:], axis=AX.X, op=ALU.add),
                    reads=[rt1_s], writes=[rm_s[2]])
            self.op("dve", lambda v: v.reciprocal(out=rm[:, 3, :], in_=rm[:, 2, :]), reads=[rm_s[2]], writes=[rm_s[3]])
            self.op("dve", lambda v: v.tensor_tensor(out=self.comb[:], in0=rt1[:], in1=bc(rm[:, 3, :]), op=ALU.mult),
                    reads=[rt1_s, rm_s[3]], writes=[comb_s])

        for i in range(NT + 2):
            gens = []
            if i < NT:
                gens.append(stA(i))
            if 0 <= i - 1 < NT:
                gens.append(stB(i - 1))
            if moe and 0 <= i - 2 < NT:
                gens.append(stR(i - 2))
            self.interleave(gens)
        if moe:
            router_finish()
        self.end_phase()

    def phase3b(self, l):
        self.begin_phase()
        self.pump_until("ffn%d" % l)
        nc = self.nc
        sb = self.sb
        moe = (l % 2 == 1)
        NE = 8 if moe else 2
        wg = [sb("wg", [128, 8, FFE], BF16) for _ in range(2)]
        wu = [sb("wu", [128, 8, FFE], BF16) for _ in range(2)]
        wd = sb("wd", [128, NCH, D], BF16)
        wgu_s = [Slot() for _ in range(2)]
        wd_s = Slot()
        wgu_d = [self.dsem() for _ in range(2)]
        wd_d = self.dsem()
        hTb = [sb("hTb", [128, 8, 512], BF16) for _ in range(2)]
        hTb_s = [Slot() for _ in range(2)]
        hTb_d = [self.dsem() for _ in range(2)]
        aT = [sb("aT", [128, NCH, 512], BF16) for _ in range(2)]
        aT_s = [Slot() for _ in range(2)]
        NSG = 3
        sgb = [sb("sgb", [128, 512], F32) for _ in range(NSG)]
        sgb_s = [Slot() for _ in range(NSG)]
        NOS = 2
        ost = [sb("ost", [128, D], F32) for _ in range(NOS)]
        ost_s = [Slot() for _ in range(NOS)]
        ost_d = self.swsems[:NOS]
        xrow_s = [Slot() for _ in range(NT)]
        GB = [0, 1]
        UB = [2, 3]
        DB = [4, 5]
        self.gi = 0
        self.di = 0
        self.oi = 0
        self.si = 0
        self.evi = 0

        def load_gu(e):
            i = e % 2
            self._deps("sp", [], [wgu_s[i]])
            if moe:
                gsrc = self.b_mwg[e].rearrange("(c p) n -> p c n", p=128)
                usrc = self.b_mwu[e].rearrange("(c p) n -> p c n", p=128)
            else:
                gsrc = self.b_dwg[:, e * FFE:(e + 1) * FFE].rearrange("(c p) n -> p c n", p=128)
                usrc = self.b_dwu[:, e * FFE:(e + 1) * FFE].rearrange("(c p) n -> p c n", p=128)
            self.dma("sp", wg[i][:], gsrc, wgu_d[i])
            tok = self.dma("sp", wu[i][:], usrc, wgu_d[i])
            wgu_s[i].wr = [tok]
            wgu_s[i].rd = []

        def load_d(e):
            if moe:
                dsrc = self.b_mwd[e].rearrange("(c p) n -> p c n", p=128)
            else:
                dsrc = self.b_dwd[e * FFE:(e + 1) * FFE, :].rearrange("(c p) n -> p c n", p=128)
            self.dma("sp", wd[:], dsrc, wd_d, writes=[wd_s])

        def load_h(bi_):
            i = bi_ % 2
            blk = bi_ % 8
            self.dma("sp", hTb[i][:], self.h2t[:, :, blk * 512:(blk + 1) * 512].rearrange("k p n -> p k n"), hTb_d[i],
                     writes=[hTb_s[i]])

        def gate_up(e, blk, bi_):
            i = e % 2
            hi = bi_ % 2
            ai = bi_ % 2
            for c in range(NCH):
                gb = GB[self.gi % 2]
                ub = UB[self.gi % 2]
                self.gi += 1
                mms = [(self.banks[gb][:, :], wg[i][:, k, c * 128:(c + 1) * 128], hTb[hi][:, k, :], k == 0, k == 7) for k in range(8)]
                self.mm_group(gb, mms, reads=[wgu_s[i], hTb_s[hi]])
                mms = [(self.banks[ub][:, :], wu[i][:, k, c * 128:(c + 1) * 128], hTb[hi][:, k, :], k == 0, k == 7) for k in range(8)]
                self.mm_group(ub, mms, reads=[wgu_s[i], hTb_s[hi]])
                si_ = self.si % NSG
                self.si += 1
                self.op("act", lambda a: a.activation(out=sgb[si_][:], in_=self.banks[gb][:, :], func=AF.Silu),
                        reads=[self.bank_s[gb]], writes=[sgb_s[si_]])
                tok = self.op("dve", lambda v: v.tensor_tensor(out=aT[ai][:, c, :], in0=sgb[si_][:], in1=self.banks[ub][:, :],
                                                               op=ALU.mult),
                              reads=[sgb_s[si_], self.bank_s[ub]], writes=[aT_s[ai]] if c == 0 else [])
                if c > 0:
                    aT_s[ai].wr.append(tok)

        def down(e, blk, bi_):
            ai = bi_ % 2
            for tq in range(4):
                t = blk * 4 + tq
                oi_ = self.oi % NOS
                self.oi += 1
                rdt = ost_s[oi_].rd + ost_s[oi_].wr
                self.wait("act", rdt)
                self.wait("dve", rdt)
                ost_s[oi_].rd = []
                ost_s[oi_].wr = []
                for hf in range(2):
                    db = DB[self.di % 2]
                    self.di += 1
                    mms = [(self.banks[db][:, :], aT[ai][:, c, tq * 128:(tq + 1) * 128], wd[:, c, hf * 512:(hf + 1) * 512],
                            c == 0, c == NCH - 1) for c in range(NCH)]
                    self.mm_group(db, mms, reads=[aT_s[ai], wd_s])
                    eng = "act" if self.evi % 2 == 0 else "dve"
                    self.evi += 1
                    dst = ost[oi_][:, hf * 512:(hf + 1) * 512]
                    wr = []
                    if moe:
                        sc = self.comb[:, t, e:e + 1]
                        if eng == "act":
                            tok = self.op("act", lambda a: a.activation(out=dst, in_=self.banks[db][:, :], func=AF.Copy, scale=sc),
                                          reads=[self.bank_s[db]], writes=wr)
                        else:
                            tok = self.op("dve", lambda v: v.tensor_scalar(out=dst, in0=self.banks[db][:, :], scalar1=sc,
                                                                           scalar2=None, op0=ALU.mult),
                                          reads=[self.bank_s[db]], writes=wr)
                    else:
                        if eng == "act":
                            tok = self.op("act", lambda a: a.copy(out=dst, in_=self.banks[db][:, :]), reads=[self.bank_s[db]],
                                          writes=wr)
                        else:
                            tok = self.op("dve", lambda v: v.tensor_copy(out=dst, in_=self.banks[db][:, :]),
                                          reads=[self.bank_s[db]], writes=wr)
                    ost_s[oi_].wr.append(tok)
                self.dma("pool", self.xr[t * 128:(t + 1) * 128, :], ost[oi_][:], ost_d[oi_], reads=[ost_s[oi_]],
                         writes=[xrow_s[t]], accum_op=ALU.add)

        seq = [(e, blk) for e in range(NE) for blk in range(8)]
        load_gu(0)
        load_d(0)
        load_h(0)
        cur_wd = 0
        for bi_, (e, blk) in enumerate(seq):
            if bi_ + 1 < len(seq):
                load_h(bi_ + 1)
            if blk == 0 and e + 1 < NE:
                load_gu(e + 1)
            if bi_ == 0:
                gate_up(e, blk, bi_)
            if bi_ + 1 < len(seq):
                gate_up(seq[bi_ + 1][0], seq[bi_ + 1][1], bi_ + 1)
            if e != cur_wd:
                load_d(e)
                cur_wd = e
            down(e, blk, bi_)
        self.end_phase()


def _rope_table(theta, rot):
    half = rot // 2
    inv_freq = (1.0 / (np.float32(theta) ** (np.arange(half, dtype=np.float32) * np.float32(2.0 / rot)))).astype(np.float32)
    ang = (np.arange(S, dtype=np.float32)[:, None] * inv_freq[None, :]).astype(np.float32)
    c = np.cos(ang.astype(np.float64)).astype(np.float32)
    s = np.sin(ang.astype(np.float64)).astype(np.float32)
    return np.ascontiguousarray(np.concatenate([c, c, -s, s], axis=1))


_CACHE = {}


def _get_nc(**kw):
    key = tuple(sorted(kw.items()))
    if key not in _CACHE:
        kb = KB(**kw)
        kb.build()
        _CACHE[key] = kb
    return _CACHE[key]


def make_in_maps(inputs, ncores=8):
    f = lambda a: np.ascontiguousarray(np.asarray(a, dtype=np.float32))
    shared = {
        "attn_norm_g": f(inputs["attn_norm_g"]),
        "w_in": f(inputs["w_in"]),
        "diff_q_norm_g": f(inputs["diff_q_norm_g"]),
        "diff_k_norm_g": f(inputs["diff_k_norm_g"]),
        "diff_lambda": f(inputs["diff_lambda"]).reshape(DEPTH, 256),
        "diff_subln_g": f(inputs["diff_subln_g"]),
        "mla_q_ln_g": f(inputs["mla_q_ln_g"]),
        "w_uq": f(inputs["w_uq"]),
        "mla_kv_ln_g": f(inputs["mla_kv_ln_g"]),
        "w_ukv": f(inputs["w_ukv"]),
        "mla_qk_norm_g": f(inputs["mla_qk_norm_g"]).reshape(DEPTH, 384),
        "w_o": f(inputs["w_o"]),
        "ffn_norm_g": f(inputs["ffn_norm_g"]),
        "dense_w_gate": f(inputs["dense_w_gate"])[0],
        "dense_w_up": f(inputs["dense_w_up"])[0],
        "dense_w_down": f(inputs["dense_w_down"])[0],
        "router_w": f(inputs["router_w"])[0],
        "moe_w_gate": f(inputs["moe_w_gate"])[0],
        "moe_w_up": f(inputs["moe_w_up"])[0],
        "moe_w_down": f(inputs["moe_w_down"])[0],
        "cs_d": _rope_table(ROPE_THETA, 16),
        "cs_m": _rope_table(MLA_ROPE_THETA, 64),
        "ident": np.eye(128, dtype=np.float32),
    }
    x = f(inputs["x"])
    maps = []
    for c in range(ncores):
        m = dict(shared)
        m["x"] = np.ascontiguousarray(x[c])
        maps.append(m)
    return maps


def kernel(**inputs):
    kb = _get_nc()
    in_maps = make_in_maps(inputs)
    res = run_bass_kernel_spmd(kb.nc, in_maps, core_ids=list(range(8)))
    out = np.stack([np.asarray(r["y"], dtype=np.float32) for r in res.results], axis=0)
    return out
```

```python
import math
from contextlib import ExitStack
import numpy as np
import ml_dtypes
import concourse.bass as bass
import concourse.mybir as mybir
from concourse.bass_utils import run_bass_kernel_spmd

F32 = mybir.dt.float32
BF16 = mybir.dt.bfloat16
ALU = mybir.AluOpType
AF = mybir.ActivationFunctionType
AX = mybir.AxisListType

S = 4096
D = 1024
NT = S // 128
DEPTH = 2
EPS = 1e-6
INC = 1984
FFE = 1408
NCH = FFE // 128
ROPE_THETA = 500000.0
MLA_ROPE_THETA = 10000.0


class DSem:
    def __init__(self, h):
        self.h = h
        self.v = 0


class Slot:
    __slots__ = ("wr", "rd")

    def __init__(self):
        self.wr = []
        self.rd = []


class KB:
    def __init__(self, depth=DEPTH, stop_after=None, debug=False):
        self.depth = depth
        self.stop_after = stop_after
        self.debug = debug
        nc = self.nc = bass.Bass("TRN2", target_bir_lowering=False)
        self.E = dict(pe=nc.tensor, dve=nc.vector, act=nc.scalar, pool=nc.gpsimd, sp=nc.sync)
        self.csem = {k: nc.alloc_semaphore("c_" + k) for k in ("pe", "dve", "act", "pool")}
        self.ccnt = dict.fromkeys(self.csem, 0)
        self.waited = {}
        self.dsems = [DSem(nc.alloc_semaphore("d%d" % i)) for i in range(72)]
        self.dptr = 0
        self.swsems = [DSem(nc.alloc_semaphore("sw%d" % i)) for i in range(3)]
        self.banks = [nc.alloc_psum_tensor("bank%d" % i, [128, 512], F32) for i in range(8)]
        self.bank_s = [Slot() for _ in range(8)]
        self.stack = None
        self.uid = 0
        self.const_s = None
        self.log = [] if debug else None

    def sig(self, e, ins):
        self.ccnt[e] += 1
        ins.then_inc(self.csem[e], 1)
        if self.log is not None:
            self.log.append(("inc", e, self.csem[e].num, 1))
        return (self.csem[e], self.ccnt[e])

    def wait(self, e, toks):
        for tok in toks:
            sem, v = tok
            key = (e, sem.num)
            if self.waited.get(key, 0) >= v:
                continue
            self.waited[key] = v
            self.E[e].wait_ge(sem, v)
            if self.log is not None:
                self.log.append(("wait", e, sem.num, v))

    def _deps(self, e, reads, writes):
        toks = []
        for s in reads:
            toks.extend(s.wr)
        for s in writes:
            toks.extend(s.rd)
            toks.extend(s.wr)
        self.wait(e, toks)

    def _commit(self, tok, reads, writes):
        for s in reads:
            if s is not self.const_s:
                s.rd.append(tok)
        for s in writes:
            s.wr = [tok]
            s.rd = []

    def op(self, e, fn, reads=(), writes=()):
        self._deps(e, reads, writes)
        ins = fn(self.E[e])
        tok = self.sig(e, ins)
        self._commit(tok, reads, writes)
        return tok

    def dsem(self):
        d = self.dsems[self.dptr]
        self.dptr += 1
        return d

    def dma(self, q, out, in_, ds, reads=(), writes=(), **kw):
        self._deps(q, reads, writes)
        ins = self.E[q].dma_start(out=out, in_=in_, **kw)
        ins.then_inc(ds.h, 16)
        ds.v += 16
        if self.log is not None:
            self.log.append(("dma", q, ds.h.num, 16))
        tok = (ds.h, ds.v)
        self._commit(tok, reads, writes)
        return tok

    def mm_group(self, bank_i, mms, reads=()):
        bs = self.bank_s[bank_i]
        self._deps("pe", reads, [bs])
        ins = None
        for (o, l, r, st, sp) in mms:
            ins = self.nc.tensor.matmul(o, lhsT=l, rhs=r, start=st, stop=sp)
        tok = self.sig("pe", ins)
        self._commit(tok, reads, [bs])
        return tok

    def tr_group(self, bank_i, trs, ident, reads=()):
        bs = self.bank_s[bank_i]
        self._deps("pe", reads, [bs])
        ins = None
        for (o, i) in trs:
            ins = self.nc.tensor.transpose(out=o, in_=i, identity=ident)
        tok = self.sig("pe", ins)
        self._commit(tok, reads, [bs])
        return tok

    def barrier(self):
        toks = [(self.csem[c], self.ccnt[c]) for c in self.csem if self.ccnt[c] > 0]
        toks += [(d.h, d.v) for d in self.dsems + self.swsems if d.v > 0]
        for e in self.E:
            self.wait(e, toks)

    def begin_phase(self):
        self.barrier()
        self.stack = ExitStack()
        self.dptr = self.dkeep
        for b in self.bank_s:
            b.wr = []
            b.rd = []

    def end_phase(self):
        self.barrier()
        self.stack.close()
        self.stack = None

    def sb(self, name, shape, dt):
        self.uid += 1
        return self.stack.enter_context(self.nc.sbuf_tensor("%s_%d" % (name, self.uid), shape, dt))

    def interleave(self, gens):
        gens = list(gens)
        while gens:
            nxt = []
            for g in gens:
                try:
                    next(g)
                    nxt.append(g)
                except StopIteration:
                    pass
            gens = nxt

    def bank_bf(self, i):
        return self.banks[i][:].bitcast(BF16)

    def build(self):
        nc = self.nc

        def din(name, shape):
            return nc.dram_tensor(name, shape, F32, kind="ExternalInput").ap()

        self.x = din("x", [S, D])
        self.attn_g = din("attn_norm_g", [DEPTH, D])
        self.w_in = din("w_in", [DEPTH, D, INC])
        self.dq_g = din("diff_q_norm_g", [DEPTH, 64])
        self.dk_g = din("diff_k_norm_g", [DEPTH, 64])
        self.dlam = din("diff_lambda", [DEPTH, 256])
        self.subln_g = din("diff_subln_g", [DEPTH, 128])
        self.qln_g = din("mla_q_ln_g", [DEPTH, 256])
        self.w_uq = din("w_uq", [DEPTH, 256, 768])
        self.kvln_g = din("mla_kv_ln_g", [DEPTH, 128])
        self.w_ukv = din("w_ukv", [DEPTH, 128, 1024])
        self.mqk_g = din("mla_qk_norm_g", [DEPTH, 2 * 192])
        self.w_o = din("w_o", [DEPTH, D, D])
        self.ffn_g = din("ffn_norm_g", [DEPTH, D])
        self.dwg = din("dense_w_gate", [D, 2816])
        self.dwu = din("dense_w_up", [D, 2816])
        self.dwd = din("dense_w_down", [2816, D])
        self.rw = din("router_w", [D, 8])
        self.mwg = din("moe_w_gate", [8, D, FFE])
        self.mwu = din("moe_w_up", [8, D, FFE])
        self.mwd = din("moe_w_down", [8, FFE, D])
        self.cs_d = din("cs_d", [S, 32])
        self.cs_m = din("cs_m", [S, 128])
        self.ident_in = din("ident", [128, 128])
        self.y = nc.dram_tensor("y", [S, D], F32, kind="ExternalOutput").ap()
        xr = self.xr = self.y

        kind = "ExternalOutput" if self.debug else "Internal"

        def dscr(name, shape, dt):
            return nc.dram_tensor(name, shape, dt, kind=kind).ap()

        self.qkt = dscr("qkt", [20, 128, S], BF16)
        self.vd = dscr("vd", [S, 512], BF16)
        self.vm = dscr("vm", [S, 512], BF16)
        self.oct_d = dscr("oct_d", [8, 128, S], BF16)
        self.h2t = dscr("h2t", [8, 128, S], BF16)
        self.b_win = dscr("b_win", [DEPTH, D, INC], BF16)
        self.b_wuq = dscr("b_wuq", [DEPTH, 256, 768], BF16)
        self.b_wukv = dscr("b_wukv", [DEPTH, 128, 1024], BF16)
        self.b_wo = dscr("b_wo", [DEPTH, D, D], BF16)
        self.b_dwg = dscr("b_dwg", [D, 2816], BF16)
        self.b_dwu = dscr("b_dwu", [D, 2816], BF16)
        self.b_dwd = dscr("b_dwd", [2816, D], BF16)
        self.b_mwg = dscr("b_mwg", [8, D, FFE], BF16)
        self.b_mwu = dscr("b_mwu", [8, D, FFE], BF16)
        self.b_mwd = dscr("b_mwd", [8, FFE, D], BF16)

        A = nc.alloc_sbuf_tensor
        self.ident_f = A("ident_f", [128, 128], F32)
        self.ident_b = A("ident_b", [128, 128], BF16)
        self.ones_b = A("ones_b", [128, 128], BF16)
        self.sc_attn = A("sc_attn", [128, DEPTH, 8], F32)
        self.sc_ffn = A("sc_ffn", [128, DEPTH, 8], F32)
        self.sc_qln = A("sc_qln", [128, DEPTH, 2], F32)
        self.sc_kvln = A("sc_kvln", [128, DEPTH, 1], F32)
        self.sc_wo = A("sc_wo", [128, DEPTH, 8], F32)
        self.ones = A("ones", [128, 24], F32)
        self.gd = A("gd", [128, DEPTH, 2, 64], F32)
        self.gm = A("gm", [128, DEPTH, 2, 192], F32)
        self.lamt = A("lamt", [128, DEPTH, 256], F32)
        self.lamw = A("lamw", [128, 8], F32)
        self.neglam = A("neglam", [128, DEPTH], F32)
        self.negM = A("negM", [128, DEPTH, 2], F32)
        self.gmax = A("gmax", [128, 8], F32)
        self.rw_s = A("rw_s", [128, 8, 8], F32)
        self.comb = A("comb", [128, NT, 8], F32)
        self.junk = A("junk", [128, 1024], BF16)
        self.const_s = Slot()

        self.setup_consts()
        self.setup_wjobs()
        self.dkeep = self.dptr
        if self.log is not None:
            snap = {self.csem[c].num: self.ccnt[c] for c in self.csem}
            snap.update({d.h.num: d.v for d in self.dsems + self.swsems + [self.xinit]})
            self.log.append(("snapshot", snap))
        for l in range(self.depth):
            self.phase1(l)
            if self.stop_after == "p1_%d" % l:
                return self.finish()
            self.phase2(l)
            if self.stop_after == "p2_%d" % l:
                return self.finish()
            self.phase3a(l)
            if self.stop_after == "p3a_%d" % l:
                return self.finish()
            self.phase3b(l)
            if self.stop_after == "p3b_%d" % l:
                return self.finish()
        return self.finish()

    def finish(self):
        self.barrier()
        return self.nc

    def setup_consts(self):
        nc = self.nc
        cs = self.const_s
        ds = self.dsem()
        toks = []

        def ld(out, in_, **kw):
            ins = nc.sync.dma_start(out=out, in_=in_, **kw)
            ins.then_inc(ds.h, 16)
            ds.v += 16

        self.xinit = self.dsem()

        ld(self.ident_f[:], self.ident_in[:, :])
        for l in range(DEPTH):
            ld(self.sc_attn[:, l, :], self.attn_g[l].rearrange("(c p) -> p c", p=128), allow_slow_non_contiguous=True)
            ld(self.sc_ffn[:, l, :], self.ffn_g[l].rearrange("(c p) -> p c", p=128), allow_slow_non_contiguous=True)
            ld(self.sc_qln[:, l, :], self.qln_g[l].rearrange("(c p) -> p c", p=128), allow_slow_non_contiguous=True)
            ld(self.sc_kvln[:, l, :], self.kvln_g[l].rearrange("(c p) -> p c", p=128), allow_slow_non_contiguous=True)
            ld(self.sc_wo[:, l, 0:1], self.subln_g[l].rearrange("(c p) -> p c", p=128), allow_slow_non_contiguous=True)
            ld(self.gd[:, l, 0, :], self.dq_g[l].partition_broadcast(128))
            ld(self.gd[:, l, 1, :], self.dk_g[l].partition_broadcast(128))
            ld(self.gm[:, l, :, :].rearrange("p a b -> p (a b)"), self.mqk_g[l].partition_broadcast(128))
            ld(self.lamt[:, l, :], self.dlam[l].partition_broadcast(128))
        ld(self.rw_s[:], self.rw.rearrange("(c p) n -> p c n", p=128))
        nc.vector.wait_ge(ds.h, ds.v)
        nc.gpsimd.wait_ge(ds.h, ds.v)
        nc.scalar.wait_ge(ds.h, ds.v)
        V = nc.vector
        sv = self.csem["dve"]

        def vop(ins):
            self.ccnt["dve"] += 1
            ins.then_inc(sv, 1)
            V.wait_ge(sv, self.ccnt["dve"])

        def aop(ins):
            self.ccnt["act"] += 1
            ins.then_inc(self.csem["act"], 1)
            nc.scalar.wait_ge(self.csem["act"], self.ccnt["act"])
            V.wait_ge(self.csem["act"], self.ccnt["act"])

        vop(V.tensor_copy(out=self.ident_b[:], in_=self.ident_f[:]))
        vop(V.memset(self.ones[:], 1.0))
        vop(V.memset(self.ones_b[:], 1.0))
        for l in range(DEPTH):
            lam_init = 0.8 - 0.6 * math.exp(-0.3 * l)
            vop(V.tensor_scalar(out=self.sc_wo[:, l, 0:1], in0=self.sc_wo[:, l, 0:1], scalar1=float(1.0 - lam_init),
                                scalar2=None, op0=ALU.mult))
            for c in range(1, 4):
                vop(V.tensor_copy(out=self.sc_wo[:, l, c:c + 1], in_=self.sc_wo[:, l, 0:1]))
            vop(V.memset(self.sc_wo[:, l, 4:8], 1.0))
        for l in range(DEPTH):
            lam_init = 0.8 - 0.6 * math.exp(-0.3 * l)
            vop(V.tensor_tensor(out=self.lamt[:, l, 0:64], in0=self.lamt[:, l, 0:64], in1=self.lamt[:, l, 64:128], op=ALU.mult))
            vop(V.tensor_tensor(out=self.lamt[:, l, 128:192], in0=self.lamt[:, l, 128:192], in1=self.lamt[:, l, 192:256], op=ALU.mult))
            vop(V.tensor_reduce(out=self.lamw[:, 0:1], in_=self.lamt[:, l, 0:64], axis=AX.X, op=ALU.add))
            vop(V.tensor_reduce(out=self.lamw[:, 1:2], in_=self.lamt[:, l, 128:192], axis=AX.X, op=ALU.add))
            nc.scalar.wait_ge(sv, self.ccnt["dve"])
            aop(nc.scalar.activation(out=self.lamw[:, 2:4], in_=self.lamw[:, 0:2], func=AF.Exp))
            vop(V.tensor_tensor(out=self.lamw[:, 4:5], in0=self.lamw[:, 3:4], in1=self.lamw[:, 2:3], op=ALU.subtract))
            vop(V.tensor_scalar(out=self.neglam[:, l:l + 1], in0=self.lamw[:, 4:5], scalar1=float(-lam_init),
                                scalar2=None, op0=ALU.add))
            vop(V.tensor_reduce(out=self.gmax[:, 0:2], in_=self.gd[:, l, :, :], axis=AX.X, op=ALU.max,
                                apply_absolute_value=True))
            vop(V.tensor_reduce(out=self.gmax[:, 2:4], in_=self.gm[:, l, :, :], axis=AX.X, op=ALU.max,
                                apply_absolute_value=True))
            vop(V.tensor_tensor(out=self.gmax[:, 4:5], in0=self.gmax[:, 0:1], in1=self.gmax[:, 1:2], op=ALU.mult))
            vop(V.tensor_tensor(out=self.gmax[:, 5:6], in0=self.gmax[:, 2:3], in1=self.gmax[:, 3:4], op=ALU.mult))
            vop(V.tensor_scalar(out=self.negM[:, l, 0:1], in0=self.gmax[:, 4:5], scalar1=float(-math.sqrt(64.0)),
                                scalar2=None, op0=ALU.mult))
            vop(V.tensor_scalar(out=self.negM[:, l, 1:2], in0=self.gmax[:, 5:6], scalar1=float(-math.sqrt(192.0)),
                                scalar2=None, op0=ALU.mult))
        for c in range(8):
            vop(V.tensor_scalar(out=self.rw_s[:, c, :], in0=self.rw_s[:, c, :], scalar1=self.sc_ffn[:, 1, c:c + 1],
                                scalar2=None, op0=ALU.mult))
        vop(V.memset(self.comb[:], 1.0))

    def setup_wjobs(self):
        nc = self.nc
        NB = 3
        CM = 1408
        self.w_fst = [nc.alloc_sbuf_tensor("wf%d" % i, [128, CM], F32) for i in range(NB)]
        self.w_bst = [nc.alloc_sbuf_tensor("wb%d" % i, [128, CM], BF16) for i in range(NB)]
        self.w_fs = [Slot() for _ in range(NB)]
        self.w_bs = [Slot() for _ in range(NB)]
        self.w_fd = [self.dsem() for _ in range(NB)]
        self.w_bd = [self.dsem() for _ in range(NB)]
        self.w_nb = NB
        self.wjobs = []
        self.wnext = 0
        self.wloadq = []
        self.wcastq = []
        self.wcall = 0
        self.wtags = {}

        def add(src, dst, R, C, sc):
            ncs = 1 if C <= CM else 2
            cw = C // ncs
            for c in range(R // 128):
                for j in range(ncs):
                    self.wjobs.append((src[c * 128:(c + 1) * 128, j * cw:(j + 1) * cw],
                                       dst[c * 128:(c + 1) * 128, j * cw:(j + 1) * cw], cw, sc(c)))

        one = lambda c: self.ones[:, 0:1]

        def attn(l):
            add(self.w_in[l], self.b_win[l], D, INC, lambda c, l=l: self.sc_attn[:, l, c:c + 1])
            add(self.w_uq[l], self.b_wuq[l], 256, 768, lambda c, l=l: self.sc_qln[:, l, c:c + 1])
            add(self.w_ukv[l], self.b_wukv[l], 128, 1024, lambda c, l=l: self.sc_kvln[:, l, c:c + 1])
            self.wtags["attn%d" % l] = len(self.wjobs)
            add(self.w_o[l], self.b_wo[l], D, D, lambda c, l=l: self.sc_wo[:, l, c:c + 1])
            self.wtags["wo%d" % l] = len(self.wjobs)

        attn(0)
        add(self.dwg, self.b_dwg, D, 2816, lambda c: self.sc_ffn[:, 0, c:c + 1])
        add(self.dwu, self.b_dwu, D, 2816, lambda c: self.sc_ffn[:, 0, c:c + 1])
        add(self.dwd, self.b_dwd, 2816, D, one)
        self.wtags["ffn0"] = len(self.wjobs)
        if self.depth > 1:
            attn(1)
            for e in range(8):
                add(self.mwg[e], self.b_mwg[e], D, FFE, lambda c: self.sc_ffn[:, 1, c:c + 1])
                add(self.mwu[e], self.b_mwu[e], D, FFE, lambda c: self.sc_ffn[:, 1, c:c + 1])
                add(self.mwd[e], self.b_mwd[e], FFE, D, one)
            self.wtags["ffn1"] = len(self.wjobs)

    def _wstage_store(self, upto):
        keep = []
        for ent in self.wcastq:
            (i, dst, C, call) = ent
            if call > upto:
                keep.append(ent)
                continue
            self.dma("sp", dst, self.w_bst[i][:, 0:C], self.w_bd[i], reads=[self.w_bs[i]])
        self.wcastq = keep

    def _wstage_cast(self, upto, engs):
        keep = []
        for ent in self.wloadq:
            (i, dst, C, scal, call, k) = ent
            if call > upto:
                keep.append(ent)
                continue
            eng = engs[k % len(engs)]
            fst, bst = self.w_fst[i], self.w_bst[i]
            if eng == "act":
                self.op("act", lambda a: a.activation(out=bst[:, 0:C], in_=fst[:, 0:C], func=AF.Copy, scale=scal),
                        reads=[self.w_fs[i]], writes=[self.w_bs[i]])
            else:
                self.op(eng, lambda v: v.tensor_scalar(out=bst[:, 0:C], in0=fst[:, 0:C], scalar1=scal, scalar2=None,
                                                       op0=ALU.mult), reads=[self.w_fs[i]], writes=[self.w_bs[i]])
            self.wcastq.append((i, dst, C, self.wcall))
        self.wloadq = keep

    def pump(self, n, engs=("dve", "act")):
        self.wcall += 1
        c = self.wcall
        self._wstage_store(c - 1)
        self._wstage_cast(c - 1, engs)
        for _ in range(n):
            if self.wnext >= len(self.wjobs):
                break
            src, dst, C, scal = self.wjobs[self.wnext]
            k = self.wnext
            i = k % self.w_nb
            self.wnext += 1
            self.dma("sp", self.w_fst[i][:, 0:C], src, self.w_fd[i], writes=[self.w_fs[i]])
            self.wloadq.append((i, dst, C, scal, c, k))

    def pump_until(self, tag, engs=("dve", "act")):
        tgt = self.wtags[tag]
        while self.wnext < tgt:
            self.pump(1, engs)
        self.wcall += 1
        self._wstage_store(self.wcall)
        self._wstage_cast(self.wcall, engs)
        self._wstage_store(self.wcall)
        self.wait("sp", [(d.h, d.v) for d in self.w_bd if d.v > 0])

    def gpump(self, rounds, engs=("dve", "act")):
        for _ in range(rounds):
            yield
        self.pump(1, engs)

    def rstd(self, src_ap, src_slot, out_ap, out_slot, tmp_ap, tmp_slot, inv_w):
        self.op("act", lambda a: a.activation(out=tmp_ap, in_=src_ap, func=AF.Ln, scale=float(inv_w), bias=float(EPS)),
                reads=[src_slot], writes=[tmp_slot])
        self.op("act", lambda a: a.activation(out=out_ap, in_=tmp_ap, func=AF.Exp, scale=-0.5),
                reads=[tmp_slot], writes=[out_slot])

    def normrope(self, W_, src3, src_slots, G, W, gain3, R, rot_lo, cs_ap, cs_slot, dst_nonrot, dst_rot, dst_slot,
                 nonrot_lo, nonrot_hi):
        sq, sq_s, ssg, ssg_s, lg, lg_s, rsg, rsg_s, tmp, tmp_s, xr_, xr_s, ac, ac_s, sw, sw_s = W_
        n = G * W
        h = R // 2
        sq3 = sq[:, 0:n].rearrange("p (g w) -> p g w", w=W)
        tmp3 = tmp[:, 0:n].rearrange("p (g w) -> p g w", w=W)
        xr3 = xr_[:, 0:G * R].rearrange("p (g r) -> p g r", r=R)
        ac3 = ac[:, 0:G * R].rearrange("p (g r) -> p g r", r=R)
        sw3 = sw[:, 0:G * R].rearrange("p (g r) -> p g r", r=R)
        self.op("act", lambda a: a.activation(out=sq3, in_=src3, func=AF.Square), reads=src_slots, writes=[sq_s])
        yield
        self.op("dve", lambda v: v.tensor_reduce(out=ssg[:, 0:G], in_=sq3, axis=AX.X, op=ALU.add),
                reads=[sq_s], writes=[ssg_s])
        yield
        self.rstd(ssg[:, 0:G], ssg_s, rsg[:, 0:G], rsg_s, lg[:, 0:G], lg_s, 1.0 / W)
        yield
        self.op("dve", lambda v: v.tensor_tensor(out=tmp3, in0=src3, in1=rsg[:, 0:G].unsqueeze(2).broadcast_to([128, G, W]),
                                                 op=ALU.mult),
                reads=list(src_slots) + [rsg_s], writes=[tmp_s])
        yield
        self.op("pool", lambda p: p.tensor_tensor(out=dst_nonrot, in0=tmp3[:, :, nonrot_lo:nonrot_hi],
                                                  in1=gain3[:, :, nonrot_lo:nonrot_hi], op=ALU.mult),
                reads=[tmp_s], writes=[dst_slot])
        yield
        self.op("pool", lambda p: p.tensor_tensor(out=xr3, in0=tmp3[:, :, rot_lo:rot_lo + R],
                                                  in1=gain3[:, :, rot_lo:rot_lo + R], op=ALU.mult),
                reads=[tmp_s], writes=[xr_s])
        yield
        cc = cs_ap[:, 0:R].unsqueeze(1).broadcast_to([128, G, R])
        ns = cs_ap[:, R:R + h].unsqueeze(1).broadcast_to([128, G, h])
        ps_ = cs_ap[:, R + h:2 * R].unsqueeze(1).broadcast_to([128, G, h])
        self.op("pool", lambda p: p.tensor_tensor(out=ac3, in0=xr3, in1=cc, op=ALU.mult),
                reads=[xr_s, cs_slot], writes=[ac_s])
        yield
        self.op("pool", lambda p: p.tensor_tensor(out=sw3[:, :, 0:h], in0=xr3[:, :, h:R], in1=ns, op=ALU.mult),
                reads=[xr_s, cs_slot], writes=[sw_s])
        yield
        self.op("pool", lambda p: p.tensor_tensor(out=sw3[:, :, h:R], in0=xr3[:, :, 0:h], in1=ps_, op=ALU.mult),
                reads=[xr_s, cs_slot], writes=[sw_s])
        yield
        tok = self.op("pool", lambda p: p.tensor_tensor(out=dst_rot, in0=ac3, in1=sw3, op=ALU.add),
                      reads=[ac_s, sw_s], writes=[dst_slot])
        yield
        return tok

    def phase1(self, l):
        self.begin_phase()
        self.pump_until("attn%d" % l)
        xsrc = self.x if l == 0 else self.xr
        nc = self.nc
        sb = self.sb
        win = sb("win", [128, 8, INC], BF16)
        wuq = sb("wuq", [128, 2, 768], BF16)
        wukv = sb("wukv", [128, 1024], BF16)
        w_s = Slot()
        d0 = self.dsem()
        self.dma("sp", win[:], self.b_win[l].rearrange("(c p) n -> p c n", p=128), d0, writes=[])
        self.dma("sp", wuq[:], self.b_wuq[l].rearrange("(c p) n -> p c n", p=128), d0, writes=[])
        tokw = self.dma("sp", wukv[:], self.b_wukv[l], d0, writes=[])
        w_s.wr = [tokw]

        def bufs(name, shape, dt, n):
            return [sb(name, shape, dt) for _ in range(n)], [Slot() for _ in range(n)]

        xt, xt_s = bufs("xt", [128, D], F32, 2)
        hb, hb_s = bufs("hb", [128, D], BF16, 2)
        hT, hT_s = bufs("hT", [128, D], BF16, 2)
        pj = [sb("pj", [128, INC], F32) for _ in range(2)]
        pj_s = [[Slot() for _ in range(4)] for _ in range(2)]
        smA = [sb("smA", [128, 4], F32) for _ in range(2)]
        ss_s, lg_s, rs_s = [Slot(), Slot()], [Slot(), Slot()], [Slot(), Slot()]
        smB = [sb("smB", [128, 8], F32) for _ in range(2)]
        ssc_s, lgc_s, rsc_s = [Slot(), Slot()], [Slot(), Slot()], [Slot(), Slot()]
        cn, cn_s = bufs("cn", [128, 384], BF16, 2)
        cT, cT_s = bufs("cT", [128, 384], BF16, 2)
        qm = [sb("qm", [128, 768], F32) for _ in range(2)]
        qm_s = [[Slot(), Slot()] for _ in range(2)]
        kvm = [sb("kvm", [128, 1024], F32) for _ in range(2)]
        kvm_s = [[Slot(), Slot()] for _ in range(2)]
        kf, kf_s = bufs("kf", [128, 768], F32, 2)
        dA, dA_s = bufs("dA", [128, 1024], BF16, 3)
        mQ, mQ_s = bufs("mQ", [128, 768], BF16, 3)
        mK, mK_s = bufs("mK", [128, 768], BF16, 3)
        vb, vb_s = bufs("vb", [128, 512], BF16, 3)
        vmb, vmb_s = bufs("vmb", [128, 512], BF16, 3)
        csd, csd_s = bufs("csd", [128, 32], F32, 4)
        csm, csm_s = bufs("csm", [128, 128], F32, 4)
        stage = [sb("stage", [128, 20, 256], BF16) for _ in range(2)]
        stage_s = [Slot(), Slot()]
        xt_d = [self.dsem() for _ in range(2)]
        cs_d_ = [self.dsem() for _ in range(4)]
        cs_m_ = [self.dsem() for _ in range(4)]
        vb_d = [self.dsem() for _ in range(3)]
        vmb_d = [self.dsem() for _ in range(3)]
        st_d = [self.dsem() for _ in range(2)]

        def mkscratch(n, gr):
            return (sb("sq", [128, n], F32), Slot(), sb("ssg", [128, 16], F32), Slot(), sb("lg", [128, 16], F32), Slot(),
                    sb("rsg", [128, 16], F32), Slot(), sb("tmp", [128, n], F32), Slot(), sb("xr_", [128, gr], F32), Slot(),
                    sb("ac", [128, gr], F32), Slot(), sb("sw", [128, gr], F32), Slot())

        W_d = mkscratch(1024, 256)
        W_q = mkscratch(768, 256)
        W_k = mkscratch(768, 256)

        gdf = sb("gdf", [128, 16, 64], F32)
        gdf_s = Slot()
        self.op("dve", lambda v: v.tensor_copy(out=gdf[:, 0:8, :], in_=self.gd[:, l, 0, :].unsqueeze(1).broadcast_to([128, 8, 64])),
                writes=[gdf_s])
        self.op("dve", lambda v: v.tensor_copy(out=gdf[:, 8:16, :], in_=self.gd[:, l, 1, :].unsqueeze(1).broadcast_to([128, 8, 64])),
                writes=[gdf_s])
        self.wait("pool", gdf_s.wr)
        gd3 = gdf[:]
        gq3 = self.gm[:, l, 0, :].unsqueeze(1).broadcast_to([128, 4, 192])
        gk3 = self.gm[:, l, 1, :].unsqueeze(1).broadcast_to([128, 4, 192])
        TPB = [0, 1]
        MMB = [2, 3, 4, 5, 6, 7]
        self.mmi = 0
        self.tpi = 0
        self.evi = 0

        def evac(bank_i, src_ap, dst_ap, writes):
            eng = "act" if self.evi % 2 == 0 else "dve"
            self.evi += 1
            if eng == "act":
                return self.op("act", lambda a: a.copy(out=dst_ap, in_=src_ap), reads=[self.bank_s[bank_i]], writes=writes)
            return self.op("dve", lambda v: v.tensor_copy(out=dst_ap, in_=src_ap), reads=[self.bank_s[bank_i]], writes=writes)

        def project(lhs_chunks, rhs_fn, ncols, dst, dst_slots, reads):
            n0 = 0
            i = 0
            while n0 < ncols:
                w = min(512, ncols - n0)
                bi = MMB[self.mmi % len(MMB)]
                self.mmi += 1
                K = len(lhs_chunks)
                mms = [(self.banks[bi][:, 0:w], lhs_chunks[k], rhs_fn(k, n0, w), k == 0, k == K - 1) for k in range(K)]
                self.mm_group(bi, mms, reads=reads)
                evac(bi, self.banks[bi][:, 0:w], dst[:, n0:n0 + w], [dst_slots[i]])
                n0 += w
                i += 1

        def transposes(src_ap, ncol_blocks, src_slots, dst_ap, dst_writes):
            bi = TPB[self.tpi % 2]
            self.tpi += 1
            bv = self.bank_bf(bi)
            trs = [(bv[:, k * 128:(k + 1) * 128], src_ap[:, k * 128:(k + 1) * 128]) for k in range(ncol_blocks)]
            self.tr_group(bi, trs, self.ident_b[:], reads=src_slots)
            src = bv[:, 0:ncol_blocks * 128]
            if len(dst_ap.shape) == 3:
                src = src.rearrange("p (k n) -> p k n", n=128)
            return evac(bi, src, dst_ap, dst_writes)

        def stA(t):
            b = t % 2
            b3 = t % 3
            b4 = t % 4
            rows = slice(t * 128, (t + 1) * 128)
            sm = smA[b]
            self.dma("sp", xt[b][:], xsrc[rows, :], xt_d[b], writes=[xt_s[b]])
            yield
            self.dma("sp", csd[b4][:], self.cs_d[rows, :], cs_d_[b4], writes=[csd_s[b4]])
            yield
            self.dma("sp", csm[b4][:], self.cs_m[rows, :], cs_m_[b4], writes=[csm_s[b4]])
            yield
            self.op("act", lambda a: a.activation(out=self.junk[:], in_=xt[b][:], func=AF.Square, accum_out=sm[:, 0:1]),
                    reads=[xt_s[b]], writes=[ss_s[b]])
            yield
            self.rstd(sm[:, 0:1], ss_s[b], sm[:, 2:3], rs_s[b], sm[:, 1:2], lg_s[b], 1.0 / D)
            yield
            self.op("dve", lambda v: v.tensor_scalar(out=hb[b][:], in0=xt[b][:], scalar1=sm[:, 2:3], scalar2=None,
                                                     op0=ALU.mult), reads=[xt_s[b], rs_s[b]], writes=[hb_s[b]])
            yield
            transposes(hb[b], 8, [hb_s[b]], hT[b][:], [hT_s[b]])
            yield

        def stA2(t):
            b = t % 2
            b3 = t % 3
            project([hT[b][:, k * 128:(k + 1) * 128] for k in range(8)],
                    lambda k, n0, w: win[:, k, n0:n0 + w], INC, pj[b], pj_s[b], [hT_s[b], w_s])
            yield
            self.op("act", lambda a: a.copy(out=vb[b3][:], in_=pj[b][:, 1024:1536]), reads=[pj_s[b][2]], writes=[vb_s[b3]])
            yield

        def stB(t):
            b = t % 2
            b3 = t % 3
            b4 = t % 4
            rows = slice(t * 128, (t + 1) * 128)
            sm = smB[b]
            self.dma("sp", self.vd[rows, :], vb[b3][:], vb_d[b3], reads=[vb_s[b3]])
            yield
            src3 = pj[b][:, 0:1024].rearrange("p (g w) -> p g w", w=64)
            dA3 = dA[b3][:].rearrange("p (g w) -> p g w", w=64)
            yield from self.normrope(W_d, src3, [pj_s[b][0], pj_s[b][1]], 16, 64, gd3, 16, 0, csd[b4], csd_s[b4],
                          dA3[:, :, 16:64], dA3[:, :, 0:16], dA_s[b3], 16, 64)

        def stB2(t):
            b = t % 2
            b3 = t % 3
            sm = smB[b]
            self.op("act", lambda a: a.activation(out=self.junk[:, 256:512], in_=pj[b][:, 1536:1792], func=AF.Square,
                                                  accum_out=sm[:, 0:1]), reads=[pj_s[b][3]], writes=[ssc_s[b]])
            yield
            self.op("act", lambda a: a.activation(out=self.junk[:, 512:640], in_=pj[b][:, 1792:1920], func=AF.Square,
                                                  accum_out=sm[:, 1:2]), reads=[pj_s[b][3]], writes=[ssc_s[b]])
            yield
            self.op("act", lambda a: a.activation(out=sm[:, 2:3], in_=sm[:, 0:1], func=AF.Ln, scale=1.0 / 256, bias=float(EPS)),
                    reads=[ssc_s[b]], writes=[lgc_s[b]])
            yield
            self.op("act", lambda a: a.activation(out=sm[:, 3:4], in_=sm[:, 1:2], func=AF.Ln, scale=1.0 / 128, bias=float(EPS)),
                    reads=[ssc_s[b]], writes=[lgc_s[b]])
            yield
            self.op("act", lambda a: a.activation(out=sm[:, 4:6], in_=sm[:, 2:4], func=AF.Exp, scale=-0.5),
                    reads=[lgc_s[b]], writes=[rsc_s[b]])
            yield
            self.op("dve", lambda v: v.tensor_scalar(out=cn[b][:, 0:256], in0=pj[b][:, 1536:1792], scalar1=sm[:, 4:5],
                                                     scalar2=None, op0=ALU.mult), reads=[pj_s[b][3], rsc_s[b]], writes=[cn_s[b]])
            yield
            self.op("dve", lambda v: v.tensor_scalar(out=cn[b][:, 256:384], in0=pj[b][:, 1792:1920], scalar1=sm[:, 5:6],
                                                     scalar2=None, op0=ALU.mult), reads=[pj_s[b][3], rsc_s[b]],
                    writes=[cn_s[b]])
            yield
            transposes(cn[b], 3, [cn_s[b]], cT[b][:], [cT_s[b]])
            yield
            project([cT[b][:, 0:128], cT[b][:, 128:256]], lambda k, n0, w: wuq[:, k, n0:n0 + w], 768, qm[b], qm_s[b],
                    [cT_s[b], w_s])
            yield
            project([cT[b][:, 256:384]], lambda k, n0, w: wukv[:, n0:n0 + w], 1024, kvm[b], kvm_s[b], [cT_s[b], w_s])
            yield
            kvm4 = kvm[b][:].rearrange("p (h c) -> p h c", c=256)
            kf3 = kf[b][:].rearrange("p (h c) -> p h c", c=192)
            self.op("pool", lambda p: p.tensor_copy(out=kf3[:, :, 0:128], in_=kvm4[:, :, 0:128]), reads=kvm_s[b],
                    writes=[kf_s[b]])
            yield
            self.op("pool", lambda p: p.tensor_copy(out=kf3[:, :, 128:192],
                                                    in_=pj[b][:, 1920:1984].unsqueeze(1).broadcast_to([128, 4, 64])),
                    reads=[pj_s[b][3]], writes=[kf_s[b]])
            yield
            self.op("act", lambda a: a.copy(out=vmb[b3][:].rearrange("p (h c) -> p h c", c=128), in_=kvm4[:, :, 128:256]),
                    reads=kvm_s[b], writes=[vmb_s[b3]])
            yield

        def stCq(t):
            b = t % 2
            b3 = t % 3
            b4 = t % 4
            rows = slice(t * 128, (t + 1) * 128)
            self.dma("sp", self.vm[rows, :], vmb[b3][:], vmb_d[b3], reads=[vmb_s[b3]])
            yield
            qm3 = qm[b][:].rearrange("p (h c) -> p h c", c=192)
            mQn = mQ[b3][:, 0:512].rearrange("p (h c) -> p h c", c=128)
            mQr = mQ[b3][:, 512:768].rearrange("p (h c) -> p h c", c=64)
            yield from self.normrope(W_q, qm3, qm_s[b], 4, 192, gq3, 64, 128, csm[b4], csm_s[b4], mQn, mQr, mQ_s[b3], 0, 128)

        def stCk(t):
            b = t % 2
            b3 = t % 3
            b4 = t % 4
            kf3 = kf[b][:].rearrange("p (h c) -> p h c", c=192)
            mKn = mK[b3][:, 0:512].rearrange("p (h c) -> p h c", c=128)
            mKr = mK[b3][:, 512:768].rearrange("p (h c) -> p h c", c=64)
            yield from self.normrope(W_k, kf3, [kf_s[b]], 4, 192, gk3, 64, 128, csm[b4], csm_s[b4], mKn, mKr, mK_s[b3], 0, 128)

        def stCt(t):
            b = t % 2
            b3 = t % 3
            b4 = t % 4
            pr = t // 2
            sg = pr % 2
            tl = t % 2
            stg = stage[sg]
            cols = slice(tl * 128, (tl + 1) * 128)
            if tl == 0:
                old = stage_s[sg].rd + stage_s[sg].wr
                self.wait("act", old)
                self.wait("dve", old)
                stage_s[sg].wr = []
                stage_s[sg].rd = []
            toks = []
            toks.append(transposes(dA[b3], 8, [dA_s[b3]], stg[:, 0:8, cols], []))
            toks.append(transposes(mQ[b3], 6, [mQ_s[b3]], stg[:, 8:14, cols], []))
            toks.append(transposes(mK[b3], 6, [mK_s[b3]], stg[:, 14:20, cols], []))
            stage_s[sg].wr.extend(toks)
            if tl == 1:
                self.dma("sp", self.qkt[:, :, pr * 256:(pr + 1) * 256].rearrange("k p n -> p k n"), stg[:], st_d[sg],
                         reads=[stage_s[sg]])

        def gCt(t):
            for _ in range(8):
                yield
            stCt(t)

        for i in range(NT + 5):
            gens = []
            if i < NT:
                gens.append(stA(i))
            if 0 <= i - 1 < NT:
                gens.append(stA2(i - 1))
            if 0 <= i - 2 < NT:
                gens.append(stB(i - 2))
                gens.append(stB2(i - 2))
            if 0 <= i - 3 < NT:
                gens.append(stCq(i - 3))
                gens.append(stCk(i - 3))
            gens.append(self.gpump(6))
            if 0 <= i - 4 < NT:
                gens.append(gCt(i - 4))
            self.interleave(gens)
        self.end_phase()

    def phase2(self, l):
        self.begin_phase()
        nc = self.nc
        sb = self.sb
        qt = [sb("qt", [128, S], BF16) for _ in range(2)]
        kt = [sb("kt", [128, S], BF16) for _ in range(2)]
        qr = [sb("qr", [128, S], BF16) for _ in range(2)]
        kr = [sb("kr", [128, S], BF16) for _ in range(2)]
        vs = [sb("vs", [128, NT, 128], BF16) for _ in range(2)]
        ld_s = [Slot() for _ in range(2)]
        ld_d = [self.dsem() for _ in range(2)]
        NP = 4
        pt = [sb("pt", [128, 512], BF16) for _ in range(NP)]
        pt_s = [Slot() for _ in range(NP)]
        o1 = [sb("o1", [128, 512], F32) for _ in range(2)]
        o1_s = [Slot() for _ in range(2)]
        rcb = [sb("rcb", [128, 512], F32) for _ in range(2)]
        rcb_s = [Slot() for _ in range(2)]
        tb = [sb("tb", [128, 512], F32) for _ in range(2)]
        tb_s = [Slot() for _ in range(2)]
        sqb = [sb("sqb", [128, 512], BF16) for _ in range(2)]
        sqb_s = [Slot() for _ in range(2)]
        rs2 = [sb("rs2", [128, 512], F32) for _ in range(2)]
        rs2_s = [Slot() for _ in range(2)]
        ocs = [sb("ocs", [128, 512], BF16) for _ in range(2)]
        ocs_s = [Slot() for _ in range(2)]
        ocs_d = [self.dsem() for _ in range(2)]
        mk = sb("mk", [128, 128], BF16)
        mk_s = Slot()
        self.op("dve", lambda v: v.memset(mk[:], 0.0), writes=[mk_s])
        self.op("pool", lambda p: p.affine_select(out=mk[:], in_=mk[:], pattern=[[1, 128]], compare_op=ALU.is_ge,
                                                  fill=-30000.0, base=0, channel_multiplier=-1), writes=[mk_s])
        for i in range(2):
            self.op("dve", lambda v: v.memset(kt[i][64:128, :], 0.0), writes=[ld_s[i]])
            self.op("dve", lambda v: v.memset(kr[i][0:64, :], 0.0), writes=[ld_s[i]])
        SB_ = [0, 1, 2]
        SSB = 3
        OB_ = [(4, 5), (6, 7)]
        qkt = self.qkt
        heads = [("d", h) for h in range(4)] + [("m", h) for h in range(4)]
        self.gi = 0
        self.oci = 0
        self.rci = 0
        self.tbi = 0
        self.gstep = 0
        deferred = []

        def defer(delay, fn, tag):
            deferred.append((self.gstep + delay, fn, tag))

        def run_due(force=False, tag=None):
            while True:
                due = [d for d in deferred if (force or d[0] <= self.gstep) and (tag is None or d[2] == tag)]
                if not due:
                    break
                d = min(due, key=lambda x: x[0])
                deferred.remove(d)
                d[1]()

        def load(hi):
            typ, h = heads[hi]
            i = hi % 2
            s = ld_s[i]
            self._deps("sp", [], [s])
            if typ == "d":
                self.dma("sp", qt[i][:], qkt[h], ld_d[i])
                self.dma("sp", kt[i][0:64, :], qkt[4 + h, 0:64, :], ld_d[i])
                self.dma("sp", kr[i][64:128, :], qkt[4 + h, 64:128, :], ld_d[i])
                vsrc = self.vd
            else:
                o = (h % 2) * 64
                self.dma("sp", qt[i][:], qkt[8 + h], ld_d[i])
                self.dma("sp", kt[i][:], qkt[14 + h], ld_d[i])
                self.dma("sp", qr[i][:], qkt[12 + h // 2], ld_d[i])
                self.dma("sp", kr[i][o:o + 64, :], qkt[18 + h // 2, o:o + 64, :], ld_d[i])
                vsrc = self.vm
            tok = self.dma("sp", vs[i][:], vsrc.rearrange("(j p) c -> p j c", p=128)[:, :, h * 128:(h + 1) * 128], ld_d[i])
            s.wr = [tok]
            s.rd = []

        def prep_mla(hi):
            typ, h = heads[hi]
            i = hi % 2
            o = (h % 2) * 64
            z = 64 - o
            self.op("dve", lambda v: v.memset(kr[i][z:z + 64, :], 0.0), writes=[ld_s[i]])

        load(0)
        for hi, (typ, h) in enumerate(heads):
            if hi + 1 < len(heads):
                if heads[hi + 1][0] == "m":
                    prep_mla(hi + 1)
                load(hi + 1)
            i = hi % 2
            lds = ld_s[i]
            if typ == "d":
                maps = [[(kt[i], qt[i])], [(kr[i], qt[i])]]
                scale = 64.0 ** -0.5
                negM = self.negM[:, l, 0:1]
                col = h
            else:
                maps = [[(kt[i], qt[i]), (kr[i], qr[i])]]
                scale = 192.0 ** -0.5
                negM = self.negM[:, l, 1:2]
                col = 4 + h
            steps = []
            for Qb in range(8):
                for m in range(len(maps)):
                    for j in range(4 * Qb + 4):
                        steps.append((Qb, m, j))
            n = len(steps)

            def qk(si):
                Qb, m, j = steps[si]
                r = max(0, j - 4 * Qb)
                N = (4 - r) * 128
                bi = SB_[si % 3]
                parts = maps[m]
                q0 = Qb * 512 + r * 128
                if j < 4 * Qb:
                    mms = [(self.banks[bi][:, 0:N], kp[:, j * 128:(j + 1) * 128], qp[:, q0:q0 + N], pi == 0, pi == len(parts) - 1)
                           for pi, (kp, qp) in enumerate(parts)]
                    self.mm_group(bi, mms, reads=[lds])
                    return
                bs = self.bank_s[bi]
                self._deps("pe", [lds, mk_s], [bs])
                for pi, (kp, qp) in enumerate(parts):
                    nc.tensor.matmul(self.banks[bi][:, 0:N], lhsT=kp[:, j * 128:(j + 1) * 128], rhs=qp[:, q0:q0 + N],
                                     start=(pi == 0), stop=False, skip_group_check=True)
                ins = nc.tensor.matmul(self.banks[bi][:, 0:128], lhsT=self.ident_b[:], rhs=mk[:], start=False, stop=True,
                                       skip_group_check=True)
                tok = self.sig("pe", ins)
                self._commit(tok, [lds], [bs])

            def epilogue(Qb, m, ot, sm, typ=typ, col=col):
                ri = self.rci % 2
                self.rci += 1
                cols = slice(Qb * 512, (Qb + 1) * 512)
                self.op("dve", lambda v: v.reciprocal(out=rcb[ri][:], in_=self.banks[sm][:, :]), reads=[self.bank_s[sm]],
                        writes=[rcb_s[ri]])
                if typ == "m":
                    ci = self.oci % 2
                    self.oci += 1
                    self.op("dve", lambda v: v.tensor_tensor(out=ocs[ci][:], in0=self.banks[ot][:, :], in1=rcb[ri][:], op=ALU.mult),
                            reads=[self.bank_s[ot], rcb_s[ri]], writes=[ocs_s[ci]])
                    self.dma("sp", self.oct_d[col][:, cols], ocs[ci][:], ocs_d[ci], reads=[ocs_s[ci]])
                elif m == 0:
                    oi = Qb % 2
                    self.op("dve", lambda v: v.tensor_tensor(out=o1[oi][:], in0=self.banks[ot][:, :], in1=rcb[ri][:], op=ALU.mult),
                            reads=[self.bank_s[ot], rcb_s[ri]], writes=[o1_s[oi]])
                else:
                    oi = Qb % 2
                    ti = self.tbi % 2
                    self.tbi += 1
                    run_due(force=True, tag=ti)
                    self.op("dve", lambda v: v.scalar_tensor_tensor(out=tb[ti][:], in0=self.banks[ot][:, :],
                                                                    scalar=self.neglam[:, l:l + 1], in1=rcb[ri][:],
                                                                    op0=ALU.mult, op1=ALU.mult),
                            reads=[self.bank_s[ot], rcb_s[ri]], writes=[tb_s[ti]])
                    self.op("dve", lambda v: v.tensor_tensor(out=tb[ti][:], in0=tb[ti][:], in1=o1[oi][:], op=ALU.add),
                            reads=[o1_s[oi]], writes=[tb_s[ti]])

                    def partB():
                        self.op("act", lambda a: a.activation(out=sqb[ti][:], in_=tb[ti][:], func=AF.Square), reads=[tb_s[ti]],
                                writes=[sqb_s[ti]])

                    def partC():
                        self.mm_group(SSB, [(self.banks[SSB][:, :], self.ones_b[:], sqb[ti][:], True, True)], reads=[sqb_s[ti]])

                    def partD():
                        self.op("act", lambda a: a.activation(out=rs2[ti][:], in_=self.banks[SSB][:, :], func=AF.Ln,
                                                              scale=1.0 / 128, bias=float(EPS)),
                                reads=[self.bank_s[SSB]], writes=[rs2_s[ti]])
                        self.op("act", lambda a: a.activation(out=rs2[ti][:], in_=rs2[ti][:], func=AF.Exp, scale=-0.5),
                                reads=[], writes=[rs2_s[ti]])

                    def partE():
                        ci = self.oci % 2
                        self.oci += 1
                        self.op("dve", lambda v: v.tensor_tensor(out=ocs[ci][:], in0=tb[ti][:], in1=rs2[ti][:], op=ALU.mult),
                                reads=[tb_s[ti], rs2_s[ti]], writes=[ocs_s[ci]])
                        self.dma("sp", self.oct_d[col][:, cols], ocs[ci][:], ocs_d[ci], reads=[ocs_s[ci]])

                    defer(8, partB, ti)
                    defer(10, partC, ti)
                    defer(12, partD, ti)
                    defer(15, partE, ti)

            qk(0)
            if n > 1:
                qk(1)
            for si in range(n):
                Qb, m, j = steps[si]
                self.gstep += 1
                run_due()
                if si % 6 == 0:
                    self.pump(1, engs=("dve",))
                if si + 2 < n:
                    qk(si + 2)
                r = max(0, j - 4 * Qb)
                N = (4 - r) * 128
                c0 = r * 128
                bi = SB_[si % 3]
                pi_ = si % NP
                if j == 0:
                    g = self.gi
                    self.gi += 1
                    self.cur_ob = OB_[g % 2]
                ot, sm = self.cur_ob
                self.op("act", lambda a: a.activation(out=pt[pi_][:, 0:N], in_=self.banks[bi][:, 0:N], func=AF.Exp,
                                                      scale=float(scale), bias=negM),
                        reads=[self.bank_s[bi]], writes=[pt_s[pi_]])
                self._deps("pe", [pt_s[pi_], lds], [])
                if j == 0:
                    self._deps("pe", [], [self.bank_s[ot], self.bank_s[sm]])
                last = (j == 4 * Qb + 3)
                nc.tensor.matmul(self.banks[ot][:, c0:512], lhsT=vs[i][:, j, :], rhs=pt[pi_][:, 0:N], start=(j == 0), stop=last,
                                 skip_group_check=True)
                ins = nc.tensor.matmul(self.banks[sm][:, c0:512], lhsT=self.ones_b[:], rhs=pt[pi_][:, 0:N], start=(j == 0),
                                       stop=last, skip_group_check=True)
                tok = self.sig("pe", ins)
                pt_s[pi_].rd.append(tok)
                lds.rd.append(tok)
                for bk in (ot, sm):
                    self.bank_s[bk].wr = [tok]
                    if j == 0:
                        self.bank_s[bk].rd = []
                if last:
                    epilogue(Qb, m, ot, sm)
        run_due(force=True)
        self.end_phase()

    def phase3a(self, l):
        self.begin_phase()
        self.pump_until("wo%d" % l)
        nc = self.nc
        sb = self.sb
        moe = (l % 2 == 1)
        xsrc = self.x if l == 0 else self.xr
        wo = sb("wo", [128, 8, D], BF16)
        w_s = Slot()
        d0 = self.dsem()
        w_s.wr = [self.dma("sp", wo[:], self.b_wo[l].rearrange("(c p) n -> p c n", p=128), d0)]

        def bufs(name, shape, dt, n):
            return [sb(name, shape, dt) for _ in range(n)], [Slot() for _ in range(n)]

        oc, oc_s = bufs("oc", [128, 8, 512], BF16, 2)
        xt, xt_s = bufs("xt", [128, D], F32, 2)
        x1 = [sb("x1", [128, D], F32) for _ in range(3)]
        x1_s = [[Slot(), Slot()] for _ in range(3)]
        smB = [sb("smB", [128, 4], F32) for _ in range(2)]
        ss2_s, lg2_s, rs2_s = [Slot(), Slot()], [Slot(), Slot()], [Slot(), Slot()]
        h2, h2_s = bufs("h2", [128, D], BF16, 2)
        stage = [sb("stage", [128, 8, 512], BF16) for _ in range(2)]
        stage_s = [Slot(), Slot()]
        if moe:
            h2f, h2f_s = bufs("h2f", [128, D], F32, 2)
            h2fT = [sb("h2fT", [128, D], F32) for _ in range(2)]
            h2fT_s = [[Slot(), Slot()] for _ in range(2)]
            lgall = sb("lgall", [128, NT, 8], F32)
            lgall_s = Slot()
            rd = sb("rd", [128, NT, 8], F32)
            rd_s = Slot()
            rt1 = sb("rt1", [128, NT, 8], F32)
            rt1_s = Slot()
            rt2 = sb("rt2", [128, NT, 8], F32)
            rt2_s = Slot()
            rm = sb("rm", [128, 4, NT], F32)
            rm_s = [Slot() for _ in range(4)]
            comb_s = Slot()
        oc_d = [self.dsem() for _ in range(2)]
        xt_d = [self.dsem() for _ in range(2)]
        x1_d = [self.dsem() for _ in range(3)]
        st_d = [self.dsem() for _ in range(2)]
        TPB = [0, 1]
        MMB = [2, 3]
        FTB = [4, 5]
        LGB = [6, 7]
        self.tpi = 0
        self.mmi = 0
        self.lgi = 0

        def stA(t):
            b = t % 2
            b3 = t % 3
            blk, tl = t // 4, t % 4
            ob = blk % 2
            rows = slice(t * 128, (t + 1) * 128)
            if tl == 0:
                self.dma("sp", oc[ob][:], self.oct_d[:, :, blk * 512:(blk + 1) * 512].rearrange("k p n -> p k n"), oc_d[ob],
                         writes=[oc_s[ob]])
            self.dma("sp", xt[b][:], xsrc[rows, :], xt_d[b], writes=[xt_s[b]])
            yield
            for hf in range(2):
                mb = MMB[self.mmi % 2]
                self.mmi += 1
                mms = [(self.banks[mb][:, :], oc[ob][:, k, tl * 128:(tl + 1) * 128], wo[:, k, hf * 512:(hf + 1) * 512], k == 0, k == 7)
                       for k in range(8)]
                self.mm_group(mb, mms, reads=[oc_s[ob], w_s])
                yield
                self.op("dve", lambda v: v.tensor_tensor(out=x1[b3][:, hf * 512:(hf + 1) * 512], in0=self.banks[mb][:, :],
                                                         in1=xt[b][:, hf * 512:(hf + 1) * 512], op=ALU.add),
                        reads=[self.bank_s[mb], xt_s[b]], writes=[x1_s[b3][hf]])
                yield

        def stB(t):
            b = t % 2
            b3 = t % 3
            blk, tl = t // 4, t % 4
            sg = blk % 2
            rows = slice(t * 128, (t + 1) * 128)
            sm = smB[b]
            self.dma("sp", self.xr[rows, :], x1[b3][:], x1_d[b3], reads=x1_s[b3])
            yield
            self.op("act", lambda a: a.activation(out=self.junk[:], in_=x1[b3][:], func=AF.Square, accum_out=sm[:, 0:1]),
                    reads=x1_s[b3], writes=[ss2_s[b]])
            yield
            self.rstd(sm[:, 0:1], ss2_s[b], sm[:, 2:3], rs2_s[b], sm[:, 1:2], lg2_s[b], 1.0 / D)
            yield
            self.op("dve", lambda v: v.tensor_scalar(out=h2[b][:], in0=x1[b3][:], scalar1=sm[:, 2:3], scalar2=None,
                                                     op0=ALU.mult), reads=x1_s[b3] + [rs2_s[b]], writes=[h2_s[b]])
            yield
            bi = TPB[self.tpi % 2]
            self.tpi += 1
            bv = self.bank_bf(bi)
            self.tr_group(bi, [(bv[:, k * 128:(k + 1) * 128], h2[b][:, k * 128:(k + 1) * 128]) for k in range(8)],
                          self.ident_b[:], reads=[h2_s[b]])
            yield
            stg = stage[sg]
            if tl == 0:
                self.wait("act", stage_s[sg].rd + stage_s[sg].wr)
                stage_s[sg].wr = []
                stage_s[sg].rd = []
            tok = self.op("act", lambda a: a.copy(out=stg[:, :, tl * 128:(tl + 1) * 128],
                                                  in_=bv[:, :].rearrange("p (k n) -> p k n", n=128)),
                          reads=[self.bank_s[bi]], writes=[])
            yield
            stage_s[sg].wr.append(tok)
            if tl == 3:
                self.dma("sp", self.h2t[:, :, blk * 512:(blk + 1) * 512].rearrange("k p n -> p k n"), stg[:], st_d[sg],
                         reads=[stage_s[sg]])
                yield

        def stR(t):
            b = t % 2
            b3 = t % 3
            sm = smB[b]
            self.op("dve", lambda v: v.tensor_scalar(out=h2f[b][:], in0=x1[b3][:], scalar1=sm[:, 2:3], scalar2=None,
                                                     op0=ALU.mult), reads=x1_s[b3] + [rs2_s[b]], writes=[h2f_s[b]])
            yield
            for hf in range(2):
                fb = FTB[hf]
                self.tr_group(fb, [(self.banks[fb][:, k * 128:(k + 1) * 128],
                                    h2f[b][:, (hf * 4 + k) * 128:(hf * 4 + k + 1) * 128]) for k in range(4)],
                              self.ident_f[:], reads=[h2f_s[b]])
                yield
                self.op("act", lambda a: a.copy(out=h2fT[b][:, hf * 512:(hf + 1) * 512], in_=self.banks[fb][:, :]),
                        reads=[self.bank_s[fb]], writes=[h2fT_s[b][hf]])
                yield
            lb = LGB[self.lgi % 2]
            self.lgi += 1
            mms = [(self.banks[lb][:, 0:8], h2fT[b][:, k * 128:(k + 1) * 128], self.rw_s[:, k, :], k == 0, k == 7)
                   for k in range(8)]
            self.mm_group(lb, mms, reads=h2fT_s[b])
            yield
            tok = self.op("dve", lambda v: v.tensor_copy(out=lgall[:, t, :], in_=self.banks[lb][:, 0:8]),
                          reads=[self.bank_s[lb]], writes=[])
            lgall_s.wr.append(tok)
            yield

        def router_finish():
            def bc(ap2):
                return ap2.unsqueeze(2).broadcast_to([128, NT, 8])
            self.op("dve", lambda v: v.tensor_reduce(out=rm[:, 0, :], in_=lgall[:], axis=AX.X, op=ALU.max),
                    reads=[lgall_s], writes=[rm_s[0]])
            self.op("dve", lambda v: v.tensor_tensor(out=rd[:], in0=lgall[:], in1=bc(rm[:, 0, :]), op=ALU.subtract),
                    reads=[lgall_s, rm_s[0]], writes=[rd_s])
            self.op("dve", lambda v: v.tensor_scalar(out=rt1[:], in0=rd[:], scalar1=0.0, scalar2=-1e30, op0=ALU.is_ge,
                                                     op1=ALU.mult), reads=[rd_s], writes=[rt1_s])
            self.op("dve", lambda v: v.tensor_tensor(out=rt1[:], in0=rt1[:], in1=rd[:], op=ALU.add),
                    reads=[rd_s], writes=[rt1_s])
            self.op("dve", lambda v: v.tensor_reduce(out=rm[:, 1, :], in_=rt1[:], axis=AX.X, op=ALU.max),
                    reads=[rt1_s], writes=[rm_s[1]])
            self.op("dve", lambda v: v.tensor_tensor(out=rt1[:], in0=rd[:], in1=bc(rm[:, 1, :]), op=ALU.is_ge),
                    reads=[rd_s, rm_s[1]], writes=[rt1_s])
            self.op("act", lambda a: a.activation(out=rt2[:], in_=rd[:], func=AF.Exp), reads=[rd_s], writes=[rt2_s])
            self.op("dve", lambda v: v.tensor_tensor(out=rt1[:], in0=rt1[:], in1=rt2[:], op=ALU.mult),
                    reads=[rt2_s], writes=[rt1_s])
            self.op("dve", lambda v: v.tensor_reduce(out=rm[:, 2, :], in_=rt1[:], axis=AX.X, op=ALU.add),
                    reads=[rt1_s], writes=[rm_s[2]])
            self.op("dve", lambda v: v.reciprocal(out=rm[:, 3, :], in_=rm[:, 2, :]), reads=[rm_s[2]], writes=[rm_s[3]])
            self.op("dve", lambda v: v.tensor_tensor(out=self.comb[:], in0=rt1[:], in1=bc(rm[:, 3, :]), op=ALU.mult),
                    reads=[rt1_s, rm_s[3]], writes=[comb_s])

        for i in range(NT + 2):
            gens = []
            if i < NT:
                gens.append(stA(i))
            if 0 <= i - 1 < NT:
                gens.append(stB(i - 1))
            if moe and 0 <= i - 2 < NT:
                gens.append(stR(i - 2))
            self.interleave(gens)
        if moe:
            router_finish()
        self.end_phase()

    def phase3b(self, l):
        self.begin_phase()
        self.pump_until("ffn%d" % l)
        nc = self.nc
        sb = self.sb
        moe = (l % 2 == 1)
        NE = 8 if moe else 2
        wg = [sb("wg", [128, 8, FFE], BF16) for _ in range(2)]
        wu = [sb("wu", [128, 8, FFE], BF16) for _ in range(2)]
        wd = sb("wd", [128, NCH, D], BF16)
        wgu_s = [Slot() for _ in range(2)]
        wd_s = Slot()
        wgu_d = [self.dsem() for _ in range(2)]
        wd_d = self.dsem()
        hTb = [sb("hTb", [128, 8, 512], BF16) for _ in range(2)]
        hTb_s = [Slot() for _ in range(2)]
        hTb_d = [self.dsem() for _ in range(2)]
        aT = [sb("aT", [128, NCH, 512], BF16) for _ in range(2)]
        aT_s = [Slot() for _ in range(2)]
        NSG = 3
        sgb = [sb("sgb", [128, 512], F32) for _ in range(NSG)]
        sgb_s = [Slot() for _ in range(NSG)]
        NOS = 2
        ost = [sb("ost", [128, D], F32) for _ in range(NOS)]
        ost_s = [Slot() for _ in range(NOS)]
        ost_d = self.swsems[:NOS]
        xrow_s = [Slot() for _ in range(NT)]
        GB = [0, 1]
        UB = [2, 3]
        DB = [4, 5]
        self.gi = 0
        self.di = 0
        self.oi = 0
        self.si = 0
        self.evi = 0

        def load_gu(e):
            i = e % 2
            self._deps("sp", [], [wgu_s[i]])
            if moe:
                gsrc = self.b_mwg[e].rearrange("(c p) n -> p c n", p=128)
                usrc = self.b_mwu[e].rearrange("(c p) n -> p c n", p=128)
            else:
                gsrc = self.b_dwg[:, e * FFE:(e + 1) * FFE].rearrange("(c p) n -> p c n", p=128)
                usrc = self.b_dwu[:, e * FFE:(e + 1) * FFE].rearrange("(c p) n -> p c n", p=128)
            self.dma("sp", wg[i][:], gsrc, wgu_d[i])
            tok = self.dma("sp", wu[i][:], usrc, wgu_d[i])
            wgu_s[i].wr = [tok]
            wgu_s[i].rd = []

        def load_d(e):
            if moe:
                dsrc = self.b_mwd[e].rearrange("(c p) n -> p c n", p=128)
            else:
                dsrc = self.b_dwd[e * FFE:(e + 1) * FFE, :].rearrange("(c p) n -> p c n", p=128)
            self.dma("sp", wd[:], dsrc, wd_d, writes=[wd_s])

        def load_h(bi_):
            i = bi_ % 2
            blk = bi_ % 8
            self.dma("sp", hTb[i][:], self.h2t[:, :, blk * 512:(blk + 1) * 512].rearrange("k p n -> p k n"), hTb_d[i],
                     writes=[hTb_s[i]])

        def gate_up(e, blk, bi_):
            i = e % 2
            hi = bi_ % 2
            ai = bi_ % 2
            for c in range(NCH):
                gb = GB[self.gi % 2]
                ub = UB[self.gi % 2]
                self.gi += 1
                mms = [(self.banks[gb][:, :], wg[i][:, k, c * 128:(c + 1) * 128], hTb[hi][:, k, :], k == 0, k == 7) for k in range(8)]
                self.mm_group(gb, mms, reads=[wgu_s[i], hTb_s[hi]])
                mms = [(self.banks[ub][:, :], wu[i][:, k, c * 128:(c + 1) * 128], hTb[hi][:, k, :], k == 0, k == 7) for k in range(8)]
                self.mm_group(ub, mms, reads=[wgu_s[i], hTb_s[hi]])
                si_ = self.si % NSG
                self.si += 1
                self.op("act", lambda a: a.activation(out=sgb[si_][:], in_=self.banks[gb][:, :], func=AF.Silu),
                        reads=[self.bank_s[gb]], writes=[sgb_s[si_]])
                tok = self.op("dve", lambda v: v.tensor_tensor(out=aT[ai][:, c, :], in0=sgb[si_][:], in1=self.banks[ub][:, :],
                                                               op=ALU.mult),
                              reads=[sgb_s[si_], self.bank_s[ub]], writes=[aT_s[ai]] if c == 0 else [])
                if c > 0:
                    aT_s[ai].wr.append(tok)

        def down(e, blk, bi_):
            ai = bi_ % 2
            for tq in range(4):
                t = blk * 4 + tq
                oi_ = self.oi % NOS
                self.oi += 1
                rdt = ost_s[oi_].rd + ost_s[oi_].wr
                self.wait("act", rdt)
                self.wait("dve", rdt)
                ost_s[oi_].rd = []
                ost_s[oi_].wr = []
                for hf in range(2):
                    db = DB[self.di % 2]
                    self.di += 1
                    mms = [(self.banks[db][:, :], aT[ai][:, c, tq * 128:(tq + 1) * 128], wd[:, c, hf * 512:(hf + 1) * 512],
                            c == 0, c == NCH - 1) for c in range(NCH)]
                    self.mm_group(db, mms, reads=[aT_s[ai], wd_s])
                    eng = "act" if self.evi % 2 == 0 else "dve"
                    self.evi += 1
                    dst = ost[oi_][:, hf * 512:(hf + 1) * 512]
                    wr = []
                    if moe:
                        sc = self.comb[:, t, e:e + 1]
                        if eng == "act":
                            tok = self.op("act", lambda a: a.activation(out=dst, in_=self.banks[db][:, :], func=AF.Copy, scale=sc),
                                          reads=[self.bank_s[db]], writes=wr)
                        else:
                            tok = self.op("dve", lambda v: v.tensor_scalar(out=dst, in0=self.banks[db][:, :], scalar1=sc,
                                                                           scalar2=None, op0=ALU.mult),
                                          reads=[self.bank_s[db]], writes=wr)
                    else:
                        if eng == "act":
                            tok = self.op("act", lambda a: a.copy(out=dst, in_=self.banks[db][:, :]), reads=[self.bank_s[db]],
                                          writes=wr)
                        else:
                            tok = self.op("dve", lambda v: v.tensor_copy(out=dst, in_=self.banks[db][:, :]),
                                          reads=[self.bank_s[db]], writes=wr)
                    ost_s[oi_].wr.append(tok)
                self.dma("pool", self.xr[t * 128:(t + 1) * 128, :], ost[oi_][:], ost_d[oi_], reads=[ost_s[oi_]],
                         writes=[xrow_s[t]], accum_op=ALU.add)

        seq = [(e, blk) for e in range(NE) for blk in range(8)]
        load_gu(0)
        load_d(0)
        load_h(0)
        cur_wd = 0
        for bi_, (e, blk) in enumerate(seq):
            if bi_ + 1 < len(seq):
                load_h(bi_ + 1)
            if blk == 0 and e + 1 < NE:
                load_gu(e + 1)
            if bi_ == 0:
                gate_up(e, blk, bi_)
            if bi_ + 1 < len(seq):
                gate_up(seq[bi_ + 1][0], seq[bi_ + 1][1], bi_ + 1)
            if e != cur_wd:
                load_d(e)
                cur_wd = e
            down(e, blk, bi_)
        self.end_phase()


def _rope_table(theta, rot):
    half = rot // 2
    inv_freq = (1.0 / (np.float32(theta) ** (np.arange(half, dtype=np.float32) * np.float32(2.0 / rot)))).astype(np.float32)
    ang = (np.arange(S, dtype=np.float32)[:, None] * inv_freq[None, :]).astype(np.float32)
    c = np.cos(ang.astype(np.float64)).astype(np.float32)
    s = np.sin(ang.astype(np.float64)).astype(np.float32)
    return np.ascontiguousarray(np.concatenate([c, c, -s, s], axis=1))


_CACHE = {}


def _get_nc(**kw):
    key = tuple(sorted(kw.items()))
    if key not in _CACHE:
        kb = KB(**kw)
        kb.build()
        _CACHE[key] = kb
    return _CACHE[key]


def make_in_maps(inputs, ncores=8):
    f = lambda a: np.ascontiguousarray(np.asarray(a, dtype=np.float32))
    shared = {
        "attn_norm_g": f(inputs["attn_norm_g"]),
        "w_in": f(inputs["w_in"]),
        "diff_q_norm_g": f(inputs["diff_q_norm_g"]),
        "diff_k_norm_g": f(inputs["diff_k_norm_g"]),
        "diff_lambda": f(inputs["diff_lambda"]).reshape(DEPTH, 256),
        "diff_subln_g": f(inputs["diff_subln_g"]),
        "mla_q_ln_g": f(inputs["mla_q_ln_g"]),
        "w_uq": f(inputs["w_uq"]),
        "mla_kv_ln_g": f(inputs["mla_kv_ln_g"]),
        "w_ukv": f(inputs["w_ukv"]),
        "mla_qk_norm_g": f(inputs["mla_qk_norm_g"]).reshape(DEPTH, 384),
        "w_o": f(inputs["w_o"]),
        "ffn_norm_g": f(inputs["ffn_norm_g"]),
        "dense_w_gate": f(inputs["dense_w_gate"])[0],
        "dense_w_up": f(inputs["dense_w_up"])[0],
        "dense_w_down": f(inputs["dense_w_down"])[0],
        "router_w": f(inputs["router_w"])[0],
        "moe_w_gate": f(inputs["moe_w_gate"])[0],
        "moe_w_up": f(inputs["moe_w_up"])[0],
        "moe_w_down": f(inputs["moe_w_down"])[0],
        "cs_d": _rope_table(ROPE_THETA, 16),
        "cs_m": _rope_table(MLA_ROPE_THETA, 64),
        "ident": np.eye(128, dtype=np.float32),
    }
    x = f(inputs["x"])
    maps = []
    for c in range(ncores):
        m = dict(shared)
        m["x"] = np.ascontiguousarray(x[c])
        maps.append(m)
    return maps


def kernel(**inputs):
    kb = _get_nc()
    in_maps = make_in_maps(inputs)
    res = run_bass_kernel_spmd(kb.nc, in_maps, core_ids=list(range(8)))
    out = np.stack([np.asarray(r["y"], dtype=np.float32) for r in res.results], axis=0)
    return out
```
